# Optimizing a Trainium2 kernel written in Bass

```python
import math
import jax, jax.numpy as jnp
from jax import lax
import numpy as np

D_MODEL = 1024
BATCH = 8
SEQ = 4096
DEPTH = 1

GRID_W = 64
ATT_HEADS = 8
ATT_KV_HEADS = 2
ATT_HEAD_DIM = 64
ATT_WIDTH = ATT_HEADS * ATT_HEAD_DIM
ATT_KV_WIDTH = ATT_KV_HEADS * ATT_HEAD_DIM
Q_BLOCK = 128
ROPE_THETA = 10000.0
HG_HEADS = 4
HG_DK = 128
HG_DV = 128
HG_WIDTH_K = HG_HEADS * HG_DK
HG_WIDTH_V = HG_HEADS * HG_DV
HG_CHUNK = 64
N_BRANCH = 2
IN_SPLITS = (ATT_WIDTH, ATT_KV_WIDTH, ATT_KV_WIDTH,
             HG_WIDTH_K, HG_WIDTH_K, HG_WIDTH_K, HG_WIDTH_V, HG_WIDTH_V,
             N_BRANCH * D_MODEL)
IN_WIDTH = sum(IN_SPLITS)
PEER_HEADS = 8
PEER_NKEYS = 128
PEER_N_EXPERTS = PEER_NKEYS * PEER_NKEYS
PEER_DKEY = 256
PEER_TOPK = 16
PEER_TOKEN_BLOCK = 128
PLE_DIM = 256
EPS = 1e-6

kernel_name = "hybrid_gqa_hgrn2_peer_encoder_block"


def rmsnorm(x, g):
    xf = x.astype(jnp.float32)
    y = xf * lax.rsqrt(jnp.mean(xf * xf, axis=-1, keepdims=True) + EPS)
    return (y * g.astype(jnp.float32)).astype(x.dtype)


def axial_rope_tables(seq_len):
    rows = seq_len // GRID_W
    row = jnp.repeat(jnp.arange(rows), GRID_W).astype(jnp.float32)
    col = jnp.tile(jnp.arange(GRID_W), rows).astype(jnp.float32)
    axis_dim = ATT_HEAD_DIM // 2
    rot_half = axis_dim // 2
    inv = ROPE_THETA ** (-jnp.arange(rot_half, dtype=jnp.float32) / rot_half)
    ang = jnp.stack([row[:, None] * inv, col[:, None] * inv], axis=1)
    return jnp.cos(ang), jnp.sin(ang)


def apply_axial_rope(x, cos, sin):
    B, S, H, hd = x.shape
    xr = x.reshape(B, S, H, 2, 2, hd // 4).astype(jnp.float32)
    x1, x2 = xr[..., 0, :], xr[..., 1, :]
    c = cos[None, :, None]
    s = sin[None, :, None]
    out = jnp.stack([x1 * c - x2 * s, x1 * s + x2 * c], axis=-2)
    return out.reshape(B, S, H, hd).astype(x.dtype)


def gqa_attention(q, k, v):
    B, S = q.shape[:2]
    G = ATT_HEADS // ATT_KV_HEADS
    nb = S // Q_BLOCK
    scale = ATT_HEAD_DIM ** -0.5
    qb = q.reshape(B, nb, Q_BLOCK, ATT_KV_HEADS, G, ATT_HEAD_DIM).transpose(1, 0, 2, 3, 4, 5)

    def one_block(qblk):
        s = jnp.einsum('bqkgd,bskd->bkgqs', qblk, k, preferred_element_type=jnp.float32) * scale
        pr = jax.nn.softmax(s, axis=-1)
        return jnp.einsum('bkgqs,bskd->bqkgd', pr.astype(v.dtype), v)

    o = lax.map(one_block, qb)
    return o.transpose(1, 0, 2, 3, 4, 5).reshape(B, S, ATT_WIDTH)


def gla_chunk_scan(q, k, v, log_f):
    B, S, H, dk = q.shape
    dv = v.shape[-1]
    C = HG_CHUNK
    nc = S // C

    def to_chunks(a):
        return a.reshape(B, nc, C, H, a.shape[-1]).transpose(1, 0, 3, 2, 4)

    qc, kc, vc, gc = to_chunks(q), to_chunks(k), to_chunks(v), to_chunks(log_f)
    lower = jnp.tril(jnp.ones((C, C), dtype=bool))[:, :, None]

    def step(state, inp):
        qi, ki, vi, gi = inp
        b = jnp.cumsum(gi.astype(jnp.float32), axis=2)
        diff = b[:, :, :, None, :] - b[:, :, None, :, :]
        decay = jnp.exp(jnp.where(lower, diff, -jnp.inf))
        A = jnp.einsum('bhtd,bhsd,bhtsd->bhts', qi.astype(jnp.float32), ki.astype(jnp.float32), decay)
        o_intra = jnp.einsum('bhts,bhse->bhte', A, vi.astype(jnp.float32))
        o_inter = jnp.einsum('bhtd,bhde->bhte', qi.astype(jnp.float32) * jnp.exp(b), state)
        b_last = b[:, :, -1:, :]
        k_dec = ki.astype(jnp.float32) * jnp.exp(b_last - b)
        new_state = state * jnp.exp(b_last[:, :, 0, :])[..., None] + \
            jnp.einsum('bhsd,bhse->bhde', k_dec, vi.astype(jnp.float32))
        return new_state, o_intra + o_inter

    state0 = jnp.zeros((B, H, dk, dv), jnp.float32)
    _, o = lax.scan(step, state0, (qc, kc, vc, gc))
    return o.transpose(1, 0, 3, 2, 4).reshape(B, S, H, dv)


def hgrn2_bidirectional(hq, hf_f, hf_b, hi, hg, lb, out_norm):
    B, S = hq.shape[:2]
    q = jax.nn.silu(hq).reshape(B, S, HG_HEADS, HG_DK)
    i_in = hi.reshape(B, S, HG_HEADS, HG_DV)

    def gates(fpre, lbd):
        f = lbd + (1.0 - lbd) * jax.nn.sigmoid(fpre.astype(jnp.float32))
        f = f.reshape(B, S, HG_HEADS, HG_DK)
        return 1.0 - f, jnp.log(f)

    k_f, lf_f = gates(hf_f, lb[0])
    k_b, lf_b = gates(hf_b, lb[1])
    o_fwd = gla_chunk_scan(q, k_f, i_in, lf_f)
    flip = lambda a: jnp.flip(a, axis=1)
    o_bwd = flip(gla_chunk_scan(flip(q), flip(k_b), flip(i_in), flip(lf_b)))
    o = rmsnorm(o_fwd + o_bwd, out_norm)
    o = o * jax.nn.silu(hg.astype(jnp.float32).reshape(B, S, HG_HEADS, HG_DV))
    return o.reshape(B, S, HG_WIDTH_V).astype(hq.dtype)


def peer_ffn(h, w_q, sub_keys, u, v):
    B, S, D = h.shape
    T = B * S
    K = PEER_TOPK
    ht = h.reshape(T, D)
    q = (ht @ w_q).reshape(T, PEER_HEADS, 2, PEER_DKEY // 2)
    s = jnp.einsum('thpc,hpnc->thpn', q, sub_keys, preferred_element_type=jnp.float32)
    v1, i1 = lax.top_k(s[:, :, 0], K)
    v2, i2 = lax.top_k(s[:, :, 1], K)
    cand = (v1[..., :, None] + v2[..., None, :]).reshape(T, PEER_HEADS, K * K)
    cand_idx = (i1[..., :, None] * PEER_NKEYS + i2[..., None, :]).reshape(T, PEER_HEADS, K * K)
    top_s, pos = lax.top_k(cand, K)
    idx = jnp.take_along_axis(cand_idx, pos, axis=-1)
    g = jax.nn.softmax(top_s, axis=-1)
    nb = T // PEER_TOKEN_BLOCK

    def one_block(args):
        xb, ib, gb = args
        ue = jnp.take(u, ib, axis=0)
        a = jnp.einsum('thkd,td->thk', ue, xb, preferred_element_type=jnp.float32)
        w = jax.nn.gelu(a) * gb
        ve = jnp.take(v, ib, axis=0)
        return jnp.einsum('thk,thkd->td', w.astype(ve.dtype), ve)

    out = lax.map(one_block, (ht.reshape(nb, PEER_TOKEN_BLOCK, D),
                              idx.reshape(nb, PEER_TOKEN_BLOCK, PEER_HEADS, K),
                              g.reshape(nb, PEER_TOKEN_BLOCK, PEER_HEADS, K)))
    return out.reshape(B, S, D).astype(h.dtype)


def setup_inputs(seed: int = 0) -> dict:
    key = jax.random.key(seed)
    ks = jax.random.split(key, 24)
    f32 = jnp.float32
    nrm = lambda k, shape, scale: jax.random.normal(k, shape, f32) * scale
    gain = lambda k, shape: 1.0 + 0.02 * jax.random.normal(k, shape, f32)
    return {
        "x": nrm(ks[0], (BATCH, SEQ, D_MODEL), 1.0),
        "p": nrm(ks[1], (DEPTH, BATCH, SEQ, PLE_DIM), 1.0),
        "norm_mix": gain(ks[2], (DEPTH, D_MODEL)),
        "w_in": nrm(ks[3], (DEPTH, D_MODEL, IN_WIDTH), D_MODEL ** -0.5),
        "q_norm": gain(ks[4], (DEPTH, ATT_HEAD_DIM)),
        "k_norm": gain(ks[5], (DEPTH, ATT_HEAD_DIM)),
        "hg_lb_raw": nrm(ks[6], (DEPTH + 1, 2, HG_WIDTH_K), 0.1),
        "hg_out_norm": gain(ks[7], (DEPTH, HG_DV)),
        "w_up_att": nrm(ks[8], (DEPTH, ATT_WIDTH, D_MODEL), ATT_WIDTH ** -0.5),
        "w_up_hg": nrm(ks[9], (DEPTH, HG_WIDTH_V, D_MODEL), HG_WIDTH_V ** -0.5),
        "w_out": nrm(ks[10], (DEPTH, D_MODEL, D_MODEL), D_MODEL ** -0.5),
        "norm_ffn": gain(ks[11], (DEPTH, D_MODEL)),
        "peer_wq": nrm(ks[12], (DEPTH, D_MODEL, PEER_HEADS * PEER_DKEY), D_MODEL ** -0.5),
        "peer_subkeys": nrm(ks[13], (DEPTH, PEER_HEADS, 2, PEER_NKEYS, PEER_DKEY // 2), (PEER_DKEY // 2) ** -0.5),
        "peer_u": nrm(ks[14], (DEPTH, PEER_N_EXPERTS, D_MODEL), D_MODEL ** -0.5),
        "peer_v": nrm(ks[15], (DEPTH, PEER_N_EXPERTS, D_MODEL), 0.3),
        "norm_ple": gain(ks[16], (DEPTH, D_MODEL)),
        "ple_gate": nrm(ks[17], (DEPTH, D_MODEL, D_MODEL), D_MODEL ** -0.5),
        "ple_proj": nrm(ks[18], (DEPTH, PLE_DIM, D_MODEL), PLE_DIM ** -0.5),
    }


def reference(x, p, norm_mix, w_in, q_norm, k_norm, hg_lb_raw, hg_out_norm, w_up_att, w_up_hg,
              w_out, norm_ffn, peer_wq, peer_subkeys, peer_u, peer_v, norm_ple, ple_gate, ple_proj):
    B, S, D = x.shape
    cos, sin = axial_rope_tables(S)
    lb_all = jnp.cumsum(jax.nn.softmax(hg_lb_raw.astype(jnp.float32), axis=0), axis=0)
    offsets = [int(o) for o in np.cumsum(IN_SPLITS)[:-1]]
    for i in range(DEPTH):
        h = rmsnorm(x, norm_mix[i])
        proj = h @ w_in[i]
        aq, ak, av, hq, hf_f, hf_b, hi, hg, gate_pre = jnp.split(proj, offsets, axis=-1)
        q = rmsnorm(aq.reshape(B, S, ATT_HEADS, ATT_HEAD_DIM), q_norm[i])
        k = rmsnorm(ak.reshape(B, S, ATT_KV_HEADS, ATT_HEAD_DIM), k_norm[i])
        v = av.reshape(B, S, ATT_KV_HEADS, ATT_HEAD_DIM)
        q = apply_axial_rope(q, cos, sin)
        k = apply_axial_rope(k, cos, sin)
        y_att = gqa_attention(q, k, v) @ w_up_att[i]
        y_hg = hgrn2_bidirectional(hq, hf_f, hf_b, hi, hg, lb_all[i], hg_out_norm[i]) @ w_up_hg[i]
        gts = jax.nn.sigmoid(gate_pre.astype(jnp.float32)).reshape(B, S, N_BRANCH, D)
        merged = (gts[:, :, 0] * y_att + gts[:, :, 1] * y_hg).astype(x.dtype)
        x = x + merged @ w_out[i]
        x = x + peer_ffn(rmsnorm(x, norm_ffn[i]), peer_wq[i], peer_subkeys[i], peer_u[i], peer_v[i])
        g_ple = jax.nn.sigmoid(rmsnorm(x, norm_ple[i]) @ ple_gate[i])
        x = x + g_ple * (p[i] @ ple_proj[i])
    return x
```

```python
import contextlib
import numpy as np
import concourse.bass as bass
import concourse.mybir as mybir
from concourse.bass_utils import run_bass_kernel_spmd

F32 = mybir.dt.float32
BF16 = mybir.dt.bfloat16
I32 = mybir.dt.int32
U32 = mybir.dt.uint32
ALU = mybir.AluOpType
AF = mybir.ActivationFunctionType
AX = mybir.AxisListType

S = 4096
D = 1024
NT = S // 128
EPS = 1e-6
EPOCH = 16000
NDMA_SLOTS = 48


class Buf:
    __slots__ = ("name", "last_w", "readers")

    def __init__(self, name=""):
        self.name = name
        self.last_w = None
        self.readers = {}


class T:
    __slots__ = ("t", "b", "regs")

    def __init__(self, t, name=""):
        self.t = t
        self.b = Buf(name)
        self.regs = {}

    def reg(self, key):
        if key not in self.regs:
            self.regs[key] = Buf(f"{self.b.name}[{key}]")
        return self.regs[key]

    def regl(self, keys):
        return [self.reg(k) for k in keys]


class Prog:
    ENGS = ("pe", "act", "dve", "pool", "sp")

    def __init__(self, nc):
        self.nc = nc
        self.ops = {e: [] for e in self.ENGS}
        self.count = {e: 0 for e in self.ENGS}
        self.known = {e: {} for e in self.ENGS}
        self.dma_count = [0] * NDMA_SLOTS
        self.dma_rr = {"hw": 0, "sw": 0}
        self.n_instr = 0
        self.pending = {e: [] for e in self.ENGS}

    def barrier(self):
        prods = [(e, self.count[e]) for e in self.ENGS if self.count[e] > 0]
        prods += [(("dma", s), c) for s, c in enumerate(self.dma_count) if c > 0]
        for e in self.ENGS:
            kn = self.known[e]
            for p, c in prods:
                if kn.get(p, 0) < c:
                    kn[p] = c
                    self.pending[e].append((p, c))

    def _deps(self, eng, reads, writes, pe_accum=False):
        deps = {}

        def add(p, s):
            if deps.get(p, 0) < s:
                deps[p] = s

        for b in reads:
            if b.last_w is not None:
                add(*b.last_w)
        for b in writes:
            if b.last_w is not None:
                if not (pe_accum and b.last_w[0] == "pe" and eng == "pe"):
                    add(*b.last_w)
            for p, s in b.readers.items():
                add(p, s)
        waits = []
        kn = self.known[eng]
        for p, s in deps.items():
            if kn.get(p, 0) < s:
                kn[p] = s
                waits.append((p, s))
        return waits

    def _commit(self, prod, seq, reads, writes):
        for b in writes:
            b.last_w = (prod, seq)
            b.readers = {}
        for b in reads:
            b.readers[prod] = max(b.readers.get(prod, 0), seq)

    @staticmethod
    def _bufs(xs):
        out = []
        for x in xs:
            if isinstance(x, T):
                out.append(x.b)
            elif isinstance(x, (list, tuple)):
                out.extend(Prog._bufs(x))
            else:
                out.append(x)
        return out

    def op(self, eng, fn, reads=(), writes=(), pe_accum=False):
        reads = self._bufs(reads)
        writes = self._bufs(writes)
        waits = self._deps(eng, reads, writes, pe_accum)
        waits = [w for w in self.pending[eng] if w not in waits] + waits
        self.pending[eng] = []
        self.count[eng] += 1
        seq = self.count[eng]
        self.ops[eng].append((waits, fn, (eng, seq)))
        self._commit(eng, seq, reads, writes)
        self.n_instr += 1

    def dma(self, q, out, in_, reads=(), writes=(), **kw):
        reads = self._bufs(reads)
        writes = self._bufs(writes)
        waits = self._deps(q, reads, writes)
        waits = [w for w in self.pending[q] if w not in waits] + waits
        self.pending[q] = []
        half = NDMA_SLOTS // 2
        kind = "sw" if q == "pool" else "hw"
        slot = self.dma_rr[kind] + (half if kind == "sw" else 0)
        self.dma_rr[kind] = (self.dma_rr[kind] + 1) % half
        prod = ("dma", slot)
        prev = self.dma_count[slot]
        kn = self.known[q]
        if prev and kn.get(prod, 0) < prev:
            kn[prod] = prev
            waits.append((prod, prev))
        self.dma_count[slot] += 1
        seq = self.dma_count[slot]
        self.ops[q].append((waits, (lambda e: e.dma_start(out=out, in_=in_, **kw)), (prod, seq)))
        self._commit(prod, seq, reads, writes)
        self.n_instr += 1

    def emit(self, final_bufs=()):
        nc = self.nc
        final_bufs = self._bufs(final_bufs)
        with contextlib.ExitStack() as st:
            sems = {}
            for e in self.ENGS:
                nep = self.count[e] // EPOCH + 1
                sems[e] = [st.enter_context(nc.semaphore(f"s_{e}_{k}")) for k in range(nep)]
            for s in range(NDMA_SLOTS):
                sems[("dma", s)] = [st.enter_context(nc.semaphore(f"s_dma{s}"))]
            fin = {}
            for b in final_bufs:
                if b.last_w is not None:
                    p, s = b.last_w
                    fin[p] = max(fin.get(p, 0), s)

            def wait(eng, p, s):
                if isinstance(p, tuple):
                    eng.wait_ge(sems[p][0], 16 * s)
                else:
                    k = (s - 1) // EPOCH
                    eng.wait_ge(sems[p][k], s - k * EPOCH)

            def run(ename, eng):
                for waits, fn, (prod, seq) in self.ops[ename]:
                    for p, s in waits:
                        wait(eng, p, s)
                    ins = fn(eng)
                    if isinstance(prod, tuple):
                        ins.then_inc(sems[prod][0], 16)
                    else:
                        k = (seq - 1) // EPOCH
                        ins.then_inc(sems[prod][k], 1)
                if ename == "sp":
                    for p, s in fin.items():
                        wait(eng, p, s)

            with nc.Block() as block:
                @block.sync
                def _(e):
                    run("sp", e)

                @block.tensor
                def _(e):
                    run("pe", e)

                @block.scalar
                def _(e):
                    run("act", e)

                @block.vector
                def _(e):
                    run("dve", e)

                @block.gpsimd
                def _(e):
                    run("pool", e)


class Ring:
    def __init__(self, items):
        self.items = items
        self.i = 0

    def next(self):
        x = self.items[self.i % len(self.items)]
        self.i += 1
        return x


class Ctx:
    def __init__(self, nc, dbg=()):
        self.nc = nc
        self.P = Prog(nc)
        self.dbg = set(dbg)
        self.ins = {}
        self.scr = {}
        self.outs = []
        self.uid = 0

    def din(self, name, shape, dt=F32):
        ap = self.nc.dram_tensor(name, list(shape), dt, kind="ExternalInput").ap()
        self.ins[name] = T(ap, name)
        return self.ins[name]

    def scratch(self, name, shape, dt):
        if name in self.dbg:
            ap = self.nc.dram_tensor(name, list(shape), dt, kind="ExternalOutput").ap()
        else:
            ap = self.nc.dram_tensor(name, list(shape), dt).ap()
        t = T(ap, name)
        self.scr[name] = t
        if name in self.dbg:
            self.outs.append(t)
        return t

    def alloc(self, st, shape, dt, name=None, psum=False):
        self.uid += 1
        name = f"{name or 't'}_{self.uid}"
        if psum:
            t = st.enter_context(self.nc.psum_tensor(name, list(shape), dt))
        else:
            t = st.enter_context(self.nc.sbuf_tensor(name, list(shape), dt))
        return T(t, name)

    def ring(self, st, n, shape, dt, name=None, psum=False):
        return Ring([self.alloc(st, shape, dt, name, psum) for _ in range(n)])


def mm(C, out_t, out_ap, lhsT_t, lhsT_ap, rhs_t, rhs_ap, start, stop):
    C.P.op("pe", lambda e: e.matmul(out_ap, lhsT=lhsT_ap, rhs=rhs_ap, start=start, stop=stop),
           reads=[lhsT_t, rhs_t], writes=[out_t], pe_accum=True)


def tr(C, out_t, out_ap, in_t, in_ap, ident):
    C.P.op("pe", lambda e: e.transpose(out=out_ap, in_=in_ap, identity=ident.t[:]),
           reads=[in_t, ident], writes=[out_t], pe_accum=True)


def act(C, out_t, out_ap, in_t, in_ap, func, reads=(), extra_w=(), **kw):
    C.P.op("act", lambda e: e.activation(out=out_ap, in_=in_ap, func=func, **kw),
           reads=[in_t] + list(reads), writes=[out_t] + list(extra_w))


def tt(C, eng, out_t, out_ap, a_t, a_ap, b_t, b_ap, op):
    C.P.op(eng, lambda e: e.tensor_tensor(out=out_ap, in0=a_ap, in1=b_ap, op=op),
           reads=[a_t, b_t], writes=[out_t])


def ts(C, eng, out_t, out_ap, a_t, a_ap, s1, s2, op0, op1=None, reads=()):
    if op1 is None:
        C.P.op(eng, lambda e: e.tensor_scalar(out=out_ap, in0=a_ap, scalar1=s1, scalar2=None, op0=op0),
               reads=[a_t] + list(reads), writes=[out_t])
    else:
        C.P.op(eng, lambda e: e.tensor_scalar(out=out_ap, in0=a_ap, scalar1=s1, scalar2=s2, op0=op0, op1=op1),
               reads=[a_t] + list(reads), writes=[out_t])


def stt(C, eng, out_t, out_ap, a_t, a_ap, scalar, b_t, b_ap, op0, op1, reads=()):
    C.P.op(eng, lambda e: e.scalar_tensor_tensor(out=out_ap, in0=a_ap, scalar=scalar, in1=b_ap, op0=op0, op1=op1),
           reads=[a_t, b_t] + list(reads), writes=[out_t])


def cp(C, eng, out_t, out_ap, in_t, in_ap):
    if eng == "act":
        C.P.op("act", lambda e: e.copy(out=out_ap, in_=in_ap), reads=[in_t], writes=[out_t])
    else:
        C.P.op(eng, lambda e: e.tensor_copy(out=out_ap, in_=in_ap), reads=[in_t], writes=[out_t])


def rsqrt_mean(C, st_t, src_ap_fn, n, scale):
    ap = src_ap_fn()
    ts(C, "dve", st_t, ap, st_t, ap, scale, EPS, ALU.mult, ALU.add)
    C.P.op("act", lambda e: e.activation(out=ap, in_=ap, func=AF.Sqrt), reads=[st_t], writes=[st_t])
    C.P.op("dve", lambda e: e.reciprocal(out=ap, in_=ap), reads=[st_t], writes=[st_t])


def build_consts(C, st):
    P = C.P
    K = {}
    idf = C.alloc(st, [128, 128], F32, "idf")
    P.op("pool", lambda e: e.memset(idf.t[:], 0.0), writes=[idf])
    P.op("pool", lambda e: e.affine_select(out=idf.t[:], in_=idf.t[:], pattern=[[-1, 128]],
                                           compare_op=ALU.not_equal, fill=1.0, base=0, channel_multiplier=1),
         reads=[idf], writes=[idf])
    ident = C.alloc(st, [128, 128], BF16, "ident")
    cp(C, "dve", ident, ident.t[:], idf, idf.t[:])
    K["ident"] = ident
    K["identf"] = idf
    return K


OFF = dict(aq=0, ak=512, av=640, hq=768, hff=1280, hfb=1792, hi=2304, hg=2816, gate=3328)


def phase_a(C, K, ntiles=NT):
    nc, P = C.nc, C.P
    I = C.ins
    Sc = C.scr
    with contextlib.ExitStack() as st:
        w_in = C.alloc(st, [128, 8, 5376], BF16, "w_in")
        wv = I["w_in"].t.rearrange("(k p) n -> p k n", p=128)
        for c0 in range(0, 5376, 672):
            P.dma("pool", w_in.t[:, :, c0:c0 + 672], wv[:, :, c0:c0 + 672], reads=[I["w_in"]], writes=[w_in])
        gmix = C.alloc(st, [128, 1024], F32, "gmix")
        P.dma("sp", gmix.t[:], I["norm_mix"].t.to_broadcast([128, 1024]), writes=[gmix])
        qg = C.alloc(st, [128, 8, 64], F32, "qg")
        P.dma("sp", qg.t[:], I["q_norm"].t.unsqueeze(1).to_broadcast([128, 8, 64]), writes=[qg])
        kg = C.alloc(st, [128, 2, 64], F32, "kg")
        P.dma("sp", kg.t[:], I["k_norm"].t.unsqueeze(1).to_broadcast([128, 2, 64]), writes=[kg])
        raw = C.alloc(st, [128, 2, 2, 512], F32, "raw")
        P.dma("sp", raw.t[:], I["hg_lb_raw"].t.unsqueeze(0).to_broadcast([128, 2, 2, 512]), writes=[raw])
        lbb = C.alloc(st, [128, 2, 512], F32, "lbb")
        omlb = C.alloc(st, [128, 2, 512], F32, "omlb")
        tt(C, "dve", lbb, lbb.t[:], raw, raw.t[:, 0], raw, raw.t[:, 1], ALU.subtract)
        act(C, omlb, omlb.t[:], lbb, lbb.t[:], AF.Sigmoid, scale=-1.0)
        act(C, lbb, lbb.t[:], lbb, lbb.t[:], AF.Sigmoid)
        rawT = C.alloc(st, [128, 2, 2, 4], F32, "rawT")
        P.dma("sp", rawT.t[:], I["hg_lb_raw"].t.rearrange("s r (c p) -> p s r c", p=128), writes=[rawT],
              allow_slow_non_contiguous=True)
        omlT = C.alloc(st, [128, 2, 4], F32, "omlT")
        tt(C, "dve", omlT, omlT.t[:], rawT, rawT.t[:, 0], rawT, rawT.t[:, 1], ALU.subtract)
        act(C, omlT, omlT.t[:], omlT, omlT.t[:], AF.Sigmoid, scale=-1.0)

        xr = C.ring(st, 3, [128, 1024], F32, "xt")
        junk = C.alloc(st, [128, 1024], BF16, "junk")
        stat = C.ring(st, 8, [128, 16], F32, "stat")
        hbr = C.ring(st, 2, [128, 1024], BF16, "hb")
        hTr = C.ring(st, 2, [128, 8, 512], BF16, "hT")
        ptr = C.ring(st, 2, [128, 8, 128], BF16, "ptr", psum=True)
        pfr = C.ring(st, 2, [128, 512], F32, "pf", psum=True)
        ptk = C.ring(st, 3, [128, 512], F32, "ptk", psum=True)
        pqt = C.alloc(st, [128, 8, 128], BF16, "pqt", psum=True)
        stg_bf = C.ring(st, 4, [128, 512], BF16, "stgb")
        stg_f = C.ring(st, 3, [128, 512], F32, "stgf")
        tmpf = C.ring(st, 4, [128, 512], F32, "tmpf")
        qn = C.ring(st, 3, [128, 512], F32, "qn")
        qr = C.ring(st, 2, [128, 512], BF16, "qr")
        kk = C.ring(st, 2, [128, 4, 64], BF16, "kk")
        kn_ = C.ring(st, 2, [128, 128], F32, "kn")
        vst = C.ring(st, 2, [128, 2, 128], BF16, "vst")
        for v_ in vst.items:
            P.op("pool", lambda e, v_=v_: e.memset(v_.t[:], 1.0), writes=[v_])
        csr = C.ring(st, 2, [128, 2, 32], F32, "cs")
        qTs = C.ring(st, 2, [128, 6, 128], BF16, "qTs")
        rt = C.ring(st, 8, [128, 8, 2, 16], F32, "rt")

        def run_rr(gens):
            gens = list(gens)
            while gens:
                for g_ in list(gens):
                    try:
                        next(g_)
                    except StopIteration:
                        gens.remove(g_)

        def rope_norm(src_ps, src_ap, nh, gain, cs, dst_t, dst_ap4):
            w = nh * 64
            sq = tmpf.next()
            act(C, sq, sq.t[:, 0:w], src_ps, src_ap, AF.Square)
            yield
            s8 = stat.next()
            P.op("dve", lambda e: e.tensor_reduce(out=s8.t[:, 0:nh], in_=sq.t[:, 0:w].rearrange("p (h d) -> p h d", d=64),
                                                  axis=AX.X, op=ALU.add), reads=[sq], writes=[s8])
            yield
            ap = s8.t[:, 0:nh]
            ts(C, "dve", s8, ap, s8, ap, 1.0 / 64, EPS, ALU.mult, ALU.add)
            yield
            C.P.op("act", lambda e: e.activation(out=ap, in_=ap, func=AF.Sqrt), reads=[s8], writes=[s8])
            yield
            C.P.op("dve", lambda e: e.reciprocal(out=ap, in_=ap), reads=[s8], writes=[s8])
            yield
            n_ = qn.next()
            nv = n_.t[:, 0:w].rearrange("p (h d) -> p h d", d=64)
            tt(C, "dve", n_, nv, src_ps, src_ap.rearrange("p (h d) -> p h d", d=64),
               s8, s8.t[:, 0:nh].unsqueeze(2).to_broadcast([128, nh, 64]), ALU.mult)
            yield
            tt(C, "pool", n_, nv, n_, nv, gain, gain.t[:, 0:nh, :], ALU.mult)
            yield
            v5 = n_.t[:, 0:w].rearrange("p (h a b f) -> p h a b f", a=2, b=2, f=16)
            x1 = v5[:, :, :, 0, :]
            x2 = v5[:, :, :, 1, :]
            cb = cs.t[:, 0, :].rearrange("p (a f) -> p a f", a=2).unsqueeze(1).to_broadcast([128, nh, 2, 16])
            sb_ = cs.t[:, 1, :].rearrange("p (a f) -> p a f", a=2).unsqueeze(1).to_broadcast([128, nh, 2, 16])
            t1, t2 = rt.next(), rt.next()
            tt(C, "dve", t1, t1.t[:, 0:nh], n_, x1, cs, cb, ALU.mult)
            tt(C, "pool", t2, t2.t[:, 0:nh], n_, x2, cs, sb_, ALU.mult)
            yield
            t3, t4 = rt.next(), rt.next()
            tt(C, "dve", t3, t3.t[:, 0:nh], n_, x1, cs, sb_, ALU.mult)
            tt(C, "pool", t4, t4.t[:, 0:nh], n_, x2, cs, cb, ALU.mult)
            yield
            tt(C, "dve", dst_t, dst_ap4[:, :, :, 0, :], t1, t1.t[:, 0:nh], t2, t2.t[:, 0:nh], ALU.subtract)
            yield
            tt(C, "pool", dst_t, dst_ap4[:, :, :, 1, :], t3, t3.t[:, 0:nh], t4, t4.t[:, 0:nh], ALU.add)
            yield

        def prep_tile(g, j, hT):
            ti = g * 4 + j
            xt = xr.next()
            P.dma("sp", xt.t[:], I["x"].t[ti * 128:(ti + 1) * 128, :], reads=[I["x"]], writes=[xt])
            s_ = stat.next()
            act(C, junk, junk.t[:], xt, xt.t[:], AF.Square, accum_out=s_.t[:, 0:1], extra_w=[s_])
            yield
            ap = s_.t[:, 0:1]
            ts(C, "dve", s_, ap, s_, ap, 1.0 / D, EPS, ALU.mult, ALU.add)
            yield
            C.P.op("act", lambda e: e.activation(out=ap, in_=ap, func=AF.Sqrt), reads=[s_], writes=[s_])
            yield
            C.P.op("dve", lambda e: e.reciprocal(out=ap, in_=ap), reads=[s_], writes=[s_])
            yield
            hb = hbr.next()
            stt(C, "dve", hb, hb.t[:], xt, xt.t[:], s_.t[:, 0:1], gmix, gmix.t[:], ALU.mult, ALU.mult, reads=[s_])
            yield
            pt = ptr.next()
            for k in range(8):
                tr(C, pt, pt.t[:, k, :], hb, hb.t[:, k * 128:(k + 1) * 128], K["ident"])
            yield
            cp(C, "act", hT, hT.t[:, :, j * 128:(j + 1) * 128], pt, pt.t[:])
            yield

        ngr = ntiles // 4
        hTs = {0: hTr.next()}
        for j in range(4):
            run_rr([prep_tile(0, j, hTs[0])])
        for g in range(ngr):
            hT = hTs[g]
            if g + 1 < ngr:
                hTs[g + 1] = hTr.next()
            cols = slice(g * 512, (g + 1) * 512)
            fm = [("hq", OFF["hq"] + c * 128, c) for c in range(4)] + \
                 [("hff", OFF["hff"] + c * 128, c) for c in range(4)] + \
                 [("hfb", OFF["hfb"] + c * 128, c) for c in range(4)] + \
                 [("gate", OFF["gate"] + c * 128, c) for c in range(16)]
            for kind, c0, c in fm:
                pf = pfr.next()
                for k in range(8):
                    mm(C, pf, pf.t[:], w_in, w_in.t[:, k, c0:c0 + 128], hT, hT.t[:, k, :], k == 0, k == 7)
                sg = stg_bf.next()
                if kind == "hq":
                    act(C, sg, sg.t[:], pf, pf.t[:], AF.Silu)
                    dst = Sc["hqT"]
                    P.dma("pool", dst.t[c, :, cols], sg.t[:], reads=[sg], writes=[dst.reg((c, g))])
                elif kind in ("hff", "hfb"):
                    d_ = 0 if kind == "hff" else 1
                    tf = tmpf.next()
                    act(C, tf, tf.t[:], pf, pf.t[:], AF.Sigmoid, scale=-1.0)
                    ts(C, "dve", sg, sg.t[:], tf, tf.t[:], omlT.t[:, d_, c:c + 1], None, ALU.mult, reads=[omlT])
                    dst = Sc["kT"]
                    P.dma("pool", dst.t[d_, c, :, cols], sg.t[:], reads=[sg], writes=[dst.reg((d_, c, g))])
                else:
                    act(C, sg, sg.t[:], pf, pf.t[:], AF.Sigmoid)
                    dst = Sc["gtsT"]
                    P.dma("pool", dst.t[c, :, cols], sg.t[:], reads=[sg], writes=[dst.reg((c, g))])
            for j in range(4):
                ti = g * 4 + j
                rows = slice(ti * 128, (ti + 1) * 128)
                lhs = lambda k, j=j: hT.t[:, k, j * 128:(j + 1) * 128]
                cs = csr.next()
                P.dma("sp", cs.t[:], I["rope"].t[:, rows, :].rearrange("c p f -> p c f"), reads=[I["rope"]], writes=[cs])
                q_ = qr.next()
                k_ = kk.next()

                def chain_q():
                    pq = ptk.next()
                    for k in range(8):
                        mm(C, pq, pq.t[:], hT, lhs(k), w_in, w_in.t[:, k, 0:512], k == 0, k == 7)
                    yield
                    yield from rope_norm(pq, pq.t[:], 8, qg, cs, q_, q_.t[:].rearrange("p (h a b f) -> p h a b f", a=2, b=2, f=16))

                def chain_kv():
                    pkv = ptk.next()
                    for k in range(8):
                        mm(C, pkv, pkv.t[:, 0:256], hT, lhs(k), w_in, w_in.t[:, k, 512:768], k == 0, k == 7)
                    yield
                    v_ = vst.next()
                    cp(C, "act", v_, v_.t[:, :, 0:64], pkv, pkv.t[:, 128:256].rearrange("p (h d) -> p h d", d=64))
                    P.dma("pool", Sc["v"].t[ti], v_.t[:], reads=[v_], writes=[Sc["v"].reg(ti)])
                    yield
                    kview = k_.t[:].rearrange("p (h r) d -> p h r d", r=2)
                    yield from rope_norm(pkv, pkv.t[:, 0:128], 2, kg, cs, k_,
                                         kview[:, :, 0, :].rearrange("p h (a b f) -> p h a b f", a=2, b=2, f=16))
                    cp(C, "pool", k_, kview[:, :, 1, :], k_, kview[:, :, 0, :])
                    yield

                def chain_gate(d_, key):
                    pg = ptk.next()
                    for k in range(8):
                        mm(C, pg, pg.t[:], hT, lhs(k), w_in, w_in.t[:, k, OFF[key]:OFF[key] + 512], k == 0, k == 7)
                    yield
                    tf = tmpf.next()
                    act(C, tf, tf.t[:], pg, pg.t[:], AF.Sigmoid)
                    yield
                    tt(C, "dve", tf, tf.t[:], tf, tf.t[:], omlb, omlb.t[:, d_, :], ALU.mult)
                    yield
                    tt(C, "dve", tf, tf.t[:], tf, tf.t[:], lbb, lbb.t[:, d_, :], ALU.add)
                    yield
                    gf = stg_f.next()
                    act(C, gf, gf.t[:], tf, tf.t[:], AF.Ln)
                    P.dma("pool", Sc["g"].t[d_, rows, :], gf.t[:], reads=[gf], writes=[Sc["g"].reg((d_, ti))])
                    kb = stg_bf.next()
                    ts(C, "pool", kb, kb.t[:], tf, tf.t[:], -1.0, 1.0, ALU.mult, ALU.add)
                    P.dma("pool", Sc["k"].t[d_, rows, :], kb.t[:], reads=[kb], writes=[Sc["k"].reg((d_, ti))])
                    yield

                def chain_h(key, dstn, fn):
                    ph = ptk.next()
                    for k in range(8):
                        mm(C, ph, ph.t[:], hT, lhs(k), w_in, w_in.t[:, k, OFF[key]:OFF[key] + 512], k == 0, k == 7)
                    yield
                    sb_ = stg_bf.next()
                    if fn is None:
                        cp(C, "act", sb_, sb_.t[:], ph, ph.t[:])
                    else:
                        act(C, sb_, sb_.t[:], ph, ph.t[:], fn)
                    P.dma("pool", Sc[dstn].t[rows, :], sb_.t[:], reads=[sb_], writes=[Sc[dstn].reg(ti)])
                    yield

                chains = [chain_q(), chain_kv(), chain_gate(0, "hff")]
                if g + 1 < ngr:
                    chains.append(prep_tile(g + 1, j, hTs[g + 1]))
                run_rr(chains)
                run_rr([chain_gate(1, "hfb"), chain_h("hi", "hi", None), chain_h("hg", "sg", AF.Silu)])
                for pr in range(4):
                    tr(C, pqt, pqt.t[:, pr, :], q_, q_.t[:, pr * 128:(pr + 1) * 128], K["ident"])
                kflat = k_.t[:].rearrange("p a d -> p (a d)")
                for kv in range(2):
                    tr(C, pqt, pqt.t[:, 4 + kv, :], k_, kflat[:, kv * 128:(kv + 1) * 128], K["ident"])
                qs = qTs.next()
                cp(C, "act", qs, qs.t[:], pqt, pqt.t[:, 0:6, :])
                P.dma("pool", Sc["qT"].t[:, :, rows].rearrange("r p t -> p r t"), qs.t[:, 0:4, :], reads=[qs], writes=[Sc["qT"].reg(ti)])
                P.dma("pool", Sc["kTa"].t[:, :, rows].rearrange("r p t -> p r t"), qs.t[:, 4:6, :], reads=[qs], writes=[Sc["kTa"].reg(ti)])


def phase_b(C, K, ngroups=8, hhs=(0, 1), bg=()):
    nc, P = C.nc, C.P
    Sc = C.scr
    with contextlib.ExitStack() as st:
        kT = [C.alloc(st, [128, S], BF16, "kTsb") for _ in range(2)]
        for kv in range(2):
            for hf in range(2):
                cs_ = slice(hf * 2048, (hf + 1) * 2048)
                P.dma("sp", kT[kv].t[:, cs_], Sc["kTa"].t[kv, :, cs_],
                      reads=Sc["kTa"].regl(range(hf * 16, hf * 16 + 16)), writes=[kT[kv]])
        vs = C.alloc(st, [128, NT, 256], BF16, "vsb")
        for hf in range(4):
            P.dma("sp", vs.t[:, hf * 8:(hf + 1) * 8, :], Sc["v"].t[hf * 8:(hf + 1) * 8].rearrange("t p h c -> p t (h c)"),
                  reads=Sc["v"].regl(range(hf * 8, hf * 8 + 8)), writes=[vs])
        if "dbgvs" in Sc:
            P.dma("pool", Sc["dbgvs"].t[:], vs.t[:], reads=[vs], writes=[Sc["dbgvs"]])
        qr_ = C.ring(st, 2, [128, 512], BF16, "qTg")
        psS = C.ring(st, 4, [128, 512], F32, "psS", psum=True)
        acc = [C.alloc(st, [128, 512], F32, "acc", psum=True) for _ in range(2)]
        ptr_ = C.ring(st, 8, [128, 512], BF16, "pT")
        rl = C.ring(st, 2, [128, 512], F32, "rl")
        obr = C.ring(st, 2, [128, 512], BF16, "ob")
        LAG = 3
        steps = [(g, pr, kt, hh) for g in range(ngroups) for pr in range(4) for kt in range(NT) for hh in hhs]
        state = {}
        accs = [acc, [C.alloc(st, [128, 512], F32, "acc2", psum=True) for _ in range(2)]]

        def stage1(g, pr, kt, hh):
            kv = pr // 2
            if kt == 0 and hh == hhs[0]:
                q = qr_.next()
                P.dma("sp", q.t[:], Sc["qT"].t[pr, :, g * 512:(g + 1) * 512], reads=Sc["qT"].regl(range(4 * g, 4 * g + 4)), writes=[q])
                state["q", g, pr] = q
            q = state["q", g, pr]
            rows = slice(hh * 64, (hh + 1) * 64)
            s_ = psS.next()
            mm(C, s_, s_.t[:], kT[kv], kT[kv].t[rows, kt * 128:(kt + 1) * 128], q, q.t[rows, :], True, True)
            p_ = ptr_.next()
            act(C, p_, p_.t[:], s_, s_.t[:], AF.Exp, scale=0.125)
            state["p", g, pr, kt, hh] = p_

        def stage2(g, pr, kt, hh):
            kv = pr // 2
            p_ = state.pop(("p", g, pr, kt, hh))
            ac = accs[(g * 4 + pr) % 2]
            mm(C, ac[hh], ac[hh].t[:], vs, vs.t[:, kt, kv * 128:(kv + 1) * 128], p_, p_.t[:], kt == 0, kt == NT - 1)
            if kt == NT - 1 and hh == hhs[-1]:
                ob = obr.next()
                for h2_ in hhs:
                    r_ = rl.next()
                    C.P.op("dve", lambda e, r_=r_, h2_=h2_, ac=ac: e.reciprocal(out=r_.t[64:128, :], in_=ac[h2_].t[64:128, :]),
                           reads=[ac[h2_]], writes=[r_])
                    tt(C, "dve", ob, ob.t[h2_ * 64:(h2_ + 1) * 64, :], ac[h2_], ac[h2_].t[0:64, :], r_, r_.t[64:128, :], ALU.mult)
                P.dma("pool", Sc["attoT"].t[pr, :, g * 512:(g + 1) * 512], ob.t[:], reads=[ob], writes=[Sc["attoT"].reg((pr, g))])

        bg = list(bg)
        LAG = 4
        for it in range(0, len(steps) + LAG, 2):
            for i_ in (it, it + 1):
                if i_ < len(steps):
                    stage1(*steps[i_])
            for i_ in (it - LAG, it - LAG + 1):
                if 0 <= i_ < len(steps):
                    stage2(*steps[i_])
            if bg and (it // 2) % 3 == 2:
                bg.pop(0)()
        for job in bg:
            job()


def build_masks(C, st):
    P = C.P
    M = {}
    specs = {
        "f_incl": (ALU.is_ge, 0, 1, -1),
        "f_excl": (ALU.is_gt, 0, -1, 1),
        "b_incl": (ALU.is_ge, 0, -1, 1),
        "b_excl": (ALU.is_gt, 0, 1, -1),
    }
    for name, (op, base, tmul, pmul) in specs.items():
        m = C.alloc(st, [128, 128], F32, "m_" + name)
        P.op("pool", lambda e, m=m: e.memset(m.t[:], 1.0), writes=[m])
        P.op("pool", lambda e, m=m, op=op, base=base, tmul=tmul, pmul=pmul: e.affine_select(
            out=m.t[:], in_=m.t[:], pattern=[[tmul, 128]], compare_op=op, fill=0.0, base=base, channel_multiplier=pmul),
            reads=[m], writes=[m])
        P.op("pool", lambda e, m=m: e.memset(m.t[0:64, 64:128], 0.0), reads=[m], writes=[m])
        P.op("pool", lambda e, m=m: e.memset(m.t[64:128, 0:64], 0.0), reads=[m], writes=[m])
        M[name] = m
    return M


def phase_c(C, K, ntiles=NT, dirs=(0, 1)):
    nc, P = C.nc, C.P
    Sc = C.scr
    I = C.ins
    with contextlib.ExitStack() as st:
        M = build_masks(C, st)
        gon = C.alloc(st, [128, 4, 128], F32, "gon")
        P.dma("sp", gon.t[:], I["hg_out_norm"].t.unsqueeze(1).to_broadcast([128, 4, 128]), writes=[gon])
        gr = C.ring(st, 2, [128, 512], F32, "g_t")
        kdr = C.ring(st, 2, [128, 512], BF16, "kd_t")
        kTr = C.ring(st, 2, [128, 4, 128], BF16, "kT_t")
        qTr = C.ring(st, 2, [128, 4, 128], BF16, "hqT_t")
        vr = C.ring(st, 2, [128, 512], BF16, "v_t")
        ofr = C.ring(st, 2, [128, 512], F32, "of_t")
        sgr = C.ring(st, 2, [128, 512], BF16, "sg_t")
        prx = C.alloc(st, [128, 512], F32, "prx", psum=True)
        pbT = C.alloc(st, [128, 4, 128], F32, "pbT", psum=True)
        pX = [C.alloc(st, [128, 4, 128], F32, "pX", psum=True) for _ in range(2)]
        pOs = [C.alloc(st, [128, 4, 128], F32, "pOs", psum=True) for _ in range(2)]
        pTr = C.alloc(st, [128, 8, 128], BF16, "pTr", psum=True)
        ebT = C.ring(st, 2, [128, 4, 128], F32, "ebT")
        enbT = C.ring(st, 2, [128, 4, 128], F32, "enbT")
        er = C.ring(st, 2, [128, 512], F32, "er")
        qfull = C.ring(st, 2, [128, 4, 128], BF16, "qfull")
        qlo = C.ring(st, 2, [128, 4, 128], BF16, "qlo")
        qhi = C.ring(st, 2, [128, 4, 128], BF16, "qhi")
        for t_ in qlo.items + qhi.items:
            P.op("pool", lambda e, t_=t_: e.memset(t_.t[:], 0.0), writes=[t_])
        ktil = C.ring(st, 2, [128, 4, 128], BF16, "ktil")
        kdec = C.ring(st, 2, [128, 512], BF16, "kdec")
        atm = C.ring(st, 4, [128, 128], BF16, "atm")
        S32 = [C.alloc(st, [128, 128], F32, "S32") for _ in range(4)]
        Sbf = [C.alloc(st, [128, 128], BF16, "Sbf") for _ in range(4)]
        osb = C.ring(st, 2, [128, 512], F32, "osb")
        tot = C.ring(st, 2, [128, 512], F32, "tot")
        sqt = C.ring(st, 2, [128, 512], F32, "sqt")
        stat = C.ring(st, 2, [128, 8], F32, "statc")
        onb = C.ring(st, 2, [128, 512], BF16, "onb")
        oTs = C.ring(st, 2, [128, 4, 128], BF16, "oTs")

        for d_ in dirs:
            Mi = M["f_incl"] if d_ == 0 else M["b_incl"]
            Me = M["f_excl"] if d_ == 0 else M["b_excl"]
            for hd in range(4):
                P.op("pool", lambda e, hd=hd: e.memset(S32[hd].t[:], 0.0), writes=[S32[hd]])
                P.op("pool", lambda e, hd=hd: e.memset(Sbf[hd].t[:], 0.0), writes=[Sbf[hd]])
            order = list(range(ntiles)) if d_ == 0 else list(range(ntiles - 1, -1, -1))
            def pro(ti):
                rows = slice(ti * 128, (ti + 1) * 128)
                g_t, kd_t, kT_t, q_t, v_t = gr.next(), kdr.next(), kTr.next(), qTr.next(), vr.next()
                P.dma("sp", g_t.t[:], Sc["g"].t[d_, rows, :], reads=[Sc["g"].reg((d_, ti))], writes=[g_t])
                P.dma("sp", kd_t.t[:], Sc["k"].t[d_, rows, :], reads=[Sc["k"].reg((d_, ti))], writes=[kd_t])
                P.dma("sp", kT_t.t[:], Sc["kT"].t[d_, :, :, rows].rearrange("h p t -> p h t"),
                      reads=[Sc["kT"].reg((d_, c, ti // 4)) for c in range(4)], writes=[kT_t])
                P.dma("sp", q_t.t[:], Sc["hqT"].t[:, :, rows].rearrange("h p t -> p h t"),
                      reads=[Sc["hqT"].reg((c, ti // 4)) for c in range(4)], writes=[q_t])
                P.dma("sp", v_t.t[:], Sc["hi"].t[rows, :], reads=[Sc["hi"].reg(ti)], writes=[v_t])
                mm(C, prx, prx.t[:], Me, Me.t[:], g_t, g_t.t[:], True, True)
                for hd in range(4):
                    mm(C, pbT, pbT.t[:, hd, :], g_t, g_t.t[:, hd * 128:(hd + 1) * 128], Mi, Mi.t[:], True, True)
                eb, enb, er_ = ebT.next(), enbT.next(), er.next()
                act(C, eb, eb.t[:], pbT, pbT.t[:], AF.Exp)
                act(C, enb, enb.t[:], pbT, pbT.t[:], AF.Exp, scale=-1.0)
                act(C, er_, er_.t[:], prx, prx.t[:], AF.Exp)
                qf, ql, qh, kt_, kdc = qfull.next(), qlo.next(), qhi.next(), ktil.next(), kdec.next()
                tt(C, "dve", qf, qf.t[:], q_t, q_t.t[:], eb, eb.t[:], ALU.mult)
                cp(C, "pool", ql, ql.t[:, :, 0:64], qf, qf.t[:, :, 0:64])
                cp(C, "pool", qh, qh.t[:, :, 64:128], qf, qf.t[:, :, 64:128])
                tt(C, "dve", kt_, kt_.t[:], kT_t, kT_t.t[:], enb, enb.t[:], ALU.mult)
                tt(C, "pool", kdc, kdc.t[:], kd_t, kd_t.t[:], er_, er_.t[:], ALU.mult)
                return dict(kd_t=kd_t, v_t=v_t, eb=eb, qf=qf, ql=ql, qh=qh, kt_=kt_, kdc=kdc)

            def tile_body(ti, B_):
                rows = slice(ti * 128, (ti + 1) * 128)
                kd_t, v_t, eb, qf, ql, qh, kt_, kdc = (B_[k_] for k_ in ('kd_t', 'v_t', 'eb', 'qf', 'ql', 'qh', 'kt_', 'kdc'))
                if d_ == 0:
                    ca, cb, qa, qb, la, lb_ = 0, 1, ql, qh, 63, 127
                else:
                    ca, cb, qa, qb, la, lb_ = 1, 0, qh, ql, 64, 0
                ra = slice(ca * 64, (ca + 1) * 64)
                rb = slice(cb * 64, (cb + 1) * 64)
                def head_chain(hd):
                    hc = slice(hd * 128, (hd + 1) * 128)
                    X, O_ = pX[hd % 2], pOs[hd % 2]
                    oa = O_.t[:, hd // 2, :]
                    mm(C, X, X.t[:, 0, :], kt_, kt_.t[:, hd, :], qf, qf.t[:, hd, :], True, True)
                    mm(C, O_, oa, qa, qa.t[:, hd, :], Sbf[hd], Sbf[hd].t[:], True, False)
                    mm(C, X, X.t[:, 1, :], kdc, kdc.t[ra, hc], v_t, v_t.t[ra, hc], True, True)
                    yield
                    am = atm.next()
                    tt(C, "dve", am, am.t[:], X, X.t[:, 0, :], Mi, Mi.t[:], ALU.mult)
                    stt(C, "dve", S32[hd], S32[hd].t[:], S32[hd], S32[hd].t[:], eb.t[:, hd, la:la + 1],
                        X, X.t[:, 1, :], ALU.mult, ALU.add, reads=[eb])
                    yield
                    cp(C, "act", Sbf[hd], Sbf[hd].t[:], S32[hd], S32[hd].t[:])
                    yield
                    mm(C, O_, oa, qb, qb.t[:, hd, :], Sbf[hd], Sbf[hd].t[:], False, False)
                    mm(C, O_, oa, am, am.t[:], v_t, v_t.t[:, hc], False, True)
                    mm(C, X, X.t[:, 2, :], kdc, kdc.t[rb, hc], v_t, v_t.t[rb, hc], True, True)
                    yield
                    stt(C, "dve", S32[hd], S32[hd].t[:], S32[hd], S32[hd].t[:], eb.t[:, hd, lb_:lb_ + 1],
                        X, X.t[:, 2, :], ALU.mult, ALU.add, reads=[eb])
                    yield
                    cp(C, "act", Sbf[hd], Sbf[hd].t[:], S32[hd], S32[hd].t[:])
                    yield

                for pair in ((0, 1), (2, 3)):
                    gens = [head_chain(hd) for hd in pair]
                    while gens:
                        for g_ in list(gens):
                            try:
                                next(g_)
                            except StopIteration:
                                gens.remove(g_)

                def ov(tile_ap, s_):
                    return tile_ap.rearrange("p (a s d) -> p a s d", s=2, d=128)[:, :, s_, :]

                if d_ == 0 and len(dirs) == 2:
                    o_ = osb.next()
                    for s_ in range(2):
                        cp(C, "act", o_, ov(o_.t[:], s_), pOs[s_], pOs[s_].t[:, 0:2, :])
                    P.dma("pool", Sc["ofwd"].t[rows, :], o_.t[:], reads=[o_], writes=[Sc["ofwd"].reg(ti)])
                    return
                t_ = tot.next()
                if len(dirs) == 2:
                    of_ = ofr.next()
                    P.dma("sp", of_.t[:], Sc["ofwd"].t[rows, :], reads=[Sc["ofwd"].reg(ti)], writes=[of_])
                    for s_ in range(2):
                        tt(C, "dve", t_, ov(t_.t[:], s_), pOs[s_], pOs[s_].t[:, 0:2, :], of_, ov(of_.t[:], s_), ALU.add)
                else:
                    for s_ in range(2):
                        cp(C, "dve", t_, ov(t_.t[:], s_), pOs[s_], pOs[s_].t[:, 0:2, :])
                if "dbgo" in Sc:
                    P.dma("pool", Sc["dbgo"].t[rows, :], t_.t[:], reads=[t_], writes=[Sc["dbgo"].reg(ti)])
                sg_ = sgr.next()
                P.dma("sp", sg_.t[:], Sc["sg"].t[rows, :], reads=[Sc["sg"].reg(ti)], writes=[sg_])
                sq = sqt.next()
                act(C, sq, sq.t[:], t_, t_.t[:], AF.Square)
                s4 = stat.next()
                P.op("dve", lambda e, s4=s4, sq=sq: e.tensor_reduce(out=s4.t[:, 0:4], in_=sq.t[:].rearrange("p (h d) -> p h d", d=128),
                                                              axis=AX.X, op=ALU.add), reads=[sq], writes=[s4])
                rsqrt_mean(C, s4, lambda s4=s4: s4.t[:, 0:4], 4, 1.0 / 128)
                t3 = t_.t[:].rearrange("p (h d) -> p h d", d=128)
                tt(C, "dve", t_, t3, t_, t3, s4, s4.t[:, 0:4].unsqueeze(2).to_broadcast([128, 4, 128]), ALU.mult)
                tt(C, "pool", t_, t3, t_, t3, gon, gon.t[:], ALU.mult)
                ob = onb.next()
                tt(C, "dve", ob, ob.t[:], t_, t_.t[:], sg_, sg_.t[:], ALU.mult)
                for hd in range(4):
                    tr(C, pTr, pTr.t[:, hd, :], ob, ob.t[:, hd * 128:(hd + 1) * 128], K["ident"])
                os_ = oTs.next()
                cp(C, "act", os_, os_.t[:], pTr, pTr.t[:, 0:4, :])
                P.dma("pool", Sc["hgoT"].t[:, :, rows].rearrange("h p t -> p h t"), os_.t[:], reads=[os_], writes=[Sc["hgoT"].reg(ti)])

            pend = pro(order[0])
            for idx_, ti in enumerate(order):
                nxt = pro(order[idx_ + 1]) if idx_ + 1 < len(order) else None
                tile_body(ti, pend)
                pend = nxt


def load_w(C, st, name, kchunks, ncols, q="pool"):
    w = C.alloc(st, [128, kchunks, ncols], BF16, name)
    src = C.ins[name].t.rearrange("(k p) n -> p k n", p=128)
    step = min(kchunks, max(1, 4096 // ncols))
    for k0 in range(0, kchunks, step):
        C.P.dma(q, w.t[:, k0:k0 + step, :], src[:, k0:k0 + step, :], reads=[C.ins[name]], writes=[w])
    return w


def norm_transpose(C, K, xt, gain, stat, junk, hb, pt, dst, dst_ap):
    s_ = stat
    act(C, junk, junk.t[:], xt, xt.t[:], AF.Square, accum_out=s_.t[:, 0:1], extra_w=[s_])
    rsqrt_mean(C, s_, lambda: s_.t[:, 0:1], 1, 1.0 / D)
    stt(C, "dve", hb, hb.t[:], xt, xt.t[:], s_.t[:, 0:1], gain, gain.t[:], ALU.mult, ALU.mult, reads=[s_])
    for k in range(8):
        tr(C, pt, pt.t[:, k, :], hb, hb.t[:, k * 128:(k + 1) * 128], K["ident"])
    cp(C, "act", dst, dst_ap, pt, pt.t[:])


def phase_d(C, K, ngroups=8):
    nc, P = C.nc, C.P
    Sc = C.scr
    I = C.ins
    with contextlib.ExitStack() as st:
        wua = load_w(C, st, "w_up_att", 4, 1024)
        wuh = load_w(C, st, "w_up_hg", 4, 1024)
        wo = load_w(C, st, "w_out", 8, 1024)
        gffn = C.alloc(st, [128, 1024], F32, "gffn")
        P.dma("sp", gffn.t[:], I["norm_ffn"].t.to_broadcast([128, 1024]), writes=[gffn])
        aTr = C.ring(st, 2, [128, 4, 512], BF16, "aT")
        hTr_ = C.ring(st, 2, [128, 4, 512], BF16, "hgT")
        gtr = C.ring(st, 2, [128, 16, 512], BF16, "gts")
        pya = C.ring(st, 2, [128, 512], F32, "pya", psum=True)
        pyh = C.ring(st, 2, [128, 512], F32, "pyh", psum=True)
        px = C.ring(st, 2, [128, 512], F32, "px", psum=True)
        pt = C.ring(st, 2, [128, 8, 128], BF16, "ptd", psum=True)
        t1r = C.ring(st, 2, [128, 512], F32, "t1")
        t2r = C.ring(st, 2, [128, 512], F32, "t2")
        mTr = C.ring(st, 2, [128, 8, 512], BF16, "mT")
        xr = C.ring(st, 2, [128, 1024], F32, "xtd")
        x1r = C.ring(st, 3, [128, 1024], F32, "x1t")
        junk = C.alloc(st, [128, 1024], BF16, "junkd")
        stat = C.ring(st, 2, [128, 8], F32, "statd")
        hbr = C.ring(st, 2, [128, 1024], BF16, "hbd")
        h2s = C.ring(st, 2, [128, 8, 128], BF16, "h2s")
        for g in range(ngroups):
            cols = slice(g * 512, (g + 1) * 512)
            aT, hT, gt = aTr.next(), hTr_.next(), gtr.next()
            P.dma("sp", aT.t[:], Sc["attoT"].t[:, :, cols].rearrange("r p t -> p r t"),
                  reads=[Sc["attoT"].reg((pr, g)) for pr in range(4)], writes=[aT])
            P.dma("sp", hT.t[:], Sc["hgoT"].t[:, :, cols].rearrange("r p t -> p r t"),
                  reads=Sc["hgoT"].regl(range(4 * g, 4 * g + 4)), writes=[hT])
            P.dma("sp", gt.t[:], Sc["gtsT"].t[:, :, cols].rearrange("r p t -> p r t"),
                  reads=[Sc["gtsT"].reg((c, g)) for c in range(16)], writes=[gt])
            mT = mTr.next()
            for m_ in range(8):
                ms = slice(m_ * 128, (m_ + 1) * 128)
                ya, yh = pya.next(), pyh.next()
                for kc in range(4):
                    mm(C, ya, ya.t[:], wua, wua.t[:, kc, ms], aT, aT.t[:, kc, :], kc == 0, kc == 3)
                for kc in range(4):
                    mm(C, yh, yh.t[:], wuh, wuh.t[:, kc, ms], hT, hT.t[:, kc, :], kc == 0, kc == 3)
                t1, t2 = t1r.next(), t2r.next()
                tt(C, "dve", t1, t1.t[:], ya, ya.t[:], gt, gt.t[:, m_, :], ALU.mult)
                tt(C, "dve", t2, t2.t[:], yh, yh.t[:], gt, gt.t[:, 8 + m_, :], ALU.mult)
                tt(C, "pool", mT, mT.t[:, m_, :], t1, t1.t[:], t2, t2.t[:], ALU.add)
            def part1(j):
                ti = g * 4 + j
                rows = slice(ti * 128, (ti + 1) * 128)
                xt = xr.next()
                P.dma("sp", xt.t[:], I["x"].t[rows, :], reads=[I["x"]], writes=[xt])
                x1 = x1r.next()
                for hf in range(2):
                    hs = slice(hf * 512, (hf + 1) * 512)
                    p_ = px.next()
                    for m_ in range(8):
                        mm(C, p_, p_.t[:], mT, mT.t[:, m_, j * 128:(j + 1) * 128], wo, wo.t[:, m_, hs], m_ == 0, m_ == 7)
                    tt(C, "dve", x1, x1.t[:, hs], p_, p_.t[:], xt, xt.t[:, hs], ALU.add)
                P.dma("pool", Sc["x1"].t[rows, :], x1.t[:], reads=[x1], writes=[Sc["x1"].reg(ti)])
                return x1

            def part2(j, x1):
                ti = g * 4 + j
                hs_ = h2s.next()
                norm_transpose(C, K, x1, gffn, stat.next(), junk, hbr.next(), pt.next(), hs_, hs_.t[:])
                P.dma("pool", Sc["h2T"].t[ti], hs_.t[:], reads=[hs_], writes=[Sc["h2T"].reg(ti)])

            pend = part1(0)
            for j in range(4):
                nxt = part1(j + 1) if j + 1 < 4 else None
                part2(j, pend)
                pend = nxt


def phase_e1(C, K, ngroups=16):
    nc, P = C.nc, C.P
    Sc = C.scr
    I = C.ins
    with contextlib.ExitStack() as st:
        wq = load_w(C, st, "peer_wq", 8, 2048)
        skT = C.alloc(st, [128, 16, 128], BF16, "skT")
        P.dma("pool", skT.t[:], I["skT"].t, reads=[I["skT"]], writes=[skT])
        io_f = C.alloc(st, [128, 128], F32, "io_f")
        P.op("pool", lambda e: e.iota(io_f.t[:], pattern=[[1, 128]], base=0, channel_multiplier=0,
                                      allow_small_or_imprecise_dtypes=True), writes=[io_f])
        io_b = C.alloc(st, [128, 128], BF16, "io_b")
        cp(C, "dve", io_b, io_b.t[:], io_f, io_f.t[:])
        io_rep = C.alloc(st, [128, 128, 16], BF16, "io_rep")
        cp(C, "dve", io_rep, io_rep.t[:], io_f, io_f.t[:].unsqueeze(2).to_broadcast([128, 128, 16]))
        h2r = C.ring(st, 2, [128, 8, 128], BF16, "h2e")
        pq = C.ring(st, 2, [128, 4, 128], F32, "pq", psum=True)
        psc = C.ring(st, 2, [128, 4, 128], F32, "psc", psum=True)
        pIG = C.alloc(st, [128, 8, 128], BF16, "pIG", psum=True)
        pG = C.ring(st, 3, [128, 4, 128], F32, "pG", psum=True)
        qpT = C.ring(st, 2, [128, 16, 128], BF16, "qpT")
        s_all = C.ring(st, 2, [128, 16, 128], F32, "s_all")
        tmp128 = C.ring(st, 2, [128, 128], F32, "tmp128")
        v16 = C.ring(st, 2, [128, 16, 16], F32, "v16")
        i16 = C.ring(st, 2, [128, 16, 16], U32, "i16")
        i16f = C.ring(st, 2, [128, 16, 16], F32, "i16f")
        cand = C.ring(st, 1, [128, 8, 256], F32, "cand")
        tmp256 = C.ring(st, 2, [128, 256], F32, "tmp256")
        tsv = C.ring(st, 2, [128, 8, 16], F32, "tsv")
        pos = C.ring(st, 2, [128, 8, 16], U32, "pos")
        k12i = C.ring(st, 2, [128, 2, 128], I32, "k12i")
        k12f = C.ring(st, 2, [128, 2, 128], F32, "k12f")
        eq = C.ring(st, 1, [128, 128, 16], F32, "eq")
        IG = C.ring(st, 2, [128, 3, 128], BF16, "IG")
        IGf = C.ring(st, 2, [128, 3, 128], F32, "IGf")
        IGT = C.ring(st, 2, [128, 3, 128], BF16, "IGT")
        ex = C.ring(st, 2, [128, 8, 16], F32, "ex")
        st8 = C.ring(st, 2, [128, 8], F32, "st8")
        A4 = C.ring(st, 3, [128, 16, 128], BF16, "A4")
        B4 = C.ring(st, 3, [128, 16, 128], BF16, "B4")
        Gst = C.ring(st, 1, [128, 128, 256], BF16, "Gst")
        est = {}

        def stageXc(grp, j2):
            ti = grp * 2 + j2
            h2 = h2r.next()
            P.dma("sp", h2.t[:], Sc["h2T"].t[ti], reads=[Sc["h2T"].reg(ti)], writes=[h2])
            qp, sa = qpT.next(), s_all.next()
            for c4 in range(4):
                p_ = pq.next()
                for cc in range(4):
                    cq = c4 * 4 + cc
                    for k in range(8):
                        mm(C, p_, p_.t[:, cc, :], wq, wq.t[:, k, cq * 128:(cq + 1) * 128], h2, h2.t[:, k, :], k == 0, k == 7)
                cp(C, "act", qp, qp.t[:, c4 * 4:(c4 + 1) * 4, :], p_, p_.t[:])
            for c4 in range(4):
                p_ = psc.next()
                for cc in range(4):
                    cq = c4 * 4 + cc
                    mm(C, p_, p_.t[:, cc, :], qp, qp.t[:, cq, :], skT, skT.t[:, cq, :], True, True)
                cp(C, "act", sa, sa.t[:, c4 * 4:(c4 + 1) * 4, :], p_, p_.t[:])
            if "dbgs" in Sc:
                P.dma("pool", Sc["dbgs"].t[ti], sa.t[:], reads=[sa], writes=[Sc["dbgs"].reg(ti)])
            est["sa", grp, j2] = sa

        def stageXt(grp, j2):
            ti = grp * 2 + j2
            sa = est.pop(("sa", grp, j2))
            v_, i_ = v16.next(), i16.next()

            def top16(src_t, src_ap, vdst_t, vdst_ap, idst_t, idst_ap, tmp):
                P.op("dve", lambda e: e.max(out=vdst_ap[:, 0:8], in_=src_ap), reads=[src_t], writes=[vdst_t])
                P.op("dve", lambda e: e.match_replace(out=tmp.t[:], in_to_replace=vdst_ap[:, 0:8], in_values=src_ap,
                                                      imm_value=-1e30), reads=[src_t, vdst_t], writes=[tmp])
                P.op("dve", lambda e: e.max(out=vdst_ap[:, 8:16], in_=tmp.t[:]), reads=[tmp, vdst_t], writes=[vdst_t])
                P.op("dve", lambda e: e.max_index(out=idst_ap[:, 0:8], in_max=vdst_ap[:, 0:8], in_values=src_ap),
                     reads=[src_t, vdst_t], writes=[idst_t])
                P.op("dve", lambda e: e.max_index(out=idst_ap[:, 8:16], in_max=vdst_ap[:, 8:16], in_values=src_ap),
                     reads=[src_t, vdst_t, idst_t], writes=[idst_t])

            for cq in range(16):
                top16(sa, sa.t[:, cq, :], v_, v_.t[:, cq, :], i_, i_.t[:, cq, :], tmp128.next())
            if_ = i16f.next()
            cp(C, "dve", if_, if_.t[:], i_, i_.t[:])
            cd = cand.next()
            vv = v_.t[:].rearrange("p (h a) k -> p h a k", a=2)
            tt(C, "dve", cd, cd.t[:].rearrange("p h (a b) -> p h a b", b=16),
               v_, vv[:, :, 0, :].unsqueeze(3).to_broadcast([128, 8, 16, 16]),
               v_, vv[:, :, 1, :].unsqueeze(2).to_broadcast([128, 8, 16, 16]), ALU.add)
            ts_, ps_ = tsv.next(), pos.next()
            for h in range(8):
                top16(cd, cd.t[:, h, :], ts_, ts_.t[:, h, :], ps_, ps_.t[:, h, :], tmp256.next())
            ki, kf = k12i.next(), k12f.next()
            posf = ps_.t[:].rearrange("p h k -> p (h k)").bitcast(I32)
            P.op("dve", lambda e, ki=ki, posf=posf: e.tensor_single_scalar(out=ki.t[:, 0, :], in_=posf, scalar=4, op=ALU.arith_shift_right),
                 reads=[ps_], writes=[ki])
            P.op("dve", lambda e, ki=ki, posf=posf: e.tensor_single_scalar(out=ki.t[:, 1, :], in_=posf, scalar=15, op=ALU.bitwise_and),
                 reads=[ps_, ki], writes=[ki])
            cp(C, "dve", kf, kf.t[:], ki, ki.t[:])
            ig = IGf.next()
            iv = if_.t[:].rearrange("p (h a) k -> p h a k", a=2)
            for a in range(2):
                e_ = eq.next()
                tt(C, "dve", e_, e_.t[:], kf, kf.t[:, a, :].unsqueeze(2).to_broadcast([128, 128, 16]),
                   io_f, io_f.t[:, 0:16].unsqueeze(1).to_broadcast([128, 128, 16]), ALU.is_equal)
                e4 = e_.t[:].rearrange("p (h k) c -> p h k c", h=8)
                tt(C, "dve", e_, e4, e_, e4, if_, iv[:, :, a, :].unsqueeze(2).to_broadcast([128, 8, 16, 16]), ALU.mult)
                P.op("dve", lambda e, e_=e_, ig=ig, a=a: e.tensor_reduce(out=ig.t[:, a, :], in_=e_.t[:], axis=AX.X, op=ALU.add),
                     reads=[e_], writes=[ig])
            x_ = ex.next()
            tt(C, "dve", x_, x_.t[:], ts_, ts_.t[:], ts_, ts_.t[:, :, 0:1].to_broadcast([128, 8, 16]), ALU.subtract)
            act(C, x_, x_.t[:], x_, x_.t[:], AF.Exp)
            s8 = st8.next()
            P.op("dve", lambda e, s8=s8, x_=x_: e.tensor_reduce(out=s8.t[:], in_=x_.t[:], axis=AX.X, op=ALU.add), reads=[x_], writes=[s8])
            P.op("dve", lambda e, s8=s8: e.reciprocal(out=s8.t[:], in_=s8.t[:]), reads=[s8], writes=[s8])
            tt(C, "dve", ig, ig.t[:, 2, :].rearrange("p (h k) -> p h k", h=8), x_, x_.t[:],
               s8, s8.t[:].unsqueeze(2).to_broadcast([128, 8, 16]), ALU.mult)
            igf_ = ig
            ig = IG.next()
            cp(C, "dve", ig, ig.t[:], igf_, igf_.t[:])
            if "dbgig" in Sc:
                P.dma("pool", Sc["dbgig"].t[ti], ig.t[:], reads=[ig], writes=[Sc["dbgig"].reg(ti)])
            for a in range(3):
                tr(C, pIG, pIG.t[:, a, :], ig, ig.t[:, a, :], K["ident"])
            igt = IGT.next()
            cp(C, "dve", igt, igt.t[:], pIG, pIG.t[:, 0:3, :])
            est["igt", grp, j2] = igt

        def stageY(grp, j2):
            if j2 == 0:
                est["G", grp] = Gst.next()
            G_ = est["G", grp]
            igt = est.pop(("igt", grp, j2))
            TB = 16
            for b16 in range(128 // TB):
                a4, bb4 = A4.next(), B4.next()
                tsl = slice(b16 * TB, (b16 + 1) * TB)
                av = a4.t[:].rearrange("p t i -> p (t i)").rearrange("p (i t) -> p i t", t=TB)
                bv = bb4.t[:].rearrange("p t i -> p (t i)").rearrange("p (i t) -> p i t", t=TB)
                tt(C, "dve", a4, av, io_rep, io_rep.t[:], igt, igt.t[:, 0, tsl].unsqueeze(1).to_broadcast([128, 128, TB]), ALU.is_equal)
                tt(C, "dve", bb4, bv, io_rep, io_rep.t[:], igt, igt.t[:, 1, tsl].unsqueeze(1).to_broadcast([128, 128, TB]), ALU.is_equal)
                tt(C, "pool", a4, av, a4, av, igt, igt.t[:, 2, tsl].unsqueeze(1).to_broadcast([128, 128, TB]), ALU.mult)
                for q4 in range(TB // 4):
                    pg = pG.next()
                    for q_ in range(4):
                        mm(C, pg, pg.t[:, q_, :], a4, av[:, :, q4 * 4 + q_], bb4, bv[:, :, q4 * 4 + q_], True, True)
                    t0 = j2 * 128 + b16 * TB + q4 * 4
                    cp(C, "act", G_, G_.t[:, :, t0:t0 + 4].rearrange("p i t -> p t i"), pg, pg.t[:])
            if j2 == 1:
                hc = slice((grp % 2) * 256, (grp % 2 + 1) * 256)
                for i0 in range(0, 128, 32):
                    P.dma("pool", Sc["G"].t[grp // 2, :, i0:i0 + 32, hc], G_.t[:, i0:i0 + 32, :], reads=[G_], writes=[Sc["G"].reg(grp)])

        tl = [(grp, j2) for grp in range(ngroups) for j2 in range(2)]
        for it in range(len(tl) + 2):
            if it < len(tl):
                stageXc(*tl[it])
            if 1 <= it < len(tl) + 1:
                stageXt(*tl[it - 1])
            if it >= 2:
                stageY(*tl[it - 2])


def phase_e0(C, K):
    P = C.P
    jobs = []
    for i2 in range(128):
        jobs.append(lambda i2=i2: P.dma("pool", C.scr["uTb"].t[i2], C.ins["uT"].t[i2], reads=[C.ins["uT"]], writes=[C.scr["uTb"].reg(i2)]))
        jobs.append(lambda i2=i2: P.dma("pool", C.scr["vLb"].t[i2], C.ins["vL"].t[i2], reads=[C.ins["vL"]], writes=[C.scr["vLb"].reg(i2)]))
    return jobs


def phase_e2(C, K, ngroups=8, ni2=128):
    nc, P = C.nc, C.P
    Sc = C.scr
    with contextlib.ExitStack() as st:
        h2r = C.ring(st, 1, [128, 8, 512], BF16, "h2g")
        po = [C.alloc(st, [128, 512], F32, "po", psum=True) for _ in range(4)]
        par = C.ring(st, 4, [128, 512], F32, "pa", psum=True)
        uch = C.ring(st, 3, [128, 2, 8, 128], BF16, "uch")
        vch = C.ring(st, 4, [128, 2, 512], BF16, "vch")
        gch = C.ring(st, 3, [128, 2, 512], BF16, "gch")
        sqr = C.ring(st, 3, [128, 512], F32, "sqe")
        t2r = C.ring(st, 3, [128, 512], F32, "t2e")
        sgr = C.ring(st, 3, [128, 512], BF16, "sge")
        agr = C.ring(st, 3, [128, 512], BF16, "age")
        Wall = C.alloc(st, [128, ni2, 512], BF16, "Wall")
        xs = C.ring(st, 2, [128, 512], F32, "xs")
        x1h = C.ring(st, 2, [128, 512], F32, "x1h")
        LAG = 3
        state = {}

        def s1(grp, i2):
            h2 = state["h2"]
            if i2 % 2 == 0:
                u_, v_, g_ = uch.next(), vch.next(), gch.next()
                P.dma("sp", u_.t[:], Sc["uTb"].t[i2:i2 + 2].rearrange("i p k c -> p i k c"),
                      reads=Sc["uTb"].regl([i2, i2 + 1]), writes=[u_])
                P.dma("sp", v_.t[:], Sc["vLb"].t[i2:i2 + 2, :, 0:512].rearrange("i p d -> p i d"),
                      reads=Sc["vLb"].regl([i2, i2 + 1]), writes=[v_])
                P.dma("sp", g_.t[:], Sc["G"].t[grp, :, i2:i2 + 2, :], reads=Sc["G"].regl([2 * grp, 2 * grp + 1]), writes=[g_])
                state["uvg"] = (u_, v_, g_)
            u_, v_, g_ = state["uvg"]
            e_ = i2 % 2
            pa = par.next()
            for k in range(8):
                mm(C, pa, pa.t[:], u_, u_.t[:, e_, k, :], h2, h2.t[:, k, :], k == 0, k == 7)
            sq, t2, sg, ag = sqr.next(), t2r.next(), sgr.next(), agr.next()
            Wb = Wall.reg(i2)
            act(C, sq, sq.t[:], pa, pa.t[:], AF.Square, scale=0.21145921592448583)
            stt(C, "dve", t2, t2.t[:], sq, sq.t[:], 1.0, pa, pa.t[:], ALU.add, ALU.mult)
            tt(C, "dve", ag, ag.t[:], pa, pa.t[:], g_, g_.t[:, e_, :], ALU.mult)
            act(C, sg, sg.t[:], t2, t2.t[:], AF.Sigmoid, scale=1.5957691216057308)
            P.op("pool", lambda e: e.tensor_tensor(out=Wall.t[:, i2, :], in0=sg.t[:], in1=ag.t[:], op=ALU.mult),
                 reads=[sg, ag], writes=[Wb])
            state["v", i2] = (v_, e_)

        def s2(grp, i2, half):
            v_, e_ = state.pop(("v", i2)) if half == 0 else state.pop(("v2", i2))
            for j in range(4):
                P.op("pe", lambda e, j=j: e.matmul(po[j].t[:], lhsT=Wall.t[:, i2, j * 128:(j + 1) * 128], rhs=v_.t[:, e_, :],
                                                    start=(i2 == 0), stop=(i2 == ni2 - 1)),
                     reads=[Wall.reg(i2), v_], writes=[po[j]], pe_accum=True)

        def evac(grp, half):
            hs = slice(half * 512, (half + 1) * 512)
            for j in range(4):
                ti = grp * 4 + j
                rows = slice(ti * 128, (ti + 1) * 128)
                x1_ = x1h.next()
                P.dma("sp", x1_.t[:], Sc["x1"].t[rows, hs], reads=[Sc["x1"].reg(ti)], writes=[x1_])
                x_ = xs.next()
                tt(C, "dve", x_, x_.t[:], po[j], po[j].t[:], x1_, x1_.t[:], ALU.add)
                P.dma("pool", Sc["x2"].t[rows, hs], x_.t[:], reads=[x_], writes=[Sc["x2"].reg((ti, half))])

        for grp in range(ngroups):
            h2 = h2r.next()
            for j in range(4):
                ti = grp * 4 + j
                P.dma("sp", h2.t[:, :, j * 128:(j + 1) * 128], Sc["h2T"].t[ti], reads=[Sc["h2T"].reg(ti)], writes=[h2])
            state["h2"] = h2
            for it in range(ni2 + LAG):
                if it < ni2:
                    s1(grp, it)
                if it >= LAG:
                    s2(grp, it - LAG, 0)
            evac(grp, 0)
            for it in range(ni2 + LAG):
                if it < ni2:
                    if it % 2 == 0:
                        v_ = vch.next()
                        P.dma("sp", v_.t[:], Sc["vLb"].t[it:it + 2, :, 512:1024].rearrange("i p d -> p i d"),
                              reads=Sc["vLb"].regl([it, it + 1]), writes=[v_])
                        state["vp"] = v_
                    state["v2", it] = (state["vp"], it % 2)
                if it >= LAG:
                    s2(grp, it - LAG, 1)
            evac(grp, 1)


def phase_f(C, K, ntiles=NT):
    nc, P = C.nc, C.P
    Sc = C.scr
    I = C.ins
    with contextlib.ExitStack() as st:
        wg = load_w(C, st, "ple_gate", 8, 1024)
        wp = load_w(C, st, "ple_proj", 2, 1024)
        gple = C.alloc(st, [128, 1024], F32, "gple")
        P.dma("sp", gple.t[:], I["norm_ple"].t.to_broadcast([128, 1024]), writes=[gple])
        x2r = C.ring(st, 3, [128, 1024], F32, "x2f")
        pr_ = C.ring(st, 2, [128, 256], F32, "pf32")
        pbr = C.ring(st, 2, [128, 256], BF16, "pbf")
        junk = C.alloc(st, [128, 1024], BF16, "junkf")
        stat = C.ring(st, 2, [128, 8], F32, "statf")
        hbr = C.ring(st, 2, [128, 1024], BF16, "hbf")
        pt = C.ring(st, 2, [128, 8, 128], BF16, "ptf", psum=True)
        ptp = C.alloc(st, [128, 8, 128], BF16, "ptp", psum=True)
        h3r = C.ring(st, 3, [128, 8, 128], BF16, "h3T")
        pTr = C.ring(st, 3, [128, 2, 128], BF16, "pT")
        pgr = C.ring(st, 2, [128, 512], F32, "pgate", psum=True)
        ppr = C.ring(st, 2, [128, 512], F32, "pproj", psum=True)
        sgr = C.ring(st, 2, [128, 512], F32, "sgf")
        tr_ = C.ring(st, 2, [128, 512], F32, "tf")
        outr = C.ring(st, 2, [128, 1024], F32, "outf")
        def pro(ti):
            rows = slice(ti * 128, (ti + 1) * 128)
            x2 = x2r.next()
            P.dma("sp", x2.t[:], Sc["x2"].t[rows, :], reads=[Sc["x2"].reg((ti, 0)), Sc["x2"].reg((ti, 1))], writes=[x2])
            pf = pr_.next()
            P.dma("sp", pf.t[:], I["p"].t[rows, :], reads=[I["p"]], writes=[pf])
            pb = pbr.next()
            cp(C, "pool", pb, pb.t[:], pf, pf.t[:])
            for k in range(2):
                tr(C, ptp, ptp.t[:, k, :], pb, pb.t[:, k * 128:(k + 1) * 128], K["ident"])
            pT = pTr.next()
            cp(C, "act", pT, pT.t[:], ptp, ptp.t[:, 0:2, :])
            h3 = h3r.next()
            norm_transpose(C, K, x2, gple, stat.next(), junk, hbr.next(), pt.next(), h3, h3.t[:])
            return x2, pT, h3

        def body(ti, x2, pT, h3):
            rows = slice(ti * 128, (ti + 1) * 128)
            o_ = outr.next()
            for hf in range(2):
                hs = slice(hf * 512, (hf + 1) * 512)
                pg, pp = pgr.next(), ppr.next()
                for k in range(8):
                    mm(C, pg, pg.t[:], h3, h3.t[:, k, :], wg, wg.t[:, k, hs], k == 0, k == 7)
                for k in range(2):
                    mm(C, pp, pp.t[:], pT, pT.t[:, k, :], wp, wp.t[:, k, hs], k == 0, k == 1)
                sg, t_ = sgr.next(), tr_.next()
                act(C, sg, sg.t[:], pg, pg.t[:], AF.Sigmoid)
                tt(C, "dve", t_, t_.t[:], pp, pp.t[:], sg, sg.t[:], ALU.mult)
                tt(C, "pool", o_, o_.t[:, hs], t_, t_.t[:], x2, x2.t[:, hs], ALU.add)
            P.dma("sp", C.y.t[rows, :], o_.t[:], reads=[o_], writes=[C.y.reg(ti)])

        pend = pro(0)
        for ti in range(ntiles):
            nxt = pro(ti + 1) if ti + 1 < ntiles else None
            body(ti, *pend)
            pend = nxt


def declare(C):
    C.din("x", [S, D])
    C.din("p", [S, 256])
    C.din("rope", [2, S, 32])
    C.din("norm_mix", [1, D])
    C.din("w_in", [D, 5376])
    C.din("q_norm", [1, 64])
    C.din("k_norm", [1, 64])
    C.din("hg_lb_raw", [2, 2, 512])
    C.din("hg_out_norm", [1, 128])
    C.din("w_up_att", [512, D])
    C.din("w_up_hg", [512, D])
    C.din("w_out", [D, D])
    C.din("norm_ffn", [1, D])
    C.din("peer_wq", [D, 2048])
    C.din("skT", [128, 16, 128])
    C.din("uT", [128, 128, 8, 128])
    C.din("vL", [128, 128, D])
    C.din("norm_ple", [1, D])
    C.din("ple_gate", [D, D])
    C.din("ple_proj", [256, D])
    sc = C.scratch
    sc("hqT", [4, 128, S], BF16)
    sc("kT", [2, 4, 128, S], BF16)
    sc("gtsT", [16, 128, S], BF16)
    sc("qT", [4, 128, S], BF16)
    sc("kTa", [2, 128, S], BF16)
    sc("v", [NT, 128, 2, 128], BF16)
    sc("g", [2, S, 512], F32)
    sc("k", [2, S, 512], BF16)
    sc("hi", [S, 512], BF16)
    sc("sg", [S, 512], BF16)
    sc("attoT", [4, 128, S], BF16)
    sc("ofwd", [S, 512], F32)
    sc("uTb", [128, 128, 8, 128], BF16)
    sc("vLb", [128, 128, D], BF16)
    sc("x2", [S, D], F32)
    sc("G", [8, 128, 128, 512], BF16)
    if "dbgs" in C.dbg:
        sc("dbgs", [NT, 128, 16, 128], F32)
        sc("dbgig", [NT, 128, 3, 128], BF16)
    sc("x1", [S, D], F32)
    sc("h2T", [NT, 128, 8, 128], BF16)
    sc("hgoT", [4, 128, S], BF16)
    if "dbgo" in C.dbg:
        sc("dbgo", [S, 512], F32)
    if "dbgacc" in C.dbg:
        sc("dbgacc", [128, 512], F32)
        sc("dbgp", [2, 128, 512], BF16)
        sc("dbgvs", [128, NT, 256], BF16)


def build(dbg=(), phases="A", ntiles=NT, **kw):
    nc = bass.Bass("TRN2", target_bir_lowering=False)
    C = Ctx(nc, dbg)
    declare(C)
    C.y = T(nc.dram_tensor("y", [S, D], F32, kind="ExternalOutput").ap(), "y")
    C.outs.append(C.y)
    with contextlib.ExitStack() as st:
        K = build_consts(C, st)
        if "A" in phases:
            phase_a(C, K, ntiles)
        if "C" in phases:
            C.P.barrier()
            phase_c(C, K, kw.get("c_tiles", NT), kw.get("c_dirs", (0, 1)))
        C.P.barrier()
        bg = phase_e0(C, K) if "2" in phases else []
        if "B" in phases:
            phase_b(C, K, kw.get("b_groups", 8), kw.get("hhs", (0, 1)), bg)
        else:
            for job in bg:
                job()
        if "D" in phases:
            C.P.barrier()
            phase_d(C, K, kw.get("d_groups", 8))
        if "E" in phases:
            C.P.barrier()
            phase_e1(C, K, kw.get("e1_groups", 16))
        if "2" in phases:
            C.P.barrier()
            phase_e2(C, K, kw.get("e2_groups", 8), kw.get("ni2", 128))
        if "F" in phases:
            C.P.barrier()
            phase_f(C, K, kw.get("f_tiles", NT))
        fin = []
        for t in C.outs:
            fin.append(t.b)
            fin.extend(t.regs.values())
        C.P.emit(final_bufs=fin)
    return nc, C


def _rope_tables():
    t = np.arange(S)
    row = (t // 64).astype(np.float32)
    col = (t % 64).astype(np.float32)
    inv = (np.float32(10000.0) ** (-np.arange(16, dtype=np.float32) / 16)).astype(np.float32)
    ang = np.concatenate([row[:, None] * inv, col[:, None] * inv], 1).astype(np.float32)
    return np.stack([np.cos(ang), np.sin(ang)], 0).astype(np.float32)


def _in_maps(inp, ncores):
    shared = {}
    shared["rope"] = _rope_tables()
    for k in ["norm_mix", "q_norm", "k_norm", "hg_out_norm", "norm_ffn", "norm_ple"]:
        shared[k] = np.ascontiguousarray(np.asarray(inp[k], np.float32)[0][None])
    for k in ["w_in", "w_up_att", "w_up_hg", "w_out", "peer_wq", "ple_gate", "ple_proj"]:
        shared[k] = np.ascontiguousarray(np.asarray(inp[k], np.float32)[0])
    shared["hg_lb_raw"] = np.ascontiguousarray(np.asarray(inp["hg_lb_raw"], np.float32))
    sk = np.asarray(inp["peer_subkeys"], np.float32)[0]
    shared["skT"] = np.ascontiguousarray(sk.transpose(3, 0, 1, 2).reshape(128, 16, 128))
    u = np.asarray(inp["peer_u"], np.float32)[0].reshape(128, 128, 8, 128)
    shared["uT"] = np.ascontiguousarray(u.transpose(1, 3, 2, 0))
    v = np.asarray(inp["peer_v"], np.float32)[0].reshape(128, 128, D)
    shared["vL"] = np.ascontiguousarray(v.transpose(1, 0, 2))
    x = np.asarray(inp["x"], np.float32)
    p = np.asarray(inp["p"], np.float32)
    maps = []
    for b in range(ncores):
        m = dict(shared)
        m["x"] = np.ascontiguousarray(x[b])
        m["p"] = np.ascontiguousarray(p[0, b])
        maps.append(m)
    return maps


_NC_CACHE = {}


def kernel(**inputs):
    ncores = 8
    if "nc" not in _NC_CACHE:
        _NC_CACHE["nc"] = build(phases="ABCDE2F")[0]
    nc = _NC_CACHE["nc"]
    maps = _in_maps(inputs, ncores)
    res = run_bass_kernel_spmd(nc, maps, core_ids=list(range(ncores)))
    out = np.stack([np.asarray(res.results[b]["y"], np.float32) for b in range(ncores)], 0)
    return out
```

```python
import contextlib
import numpy as np
import concourse.bass as bass
import concourse.mybir as mybir
from concourse.bass_utils import run_bass_kernel_spmd

F32 = mybir.dt.float32
BF16 = mybir.dt.bfloat16
I32 = mybir.dt.int32
U32 = mybir.dt.uint32
ALU = mybir.AluOpType
AF = mybir.ActivationFunctionType
AX = mybir.AxisListType

S = 4096
D = 1024
NT = S // 128
EPS = 1e-6
EPOCH = 16000
NDMA_SLOTS = 48


class Buf:
    __slots__ = ("name", "last_w", "readers")

    def __init__(self, name=""):
        self.name = name
        self.last_w = None
        self.readers = {}


class T:
    __slots__ = ("t", "b", "regs")

    def __init__(self, t, name=""):
        self.t = t
        self.b = Buf(name)
        self.regs = {}

    def reg(self, key):
        if key not in self.regs:
            self.regs[key] = Buf(f"{self.b.name}[{key}]")
        return self.regs[key]

    def regl(self, keys):
        return [self.reg(k) for k in keys]


class Prog:
    ENGS = ("pe", "act", "dve", "pool", "sp")

    def __init__(self, nc):
        self.nc = nc
        self.ops = {e: [] for e in self.ENGS}
        self.count = {e: 0 for e in self.ENGS}
        self.known = {e: {} for e in self.ENGS}
        self.dma_count = [0] * NDMA_SLOTS
        self.dma_rr = {"hw": 0, "sw": 0}
        self.n_instr = 0
        self.pending = {e: [] for e in self.ENGS}

    def barrier(self):
        prods = [(e, self.count[e]) for e in self.ENGS if self.count[e] > 0]
        prods += [(("dma", s), c) for s, c in enumerate(self.dma_count) if c > 0]
        for e in self.ENGS:
            kn = self.known[e]
            for p, c in prods:
                if kn.get(p, 0) < c:
                    kn[p] = c
                    self.pending[e].append((p, c))

    def _deps(self, eng, reads, writes, pe_accum=False):
        deps = {}

        def add(p, s):
            if deps.get(p, 0) < s:
                deps[p] = s

        for b in reads:
            if b.last_w is not None:
                add(*b.last_w)
        for b in writes:
            if b.last_w is not None:
                if not (pe_accum and b.last_w[0] == "pe" and eng == "pe"):
                    add(*b.last_w)
            for p, s in b.readers.items():
                add(p, s)
        waits = []
        kn = self.known[eng]
        for p, s in deps.items():
            if kn.get(p, 0) < s:
                kn[p] = s
                waits.append((p, s))
        return waits

    def _commit(self, prod, seq, reads, writes):
        for b in writes:
            b.last_w = (prod, seq)
            b.readers = {}
        for b in reads:
            b.readers[prod] = max(b.readers.get(prod, 0), seq)

    @staticmethod
    def _bufs(xs):
        out = []
        for x in xs:
            if isinstance(x, T):
                out.append(x.b)
            elif isinstance(x, (list, tuple)):
                out.extend(Prog._bufs(x))
            else:
                out.append(x)
        return out

    def op(self, eng, fn, reads=(), writes=(), pe_accum=False):
        reads = self._bufs(reads)
        writes = self._bufs(writes)
        waits = self._deps(eng, reads, writes, pe_accum)
        waits = [w for w in self.pending[eng] if w not in waits] + waits
        self.pending[eng] = []
        self.count[eng] += 1
        seq = self.count[eng]
        self.ops[eng].append((waits, fn, (eng, seq)))
        self._commit(eng, seq, reads, writes)
        self.n_instr += 1

    def dma(self, q, out, in_, reads=(), writes=(), **kw):
        reads = self._bufs(reads)
        writes = self._bufs(writes)
        waits = self._deps(q, reads, writes)
        waits = [w for w in self.pending[q] if w not in waits] + waits
        self.pending[q] = []
        half = NDMA_SLOTS // 2
        kind = "sw" if q == "pool" else "hw"
        slot = self.dma_rr[kind] + (half if kind == "sw" else 0)
        self.dma_rr[kind] = (self.dma_rr[kind] + 1) % half
        prod = ("dma", slot)
        prev = self.dma_count[slot]
        kn = self.known[q]
        if prev and kn.get(prod, 0) < prev:
            kn[prod] = prev
            waits.append((prod, prev))
        self.dma_count[slot] += 1
        seq = self.dma_count[slot]
        self.ops[q].append((waits, (lambda e: e.dma_start(out=out, in_=in_, **kw)), (prod, seq)))
        self._commit(prod, seq, reads, writes)
        self.n_instr += 1

    def emit(self, final_bufs=()):
        nc = self.nc
        final_bufs = self._bufs(final_bufs)
        with contextlib.ExitStack() as st:
            sems = {}
            for e in self.ENGS:
                nep = self.count[e] // EPOCH + 1
                sems[e] = [st.enter_context(nc.semaphore(f"s_{e}_{k}")) for k in range(nep)]
            for s in range(NDMA_SLOTS):
                sems[("dma", s)] = [st.enter_context(nc.semaphore(f"s_dma{s}"))]
            fin = {}
            for b in final_bufs:
                if b.last_w is not None:
                    p, s = b.last_w
                    fin[p] = max(fin.get(p, 0), s)

            def wait(eng, p, s):
                if isinstance(p, tuple):
                    eng.wait_ge(sems[p][0], 16 * s)
                else:
                    k = (s - 1) // EPOCH
                    eng.wait_ge(sems[p][k], s - k * EPOCH)

            def run(ename, eng):
                for waits, fn, (prod, seq) in self.ops[ename]:
                    for p, s in waits:
                        wait(eng, p, s)
                    ins = fn(eng)
                    if isinstance(prod, tuple):
                        ins.then_inc(sems[prod][0], 16)
                    else:
                        k = (seq - 1) // EPOCH
                        ins.then_inc(sems[prod][k], 1)
                if ename == "sp":
                    for p, s in fin.items():
                        wait(eng, p, s)

            with nc.Block() as block:
                @block.sync
                def _(e):
                    run("sp", e)

                @block.tensor
                def _(e):
                    run("pe", e)

                @block.scalar
                def _(e):
                    run("act", e)

                @block.vector
                def _(e):
                    run("dve", e)

                @block.gpsimd
                def _(e):
                    run("pool", e)


class Ring:
    def __init__(self, items):
        self.items = items
        self.i = 0

    def next(self):
        x = self.items[self.i % len(self.items)]
        self.i += 1
        return x


class Ctx:
    def __init__(self, nc, dbg=()):
        self.nc = nc
        self.P = Prog(nc)
        self.dbg = set(dbg)
        self.ins = {}
        self.scr = {}
        self.outs = []
        self.uid = 0

    def din(self, name, shape, dt=F32):
        ap = self.nc.dram_tensor(name, list(shape), dt, kind="ExternalInput").ap()
        self.ins[name] = T(ap, name)
        return self.ins[name]

    def scratch(self, name, shape, dt):
        if name in self.dbg:
            ap = self.nc.dram_tensor(name, list(shape), dt, kind="ExternalOutput").ap()
        else:
            ap = self.nc.dram_tensor(name, list(shape), dt).ap()
        t = T(ap, name)
        self.scr[name] = t
        if name in self.dbg:
            self.outs.append(t)
        return t

    def alloc(self, st, shape, dt, name=None, psum=False):
        self.uid += 1
        name = f"{name or 't'}_{self.uid}"
        if psum:
            t = st.enter_context(self.nc.psum_tensor(name, list(shape), dt))
        else:
            t = st.enter_context(self.nc.sbuf_tensor(name, list(shape), dt))
        return T(t, name)

    def ring(self, st, n, shape, dt, name=None, psum=False):
        return Ring([self.alloc(st, shape, dt, name, psum) for _ in range(n)])


def mm(C, out_t, out_ap, lhsT_t, lhsT_ap, rhs_t, rhs_ap, start, stop):
    C.P.op("pe", lambda e: e.matmul(out_ap, lhsT=lhsT_ap, rhs=rhs_ap, start=start, stop=stop),
           reads=[lhsT_t, rhs_t], writes=[out_t], pe_accum=True)


def tr(C, out_t, out_ap, in_t, in_ap, ident):
    C.P.op("pe", lambda e: e.transpose(out=out_ap, in_=in_ap, identity=ident.t[:]),
           reads=[in_t, ident], writes=[out_t], pe_accum=True)


def act(C, out_t, out_ap, in_t, in_ap, func, reads=(), extra_w=(), **kw):
    C.P.op("act", lambda e: e.activation(out=out_ap, in_=in_ap, func=func, **kw),
           reads=[in_t] + list(reads), writes=[out_t] + list(extra_w))


def tt(C, eng, out_t, out_ap, a_t, a_ap, b_t, b_ap, op):
    C.P.op(eng, lambda e: e.tensor_tensor(out=out_ap, in0=a_ap, in1=b_ap, op=op),
           reads=[a_t, b_t], writes=[out_t])


def ts(C, eng, out_t, out_ap, a_t, a_ap, s1, s2, op0, op1=None, reads=()):
    if op1 is None:
        C.P.op(eng, lambda e: e.tensor_scalar(out=out_ap, in0=a_ap, scalar1=s1, scalar2=None, op0=op0),
               reads=[a_t] + list(reads), writes=[out_t])
    else:
        C.P.op(eng, lambda e: e.tensor_scalar(out=out_ap, in0=a_ap, scalar1=s1, scalar2=s2, op0=op0, op1=op1),
               reads=[a_t] + list(reads), writes=[out_t])


def stt(C, eng, out_t, out_ap, a_t, a_ap, scalar, b_t, b_ap, op0, op1, reads=()):
    C.P.op(eng, lambda e: e.scalar_tensor_tensor(out=out_ap, in0=a_ap, scalar=scalar, in1=b_ap, op0=op0, op1=op1),
           reads=[a_t, b_t] + list(reads), writes=[out_t])


def cp(C, eng, out_t, out_ap, in_t, in_ap):
    if eng == "act":
        C.P.op("act", lambda e: e.copy(out=out_ap, in_=in_ap), reads=[in_t], writes=[out_t])
    else:
        C.P.op(eng, lambda e: e.tensor_copy(out=out_ap, in_=in_ap), reads=[in_t], writes=[out_t])


def rsqrt_mean(C, st_t, src_ap_fn, n, scale):
    ap = src_ap_fn()
    ts(C, "dve", st_t, ap, st_t, ap, scale, EPS, ALU.mult, ALU.add)
    C.P.op("act", lambda e: e.activation(out=ap, in_=ap, func=AF.Sqrt), reads=[st_t], writes=[st_t])
    C.P.op("dve", lambda e: e.reciprocal(out=ap, in_=ap), reads=[st_t], writes=[st_t])


def build_consts(C, st):
    P = C.P
    K = {}
    idf = C.alloc(st, [128, 128], F32, "idf")
    P.op("pool", lambda e: e.memset(idf.t[:], 0.0), writes=[idf])
    P.op("pool", lambda e: e.affine_select(out=idf.t[:], in_=idf.t[:], pattern=[[-1, 128]],
                                           compare_op=ALU.not_equal, fill=1.0, base=0, channel_multiplier=1),
         reads=[idf], writes=[idf])
    ident = C.alloc(st, [128, 128], BF16, "ident")
    cp(C, "dve", ident, ident.t[:], idf, idf.t[:])
    K["ident"] = ident
    K["identf"] = idf
    return K


OFF = dict(aq=0, ak=512, av=640, hq=768, hff=1280, hfb=1792, hi=2304, hg=2816, gate=3328)


def phase_a(C, K, ntiles=NT):
    nc, P = C.nc, C.P
    I = C.ins
    Sc = C.scr
    with contextlib.ExitStack() as st:
        w_in = C.alloc(st, [128, 8, 5376], BF16, "w_in")
        wv = I["w_in"].t.rearrange("(k p) n -> p k n", p=128)
        for c0 in range(0, 5376, 672):
            P.dma("pool", w_in.t[:, :, c0:c0 + 672], wv[:, :, c0:c0 + 672], reads=[I["w_in"]], writes=[w_in])
        gmix = C.alloc(st, [128, 1024], F32, "gmix")
        P.dma("sp", gmix.t[:], I["norm_mix"].t.to_broadcast([128, 1024]), writes=[gmix])
        qg = C.alloc(st, [128, 8, 64], F32, "qg")
        P.dma("sp", qg.t[:], I["q_norm"].t.unsqueeze(1).to_broadcast([128, 8, 64]), writes=[qg])
        kg = C.alloc(st, [128, 2, 64], F32, "kg")
        P.dma("sp", kg.t[:], I["k_norm"].t.unsqueeze(1).to_broadcast([128, 2, 64]), writes=[kg])
        raw = C.alloc(st, [128, 2, 2, 512], F32, "raw")
        P.dma("sp", raw.t[:], I["hg_lb_raw"].t.unsqueeze(0).to_broadcast([128, 2, 2, 512]), writes=[raw])
        lbb = C.alloc(st, [128, 2, 512], F32, "lbb")
        omlb = C.alloc(st, [128, 2, 512], F32, "omlb")
        tt(C, "dve", lbb, lbb.t[:], raw, raw.t[:, 0], raw, raw.t[:, 1], ALU.subtract)
        act(C, omlb, omlb.t[:], lbb, lbb.t[:], AF.Sigmoid, scale=-1.0)
        act(C, lbb, lbb.t[:], lbb, lbb.t[:], AF.Sigmoid)
        rawT = C.alloc(st, [128, 2, 2, 4], F32, "rawT")
        P.dma("sp", rawT.t[:], I["hg_lb_raw"].t.rearrange("s r (c p) -> p s r c", p=128), writes=[rawT],
              allow_slow_non_contiguous=True)
        omlT = C.alloc(st, [128, 2, 4], F32, "omlT")
        tt(C, "dve", omlT, omlT.t[:], rawT, rawT.t[:, 0], rawT, rawT.t[:, 1], ALU.subtract)
        act(C, omlT, omlT.t[:], omlT, omlT.t[:], AF.Sigmoid, scale=-1.0)

        xr = C.ring(st, 3, [128, 1024], F32, "xt")
        junk = C.alloc(st, [128, 1024], BF16, "junk")
        stat = C.ring(st, 8, [128, 16], F32, "stat")
        hbr = C.ring(st, 2, [128, 1024], BF16, "hb")
        hTr = C.ring(st, 2, [128, 8, 512], BF16, "hT")
        ptr = C.ring(st, 2, [128, 8, 128], BF16, "ptr", psum=True)
        pfr = C.ring(st, 2, [128, 512], F32, "pf", psum=True)
        ptk = C.ring(st, 3, [128, 512], F32, "ptk", psum=True)
        pqt = C.alloc(st, [128, 8, 128], BF16, "pqt", psum=True)
        stg_bf = C.ring(st, 4, [128, 512], BF16, "stgb")
        stg_f = C.ring(st, 3, [128, 512], F32, "stgf")
        tmpf = C.ring(st, 4, [128, 512], F32, "tmpf")
        qn = C.ring(st, 3, [128, 512], F32, "qn")
        qr = C.ring(st, 2, [128, 512], BF16, "qr")
        kk = C.ring(st, 2, [128, 4, 64], BF16, "kk")
        kn_ = C.ring(st, 2, [128, 128], F32, "kn")
        vst = C.ring(st, 2, [128, 2, 128], BF16, "vst")
        for v_ in vst.items:
            P.op("pool", lambda e, v_=v_: e.memset(v_.t[:], 1.0), writes=[v_])
        csr = C.ring(st, 2, [128, 2, 32], F32, "cs")
        qTs = C.ring(st, 2, [128, 6, 128], BF16, "qTs")
        rt = C.ring(st, 8, [128, 8, 2, 16], F32, "rt")

        def run_rr(gens):
            gens = list(gens)
            while gens:
                for g_ in list(gens):
                    try:
                        next(g_)
                    except StopIteration:
                        gens.remove(g_)

        def rope_norm(src_ps, src_ap, nh, gain, cs, dst_t, dst_ap4):
            w = nh * 64
            sq = tmpf.next()
            act(C, sq, sq.t[:, 0:w], src_ps, src_ap, AF.Square)
            yield
            s8 = stat.next()
            P.op("dve", lambda e: e.tensor_reduce(out=s8.t[:, 0:nh], in_=sq.t[:, 0:w].rearrange("p (h d) -> p h d", d=64),
                                                  axis=AX.X, op=ALU.add), reads=[sq], writes=[s8])
            yield
            ap = s8.t[:, 0:nh]
            ts(C, "dve", s8, ap, s8, ap, 1.0 / 64, EPS, ALU.mult, ALU.add)
            yield
            C.P.op("act", lambda e: e.activation(out=ap, in_=ap, func=AF.Sqrt), reads=[s8], writes=[s8])
            yield
            C.P.op("dve", lambda e: e.reciprocal(out=ap, in_=ap), reads=[s8], writes=[s8])
            yield
            n_ = qn.next()
            nv = n_.t[:, 0:w].rearrange("p (h d) -> p h d", d=64)
            tt(C, "dve", n_, nv, src_ps, src_ap.rearrange("p (h d) -> p h d", d=64),
               s8, s8.t[:, 0:nh].unsqueeze(2).to_broadcast([128, nh, 64]), ALU.mult)
            yield
            tt(C, "pool", n_, nv, n_, nv, gain, gain.t[:, 0:nh, :], ALU.mult)
            yield
            v5 = n_.t[:, 0:w].rearrange("p (h a b f) -> p h a b f", a=2, b=2, f=16)
            x1 = v5[:, :, :, 0, :]
            x2 = v5[:, :, :, 1, :]
            cb = cs.t[:, 0, :].rearrange("p (a f) -> p a f", a=2).unsqueeze(1).to_broadcast([128, nh, 2, 16])
            sb_ = cs.t[:, 1, :].rearrange("p (a f) -> p a f", a=2).unsqueeze(1).to_broadcast([128, nh, 2, 16])
            t1, t2 = rt.next(), rt.next()
            tt(C, "dve", t1, t1.t[:, 0:nh], n_, x1, cs, cb, ALU.mult)
            tt(C, "pool", t2, t2.t[:, 0:nh], n_, x2, cs, sb_, ALU.mult)
            yield
            t3, t4 = rt.next(), rt.next()
            tt(C, "dve", t3, t3.t[:, 0:nh], n_, x1, cs, sb_, ALU.mult)
            tt(C, "pool", t4, t4.t[:, 0:nh], n_, x2, cs, cb, ALU.mult)
            yield
            tt(C, "dve", dst_t, dst_ap4[:, :, :, 0, :], t1, t1.t[:, 0:nh], t2, t2.t[:, 0:nh], ALU.subtract)
            yield
            tt(C, "pool", dst_t, dst_ap4[:, :, :, 1, :], t3, t3.t[:, 0:nh], t4, t4.t[:, 0:nh], ALU.add)
            yield

        def prep_tile(g, j, hT):
            ti = g * 4 + j
            xt = xr.next()
            P.dma("sp", xt.t[:], I["x"].t[ti * 128:(ti + 1) * 128, :], reads=[I["x"]], writes=[xt])
            s_ = stat.next()
            act(C, junk, junk.t[:], xt, xt.t[:], AF.Square, accum_out=s_.t[:, 0:1], extra_w=[s_])
            yield
            ap = s_.t[:, 0:1]
            ts(C, "dve", s_, ap, s_, ap, 1.0 / D, EPS, ALU.mult, ALU.add)
            yield
            C.P.op("act", lambda e: e.activation(out=ap, in_=ap, func=AF.Sqrt), reads=[s_], writes=[s_])
            yield
            C.P.op("dve", lambda e: e.reciprocal(out=ap, in_=ap), reads=[s_], writes=[s_])
            yield
            hb = hbr.next()
            stt(C, "dve", hb, hb.t[:], xt, xt.t[:], s_.t[:, 0:1], gmix, gmix.t[:], ALU.mult, ALU.mult, reads=[s_])
            yield
            pt = ptr.next()
            for k in range(8):
                tr(C, pt, pt.t[:, k, :], hb, hb.t[:, k * 128:(k + 1) * 128], K["ident"])
            yield
            cp(C, "act", hT, hT.t[:, :, j * 128:(j + 1) * 128], pt, pt.t[:])
            yield

        ngr = ntiles // 4
        hTs = {0: hTr.next()}
        for j in range(4):
            run_rr([prep_tile(0, j, hTs[0])])
        for g in range(ngr):
            hT = hTs[g]
            if g + 1 < ngr:
                hTs[g + 1] = hTr.next()
            cols = slice(g * 512, (g + 1) * 512)
            fm = [("hq", OFF["hq"] + c * 128, c) for c in range(4)] + \
                 [("hff", OFF["hff"] + c * 128, c) for c in range(4)] + \
                 [("hfb", OFF["hfb"] + c * 128, c) for c in range(4)] + \
                 [("gate", OFF["gate"] + c * 128, c) for c in range(16)]
            for kind, c0, c in fm:
                pf = pfr.next()
                for k in range(8):
                    mm(C, pf, pf.t[:], w_in, w_in.t[:, k, c0:c0 + 128], hT, hT.t[:, k, :], k == 0, k == 7)
                sg = stg_bf.next()
                if kind == "hq":
                    act(C, sg, sg.t[:], pf, pf.t[:], AF.Silu)
                    dst = Sc["hqT"]
                    P.dma("pool", dst.t[c, :, cols], sg.t[:], reads=[sg], writes=[dst.reg((c, g))])
                elif kind in ("hff", "hfb"):
                    d_ = 0 if kind == "hff" else 1
                    tf = tmpf.next()
                    act(C, tf, tf.t[:], pf, pf.t[:], AF.Sigmoid, scale=-1.0)
                    ts(C, "dve", sg, sg.t[:], tf, tf.t[:], omlT.t[:, d_, c:c + 1], None, ALU.mult, reads=[omlT])
                    dst = Sc["kT"]
                    P.dma("pool", dst.t[d_, c, :, cols], sg.t[:], reads=[sg], writes=[dst.reg((d_, c, g))])
                else:
                    act(C, sg, sg.t[:], pf, pf.t[:], AF.Sigmoid)
                    dst = Sc["gtsT"]
                    P.dma("pool", dst.t[c, :, cols], sg.t[:], reads=[sg], writes=[dst.reg((c, g))])
            for j in range(4):
                ti = g * 4 + j
                rows = slice(ti * 128, (ti + 1) * 128)
                lhs = lambda k, j=j: hT.t[:, k, j * 128:(j + 1) * 128]
                cs = csr.next()
                P.dma("sp", cs.t[:], I["rope"].t[:, rows, :].rearrange("c p f -> p c f"), reads=[I["rope"]], writes=[cs])
                q_ = qr.next()
                k_ = kk.next()

                def chain_q():
                    pq = ptk.next()
                    for k in range(8):
                        mm(C, pq, pq.t[:], hT, lhs(k), w_in, w_in.t[:, k, 0:512], k == 0, k == 7)
                    yield
                    yield from rope_norm(pq, pq.t[:], 8, qg, cs, q_, q_.t[:].rearrange("p (h a b f) -> p h a b f", a=2, b=2, f=16))

                def chain_kv():
                    pkv = ptk.next()
                    for k in range(8):
                        mm(C, pkv, pkv.t[:, 0:256], hT, lhs(k), w_in, w_in.t[:, k, 512:768], k == 0, k == 7)
                    yield
                    v_ = vst.next()
                    cp(C, "act", v_, v_.t[:, :, 0:64], pkv, pkv.t[:, 128:256].rearrange("p (h d) -> p h d", d=64))
                    P.dma("pool", Sc["v"].t[ti], v_.t[:], reads=[v_], writes=[Sc["v"].reg(ti)])
                    yield
                    kview = k_.t[:].rearrange("p (h r) d -> p h r d", r=2)
                    yield from rope_norm(pkv, pkv.t[:, 0:128], 2, kg, cs, k_,
                                         kview[:, :, 0, :].rearrange("p h (a b f) -> p h a b f", a=2, b=2, f=16))
                    cp(C, "pool", k_, kview[:, :, 1, :], k_, kview[:, :, 0, :])
                    yield

                def chain_gate(d_, key):
                    pg = ptk.next()
                    for k in range(8):
                        mm(C, pg, pg.t[:], hT, lhs(k), w_in, w_in.t[:, k, OFF[key]:OFF[key] + 512], k == 0, k == 7)
                    yield
                    tf = tmpf.next()
                    act(C, tf, tf.t[:], pg, pg.t[:], AF.Sigmoid)
                    yield
                    tt(C, "dve", tf, tf.t[:], tf, tf.t[:], omlb, omlb.t[:, d_, :], ALU.mult)
                    yield
                    tt(C, "dve", tf, tf.t[:], tf, tf.t[:], lbb, lbb.t[:, d_, :], ALU.add)
                    yield
                    gf = stg_f.next()
                    act(C, gf, gf.t[:], tf, tf.t[:], AF.Ln)
                    P.dma("pool", Sc["g"].t[d_, rows, :], gf.t[:], reads=[gf], writes=[Sc["g"].reg((d_, ti))])
                    kb = stg_bf.next()
                    ts(C, "pool", kb, kb.t[:], tf, tf.t[:], -1.0, 1.0, ALU.mult, ALU.add)
                    P.dma("pool", Sc["k"].t[d_, rows, :], kb.t[:], reads=[kb], writes=[Sc["k"].reg((d_, ti))])
                    yield

                def chain_h(key, dstn, fn):
                    ph = ptk.next()
                    for k in range(8):
                        mm(C, ph, ph.t[:], hT, lhs(k), w_in, w_in.t[:, k, OFF[key]:OFF[key] + 512], k == 0, k == 7)
                    yield
                    sb_ = stg_bf.next()
                    if fn is None:
                        cp(C, "act", sb_, sb_.t[:], ph, ph.t[:])
                    else:
                        act(C, sb_, sb_.t[:], ph, ph.t[:], fn)
                    P.dma("pool", Sc[dstn].t[rows, :], sb_.t[:], reads=[sb_], writes=[Sc[dstn].reg(ti)])
                    yield

                chains = [chain_q(), chain_kv(), chain_gate(0, "hff")]
                if g + 1 < ngr:
                    chains.append(prep_tile(g + 1, j, hTs[g + 1]))
                run_rr(chains)
                run_rr([chain_gate(1, "hfb"), chain_h("hi", "hi", None), chain_h("hg", "sg", AF.Silu)])
                for pr in range(4):
                    tr(C, pqt, pqt.t[:, pr, :], q_, q_.t[:, pr * 128:(pr + 1) * 128], K["ident"])
                kflat = k_.t[:].rearrange("p a d -> p (a d)")
                for kv in range(2):
                    tr(C, pqt, pqt.t[:, 4 + kv, :], k_, kflat[:, kv * 128:(kv + 1) * 128], K["ident"])
                qs = qTs.next()
                cp(C, "act", qs, qs.t[:], pqt, pqt.t[:, 0:6, :])
                P.dma("pool", Sc["qT"].t[:, :, rows].rearrange("r p t -> p r t"), qs.t[:, 0:4, :], reads=[qs], writes=[Sc["qT"].reg(ti)])
                P.dma("pool", Sc["kTa"].t[:, :, rows].rearrange("r p t -> p r t"), qs.t[:, 4:6, :], reads=[qs], writes=[Sc["kTa"].reg(ti)])


def phase_b(C, K, ngroups=8, hhs=(0, 1), bg=()):
    nc, P = C.nc, C.P
    Sc = C.scr
    with contextlib.ExitStack() as st:
        kT = [C.alloc(st, [128, S], BF16, "kTsb") for _ in range(2)]
        for kv in range(2):
            for hf in range(2):
                cs_ = slice(hf * 2048, (hf + 1) * 2048)
                P.dma("sp", kT[kv].t[:, cs_], Sc["kTa"].t[kv, :, cs_],
                      reads=Sc["kTa"].regl(range(hf * 16, hf * 16 + 16)), writes=[kT[kv]])
        vs = C.alloc(st, [128, NT, 256], BF16, "vsb")
        for hf in range(4):
            P.dma("sp", vs.t[:, hf * 8:(hf + 1) * 8, :], Sc["v"].t[hf * 8:(hf + 1) * 8].rearrange("t p h c -> p t (h c)"),
                  reads=Sc["v"].regl(range(hf * 8, hf * 8 + 8)), writes=[vs])
        if "dbgvs" in Sc:
            P.dma("pool", Sc["dbgvs"].t[:], vs.t[:], reads=[vs], writes=[Sc["dbgvs"]])
        qr_ = C.ring(st, 2, [128, 512], BF16, "qTg")
        psS = C.ring(st, 4, [128, 512], F32, "psS", psum=True)
        acc = [C.alloc(st, [128, 512], F32, "acc", psum=True) for _ in range(2)]
        ptr_ = C.ring(st, 8, [128, 512], BF16, "pT")
        rl = C.ring(st, 2, [128, 512], F32, "rl")
        obr = C.ring(st, 2, [128, 512], BF16, "ob")
        LAG = 3
        steps = [(g, pr, kt, hh) for g in range(ngroups) for pr in range(4) for kt in range(NT) for hh in hhs]
        state = {}
        accs = [acc, [C.alloc(st, [128, 512], F32, "acc2", psum=True) for _ in range(2)]]

        def stage1(g, pr, kt, hh):
            kv = pr // 2
            if kt == 0 and hh == hhs[0]:
                q = qr_.next()
                P.dma("sp", q.t[:], Sc["qT"].t[pr, :, g * 512:(g + 1) * 512], reads=Sc["qT"].regl(range(4 * g, 4 * g + 4)), writes=[q])
                state["q", g, pr] = q
            q = state["q", g, pr]
            rows = slice(hh * 64, (hh + 1) * 64)
            s_ = psS.next()
            mm(C, s_, s_.t[:], kT[kv], kT[kv].t[rows, kt * 128:(kt + 1) * 128], q, q.t[rows, :], True, True)
            p_ = ptr_.next()
            act(C, p_, p_.t[:], s_, s_.t[:], AF.Exp, scale=0.125)
            state["p", g, pr, kt, hh] = p_

        def stage2(g, pr, kt, hh):
            kv = pr // 2
            p_ = state.pop(("p", g, pr, kt, hh))
            ac = accs[(g * 4 + pr) % 2]
            mm(C, ac[hh], ac[hh].t[:], vs, vs.t[:, kt, kv * 128:(kv + 1) * 128], p_, p_.t[:], kt == 0, kt == NT - 1)
            if kt == NT - 1 and hh == hhs[-1]:
                ob = obr.next()
                for h2_ in hhs:
                    r_ = rl.next()
                    C.P.op("dve", lambda e, r_=r_, h2_=h2_, ac=ac: e.reciprocal(out=r_.t[64:128, :], in_=ac[h2_].t[64:128, :]),
                           reads=[ac[h2_]], writes=[r_])
                    tt(C, "dve", ob, ob.t[h2_ * 64:(h2_ + 1) * 64, :], ac[h2_], ac[h2_].t[0:64, :], r_, r_.t[64:128, :], ALU.mult)
                P.dma("pool", Sc["attoT"].t[pr, :, g * 512:(g + 1) * 512], ob.t[:], reads=[ob], writes=[Sc["attoT"].reg((pr, g))])

        bg = list(bg)
        LAG = 4
        for it in range(0, len(steps) + LAG, 2):
            for i_ in (it, it + 1):
                if i_ < len(steps):
                    stage1(*steps[i_])
            for i_ in (it - LAG, it - LAG + 1):
                if 0 <= i_ < len(steps):
                    stage2(*steps[i_])
            if bg and (it // 2) % 3 == 2:
                bg.pop(0)()
        for job in bg:
            job()


def build_masks(C, st):
    P = C.P
    M = {}
    specs = {
        "f_incl": (ALU.is_ge, 0, 1, -1),
        "f_excl": (ALU.is_gt, 0, -1, 1),
        "b_incl": (ALU.is_ge, 0, -1, 1),
        "b_excl": (ALU.is_gt, 0, 1, -1),
    }
    for name, (op, base, tmul, pmul) in specs.items():
        m = C.alloc(st, [128, 128], F32, "m_" + name)
        P.op("pool", lambda e, m=m: e.memset(m.t[:], 1.0), writes=[m])
        P.op("pool", lambda e, m=m, op=op, base=base, tmul=tmul, pmul=pmul: e.affine_select(
            out=m.t[:], in_=m.t[:], pattern=[[tmul, 128]], compare_op=op, fill=0.0, base=base, channel_multiplier=pmul),
            reads=[m], writes=[m])
        P.op("pool", lambda e, m=m: e.memset(m.t[0:64, 64:128], 0.0), reads=[m], writes=[m])
        P.op("pool", lambda e, m=m: e.memset(m.t[64:128, 0:64], 0.0), reads=[m], writes=[m])
        M[name] = m
    return M


def phase_c(C, K, ntiles=NT, dirs=(0, 1)):
    nc, P = C.nc, C.P
    Sc = C.scr
    I = C.ins
    with contextlib.ExitStack() as st:
        M = build_masks(C, st)
        gon = C.alloc(st, [128, 4, 128], F32, "gon")
        P.dma("sp", gon.t[:], I["hg_out_norm"].t.unsqueeze(1).to_broadcast([128, 4, 128]), writes=[gon])
        gr = C.ring(st, 2, [128, 512], F32, "g_t")
        kdr = C.ring(st, 2, [128, 512], BF16, "kd_t")
        kTr = C.ring(st, 2, [128, 4, 128], BF16, "kT_t")
        qTr = C.ring(st, 2, [128, 4, 128], BF16, "hqT_t")
        vr = C.ring(st, 2, [128, 512], BF16, "v_t")
        ofr = C.ring(st, 2, [128, 512], F32, "of_t")
        sgr = C.ring(st, 2, [128, 512], BF16, "sg_t")
        prx = C.alloc(st, [128, 512], F32, "prx", psum=True)
        pbT = C.alloc(st, [128, 4, 128], F32, "pbT", psum=True)
        pX = [C.alloc(st, [128, 4, 128], F32, "pX", psum=True) for _ in range(2)]
        pOs = [C.alloc(st, [128, 4, 128], F32, "pOs", psum=True) for _ in range(2)]
        pTr = C.alloc(st, [128, 8, 128], BF16, "pTr", psum=True)
        ebT = C.ring(st, 2, [128, 4, 128], F32, "ebT")
        enbT = C.ring(st, 2, [128, 4, 128], F32, "enbT")
        er = C.ring(st, 2, [128, 512], F32, "er")
        qfull = C.ring(st, 2, [128, 4, 128], BF16, "qfull")
        qlo = C.ring(st, 2, [128, 4, 128], BF16, "qlo")
        qhi = C.ring(st, 2, [128, 4, 128], BF16, "qhi")
        for t_ in qlo.items + qhi.items:
            P.op("pool", lambda e, t_=t_: e.memset(t_.t[:], 0.0), writes=[t_])
        ktil = C.ring(st, 2, [128, 4, 128], BF16, "ktil")
        kdec = C.ring(st, 2, [128, 512], BF16, "kdec")
        atm = C.ring(st, 4, [128, 128], BF16, "atm")
        S32 = [C.alloc(st, [128, 128], F32, "S32") for _ in range(4)]
        Sbf = [C.alloc(st, [128, 128], BF16, "Sbf") for _ in range(4)]
        osb = C.ring(st, 2, [128, 512], F32, "osb")
        tot = C.ring(st, 2, [128, 512], F32, "tot")
        sqt = C.ring(st, 2, [128, 512], F32, "sqt")
        stat = C.ring(st, 2, [128, 8], F32, "statc")
        onb = C.ring(st, 2, [128, 512], BF16, "onb")
        oTs = C.ring(st, 2, [128, 4, 128], BF16, "oTs")

        for d_ in dirs:
            Mi = M["f_incl"] if d_ == 0 else M["b_incl"]
            Me = M["f_excl"] if d_ == 0 else M["b_excl"]
            for hd in range(4):
                P.op("pool", lambda e, hd=hd: e.memset(S32[hd].t[:], 0.0), writes=[S32[hd]])
                P.op("pool", lambda e, hd=hd: e.memset(Sbf[hd].t[:], 0.0), writes=[Sbf[hd]])
            order = list(range(ntiles)) if d_ == 0 else list(range(ntiles - 1, -1, -1))
            def pro(ti):
                rows = slice(ti * 128, (ti + 1) * 128)
                g_t, kd_t, kT_t, q_t, v_t = gr.next(), kdr.next(), kTr.next(), qTr.next(), vr.next()
                P.dma("sp", g_t.t[:], Sc["g"].t[d_, rows, :], reads=[Sc["g"].reg((d_, ti))], writes=[g_t])
                P.dma("sp", kd_t.t[:], Sc["k"].t[d_, rows, :], reads=[Sc["k"].reg((d_, ti))], writes=[kd_t])
                P.dma("sp", kT_t.t[:], Sc["kT"].t[d_, :, :, rows].rearrange("h p t -> p h t"),
                      reads=[Sc["kT"].reg((d_, c, ti // 4)) for c in range(4)], writes=[kT_t])
                P.dma("sp", q_t.t[:], Sc["hqT"].t[:, :, rows].rearrange("h p t -> p h t"),
                      reads=[Sc["hqT"].reg((c, ti // 4)) for c in range(4)], writes=[q_t])
                P.dma("sp", v_t.t[:], Sc["hi"].t[rows, :], reads=[Sc["hi"].reg(ti)], writes=[v_t])
                mm(C, prx, prx.t[:], Me, Me.t[:], g_t, g_t.t[:], True, True)
                for hd in range(4):
                    mm(C, pbT, pbT.t[:, hd, :], g_t, g_t.t[:, hd * 128:(hd + 1) * 128], Mi, Mi.t[:], True, True)
                eb, enb, er_ = ebT.next(), enbT.next(), er.next()
                act(C, eb, eb.t[:], pbT, pbT.t[:], AF.Exp)
                act(C, enb, enb.t[:], pbT, pbT.t[:], AF.Exp, scale=-1.0)
                act(C, er_, er_.t[:], prx, prx.t[:], AF.Exp)
                qf, ql, qh, kt_, kdc = qfull.next(), qlo.next(), qhi.next(), ktil.next(), kdec.next()
                tt(C, "dve", qf, qf.t[:], q_t, q_t.t[:], eb, eb.t[:], ALU.mult)
                cp(C, "pool", ql, ql.t[:, :, 0:64], qf, qf.t[:, :, 0:64])
                cp(C, "pool", qh, qh.t[:, :, 64:128], qf, qf.t[:, :, 64:128])
                tt(C, "dve", kt_, kt_.t[:], kT_t, kT_t.t[:], enb, enb.t[:], ALU.mult)
                tt(C, "pool", kdc, kdc.t[:], kd_t, kd_t.t[:], er_, er_.t[:], ALU.mult)
                return dict(kd_t=kd_t, v_t=v_t, eb=eb, qf=qf, ql=ql, qh=qh, kt_=kt_, kdc=kdc)

            def tile_body(ti, B_):
                rows = slice(ti * 128, (ti + 1) * 128)
                kd_t, v_t, eb, qf, ql, qh, kt_, kdc = (B_[k_] for k_ in ('kd_t', 'v_t', 'eb', 'qf', 'ql', 'qh', 'kt_', 'kdc'))
                if d_ == 0:
                    ca, cb, qa, qb, la, lb_ = 0, 1, ql, qh, 63, 127
                else:
                    ca, cb, qa, qb, la, lb_ = 1, 0, qh, ql, 64, 0
                ra = slice(ca * 64, (ca + 1) * 64)
                rb = slice(cb * 64, (cb + 1) * 64)
                def head_chain(hd):
                    hc = slice(hd * 128, (hd + 1) * 128)
                    X, O_ = pX[hd % 2], pOs[hd % 2]
                    oa = O_.t[:, hd // 2, :]
                    mm(C, X, X.t[:, 0, :], kt_, kt_.t[:, hd, :], qf, qf.t[:, hd, :], True, True)
                    mm(C, O_, oa, qa, qa.t[:, hd, :], Sbf[hd], Sbf[hd].t[:], True, False)
                    mm(C, X, X.t[:, 1, :], kdc, kdc.t[ra, hc], v_t, v_t.t[ra, hc], True, True)
                    yield
                    am = atm.next()
                    tt(C, "dve", am, am.t[:], X, X.t[:, 0, :], Mi, Mi.t[:], ALU.mult)
                    stt(C, "dve", S32[hd], S32[hd].t[:], S32[hd], S32[hd].t[:], eb.t[:, hd, la:la + 1],
                        X, X.t[:, 1, :], ALU.mult, ALU.add, reads=[eb])
                    yield
                    cp(C, "act", Sbf[hd], Sbf[hd].t[:], S32[hd], S32[hd].t[:])
                    yield
                    mm(C, O_, oa, qb, qb.t[:, hd, :], Sbf[hd], Sbf[hd].t[:], False, False)
                    mm(C, O_, oa, am, am.t[:], v_t, v_t.t[:, hc], False, True)
                    mm(C, X, X.t[:, 2, :], kdc, kdc.t[rb, hc], v_t, v_t.t[rb, hc], True, True)
                    yield
                    stt(C, "dve", S32[hd], S32[hd].t[:], S32[hd], S32[hd].t[:], eb.t[:, hd, lb_:lb_ + 1],
                        X, X.t[:, 2, :], ALU.mult, ALU.add, reads=[eb])
                    yield
                    cp(C, "act", Sbf[hd], Sbf[hd].t[:], S32[hd], S32[hd].t[:])
                    yield

                for pair in ((0, 1), (2, 3)):
                    gens = [head_chain(hd) for hd in pair]
                    while gens:
                        for g_ in list(gens):
                            try:
                                next(g_)
                            except StopIteration:
                                gens.remove(g_)

                def ov(tile_ap, s_):
                    return tile_ap.rearrange("p (a s d) -> p a s d", s=2, d=128)[:, :, s_, :]

                if d_ == 0 and len(dirs) == 2:
                    o_ = osb.next()
                    for s_ in range(2):
                        cp(C, "act", o_, ov(o_.t[:], s_), pOs[s_], pOs[s_].t[:, 0:2, :])
                    P.dma("pool", Sc["ofwd"].t[rows, :], o_.t[:], reads=[o_], writes=[Sc["ofwd"].reg(ti)])
                    return
                t_ = tot.next()
                if len(dirs) == 2:
                    of_ = ofr.next()
                    P.dma("sp", of_.t[:], Sc["ofwd"].t[rows, :], reads=[Sc["ofwd"].reg(ti)], writes=[of_])
                    for s_ in range(2):
                        tt(C, "dve", t_, ov(t_.t[:], s_), pOs[s_], pOs[s_].t[:, 0:2, :], of_, ov(of_.t[:], s_), ALU.add)
                else:
                    for s_ in range(2):
                        cp(C, "dve", t_, ov(t_.t[:], s_), pOs[s_], pOs[s_].t[:, 0:2, :])
                if "dbgo" in Sc:
                    P.dma("pool", Sc["dbgo"].t[rows, :], t_.t[:], reads=[t_], writes=[Sc["dbgo"].reg(ti)])
                sg_ = sgr.next()
                P.dma("sp", sg_.t[:], Sc["sg"].t[rows, :], reads=[Sc["sg"].reg(ti)], writes=[sg_])
                sq = sqt.next()
                act(C, sq, sq.t[:], t_, t_.t[:], AF.Square)
                s4 = stat.next()
                P.op("dve", lambda e, s4=s4, sq=sq: e.tensor_reduce(out=s4.t[:, 0:4], in_=sq.t[:].rearrange("p (h d) -> p h d", d=128),
                                                              axis=AX.X, op=ALU.add), reads=[sq], writes=[s4])
                rsqrt_mean(C, s4, lambda s4=s4: s4.t[:, 0:4], 4, 1.0 / 128)
                t3 = t_.t[:].rearrange("p (h d) -> p h d", d=128)
                tt(C, "dve", t_, t3, t_, t3, s4, s4.t[:, 0:4].unsqueeze(2).to_broadcast([128, 4, 128]), ALU.mult)
                tt(C, "pool", t_, t3, t_, t3, gon, gon.t[:], ALU.mult)
                ob = onb.next()
                tt(C, "dve", ob, ob.t[:], t_, t_.t[:], sg_, sg_.t[:], ALU.mult)
                for hd in range(4):
                    tr(C, pTr, pTr.t[:, hd, :], ob, ob.t[:, hd * 128:(hd + 1) * 128], K["ident"])
                os_ = oTs.next()
                cp(C, "act", os_, os_.t[:], pTr, pTr.t[:, 0:4, :])
                P.dma("pool", Sc["hgoT"].t[:, :, rows].rearrange("h p t -> p h t"), os_.t[:], reads=[os_], writes=[Sc["hgoT"].reg(ti)])

            pend = pro(order[0])
            for idx_, ti in enumerate(order):
                nxt = pro(order[idx_ + 1]) if idx_ + 1 < len(order) else None
                tile_body(ti, pend)
                pend = nxt


def load_w(C, st, name, kchunks, ncols, q="pool"):
    w = C.alloc(st, [128, kchunks, ncols], BF16, name)
    src = C.ins[name].t.rearrange("(k p) n -> p k n", p=128)
    step = min(kchunks, max(1, 4096 // ncols))
    for k0 in range(0, kchunks, step):
        C.P.dma(q, w.t[:, k0:k0 + step, :], src[:, k0:k0 + step, :], reads=[C.ins[name]], writes=[w])
    return w


def norm_transpose(C, K, xt, gain, stat, junk, hb, pt, dst, dst_ap):
    s_ = stat
    act(C, junk, junk.t[:], xt, xt.t[:], AF.Square, accum_out=s_.t[:, 0:1], extra_w=[s_])
    rsqrt_mean(C, s_, lambda: s_.t[:, 0:1], 1, 1.0 / D)
    stt(C, "dve", hb, hb.t[:], xt, xt.t[:], s_.t[:, 0:1], gain, gain.t[:], ALU.mult, ALU.mult, reads=[s_])
    for k in range(8):
        tr(C, pt, pt.t[:, k, :], hb, hb.t[:, k * 128:(k + 1) * 128], K["ident"])
    cp(C, "act", dst, dst_ap, pt, pt.t[:])


def phase_d(C, K, ngroups=8):
    nc, P = C.nc, C.P
    Sc = C.scr
    I = C.ins
    with contextlib.ExitStack() as st:
        wua = load_w(C, st, "w_up_att", 4, 1024)
        wuh = load_w(C, st, "w_up_hg", 4, 1024)
        wo = load_w(C, st, "w_out", 8, 1024)
        gffn = C.alloc(st, [128, 1024], F32, "gffn")
        P.dma("sp", gffn.t[:], I["norm_ffn"].t.to_broadcast([128, 1024]), writes=[gffn])
        aTr = C.ring(st, 2, [128, 4, 512], BF16, "aT")
        hTr_ = C.ring(st, 2, [128, 4, 512], BF16, "hgT")
        gtr = C.ring(st, 2, [128, 16, 512], BF16, "gts")
        pya = C.ring(st, 2, [128, 512], F32, "pya", psum=True)
        pyh = C.ring(st, 2, [128, 512], F32, "pyh", psum=True)
        px = C.ring(st, 2, [128, 512], F32, "px", psum=True)
        pt = C.ring(st, 2, [128, 8, 128], BF16, "ptd", psum=True)
        t1r = C.ring(st, 2, [128, 512], F32, "t1")
        t2r = C.ring(st, 2, [128, 512], F32, "t2")
        mTr = C.ring(st, 2, [128, 8, 512], BF16, "mT")
        xr = C.ring(st, 2, [128, 1024], F32, "xtd")
        x1r = C.ring(st, 3, [128, 1024], F32, "x1t")
        junk = C.alloc(st, [128, 1024], BF16, "junkd")
        stat = C.ring(st, 2, [128, 8], F32, "statd")
        hbr = C.ring(st, 2, [128, 1024], BF16, "hbd")
        h2s = C.ring(st, 2, [128, 8, 128], BF16, "h2s")
        for g in range(ngroups):
            cols = slice(g * 512, (g + 1) * 512)
            aT, hT, gt = aTr.next(), hTr_.next(), gtr.next()
            P.dma("sp", aT.t[:], Sc["attoT"].t[:, :, cols].rearrange("r p t -> p r t"),
                  reads=[Sc["attoT"].reg((pr, g)) for pr in range(4)], writes=[aT])
            P.dma("sp", hT.t[:], Sc["hgoT"].t[:, :, cols].rearrange("r p t -> p r t"),
                  reads=Sc["hgoT"].regl(range(4 * g, 4 * g + 4)), writes=[hT])
            P.dma("sp", gt.t[:], Sc["gtsT"].t[:, :, cols].rearrange("r p t -> p r t"),
                  reads=[Sc["gtsT"].reg((c, g)) for c in range(16)], writes=[gt])
            mT = mTr.next()
            for m_ in range(8):
                ms = slice(m_ * 128, (m_ + 1) * 128)
                ya, yh = pya.next(), pyh.next()
                for kc in range(4):
                    mm(C, ya, ya.t[:], wua, wua.t[:, kc, ms], aT, aT.t[:, kc, :], kc == 0, kc == 3)
                for kc in range(4):
                    mm(C, yh, yh.t[:], wuh, wuh.t[:, kc, ms], hT, hT.t[:, kc, :], kc == 0, kc == 3)
                t1, t2 = t1r.next(), t2r.next()
                tt(C, "dve", t1, t1.t[:], ya, ya.t[:], gt, gt.t[:, m_, :], ALU.mult)
                tt(C, "dve", t2, t2.t[:], yh, yh.t[:], gt, gt.t[:, 8 + m_, :], ALU.mult)
                tt(C, "pool", mT, mT.t[:, m_, :], t1, t1.t[:], t2, t2.t[:], ALU.add)
            def part1(j):
                ti = g * 4 + j
                rows = slice(ti * 128, (ti + 1) * 128)
                xt = xr.next()
                P.dma("sp", xt.t[:], I["x"].t[rows, :], reads=[I["x"]], writes=[xt])
                x1 = x1r.next()
                for hf in range(2):
                    hs = slice(hf * 512, (hf + 1) * 512)
                    p_ = px.next()
                    for m_ in range(8):
                        mm(C, p_, p_.t[:], mT, mT.t[:, m_, j * 128:(j + 1) * 128], wo, wo.t[:, m_, hs], m_ == 0, m_ == 7)
                    tt(C, "dve", x1, x1.t[:, hs], p_, p_.t[:], xt, xt.t[:, hs], ALU.add)
                P.dma("pool", Sc["x1"].t[rows, :], x1.t[:], reads=[x1], writes=[Sc["x1"].reg(ti)])
                return x1

            def part2(j, x1):
                ti = g * 4 + j
                hs_ = h2s.next()
                norm_transpose(C, K, x1, gffn, stat.next(), junk, hbr.next(), pt.next(), hs_, hs_.t[:])
                P.dma("pool", Sc["h2T"].t[ti], hs_.t[:], reads=[hs_], writes=[Sc["h2T"].reg(ti)])

            pend = part1(0)
            for j in range(4):
                nxt = part1(j + 1) if j + 1 < 4 else None
                part2(j, pend)
                pend = nxt


def phase_e1(C, K, ngroups=16):
    nc, P = C.nc, C.P
    Sc = C.scr
    I = C.ins
    with contextlib.ExitStack() as st:
        wq = load_w(C, st, "peer_wq", 8, 2048)
        skT = C.alloc(st, [128, 16, 128], BF16, "skT")
        P.dma("pool", skT.t[:], I["skT"].t, reads=[I["skT"]], writes=[skT])
        io_f = C.alloc(st, [128, 128], F32, "io_f")
        P.op("pool", lambda e: e.iota(io_f.t[:], pattern=[[1, 128]], base=0, channel_multiplier=0,
                                      allow_small_or_imprecise_dtypes=True), writes=[io_f])
        io_b = C.alloc(st, [128, 128], BF16, "io_b")
        cp(C, "dve", io_b, io_b.t[:], io_f, io_f.t[:])
        io_rep = C.alloc(st, [128, 128, 16], BF16, "io_rep")
        cp(C, "dve", io_rep, io_rep.t[:], io_f, io_f.t[:].unsqueeze(2).to_broadcast([128, 128, 16]))
        h2r = C.ring(st, 2, [128, 8, 128], BF16, "h2e")
        pq = C.ring(st, 2, [128, 4, 128], F32, "pq", psum=True)
        psc = C.ring(st, 2, [128, 4, 128], F32, "psc", psum=True)
        pIG = C.alloc(st, [128, 8, 128], BF16, "pIG", psum=True)
        pG = C.ring(st, 3, [128, 4, 128], F32, "pG", psum=True)
        qpT = C.ring(st, 2, [128, 16, 128], BF16, "qpT")
        s_all = C.ring(st, 2, [128, 16, 128], F32, "s_all")
        tmp128 = C.ring(st, 4, [128, 128], F32, "tmp128")
        v16 = C.ring(st, 2, [128, 16, 16], F32, "v16")
        i16 = C.ring(st, 2, [128, 16, 16], U32, "i16")
        i16f = C.ring(st, 2, [128, 16, 16], F32, "i16f")
        cand = C.ring(st, 1, [128, 8, 256], F32, "cand")
        tmp256 = C.ring(st, 4, [128, 256], F32, "tmp256")
        tsv = C.ring(st, 2, [128, 8, 16], F32, "tsv")
        pos = C.ring(st, 2, [128, 8, 16], U32, "pos")
        k12i = C.ring(st, 2, [128, 2, 128], I32, "k12i")
        k12f = C.ring(st, 2, [128, 2, 128], F32, "k12f")
        eq = C.ring(st, 2, [128, 128, 16], F32, "eq")
        IG = C.ring(st, 2, [128, 3, 128], BF16, "IG")
        IGf = C.ring(st, 2, [128, 3, 128], F32, "IGf")
        IGT = C.ring(st, 2, [128, 3, 128], BF16, "IGT")
        ex = C.ring(st, 2, [128, 8, 16], F32, "ex")
        st8 = C.ring(st, 2, [128, 8], F32, "st8")
        A4 = C.ring(st, 3, [128, 16, 128], BF16, "A4")
        B4 = C.ring(st, 3, [128, 16, 128], BF16, "B4")
        Gst = C.ring(st, 1, [128, 128, 256], BF16, "Gst")
        est = {}

        def stageXc(grp, j2):
            ti = grp * 2 + j2
            h2 = h2r.next()
            P.dma("sp", h2.t[:], Sc["h2T"].t[ti], reads=[Sc["h2T"].reg(ti)], writes=[h2])
            qp, sa = qpT.next(), s_all.next()
            for c4 in range(4):
                p_ = pq.next()
                for cc in range(4):
                    cq = c4 * 4 + cc
                    for k in range(8):
                        mm(C, p_, p_.t[:, cc, :], wq, wq.t[:, k, cq * 128:(cq + 1) * 128], h2, h2.t[:, k, :], k == 0, k == 7)
                cp(C, "act", qp, qp.t[:, c4 * 4:(c4 + 1) * 4, :], p_, p_.t[:])
            for c4 in range(4):
                p_ = psc.next()
                for cc in range(4):
                    cq = c4 * 4 + cc
                    mm(C, p_, p_.t[:, cc, :], qp, qp.t[:, cq, :], skT, skT.t[:, cq, :], True, True)
                cp(C, "act", sa, sa.t[:, c4 * 4:(c4 + 1) * 4, :], p_, p_.t[:])
            if "dbgs" in Sc:
                P.dma("pool", Sc["dbgs"].t[ti], sa.t[:], reads=[sa], writes=[Sc["dbgs"].reg(ti)])
            est["sa", grp, j2] = sa

        def stageXt(grp, j2):
            ti = grp * 2 + j2
            sa = est.pop(("sa", grp, j2))
            v_, i_ = v16.next(), i16.next()

            def top16(src_t, src_ap, vdst_t, vdst_ap, idst_t, idst_ap, tmp):
                P.op("dve", lambda e: e.max(out=vdst_ap[:, 0:8], in_=src_ap), reads=[src_t], writes=[vdst_t])
                P.op("dve", lambda e: e.match_replace(out=tmp.t[:], in_to_replace=vdst_ap[:, 0:8], in_values=src_ap,
                                                      imm_value=-1e30), reads=[src_t, vdst_t], writes=[tmp])
                P.op("dve", lambda e: e.max(out=vdst_ap[:, 8:16], in_=tmp.t[:]), reads=[tmp, vdst_t], writes=[vdst_t])
                P.op("dve", lambda e: e.max_index(out=idst_ap[:, 0:8], in_max=vdst_ap[:, 0:8], in_values=src_ap),
                     reads=[src_t, vdst_t], writes=[idst_t])
                P.op("dve", lambda e: e.max_index(out=idst_ap[:, 8:16], in_max=vdst_ap[:, 8:16], in_values=src_ap),
                     reads=[src_t, vdst_t, idst_t], writes=[idst_t])

            for cq in range(16):
                top16(sa, sa.t[:, cq, :], v_.reg(cq), v_.t[:, cq, :], i_.reg(cq), i_.t[:, cq, :], tmp128.next())
            if_ = i16f.next()
            cp(C, "dve", if_, if_.t[:], i_.regl(range(16)), i_.t[:])
            cd = cand.next()
            vv = v_.t[:].rearrange("p (h a) k -> p h a k", a=2)
            tt(C, "dve", cd, cd.t[:].rearrange("p h (a b) -> p h a b", b=16),
               v_.regl(range(16)), vv[:, :, 0, :].unsqueeze(3).to_broadcast([128, 8, 16, 16]),
               v_.regl(range(16)), vv[:, :, 1, :].unsqueeze(2).to_broadcast([128, 8, 16, 16]), ALU.add)
            ts_, ps_ = tsv.next(), pos.next()
            for h in range(8):
                top16(cd, cd.t[:, h, :], ts_.reg(h), ts_.t[:, h, :], ps_.reg(h), ps_.t[:, h, :], tmp256.next())
            ki, kf = k12i.next(), k12f.next()
            posf = ps_.t[:].rearrange("p h k -> p (h k)").bitcast(I32)
            P.op("dve", lambda e, ki=ki, posf=posf: e.tensor_single_scalar(out=ki.t[:, 0, :], in_=posf, scalar=4, op=ALU.arith_shift_right),
                 reads=ps_.regl(range(8)), writes=[ki])
            P.op("dve", lambda e, ki=ki, posf=posf: e.tensor_single_scalar(out=ki.t[:, 1, :], in_=posf, scalar=15, op=ALU.bitwise_and),
                 reads=ps_.regl(range(8)) + [ki], writes=[ki])
            cp(C, "dve", kf, kf.t[:], ki, ki.t[:])
            ig = IGf.next()
            iv = if_.t[:].rearrange("p (h a) k -> p h a k", a=2)
            for a in range(2):
                e_ = eq.next()
                tt(C, "dve", e_, e_.t[:], kf, kf.t[:, a, :].unsqueeze(2).to_broadcast([128, 128, 16]),
                   io_f, io_f.t[:, 0:16].unsqueeze(1).to_broadcast([128, 128, 16]), ALU.is_equal)
                e4 = e_.t[:].rearrange("p (h k) c -> p h k c", h=8)
                tt(C, "dve", e_, e4, e_, e4, if_, iv[:, :, a, :].unsqueeze(2).to_broadcast([128, 8, 16, 16]), ALU.mult)
                P.op("dve", lambda e, e_=e_, ig=ig, a=a: e.tensor_reduce(out=ig.t[:, a, :], in_=e_.t[:], axis=AX.X, op=ALU.add),
                     reads=[e_], writes=[ig])
            x_ = ex.next()
            tt(C, "dve", x_, x_.t[:], ts_.regl(range(8)), ts_.t[:], ts_.regl(range(8)), ts_.t[:, :, 0:1].to_broadcast([128, 8, 16]), ALU.subtract)
            act(C, x_, x_.t[:], x_, x_.t[:], AF.Exp)
            s8 = st8.next()
            P.op("dve", lambda e, s8=s8, x_=x_: e.tensor_reduce(out=s8.t[:], in_=x_.t[:], axis=AX.X, op=ALU.add), reads=[x_], writes=[s8])
            P.op("dve", lambda e, s8=s8: e.reciprocal(out=s8.t[:], in_=s8.t[:]), reads=[s8], writes=[s8])
            tt(C, "dve", ig, ig.t[:, 2, :].rearrange("p (h k) -> p h k", h=8), x_, x_.t[:],
               s8, s8.t[:].unsqueeze(2).to_broadcast([128, 8, 16]), ALU.mult)
            igf_ = ig
            ig = IG.next()
            cp(C, "dve", ig, ig.t[:], igf_, igf_.t[:])
            if "dbgig" in Sc:
                P.dma("pool", Sc["dbgig"].t[ti], ig.t[:], reads=[ig], writes=[Sc["dbgig"].reg(ti)])
            for a in range(3):
                tr(C, pIG, pIG.t[:, a, :], ig, ig.t[:, a, :], K["ident"])
            igt = IGT.next()
            cp(C, "dve", igt, igt.t[:], pIG, pIG.t[:, 0:3, :])
            est["igt", grp, j2] = igt

        def stageY(grp, j2):
            if j2 == 0:
                est["G", grp] = Gst.next()
            G_ = est["G", grp]
            igt = est.pop(("igt", grp, j2))
            TB = 16
            for b16 in range(128 // TB):
                a4, bb4 = A4.next(), B4.next()
                tsl = slice(b16 * TB, (b16 + 1) * TB)
                av = a4.t[:].rearrange("p t i -> p (t i)").rearrange("p (i t) -> p i t", t=TB)
                bv = bb4.t[:].rearrange("p t i -> p (t i)").rearrange("p (i t) -> p i t", t=TB)
                tt(C, "dve", a4, av, io_rep, io_rep.t[:], igt, igt.t[:, 0, tsl].unsqueeze(1).to_broadcast([128, 128, TB]), ALU.is_equal)
                tt(C, "dve", bb4, bv, io_rep, io_rep.t[:], igt, igt.t[:, 1, tsl].unsqueeze(1).to_broadcast([128, 128, TB]), ALU.is_equal)
                tt(C, "pool", a4, av, a4, av, igt, igt.t[:, 2, tsl].unsqueeze(1).to_broadcast([128, 128, TB]), ALU.mult)
                for q4 in range(TB // 4):
                    pg = pG.next()
                    for q_ in range(4):
                        mm(C, pg, pg.t[:, q_, :], a4, av[:, :, q4 * 4 + q_], bb4, bv[:, :, q4 * 4 + q_], True, True)
                    t0 = j2 * 128 + b16 * TB + q4 * 4
                    cp(C, "act", G_, G_.t[:, :, t0:t0 + 4].rearrange("p i t -> p t i"), pg, pg.t[:])
            if j2 == 1:
                hc = slice((grp % 2) * 256, (grp % 2 + 1) * 256)
                for i0 in range(0, 128, 32):
                    P.dma("pool", Sc["G"].t[grp // 2, :, i0:i0 + 32, hc], G_.t[:, i0:i0 + 32, :], reads=[G_], writes=[Sc["G"].reg(grp)])

        tl = [(grp, j2) for grp in range(ngroups) for j2 in range(2)]
        for it in range(len(tl) + 2):
            if it < len(tl):
                stageXc(*tl[it])
            if 1 <= it < len(tl) + 1:
                stageXt(*tl[it - 1])
            if it >= 2:
                stageY(*tl[it - 2])


def phase_e0(C, K):
    P = C.P
    jobs = []
    for i2 in range(128):
        jobs.append(lambda i2=i2: P.dma("pool", C.scr["uTb"].t[i2], C.ins["uT"].t[i2], reads=[C.ins["uT"]], writes=[C.scr["uTb"].reg(i2)]))
        jobs.append(lambda i2=i2: P.dma("pool", C.scr["vLb"].t[i2], C.ins["vL"].t[i2], reads=[C.ins["vL"]], writes=[C.scr["vLb"].reg(i2)]))
    return jobs


def phase_e2(C, K, ngroups=8, ni2=128):
    nc, P = C.nc, C.P
    Sc = C.scr
    with contextlib.ExitStack() as st:
        h2r = C.ring(st, 1, [128, 8, 512], BF16, "h2g")
        po = [C.alloc(st, [128, 512], F32, "po", psum=True) for _ in range(4)]
        par = C.ring(st, 4, [128, 512], F32, "pa", psum=True)
        uch = C.ring(st, 3, [128, 2, 8, 128], BF16, "uch")
        vch = C.ring(st, 4, [128, 2, 512], BF16, "vch")
        gch = C.ring(st, 3, [128, 2, 512], BF16, "gch")
        sqr = C.ring(st, 3, [128, 512], F32, "sqe")
        t2r = C.ring(st, 3, [128, 512], F32, "t2e")
        sgr = C.ring(st, 3, [128, 512], BF16, "sge")
        agr = C.ring(st, 3, [128, 512], BF16, "age")
        Wall = C.alloc(st, [128, ni2, 512], BF16, "Wall")
        xs = C.ring(st, 2, [128, 512], F32, "xs")
        x1h = C.ring(st, 2, [128, 512], F32, "x1h")
        LAG = 3
        state = {}

        def s1(grp, i2):
            h2 = state["h2"]
            if i2 % 2 == 0:
                u_, v_, g_ = uch.next(), vch.next(), gch.next()
                P.dma("sp", u_.t[:], Sc["uTb"].t[i2:i2 + 2].rearrange("i p k c -> p i k c"),
                      reads=Sc["uTb"].regl([i2, i2 + 1]), writes=[u_])
                P.dma("sp", v_.t[:], Sc["vLb"].t[i2:i2 + 2, :, 0:512].rearrange("i p d -> p i d"),
                      reads=Sc["vLb"].regl([i2, i2 + 1]), writes=[v_])
                P.dma("sp", g_.t[:], Sc["G"].t[grp, :, i2:i2 + 2, :], reads=Sc["G"].regl([2 * grp, 2 * grp + 1]), writes=[g_])
                state["uvg"] = (u_, v_, g_)
            u_, v_, g_ = state["uvg"]
            e_ = i2 % 2
            pa = par.next()
            for k in range(8):
                mm(C, pa, pa.t[:], u_, u_.t[:, e_, k, :], h2, h2.t[:, k, :], k == 0, k == 7)
            sq, t2, sg, ag = sqr.next(), t2r.next(), sgr.next(), agr.next()
            Wb = Wall.reg(i2)
            act(C, sq, sq.t[:], pa, pa.t[:], AF.Square, scale=0.21145921592448583)
            stt(C, "dve", t2, t2.t[:], sq, sq.t[:], 1.0, pa, pa.t[:], ALU.add, ALU.mult)
            tt(C, "dve", ag, ag.t[:], pa, pa.t[:], g_, g_.t[:, e_, :], ALU.mult)
            act(C, sg, sg.t[:], t2, t2.t[:], AF.Sigmoid, scale=1.5957691216057308)
            P.op("pool", lambda e: e.tensor_tensor(out=Wall.t[:, i2, :], in0=sg.t[:], in1=ag.t[:], op=ALU.mult),
                 reads=[sg, ag], writes=[Wb])
            state["v", i2] = (v_, e_)

        def s2(grp, i2, half):
            v_, e_ = state.pop(("v", i2)) if half == 0 else state.pop(("v2", i2))
            for j in range(4):
                P.op("pe", lambda e, j=j: e.matmul(po[j].t[:], lhsT=Wall.t[:, i2, j * 128:(j + 1) * 128], rhs=v_.t[:, e_, :],
                                                    start=(i2 == 0), stop=(i2 == ni2 - 1)),
                     reads=[Wall.reg(i2), v_], writes=[po[j]], pe_accum=True)

        def evac(grp, half):
            hs = slice(half * 512, (half + 1) * 512)
            for j in range(4):
                ti = grp * 4 + j
                rows = slice(ti * 128, (ti + 1) * 128)
                x1_ = x1h.next()
                P.dma("sp", x1_.t[:], Sc["x1"].t[rows, hs], reads=[Sc["x1"].reg(ti)], writes=[x1_])
                x_ = xs.next()
                tt(C, "dve", x_, x_.t[:], po[j], po[j].t[:], x1_, x1_.t[:], ALU.add)
                P.dma("pool", Sc["x2"].t[rows, hs], x_.t[:], reads=[x_], writes=[Sc["x2"].reg((ti, half))])

        for grp in range(ngroups):
            h2 = h2r.next()
            for j in range(4):
                ti = grp * 4 + j
                P.dma("sp", h2.t[:, :, j * 128:(j + 1) * 128], Sc["h2T"].t[ti], reads=[Sc["h2T"].reg(ti)], writes=[h2])
            state["h2"] = h2
            for it in range(ni2 + LAG):
                if it < ni2:
                    s1(grp, it)
                if it >= LAG:
                    s2(grp, it - LAG, 0)
            evac(grp, 0)
            for it in range(ni2 + LAG):
                if it < ni2:
                    if it % 2 == 0:
                        v_ = vch.next()
                        P.dma("sp", v_.t[:], Sc["vLb"].t[it:it + 2, :, 512:1024].rearrange("i p d -> p i d"),
                              reads=Sc["vLb"].regl([it, it + 1]), writes=[v_])
                        state["vp"] = v_
                    state["v2", it] = (state["vp"], it % 2)
                if it >= LAG:
                    s2(grp, it - LAG, 1)
            evac(grp, 1)


def phase_f(C, K, ntiles=NT):
    nc, P = C.nc, C.P
    Sc = C.scr
    I = C.ins
    with contextlib.ExitStack() as st:
        wg = load_w(C, st, "ple_gate", 8, 1024)
        wp = load_w(C, st, "ple_proj", 2, 1024)
        gple = C.alloc(st, [128, 1024], F32, "gple")
        P.dma("sp", gple.t[:], I["norm_ple"].t.to_broadcast([128, 1024]), writes=[gple])
        x2r = C.ring(st, 3, [128, 1024], F32, "x2f")
        pr_ = C.ring(st, 2, [128, 256], F32, "pf32")
        pbr = C.ring(st, 2, [128, 256], BF16, "pbf")
        junk = C.alloc(st, [128, 1024], BF16, "junkf")
        stat = C.ring(st, 2, [128, 8], F32, "statf")
        hbr = C.ring(st, 2, [128, 1024], BF16, "hbf")
        pt = C.ring(st, 2, [128, 8, 128], BF16, "ptf", psum=True)
        ptp = C.alloc(st, [128, 8, 128], BF16, "ptp", psum=True)
        h3r = C.ring(st, 3, [128, 8, 128], BF16, "h3T")
        pTr = C.ring(st, 3, [128, 2, 128], BF16, "pT")
        pgr = C.ring(st, 2, [128, 512], F32, "pgate", psum=True)
        ppr = C.ring(st, 2, [128, 512], F32, "pproj", psum=True)
        sgr = C.ring(st, 2, [128, 512], F32, "sgf")
        tr_ = C.ring(st, 2, [128, 512], F32, "tf")
        outr = C.ring(st, 2, [128, 1024], F32, "outf")
        def pro(ti):
            rows = slice(ti * 128, (ti + 1) * 128)
            x2 = x2r.next()
            P.dma("sp", x2.t[:], Sc["x2"].t[rows, :], reads=[Sc["x2"].reg((ti, 0)), Sc["x2"].reg((ti, 1))], writes=[x2])
            pf = pr_.next()
            P.dma("sp", pf.t[:], I["p"].t[rows, :], reads=[I["p"]], writes=[pf])
            pb = pbr.next()
            cp(C, "pool", pb, pb.t[:], pf, pf.t[:])
            for k in range(2):
                tr(C, ptp, ptp.t[:, k, :], pb, pb.t[:, k * 128:(k + 1) * 128], K["ident"])
            pT = pTr.next()
            cp(C, "act", pT, pT.t[:], ptp, ptp.t[:, 0:2, :])
            h3 = h3r.next()
            norm_transpose(C, K, x2, gple, stat.next(), junk, hbr.next(), pt.next(), h3, h3.t[:])
            return x2, pT, h3

        def body(ti, x2, pT, h3):
            rows = slice(ti * 128, (ti + 1) * 128)
            o_ = outr.next()
            for hf in range(2):
                hs = slice(hf * 512, (hf + 1) * 512)
                pg, pp = pgr.next(), ppr.next()
                for k in range(8):
                    mm(C, pg, pg.t[:], h3, h3.t[:, k, :], wg, wg.t[:, k, hs], k == 0, k == 7)
                for k in range(2):
                    mm(C, pp, pp.t[:], pT, pT.t[:, k, :], wp, wp.t[:, k, hs], k == 0, k == 1)
                sg, t_ = sgr.next(), tr_.next()
                act(C, sg, sg.t[:], pg, pg.t[:], AF.Sigmoid)
                tt(C, "dve", t_, t_.t[:], pp, pp.t[:], sg, sg.t[:], ALU.mult)
                tt(C, "pool", o_, o_.t[:, hs], t_, t_.t[:], x2, x2.t[:, hs], ALU.add)
            P.dma("sp", C.y.t[rows, :], o_.t[:], reads=[o_], writes=[C.y.reg(ti)])

        pend = pro(0)
        for ti in range(ntiles):
            nxt = pro(ti + 1) if ti + 1 < ntiles else None
            body(ti, *pend)
            pend = nxt


def declare(C):
    C.din("x", [S, D])
    C.din("p", [S, 256])
    C.din("rope", [2, S, 32])
    C.din("norm_mix", [1, D])
    C.din("w_in", [D, 5376])
    C.din("q_norm", [1, 64])
    C.din("k_norm", [1, 64])
    C.din("hg_lb_raw", [2, 2, 512])
    C.din("hg_out_norm", [1, 128])
    C.din("w_up_att", [512, D])
    C.din("w_up_hg", [512, D])
    C.din("w_out", [D, D])
    C.din("norm_ffn", [1, D])
    C.din("peer_wq", [D, 2048])
    C.din("skT", [128, 16, 128])
    C.din("uT", [128, 128, 8, 128])
    C.din("vL", [128, 128, D])
    C.din("norm_ple", [1, D])
    C.din("ple_gate", [D, D])
    C.din("ple_proj", [256, D])
    sc = C.scratch
    sc("hqT", [4, 128, S], BF16)
    sc("kT", [2, 4, 128, S], BF16)
    sc("gtsT", [16, 128, S], BF16)
    sc("qT", [4, 128, S], BF16)
    sc("kTa", [2, 128, S], BF16)
    sc("v", [NT, 128, 2, 128], BF16)
    sc("g", [2, S, 512], F32)
    sc("k", [2, S, 512], BF16)
    sc("hi", [S, 512], BF16)
    sc("sg", [S, 512], BF16)
    sc("attoT", [4, 128, S], BF16)
    sc("ofwd", [S, 512], F32)
    sc("uTb", [128, 128, 8, 128], BF16)
    sc("vLb", [128, 128, D], BF16)
    sc("x2", [S, D], F32)
    sc("G", [8, 128, 128, 512], BF16)
    if "dbgs" in C.dbg:
        sc("dbgs", [NT, 128, 16, 128], F32)
        sc("dbgig", [NT, 128, 3, 128], BF16)
    sc("x1", [S, D], F32)
    sc("h2T", [NT, 128, 8, 128], BF16)
    sc("hgoT", [4, 128, S], BF16)
    if "dbgo" in C.dbg:
        sc("dbgo", [S, 512], F32)
    if "dbgacc" in C.dbg:
        sc("dbgacc", [128, 512], F32)
        sc("dbgp", [2, 128, 512], BF16)
        sc("dbgvs", [128, NT, 256], BF16)


def build(dbg=(), phases="A", ntiles=NT, **kw):
    nc = bass.Bass("TRN2", target_bir_lowering=False)
    C = Ctx(nc, dbg)
    declare(C)
    C.y = T(nc.dram_tensor("y", [S, D], F32, kind="ExternalOutput").ap(), "y")
    C.outs.append(C.y)
    with contextlib.ExitStack() as st:
        K = build_consts(C, st)
        if "A" in phases:
            phase_a(C, K, ntiles)
        if "C" in phases:
            C.P.barrier()
            phase_c(C, K, kw.get("c_tiles", NT), kw.get("c_dirs", (0, 1)))
        C.P.barrier()
        bg = phase_e0(C, K) if "2" in phases else []
        if "B" in phases:
            phase_b(C, K, kw.get("b_groups", 8), kw.get("hhs", (0, 1)), bg)
        else:
            for job in bg:
                job()
        if "D" in phases:
            C.P.barrier()
            phase_d(C, K, kw.get("d_groups", 8))
        if "E" in phases:
            C.P.barrier()
            phase_e1(C, K, kw.get("e1_groups", 16))
        if "2" in phases:
            C.P.barrier()
            phase_e2(C, K, kw.get("e2_groups", 8), kw.get("ni2", 128))
        if "F" in phases:
            C.P.barrier()
            phase_f(C, K, kw.get("f_tiles", NT))
        fin = []
        for t in C.outs:
            fin.append(t.b)
            fin.extend(t.regs.values())
        C.P.emit(final_bufs=fin)
    return nc, C


def _rope_tables():
    t = np.arange(S)
    row = (t // 64).astype(np.float32)
    col = (t % 64).astype(np.float32)
    inv = (np.float32(10000.0) ** (-np.arange(16, dtype=np.float32) / 16)).astype(np.float32)
    ang = np.concatenate([row[:, None] * inv, col[:, None] * inv], 1).astype(np.float32)
    return np.stack([np.cos(ang), np.sin(ang)], 0).astype(np.float32)


def _in_maps(inp, ncores):
    shared = {}
    shared["rope"] = _rope_tables()
    for k in ["norm_mix", "q_norm", "k_norm", "hg_out_norm", "norm_ffn", "norm_ple"]:
        shared[k] = np.ascontiguousarray(np.asarray(inp[k], np.float32)[0][None])
    for k in ["w_in", "w_up_att", "w_up_hg", "w_out", "peer_wq", "ple_gate", "ple_proj"]:
        shared[k] = np.ascontiguousarray(np.asarray(inp[k], np.float32)[0])
    shared["hg_lb_raw"] = np.ascontiguousarray(np.asarray(inp["hg_lb_raw"], np.float32))
    sk = np.asarray(inp["peer_subkeys"], np.float32)[0]
    shared["skT"] = np.ascontiguousarray(sk.transpose(3, 0, 1, 2).reshape(128, 16, 128))
    u = np.asarray(inp["peer_u"], np.float32)[0].reshape(128, 128, 8, 128)
    shared["uT"] = np.ascontiguousarray(u.transpose(1, 3, 2, 0))
    v = np.asarray(inp["peer_v"], np.float32)[0].reshape(128, 128, D)
    shared["vL"] = np.ascontiguousarray(v.transpose(1, 0, 2))
    x = np.asarray(inp["x"], np.float32)
    p = np.asarray(inp["p"], np.float32)
    maps = []
    for b in range(ncores):
        m = dict(shared)
        m["x"] = np.ascontiguousarray(x[b])
        m["p"] = np.ascontiguousarray(p[0, b])
        maps.append(m)
    return maps


_NC_CACHE = {}


def kernel(**inputs):
    ncores = 8
    if "nc" not in _NC_CACHE:
        _NC_CACHE["nc"] = build(phases="ABCDE2F")[0]
    nc = _NC_CACHE["nc"]
    maps = _in_maps(inputs, ncores)
    res = run_bass_kernel_spmd(nc, maps, core_ids=list(range(ncores)))
    out = np.stack([np.asarray(res.results[b]["y"], np.float32) for b in range(ncores)], 0)
    return out
```

```python
import contextlib
import numpy as np
import concourse.bass as bass
import concourse.mybir as mybir
from concourse.bass_utils import run_bass_kernel_spmd

F32 = mybir.dt.float32
BF16 = mybir.dt.bfloat16
I32 = mybir.dt.int32
U32 = mybir.dt.uint32
ALU = mybir.AluOpType
AF = mybir.ActivationFunctionType
AX = mybir.AxisListType

S = 4096
D = 1024
NT = S // 128
EPS = 1e-6
EPOCH = 16000
NDMA_SLOTS = 48


class Buf:
    __slots__ = ("name", "last_w", "readers")

    def __init__(self, name=""):
        self.name = name
        self.last_w = None
        self.readers = {}


class T:
    __slots__ = ("t", "b", "regs")

    def __init__(self, t, name=""):
        self.t = t
        self.b = Buf(name)
        self.regs = {}

    def reg(self, key):
        if key not in self.regs:
            self.regs[key] = Buf(f"{self.b.name}[{key}]")
        return self.regs[key]

    def regl(self, keys):
        return [self.reg(k) for k in keys]


class Prog:
    ENGS = ("pe", "act", "dve", "pool", "sp")

    def __init__(self, nc):
        self.nc = nc
        self.ops = {e: [] for e in self.ENGS}
        self.count = {e: 0 for e in self.ENGS}
        self.known = {e: {} for e in self.ENGS}
        self.dma_count = [0] * NDMA_SLOTS
        self.dma_rr = {"hw": 0, "sw": 0}
        self.n_instr = 0
        self.pending = {e: [] for e in self.ENGS}

    def barrier(self):
        prods = [(e, self.count[e]) for e in self.ENGS if self.count[e] > 0]
        prods += [(("dma", s), c) for s, c in enumerate(self.dma_count) if c > 0]
        for e in self.ENGS:
            kn = self.known[e]
            for p, c in prods:
                if kn.get(p, 0) < c:
                    kn[p] = c
                    self.pending[e].append((p, c))

    def _deps(self, eng, reads, writes, pe_accum=False):
        deps = {}

        def add(p, s):
            if deps.get(p, 0) < s:
                deps[p] = s

        for b in reads:
            if b.last_w is not None:
                add(*b.last_w)
        for b in writes:
            if b.last_w is not None:
                if not (pe_accum and b.last_w[0] == "pe" and eng == "pe"):
                    add(*b.last_w)
            for p, s in b.readers.items():
                add(p, s)
        waits = []
        kn = self.known[eng]
        for p, s in deps.items():
            if kn.get(p, 0) < s:
                kn[p] = s
                waits.append((p, s))
        return waits

    def _commit(self, prod, seq, reads, writes):
        for b in writes:
            b.last_w = (prod, seq)
            b.readers = {}
        for b in reads:
            b.readers[prod] = max(b.readers.get(prod, 0), seq)

    @staticmethod
    def _bufs(xs):
        out = []
        for x in xs:
            if isinstance(x, T):
                out.append(x.b)
            elif isinstance(x, (list, tuple)):
                out.extend(Prog._bufs(x))
            else:
                out.append(x)
        return out

    def op(self, eng, fn, reads=(), writes=(), pe_accum=False):
        reads = self._bufs(reads)
        writes = self._bufs(writes)
        waits = self._deps(eng, reads, writes, pe_accum)
        waits = [w for w in self.pending[eng] if w not in waits] + waits
        self.pending[eng] = []
        self.count[eng] += 1
        seq = self.count[eng]
        self.ops[eng].append((waits, fn, (eng, seq)))
        self._commit(eng, seq, reads, writes)
        self.n_instr += 1

    def dma(self, q, out, in_, reads=(), writes=(), **kw):
        reads = self._bufs(reads)
        writes = self._bufs(writes)
        waits = self._deps(q, reads, writes)
        waits = [w for w in self.pending[q] if w not in waits] + waits
        self.pending[q] = []
        half = NDMA_SLOTS // 2
        kind = "sw" if q == "pool" else "hw"
        slot = self.dma_rr[kind] + (half if kind == "sw" else 0)
        self.dma_rr[kind] = (self.dma_rr[kind] + 1) % half
        prod = ("dma", slot)
        prev = self.dma_count[slot]
        kn = self.known[q]
        if prev and kn.get(prod, 0) < prev:
            kn[prod] = prev
            waits.append((prod, prev))
        self.dma_count[slot] += 1
        seq = self.dma_count[slot]
        self.ops[q].append((waits, (lambda e: e.dma_start(out=out, in_=in_, **kw)), (prod, seq)))
        self._commit(prod, seq, reads, writes)
        self.n_instr += 1

    def emit(self, final_bufs=()):
        nc = self.nc
        final_bufs = self._bufs(final_bufs)
        with contextlib.ExitStack() as st:
            sems = {}
            for e in self.ENGS:
                nep = self.count[e] // EPOCH + 1
                sems[e] = [st.enter_context(nc.semaphore(f"s_{e}_{k}")) for k in range(nep)]
            for s in range(NDMA_SLOTS):
                sems[("dma", s)] = [st.enter_context(nc.semaphore(f"s_dma{s}"))]
            fin = {}
            for b in final_bufs:
                if b.last_w is not None:
                    p, s = b.last_w
                    fin[p] = max(fin.get(p, 0), s)

            def wait(eng, p, s):
                if isinstance(p, tuple):
                    eng.wait_ge(sems[p][0], 16 * s)
                else:
                    k = (s - 1) // EPOCH
                    eng.wait_ge(sems[p][k], s - k * EPOCH)

            def run(ename, eng):
                for waits, fn, (prod, seq) in self.ops[ename]:
                    for p, s in waits:
                        wait(eng, p, s)
                    ins = fn(eng)
                    if isinstance(prod, tuple):
                        ins.then_inc(sems[prod][0], 16)
                    else:
                        k = (seq - 1) // EPOCH
                        ins.then_inc(sems[prod][k], 1)
                if ename == "sp":
                    for p, s in fin.items():
                        wait(eng, p, s)

            with nc.Block() as block:
                @block.sync
                def _(e):
                    run("sp", e)

                @block.tensor
                def _(e):
                    run("pe", e)

                @block.scalar
                def _(e):
                    run("act", e)

                @block.vector
                def _(e):
                    run("dve", e)

                @block.gpsimd
                def _(e):
                    run("pool", e)


class Ring:
    def __init__(self, items):
        self.items = items
        self.i = 0

    def next(self):
        x = self.items[self.i % len(self.items)]
        self.i += 1
        return x


class Ctx:
    def __init__(self, nc, dbg=()):
        self.nc = nc
        self.P = Prog(nc)
        self.dbg = set(dbg)
        self.ins = {}
        self.scr = {}
        self.outs = []
        self.uid = 0

    def din(self, name, shape, dt=F32):
        ap = self.nc.dram_tensor(name, list(shape), dt, kind="ExternalInput").ap()
        self.ins[name] = T(ap, name)
        return self.ins[name]

    def scratch(self, name, shape, dt):
        if name in self.dbg:
            ap = self.nc.dram_tensor(name, list(shape), dt, kind="ExternalOutput").ap()
        else:
            ap = self.nc.dram_tensor(name, list(shape), dt).ap()
        t = T(ap, name)
        self.scr[name] = t
        if name in self.dbg:
            self.outs.append(t)
        return t

    def alloc(self, st, shape, dt, name=None, psum=False):
        self.uid += 1
        name = f"{name or 't'}_{self.uid}"
        if psum:
            t = st.enter_context(self.nc.psum_tensor(name, list(shape), dt))
        else:
            t = st.enter_context(self.nc.sbuf_tensor(name, list(shape), dt))
        return T(t, name)

    def ring(self, st, n, shape, dt, name=None, psum=False):
        return Ring([self.alloc(st, shape, dt, name, psum) for _ in range(n)])


def mm(C, out_t, out_ap, lhsT_t, lhsT_ap, rhs_t, rhs_ap, start, stop):
    C.P.op("pe", lambda e: e.matmul(out_ap, lhsT=lhsT_ap, rhs=rhs_ap, start=start, stop=stop),
           reads=[lhsT_t, rhs_t], writes=[out_t], pe_accum=True)


def tr(C, out_t, out_ap, in_t, in_ap, ident):
    C.P.op("pe", lambda e: e.transpose(out=out_ap, in_=in_ap, identity=ident.t[:]),
           reads=[in_t, ident], writes=[out_t], pe_accum=True)


def act(C, out_t, out_ap, in_t, in_ap, func, reads=(), extra_w=(), **kw):
    C.P.op("act", lambda e: e.activation(out=out_ap, in_=in_ap, func=func, **kw),
           reads=[in_t] + list(reads), writes=[out_t] + list(extra_w))


def tt(C, eng, out_t, out_ap, a_t, a_ap, b_t, b_ap, op):
    C.P.op(eng, lambda e: e.tensor_tensor(out=out_ap, in0=a_ap, in1=b_ap, op=op),
           reads=[a_t, b_t], writes=[out_t])


def ts(C, eng, out_t, out_ap, a_t, a_ap, s1, s2, op0, op1=None, reads=()):
    if op1 is None:
        C.P.op(eng, lambda e: e.tensor_scalar(out=out_ap, in0=a_ap, scalar1=s1, scalar2=None, op0=op0),
               reads=[a_t] + list(reads), writes=[out_t])
    else:
        C.P.op(eng, lambda e: e.tensor_scalar(out=out_ap, in0=a_ap, scalar1=s1, scalar2=s2, op0=op0, op1=op1),
               reads=[a_t] + list(reads), writes=[out_t])


def stt(C, eng, out_t, out_ap, a_t, a_ap, scalar, b_t, b_ap, op0, op1, reads=()):
    C.P.op(eng, lambda e: e.scalar_tensor_tensor(out=out_ap, in0=a_ap, scalar=scalar, in1=b_ap, op0=op0, op1=op1),
           reads=[a_t, b_t] + list(reads), writes=[out_t])


def cp(C, eng, out_t, out_ap, in_t, in_ap):
    if eng == "act":
        C.P.op("act", lambda e: e.copy(out=out_ap, in_=in_ap), reads=[in_t], writes=[out_t])
    else:
        C.P.op(eng, lambda e: e.tensor_copy(out=out_ap, in_=in_ap), reads=[in_t], writes=[out_t])


def rsqrt_mean(C, st_t, src_ap_fn, n, scale):
    ap = src_ap_fn()
    ts(C, "dve", st_t, ap, st_t, ap, scale, EPS, ALU.mult, ALU.add)
    C.P.op("act", lambda e: e.activation(out=ap, in_=ap, func=AF.Sqrt), reads=[st_t], writes=[st_t])
    C.P.op("dve", lambda e: e.reciprocal(out=ap, in_=ap), reads=[st_t], writes=[st_t])


def build_consts(C, st):
    P = C.P
    K = {}
    idf = C.alloc(st, [128, 128], F32, "idf")
    P.op("pool", lambda e: e.memset(idf.t[:], 0.0), writes=[idf])
    P.op("pool", lambda e: e.affine_select(out=idf.t[:], in_=idf.t[:], pattern=[[-1, 128]],
                                           compare_op=ALU.not_equal, fill=1.0, base=0, channel_multiplier=1),
         reads=[idf], writes=[idf])
    ident = C.alloc(st, [128, 128], BF16, "ident")
    cp(C, "dve", ident, ident.t[:], idf, idf.t[:])
    K["ident"] = ident
    K["identf"] = idf
    return K


def build_rope(C, st):
    import math
    P = C.P
    rope = C.alloc(st, [128, NT, 2, 32], F32, "rope")
    with contextlib.ExitStack() as tmp:
        pf = C.alloc(tmp, [128, 1], F32, "pf")
        P.op("pool", lambda e: e.iota(pf.t[:], pattern=[[0, 1]], base=0, channel_multiplier=1,
                                      allow_small_or_imprecise_dtypes=True), writes=[pf])
        pi = C.alloc(tmp, [128, 2], I32, "pi")
        hl = C.alloc(tmp, [128, 2], I32, "hl")
        hlf = C.alloc(tmp, [128, 2], F32, "hlf")
        cp(C, "dve", pi, pi.t[:, 0:1], pf, pf.t[:])
        P.op("dve", lambda e: e.tensor_single_scalar(out=hl.t[:, 0:1], in_=pi.t[:, 0:1], scalar=6, op=ALU.arith_shift_right),
             reads=[pi], writes=[hl])
        P.op("dve", lambda e: e.tensor_single_scalar(out=hl.t[:, 1:2], in_=pi.t[:, 0:1], scalar=63, op=ALU.bitwise_and),
             reads=[pi, hl], writes=[hl])
        cp(C, "dve", hlf, hlf.t[:], hl, hl.t[:])
        jf = C.alloc(tmp, [128, 16], F32, "jf")
        P.op("pool", lambda e: e.iota(jf.t[:], pattern=[[1, 16]], base=0, channel_multiplier=0,
                                      allow_small_or_imprecise_dtypes=True), writes=[jf])
        inv = C.alloc(tmp, [128, 16], F32, "inv")
        act(C, inv, inv.t[:], jf, jf.t[:], AF.Exp, scale=-math.log(10000.0) / 16.0)
        rowp = C.alloc(tmp, [128, NT], F32, "rowp")
        P.op("pool", lambda e: e.iota(rowp.t[:], pattern=[[2, NT]], base=0, channel_multiplier=0,
                                      allow_small_or_imprecise_dtypes=True), writes=[rowp])
        ts(C, "dve", rowp, rowp.t[:], rowp, rowp.t[:], hlf.t[:, 0:1], None, ALU.add, reads=[hlf])
        ang = C.alloc(tmp, [128, NT, 32], F32, "ang")
        tt(C, "dve", ang, ang.t[:, :, 0:16], rowp, rowp.t[:].unsqueeze(2).to_broadcast([128, NT, 16]),
           inv, inv.t[:].unsqueeze(1).to_broadcast([128, NT, 16]), ALU.mult)
        colang = C.alloc(tmp, [128, 16], F32, "colang")
        ts(C, "dve", colang, colang.t[:], inv, inv.t[:], hlf.t[:, 1:2], None, ALU.mult, reads=[hlf])
        cp(C, "dve", ang, ang.t[:, :, 16:32], colang, colang.t[:].unsqueeze(1).to_broadcast([128, NT, 16]))
        ni = C.alloc(tmp, [128, NT, 32], I32, "ni")
        nf = C.alloc(tmp, [128, NT, 32], F32, "nf")
        red = C.alloc(tmp, [128, NT, 32], F32, "red")
        two_pi = 2.0 * math.pi
        for which, shift in ((1, 0.0), (0, math.pi / 2.0)):
            ts(C, "dve", nf, nf.t[:], ang, ang.t[:], shift, 1.0 / two_pi, ALU.add, ALU.mult)
            cp(C, "dve", ni, ni.t[:], nf, nf.t[:])
            cp(C, "dve", nf, nf.t[:], ni, ni.t[:])
            stt(C, "dve", red, red.t[:], nf, nf.t[:], -two_pi, ang, ang.t[:], ALU.mult, ALU.add)
            ts(C, "dve", red, red.t[:], red, red.t[:], shift, 3.1415925, ALU.add, ALU.min)
            ts(C, "dve", red, red.t[:], red, red.t[:], -3.1415925, None, ALU.max)
            act(C, rope, rope.t[:, :, which, :], red, red.t[:], AF.Sin)
    return rope


OFF = dict(aq=0, ak=512, av=640, hq=768, hff=1280, hfb=1792, hi=2304, hg=2816, gate=3328)


def phase_a(C, K, ntiles=NT):
    nc, P = C.nc, C.P
    I = C.ins
    Sc = C.scr
    with contextlib.ExitStack() as st:
        K = dict(K)
        K["rope"] = build_rope(C, st)
        P.barrier()
        w_in = C.alloc(st, [128, 8, 5376], BF16, "w_in")
        wv = I["w_in"].t.rearrange("(k p) n -> p k n", p=128)
        for c0 in range(0, 5376, 672):
            P.dma("pool", w_in.t[:, :, c0:c0 + 672], wv[:, :, c0:c0 + 672], reads=[I["w_in"]], writes=[w_in])
        gmix = C.alloc(st, [128, 1024], F32, "gmix")
        P.dma("sp", gmix.t[:], I["norm_mix"].t.to_broadcast([128, 1024]), writes=[gmix])
        qg = C.alloc(st, [128, 8, 64], F32, "qg")
        P.dma("sp", qg.t[:], I["q_norm"].t.unsqueeze(1).to_broadcast([128, 8, 64]), writes=[qg])
        kg = C.alloc(st, [128, 2, 64], F32, "kg")
        P.dma("sp", kg.t[:], I["k_norm"].t.unsqueeze(1).to_broadcast([128, 2, 64]), writes=[kg])
        raw = C.alloc(st, [128, 2, 2, 512], F32, "raw")
        P.dma("sp", raw.t[:], I["hg_lb_raw"].t.unsqueeze(0).to_broadcast([128, 2, 2, 512]), writes=[raw])
        lbb = C.alloc(st, [128, 2, 512], F32, "lbb")
        omlb = C.alloc(st, [128, 2, 512], F32, "omlb")
        tt(C, "dve", lbb, lbb.t[:], raw, raw.t[:, 0], raw, raw.t[:, 1], ALU.subtract)
        act(C, omlb, omlb.t[:], lbb, lbb.t[:], AF.Sigmoid, scale=-1.0)
        act(C, lbb, lbb.t[:], lbb, lbb.t[:], AF.Sigmoid)
        rawT = C.alloc(st, [128, 2, 2, 4], F32, "rawT")
        P.dma("sp", rawT.t[:], I["hg_lb_raw"].t.rearrange("s r (c p) -> p s r c", p=128), writes=[rawT],
              allow_slow_non_contiguous=True)
        omlT = C.alloc(st, [128, 2, 4], F32, "omlT")
        tt(C, "dve", omlT, omlT.t[:], rawT, rawT.t[:, 0], rawT, rawT.t[:, 1], ALU.subtract)
        act(C, omlT, omlT.t[:], omlT, omlT.t[:], AF.Sigmoid, scale=-1.0)

        xr = C.ring(st, 3, [128, 1024], F32, "xt")
        junk = C.alloc(st, [128, 1024], BF16, "junk")
        stat = C.ring(st, 8, [128, 16], F32, "stat")
        hbr = C.ring(st, 2, [128, 1024], BF16, "hb")
        hTr = C.ring(st, 2, [128, 8, 512], BF16, "hT")
        ptr = C.ring(st, 2, [128, 8, 128], BF16, "ptr", psum=True)
        pfr = C.ring(st, 2, [128, 512], F32, "pf", psum=True)
        ptk = C.ring(st, 3, [128, 512], F32, "ptk", psum=True)
        pqt = C.alloc(st, [128, 8, 128], BF16, "pqt", psum=True)
        stg_bf = C.ring(st, 4, [128, 512], BF16, "stgb")
        stg_f = C.ring(st, 3, [128, 512], F32, "stgf")
        tmpf = C.ring(st, 4, [128, 512], F32, "tmpf")
        qn = C.ring(st, 3, [128, 512], F32, "qn")
        qr = C.ring(st, 2, [128, 512], BF16, "qr")
        kk = C.ring(st, 2, [128, 4, 64], BF16, "kk")
        kn_ = C.ring(st, 2, [128, 128], F32, "kn")
        vst = C.ring(st, 2, [128, 2, 128], BF16, "vst")
        for v_ in vst.items:
            P.op("pool", lambda e, v_=v_: e.memset(v_.t[:], 1.0), writes=[v_])
        qTs = C.ring(st, 2, [128, 6, 128], BF16, "qTs")
        rt = C.ring(st, 8, [128, 8, 2, 16], F32, "rt")

        def run_rr(gens):
            gens = list(gens)
            while gens:
                for g_ in list(gens):
                    try:
                        next(g_)
                    except StopIteration:
                        gens.remove(g_)

        def rope_norm(src_ps, src_ap, nh, gain, cs, dst_t, dst_ap4):
            w = nh * 64
            sq = tmpf.next()
            act(C, sq, sq.t[:, 0:w], src_ps, src_ap, AF.Square)
            yield
            s8 = stat.next()
            P.op("dve", lambda e: e.tensor_reduce(out=s8.t[:, 0:nh], in_=sq.t[:, 0:w].rearrange("p (h d) -> p h d", d=64),
                                                  axis=AX.X, op=ALU.add), reads=[sq], writes=[s8])
            yield
            ap = s8.t[:, 0:nh]
            ts(C, "dve", s8, ap, s8, ap, 1.0 / 64, EPS, ALU.mult, ALU.add)
            yield
            C.P.op("act", lambda e: e.activation(out=ap, in_=ap, func=AF.Sqrt), reads=[s8], writes=[s8])
            yield
            C.P.op("dve", lambda e: e.reciprocal(out=ap, in_=ap), reads=[s8], writes=[s8])
            yield
            n_ = qn.next()
            nv = n_.t[:, 0:w].rearrange("p (h d) -> p h d", d=64)
            tt(C, "dve", n_, nv, src_ps, src_ap.rearrange("p (h d) -> p h d", d=64),
               s8, s8.t[:, 0:nh].unsqueeze(2).to_broadcast([128, nh, 64]), ALU.mult)
            yield
            tt(C, "pool", n_, nv, n_, nv, gain, gain.t[:, 0:nh, :], ALU.mult)
            yield
            v5 = n_.t[:, 0:w].rearrange("p (h a b f) -> p h a b f", a=2, b=2, f=16)
            x1 = v5[:, :, :, 0, :]
            x2 = v5[:, :, :, 1, :]
            cb = cs.t[:, 0, :].rearrange("p (a f) -> p a f", a=2).unsqueeze(1).to_broadcast([128, nh, 2, 16])
            sb_ = cs.t[:, 1, :].rearrange("p (a f) -> p a f", a=2).unsqueeze(1).to_broadcast([128, nh, 2, 16])
            t1, t2 = rt.next(), rt.next()
            tt(C, "dve", t1, t1.t[:, 0:nh], n_, x1, cs, cb, ALU.mult)
            tt(C, "pool", t2, t2.t[:, 0:nh], n_, x2, cs, sb_, ALU.mult)
            yield
            t3, t4 = rt.next(), rt.next()
            tt(C, "dve", t3, t3.t[:, 0:nh], n_, x1, cs, sb_, ALU.mult)
            tt(C, "pool", t4, t4.t[:, 0:nh], n_, x2, cs, cb, ALU.mult)
            yield
            tt(C, "dve", dst_t, dst_ap4[:, :, :, 0, :], t1, t1.t[:, 0:nh], t2, t2.t[:, 0:nh], ALU.subtract)
            yield
            tt(C, "pool", dst_t, dst_ap4[:, :, :, 1, :], t3, t3.t[:, 0:nh], t4, t4.t[:, 0:nh], ALU.add)
            yield

        def prep_tile(g, j, hT):
            ti = g * 4 + j
            xt = xr.next()
            P.dma("sp", xt.t[:], I["x"].t[ti * 128:(ti + 1) * 128, :], reads=[I["x"]], writes=[xt])
            s_ = stat.next()
            act(C, junk, junk.t[:], xt, xt.t[:], AF.Square, accum_out=s_.t[:, 0:1], extra_w=[s_])
            yield
            ap = s_.t[:, 0:1]
            ts(C, "dve", s_, ap, s_, ap, 1.0 / D, EPS, ALU.mult, ALU.add)
            yield
            C.P.op("act", lambda e: e.activation(out=ap, in_=ap, func=AF.Sqrt), reads=[s_], writes=[s_])
            yield
            C.P.op("dve", lambda e: e.reciprocal(out=ap, in_=ap), reads=[s_], writes=[s_])
            yield
            hb = hbr.next()
            stt(C, "dve", hb, hb.t[:], xt, xt.t[:], s_.t[:, 0:1], gmix, gmix.t[:], ALU.mult, ALU.mult, reads=[s_])
            yield
            pt = ptr.next()
            for k in range(8):
                tr(C, pt, pt.t[:, k, :], hb, hb.t[:, k * 128:(k + 1) * 128], K["ident"])
            yield
            cp(C, "act", hT, hT.t[:, :, j * 128:(j + 1) * 128], pt, pt.t[:])
            yield

        ngr = ntiles // 4
        hTs = {0: hTr.next()}
        for j in range(4):
            run_rr([prep_tile(0, j, hTs[0])])
        for g in range(ngr):
            hT = hTs[g]
            if g + 1 < ngr:
                hTs[g + 1] = hTr.next()
            cols = slice(g * 512, (g + 1) * 512)
            fm = [("hq", OFF["hq"] + c * 128, c) for c in range(4)] + \
                 [("hff", OFF["hff"] + c * 128, c) for c in range(4)] + \
                 [("hfb", OFF["hfb"] + c * 128, c) for c in range(4)] + \
                 [("gate", OFF["gate"] + c * 128, c) for c in range(16)]
            for kind, c0, c in fm:
                pf = pfr.next()
                for k in range(8):
                    mm(C, pf, pf.t[:], w_in, w_in.t[:, k, c0:c0 + 128], hT, hT.t[:, k, :], k == 0, k == 7)
                sg = stg_bf.next()
                if kind == "hq":
                    act(C, sg, sg.t[:], pf, pf.t[:], AF.Silu)
                    dst = Sc["hqT"]
                    P.dma("pool", dst.t[c, :, cols], sg.t[:], reads=[sg], writes=[dst.reg((c, g))])
                elif kind in ("hff", "hfb"):
                    d_ = 0 if kind == "hff" else 1
                    tf = tmpf.next()
                    act(C, tf, tf.t[:], pf, pf.t[:], AF.Sigmoid, scale=-1.0)
                    ts(C, "dve", sg, sg.t[:], tf, tf.t[:], omlT.t[:, d_, c:c + 1], None, ALU.mult, reads=[omlT])
                    dst = Sc["kT"]
                    P.dma("pool", dst.t[d_, c, :, cols], sg.t[:], reads=[sg], writes=[dst.reg((d_, c, g))])
                else:
                    act(C, sg, sg.t[:], pf, pf.t[:], AF.Sigmoid)
                    dst = Sc["gtsT"]
                    P.dma("pool", dst.t[c, :, cols], sg.t[:], reads=[sg], writes=[dst.reg((c, g))])
            for j in range(4):
                ti = g * 4 + j
                rows = slice(ti * 128, (ti + 1) * 128)
                lhs = lambda k, j=j: hT.t[:, k, j * 128:(j + 1) * 128]
                cs = T(K["rope"].t[:, ti], "rope_v")
                cs.b = K["rope"].b
                q_ = qr.next()
                k_ = kk.next()

                def chain_q():
                    pq = ptk.next()
                    for k in range(8):
                        mm(C, pq, pq.t[:], hT, lhs(k), w_in, w_in.t[:, k, 0:512], k == 0, k == 7)
                    yield
                    yield from rope_norm(pq, pq.t[:], 8, qg, cs, q_, q_.t[:].rearrange("p (h a b f) -> p h a b f", a=2, b=2, f=16))

                def chain_kv():
                    pkv = ptk.next()
                    for k in range(8):
                        mm(C, pkv, pkv.t[:, 0:256], hT, lhs(k), w_in, w_in.t[:, k, 512:768], k == 0, k == 7)
                    yield
                    v_ = vst.next()
                    cp(C, "act", v_, v_.t[:, :, 0:64], pkv, pkv.t[:, 128:256].rearrange("p (h d) -> p h d", d=64))
                    P.dma("pool", Sc["v"].t[ti], v_.t[:], reads=[v_], writes=[Sc["v"].reg(ti)])
                    yield
                    kview = k_.t[:].rearrange("p (h r) d -> p h r d", r=2)
                    yield from rope_norm(pkv, pkv.t[:, 0:128], 2, kg, cs, k_,
                                         kview[:, :, 0, :].rearrange("p h (a b f) -> p h a b f", a=2, b=2, f=16))
                    cp(C, "pool", k_, kview[:, :, 1, :], k_, kview[:, :, 0, :])
                    yield

                def chain_gate(d_, key):
                    pg = ptk.next()
                    for k in range(8):
                        mm(C, pg, pg.t[:], hT, lhs(k), w_in, w_in.t[:, k, OFF[key]:OFF[key] + 512], k == 0, k == 7)
                    yield
                    tf = tmpf.next()
                    act(C, tf, tf.t[:], pg, pg.t[:], AF.Sigmoid)
                    yield
                    tt(C, "dve", tf, tf.t[:], tf, tf.t[:], omlb, omlb.t[:, d_, :], ALU.mult)
                    yield
                    tt(C, "dve", tf, tf.t[:], tf, tf.t[:], lbb, lbb.t[:, d_, :], ALU.add)
                    yield
                    gf = stg_f.next()
                    act(C, gf, gf.t[:], tf, tf.t[:], AF.Ln)
                    P.dma("pool", Sc["g"].t[d_, rows, :], gf.t[:], reads=[gf], writes=[Sc["g"].reg((d_, ti))])
                    kb = stg_bf.next()
                    ts(C, "pool", kb, kb.t[:], tf, tf.t[:], -1.0, 1.0, ALU.mult, ALU.add)
                    P.dma("pool", Sc["k"].t[d_, rows, :], kb.t[:], reads=[kb], writes=[Sc["k"].reg((d_, ti))])
                    yield

                def chain_h(key, dstn, fn):
                    ph = ptk.next()
                    for k in range(8):
                        mm(C, ph, ph.t[:], hT, lhs(k), w_in, w_in.t[:, k, OFF[key]:OFF[key] + 512], k == 0, k == 7)
                    yield
                    sb_ = stg_bf.next()
                    if fn is None:
                        cp(C, "act", sb_, sb_.t[:], ph, ph.t[:])
                    else:
                        act(C, sb_, sb_.t[:], ph, ph.t[:], fn)
                    P.dma("pool", Sc[dstn].t[rows, :], sb_.t[:], reads=[sb_], writes=[Sc[dstn].reg(ti)])
                    yield

                chains = [chain_q(), chain_kv(), chain_gate(0, "hff")]
                if g + 1 < ngr:
                    chains.append(prep_tile(g + 1, j, hTs[g + 1]))
                run_rr(chains)
                run_rr([chain_gate(1, "hfb"), chain_h("hi", "hi", None), chain_h("hg", "sg", AF.Silu)])
                for pr in range(4):
                    tr(C, pqt, pqt.t[:, pr, :], q_, q_.t[:, pr * 128:(pr + 1) * 128], K["ident"])
                kflat = k_.t[:].rearrange("p a d -> p (a d)")
                for kv in range(2):
                    tr(C, pqt, pqt.t[:, 4 + kv, :], k_, kflat[:, kv * 128:(kv + 1) * 128], K["ident"])
                qs = qTs.next()
                cp(C, "act", qs, qs.t[:], pqt, pqt.t[:, 0:6, :])
                P.dma("pool", Sc["qT"].t[:, :, rows].rearrange("r p t -> p r t"), qs.t[:, 0:4, :], reads=[qs], writes=[Sc["qT"].reg(ti)])
                P.dma("pool", Sc["kTa"].t[:, :, rows].rearrange("r p t -> p r t"), qs.t[:, 4:6, :], reads=[qs], writes=[Sc["kTa"].reg(ti)])


def phase_b(C, K, ngroups=8, hhs=(0, 1), bg=()):
    nc, P = C.nc, C.P
    Sc = C.scr
    with contextlib.ExitStack() as st:
        kT = [C.alloc(st, [128, S], BF16, "kTsb") for _ in range(2)]
        for kv in range(2):
            for hf in range(2):
                cs_ = slice(hf * 2048, (hf + 1) * 2048)
                P.dma("sp", kT[kv].t[:, cs_], Sc["kTa"].t[kv, :, cs_],
                      reads=Sc["kTa"].regl(range(hf * 16, hf * 16 + 16)), writes=[kT[kv]])
        vs = C.alloc(st, [128, NT, 256], BF16, "vsb")
        for hf in range(4):
            P.dma("sp", vs.t[:, hf * 8:(hf + 1) * 8, :], Sc["v"].t[hf * 8:(hf + 1) * 8].rearrange("t p h c -> p t (h c)"),
                  reads=Sc["v"].regl(range(hf * 8, hf * 8 + 8)), writes=[vs])
        if "dbgvs" in Sc:
            P.dma("pool", Sc["dbgvs"].t[:], vs.t[:], reads=[vs], writes=[Sc["dbgvs"]])
        qr_ = C.ring(st, 2, [128, 512], BF16, "qTg")
        psS = C.ring(st, 4, [128, 512], F32, "psS", psum=True)
        acc = [C.alloc(st, [128, 512], F32, "acc", psum=True) for _ in range(2)]
        ptr_ = C.ring(st, 8, [128, 512], BF16, "pT")
        rl = C.ring(st, 2, [128, 512], F32, "rl")
        obr = C.ring(st, 2, [128, 512], BF16, "ob")
        LAG = 3
        steps = [(g, pr, kt, hh) for g in range(ngroups) for pr in range(4) for kt in range(NT) for hh in hhs]
        state = {}
        accs = [acc, [C.alloc(st, [128, 512], F32, "acc2", psum=True) for _ in range(2)]]

        def stage1(g, pr, kt, hh):
            kv = pr // 2
            if kt == 0 and hh == hhs[0]:
                q = qr_.next()
                P.dma("sp", q.t[:], Sc["qT"].t[pr, :, g * 512:(g + 1) * 512], reads=Sc["qT"].regl(range(4 * g, 4 * g + 4)), writes=[q])
                state["q", g, pr] = q
            q = state["q", g, pr]
            rows = slice(hh * 64, (hh + 1) * 64)
            s_ = psS.next()
            mm(C, s_, s_.t[:], kT[kv], kT[kv].t[rows, kt * 128:(kt + 1) * 128], q, q.t[rows, :], True, True)
            p_ = ptr_.next()
            act(C, p_, p_.t[:], s_, s_.t[:], AF.Exp, scale=0.125)
            state["p", g, pr, kt, hh] = p_

        def stage2(g, pr, kt, hh):
            kv = pr // 2
            p_ = state.pop(("p", g, pr, kt, hh))
            ac = accs[(g * 4 + pr) % 2]
            mm(C, ac[hh], ac[hh].t[:], vs, vs.t[:, kt, kv * 128:(kv + 1) * 128], p_, p_.t[:], kt == 0, kt == NT - 1)
            if kt == NT - 1 and hh == hhs[-1]:
                ob = obr.next()
                for h2_ in hhs:
                    r_ = rl.next()
                    C.P.op("dve", lambda e, r_=r_, h2_=h2_, ac=ac: e.reciprocal(out=r_.t[64:128, :], in_=ac[h2_].t[64:128, :]),
                           reads=[ac[h2_]], writes=[r_])
                    tt(C, "dve", ob, ob.t[h2_ * 64:(h2_ + 1) * 64, :], ac[h2_], ac[h2_].t[0:64, :], r_, r_.t[64:128, :], ALU.mult)
                P.dma("pool", Sc["attoT"].t[pr, :, g * 512:(g + 1) * 512], ob.t[:], reads=[ob], writes=[Sc["attoT"].reg((pr, g))])

        bg = list(bg)
        LAG = 4
        for it in range(0, len(steps) + LAG, 2):
            for i_ in (it, it + 1):
                if i_ < len(steps):
                    stage1(*steps[i_])
            for i_ in (it - LAG, it - LAG + 1):
                if 0 <= i_ < len(steps):
                    stage2(*steps[i_])
            if bg and (it // 2) % 3 == 2:
                bg.pop(0)()
        for job in bg:
            job()


def build_masks(C, st):
    P = C.P
    M = {}
    specs = {
        "f_incl": (ALU.is_ge, 0, 1, -1),
        "f_excl": (ALU.is_gt, 0, -1, 1),
        "b_incl": (ALU.is_ge, 0, -1, 1),
        "b_excl": (ALU.is_gt, 0, 1, -1),
    }
    for name, (op, base, tmul, pmul) in specs.items():
        m = C.alloc(st, [128, 128], F32, "m_" + name)
        P.op("pool", lambda e, m=m: e.memset(m.t[:], 1.0), writes=[m])
        P.op("pool", lambda e, m=m, op=op, base=base, tmul=tmul, pmul=pmul: e.affine_select(
            out=m.t[:], in_=m.t[:], pattern=[[tmul, 128]], compare_op=op, fill=0.0, base=base, channel_multiplier=pmul),
            reads=[m], writes=[m])
        P.op("pool", lambda e, m=m: e.memset(m.t[0:64, 64:128], 0.0), reads=[m], writes=[m])
        P.op("pool", lambda e, m=m: e.memset(m.t[64:128, 0:64], 0.0), reads=[m], writes=[m])
        M[name] = m
    return M


def phase_c(C, K, ntiles=NT, dirs=(0, 1)):
    nc, P = C.nc, C.P
    Sc = C.scr
    I = C.ins
    with contextlib.ExitStack() as st:
        M = build_masks(C, st)
        gon = C.alloc(st, [128, 4, 128], F32, "gon")
        P.dma("sp", gon.t[:], I["hg_out_norm"].t.unsqueeze(1).to_broadcast([128, 4, 128]), writes=[gon])
        gr = C.ring(st, 2, [128, 512], F32, "g_t")
        kdr = C.ring(st, 2, [128, 512], BF16, "kd_t")
        kTr = C.ring(st, 2, [128, 4, 128], BF16, "kT_t")
        qTr = C.ring(st, 2, [128, 4, 128], BF16, "hqT_t")
        vr = C.ring(st, 2, [128, 512], BF16, "v_t")
        ofr = C.ring(st, 2, [128, 512], F32, "of_t")
        sgr = C.ring(st, 2, [128, 512], BF16, "sg_t")
        prx = C.alloc(st, [128, 512], F32, "prx", psum=True)
        pbT = C.alloc(st, [128, 4, 128], F32, "pbT", psum=True)
        pX = [C.alloc(st, [128, 4, 128], F32, "pX", psum=True) for _ in range(2)]
        pOs = [C.alloc(st, [128, 4, 128], F32, "pOs", psum=True) for _ in range(2)]
        pTr = C.alloc(st, [128, 8, 128], BF16, "pTr", psum=True)
        ebT = C.ring(st, 2, [128, 4, 128], F32, "ebT")
        enbT = C.ring(st, 2, [128, 4, 128], F32, "enbT")
        er = C.ring(st, 2, [128, 512], F32, "er")
        qfull = C.ring(st, 2, [128, 4, 128], BF16, "qfull")
        qlo = C.ring(st, 2, [128, 4, 128], BF16, "qlo")
        qhi = C.ring(st, 2, [128, 4, 128], BF16, "qhi")
        for t_ in qlo.items + qhi.items:
            P.op("pool", lambda e, t_=t_: e.memset(t_.t[:], 0.0), writes=[t_])
        ktil = C.ring(st, 2, [128, 4, 128], BF16, "ktil")
        kdec = C.ring(st, 2, [128, 512], BF16, "kdec")
        atm = C.ring(st, 4, [128, 128], BF16, "atm")
        S32 = [C.alloc(st, [128, 128], F32, "S32") for _ in range(4)]
        Sbf = [C.alloc(st, [128, 128], BF16, "Sbf") for _ in range(4)]
        osb = C.ring(st, 2, [128, 512], F32, "osb")
        tot = C.ring(st, 2, [128, 512], F32, "tot")
        sqt = C.ring(st, 2, [128, 512], F32, "sqt")
        stat = C.ring(st, 2, [128, 8], F32, "statc")
        onb = C.ring(st, 2, [128, 512], BF16, "onb")
        oTs = C.ring(st, 2, [128, 4, 128], BF16, "oTs")

        for d_ in dirs:
            Mi = M["f_incl"] if d_ == 0 else M["b_incl"]
            Me = M["f_excl"] if d_ == 0 else M["b_excl"]
            for hd in range(4):
                P.op("pool", lambda e, hd=hd: e.memset(S32[hd].t[:], 0.0), writes=[S32[hd]])
                P.op("pool", lambda e, hd=hd: e.memset(Sbf[hd].t[:], 0.0), writes=[Sbf[hd]])
            order = list(range(ntiles)) if d_ == 0 else list(range(ntiles - 1, -1, -1))
            def pro(ti):
                rows = slice(ti * 128, (ti + 1) * 128)
                g_t, kd_t, kT_t, q_t, v_t = gr.next(), kdr.next(), kTr.next(), qTr.next(), vr.next()
                P.dma("sp", g_t.t[:], Sc["g"].t[d_, rows, :], reads=[Sc["g"].reg((d_, ti))], writes=[g_t])
                P.dma("sp", kd_t.t[:], Sc["k"].t[d_, rows, :], reads=[Sc["k"].reg((d_, ti))], writes=[kd_t])
                P.dma("sp", kT_t.t[:], Sc["kT"].t[d_, :, :, rows].rearrange("h p t -> p h t"),
                      reads=[Sc["kT"].reg((d_, c, ti // 4)) for c in range(4)], writes=[kT_t])
                P.dma("sp", q_t.t[:], Sc["hqT"].t[:, :, rows].rearrange("h p t -> p h t"),
                      reads=[Sc["hqT"].reg((c, ti // 4)) for c in range(4)], writes=[q_t])
                P.dma("sp", v_t.t[:], Sc["hi"].t[rows, :], reads=[Sc["hi"].reg(ti)], writes=[v_t])
                mm(C, prx, prx.t[:], Me, Me.t[:], g_t, g_t.t[:], True, True)
                for hd in range(4):
                    mm(C, pbT, pbT.t[:, hd, :], g_t, g_t.t[:, hd * 128:(hd + 1) * 128], Mi, Mi.t[:], True, True)
                eb, enb, er_ = ebT.next(), enbT.next(), er.next()
                act(C, eb, eb.t[:], pbT, pbT.t[:], AF.Exp)
                act(C, enb, enb.t[:], pbT, pbT.t[:], AF.Exp, scale=-1.0)
                act(C, er_, er_.t[:], prx, prx.t[:], AF.Exp)
                qf, ql, qh, kt_, kdc = qfull.next(), qlo.next(), qhi.next(), ktil.next(), kdec.next()
                tt(C, "dve", qf, qf.t[:], q_t, q_t.t[:], eb, eb.t[:], ALU.mult)
                cp(C, "pool", ql, ql.t[:, :, 0:64], qf, qf.t[:, :, 0:64])
                cp(C, "pool", qh, qh.t[:, :, 64:128], qf, qf.t[:, :, 64:128])
                tt(C, "dve", kt_, kt_.t[:], kT_t, kT_t.t[:], enb, enb.t[:], ALU.mult)
                tt(C, "pool", kdc, kdc.t[:], kd_t, kd_t.t[:], er_, er_.t[:], ALU.mult)
                return dict(kd_t=kd_t, v_t=v_t, eb=eb, qf=qf, ql=ql, qh=qh, kt_=kt_, kdc=kdc)

            def tile_body(ti, B_):
                rows = slice(ti * 128, (ti + 1) * 128)
                kd_t, v_t, eb, qf, ql, qh, kt_, kdc = (B_[k_] for k_ in ('kd_t', 'v_t', 'eb', 'qf', 'ql', 'qh', 'kt_', 'kdc'))
                if d_ == 0:
                    ca, cb, qa, qb, la, lb_ = 0, 1, ql, qh, 63, 127
                else:
                    ca, cb, qa, qb, la, lb_ = 1, 0, qh, ql, 64, 0
                ra = slice(ca * 64, (ca + 1) * 64)
                rb = slice(cb * 64, (cb + 1) * 64)
                def head_chain(hd):
                    hc = slice(hd * 128, (hd + 1) * 128)
                    X, O_ = pX[hd % 2], pOs[hd % 2]
                    oa = O_.t[:, hd // 2, :]
                    mm(C, X, X.t[:, 0, :], kt_, kt_.t[:, hd, :], qf, qf.t[:, hd, :], True, True)
                    mm(C, O_, oa, qa, qa.t[:, hd, :], Sbf[hd], Sbf[hd].t[:], True, False)
                    mm(C, X, X.t[:, 1, :], kdc, kdc.t[ra, hc], v_t, v_t.t[ra, hc], True, True)
                    yield
                    am = atm.next()
                    tt(C, "dve", am, am.t[:], X, X.t[:, 0, :], Mi, Mi.t[:], ALU.mult)
                    stt(C, "dve", S32[hd], S32[hd].t[:], S32[hd], S32[hd].t[:], eb.t[:, hd, la:la + 1],
                        X, X.t[:, 1, :], ALU.mult, ALU.add, reads=[eb])
                    yield
                    cp(C, "act", Sbf[hd], Sbf[hd].t[:], S32[hd], S32[hd].t[:])
                    yield
                    mm(C, O_, oa, qb, qb.t[:, hd, :], Sbf[hd], Sbf[hd].t[:], False, False)
                    mm(C, O_, oa, am, am.t[:], v_t, v_t.t[:, hc], False, True)
                    mm(C, X, X.t[:, 2, :], kdc, kdc.t[rb, hc], v_t, v_t.t[rb, hc], True, True)
                    yield
                    stt(C, "dve", S32[hd], S32[hd].t[:], S32[hd], S32[hd].t[:], eb.t[:, hd, lb_:lb_ + 1],
                        X, X.t[:, 2, :], ALU.mult, ALU.add, reads=[eb])
                    yield
                    cp(C, "act", Sbf[hd], Sbf[hd].t[:], S32[hd], S32[hd].t[:])
                    yield

                for pair in ((0, 1), (2, 3)):
                    gens = [head_chain(hd) for hd in pair]
                    while gens:
                        for g_ in list(gens):
                            try:
                                next(g_)
                            except StopIteration:
                                gens.remove(g_)

                def ov(tile_ap, s_):
                    return tile_ap.rearrange("p (a s d) -> p a s d", s=2, d=128)[:, :, s_, :]

                if d_ == 0 and len(dirs) == 2:
                    o_ = osb.next()
                    for s_ in range(2):
                        cp(C, "act", o_, ov(o_.t[:], s_), pOs[s_], pOs[s_].t[:, 0:2, :])
                    P.dma("pool", Sc["ofwd"].t[rows, :], o_.t[:], reads=[o_], writes=[Sc["ofwd"].reg(ti)])
                    return
                t_ = tot.next()
                if len(dirs) == 2:
                    of_ = ofr.next()
                    P.dma("sp", of_.t[:], Sc["ofwd"].t[rows, :], reads=[Sc["ofwd"].reg(ti)], writes=[of_])
                    for s_ in range(2):
                        tt(C, "dve", t_, ov(t_.t[:], s_), pOs[s_], pOs[s_].t[:, 0:2, :], of_, ov(of_.t[:], s_), ALU.add)
                else:
                    for s_ in range(2):
                        cp(C, "dve", t_, ov(t_.t[:], s_), pOs[s_], pOs[s_].t[:, 0:2, :])
                if "dbgo" in Sc:
                    P.dma("pool", Sc["dbgo"].t[rows, :], t_.t[:], reads=[t_], writes=[Sc["dbgo"].reg(ti)])
                sg_ = sgr.next()
                P.dma("sp", sg_.t[:], Sc["sg"].t[rows, :], reads=[Sc["sg"].reg(ti)], writes=[sg_])
                sq = sqt.next()
                act(C, sq, sq.t[:], t_, t_.t[:], AF.Square)
                s4 = stat.next()
                P.op("dve", lambda e, s4=s4, sq=sq: e.tensor_reduce(out=s4.t[:, 0:4], in_=sq.t[:].rearrange("p (h d) -> p h d", d=128),
                                                              axis=AX.X, op=ALU.add), reads=[sq], writes=[s4])
                rsqrt_mean(C, s4, lambda s4=s4: s4.t[:, 0:4], 4, 1.0 / 128)
                t3 = t_.t[:].rearrange("p (h d) -> p h d", d=128)
                tt(C, "dve", t_, t3, t_, t3, s4, s4.t[:, 0:4].unsqueeze(2).to_broadcast([128, 4, 128]), ALU.mult)
                tt(C, "pool", t_, t3, t_, t3, gon, gon.t[:], ALU.mult)
                ob = onb.next()
                tt(C, "dve", ob, ob.t[:], t_, t_.t[:], sg_, sg_.t[:], ALU.mult)
                for hd in range(4):
                    tr(C, pTr, pTr.t[:, hd, :], ob, ob.t[:, hd * 128:(hd + 1) * 128], K["ident"])
                os_ = oTs.next()
                cp(C, "act", os_, os_.t[:], pTr, pTr.t[:, 0:4, :])
                P.dma("pool", Sc["hgoT"].t[:, :, rows].rearrange("h p t -> p h t"), os_.t[:], reads=[os_], writes=[Sc["hgoT"].reg(ti)])

            pend = pro(order[0])
            for idx_, ti in enumerate(order):
                nxt = pro(order[idx_ + 1]) if idx_ + 1 < len(order) else None
                tile_body(ti, pend)
                pend = nxt


def load_w(C, st, name, kchunks, ncols, q="pool"):
    w = C.alloc(st, [128, kchunks, ncols], BF16, name)
    src = C.ins[name].t.rearrange("(k p) n -> p k n", p=128)
    step = min(kchunks, max(1, 4096 // ncols))
    for k0 in range(0, kchunks, step):
        C.P.dma(q, w.t[:, k0:k0 + step, :], src[:, k0:k0 + step, :], reads=[C.ins[name]], writes=[w])
    return w


def norm_transpose(C, K, xt, gain, stat, junk, hb, pt, dst, dst_ap):
    s_ = stat
    act(C, junk, junk.t[:], xt, xt.t[:], AF.Square, accum_out=s_.t[:, 0:1], extra_w=[s_])
    rsqrt_mean(C, s_, lambda: s_.t[:, 0:1], 1, 1.0 / D)
    stt(C, "dve", hb, hb.t[:], xt, xt.t[:], s_.t[:, 0:1], gain, gain.t[:], ALU.mult, ALU.mult, reads=[s_])
    for k in range(8):
        tr(C, pt, pt.t[:, k, :], hb, hb.t[:, k * 128:(k + 1) * 128], K["ident"])
    cp(C, "act", dst, dst_ap, pt, pt.t[:])


def phase_d(C, K, ngroups=8):
    nc, P = C.nc, C.P
    Sc = C.scr
    I = C.ins
    with contextlib.ExitStack() as st:
        wua = load_w(C, st, "w_up_att", 4, 1024)
        wuh = load_w(C, st, "w_up_hg", 4, 1024)
        wo = load_w(C, st, "w_out", 8, 1024)
        gffn = C.alloc(st, [128, 1024], F32, "gffn")
        P.dma("sp", gffn.t[:], I["norm_ffn"].t.to_broadcast([128, 1024]), writes=[gffn])
        aTr = C.ring(st, 2, [128, 4, 512], BF16, "aT")
        hTr_ = C.ring(st, 2, [128, 4, 512], BF16, "hgT")
        gtr = C.ring(st, 2, [128, 16, 512], BF16, "gts")
        pya = C.ring(st, 2, [128, 512], F32, "pya", psum=True)
        pyh = C.ring(st, 2, [128, 512], F32, "pyh", psum=True)
        px = C.ring(st, 2, [128, 512], F32, "px", psum=True)
        pt = C.ring(st, 2, [128, 8, 128], BF16, "ptd", psum=True)
        t1r = C.ring(st, 2, [128, 512], F32, "t1")
        t2r = C.ring(st, 2, [128, 512], F32, "t2")
        mTr = C.ring(st, 2, [128, 8, 512], BF16, "mT")
        xr = C.ring(st, 2, [128, 1024], F32, "xtd")
        x1r = C.ring(st, 3, [128, 1024], F32, "x1t")
        junk = C.alloc(st, [128, 1024], BF16, "junkd")
        stat = C.ring(st, 2, [128, 8], F32, "statd")
        hbr = C.ring(st, 2, [128, 1024], BF16, "hbd")
        h2s = C.ring(st, 2, [128, 8, 128], BF16, "h2s")
        for g in range(ngroups):
            cols = slice(g * 512, (g + 1) * 512)
            aT, hT, gt = aTr.next(), hTr_.next(), gtr.next()
            P.dma("sp", aT.t[:], Sc["attoT"].t[:, :, cols].rearrange("r p t -> p r t"),
                  reads=[Sc["attoT"].reg((pr, g)) for pr in range(4)], writes=[aT])
            P.dma("sp", hT.t[:], Sc["hgoT"].t[:, :, cols].rearrange("r p t -> p r t"),
                  reads=Sc["hgoT"].regl(range(4 * g, 4 * g + 4)), writes=[hT])
            P.dma("sp", gt.t[:], Sc["gtsT"].t[:, :, cols].rearrange("r p t -> p r t"),
                  reads=[Sc["gtsT"].reg((c, g)) for c in range(16)], writes=[gt])
            mT = mTr.next()
            for m_ in range(8):
                ms = slice(m_ * 128, (m_ + 1) * 128)
                ya, yh = pya.next(), pyh.next()
                for kc in range(4):
                    mm(C, ya, ya.t[:], wua, wua.t[:, kc, ms], aT, aT.t[:, kc, :], kc == 0, kc == 3)
                for kc in range(4):
                    mm(C, yh, yh.t[:], wuh, wuh.t[:, kc, ms], hT, hT.t[:, kc, :], kc == 0, kc == 3)
                t1, t2 = t1r.next(), t2r.next()
                tt(C, "dve", t1, t1.t[:], ya, ya.t[:], gt, gt.t[:, m_, :], ALU.mult)
                tt(C, "dve", t2, t2.t[:], yh, yh.t[:], gt, gt.t[:, 8 + m_, :], ALU.mult)
                tt(C, "pool", mT, mT.t[:, m_, :], t1, t1.t[:], t2, t2.t[:], ALU.add)
            def part1(j):
                ti = g * 4 + j
                rows = slice(ti * 128, (ti + 1) * 128)
                xt = xr.next()
                P.dma("sp", xt.t[:], I["x"].t[rows, :], reads=[I["x"]], writes=[xt])
                x1 = x1r.next()
                for hf in range(2):
                    hs = slice(hf * 512, (hf + 1) * 512)
                    p_ = px.next()
                    for m_ in range(8):
                        mm(C, p_, p_.t[:], mT, mT.t[:, m_, j * 128:(j + 1) * 128], wo, wo.t[:, m_, hs], m_ == 0, m_ == 7)
                    tt(C, "dve", x1, x1.t[:, hs], p_, p_.t[:], xt, xt.t[:, hs], ALU.add)
                P.dma("pool", Sc["x1"].t[rows, :], x1.t[:], reads=[x1], writes=[Sc["x1"].reg(ti)])
                return x1

            def part2(j, x1):
                ti = g * 4 + j
                hs_ = h2s.next()
                norm_transpose(C, K, x1, gffn, stat.next(), junk, hbr.next(), pt.next(), hs_, hs_.t[:])
                P.dma("pool", Sc["h2T"].t[ti], hs_.t[:], reads=[hs_], writes=[Sc["h2T"].reg(ti)])

            pend = part1(0)
            for j in range(4):
                nxt = part1(j + 1) if j + 1 < 4 else None
                part2(j, pend)
                pend = nxt


def phase_e1(C, K, ngroups=16):
    nc, P = C.nc, C.P
    Sc = C.scr
    I = C.ins
    with contextlib.ExitStack() as st:
        wq = load_w(C, st, "peer_wq", 8, 2048)
        skT = C.alloc(st, [128, 16, 128], BF16, "skT")
        P.dma("pool", skT.t[:], I["skT"].t, reads=[I["skT"]], writes=[skT])
        io_f = C.alloc(st, [128, 128], F32, "io_f")
        P.op("pool", lambda e: e.iota(io_f.t[:], pattern=[[1, 128]], base=0, channel_multiplier=0,
                                      allow_small_or_imprecise_dtypes=True), writes=[io_f])
        io_b = C.alloc(st, [128, 128], BF16, "io_b")
        cp(C, "dve", io_b, io_b.t[:], io_f, io_f.t[:])
        io_rep = C.alloc(st, [128, 128, 16], BF16, "io_rep")
        cp(C, "dve", io_rep, io_rep.t[:], io_f, io_f.t[:].unsqueeze(2).to_broadcast([128, 128, 16]))
        h2r = C.ring(st, 2, [128, 8, 128], BF16, "h2e")
        pq = C.ring(st, 2, [128, 4, 128], F32, "pq", psum=True)
        psc = C.ring(st, 2, [128, 4, 128], F32, "psc", psum=True)
        pIG = C.alloc(st, [128, 8, 128], BF16, "pIG", psum=True)
        pG = C.ring(st, 3, [128, 4, 128], F32, "pG", psum=True)
        qpT = C.ring(st, 2, [128, 16, 128], BF16, "qpT")
        s_all = C.ring(st, 2, [128, 16, 128], F32, "s_all")
        tmp128 = C.ring(st, 4, [128, 128], F32, "tmp128")
        v16 = C.ring(st, 2, [128, 16, 16], F32, "v16")
        i16 = C.ring(st, 2, [128, 16, 16], U32, "i16")
        i16f = C.ring(st, 2, [128, 16, 16], F32, "i16f")
        cand = C.ring(st, 1, [128, 8, 256], F32, "cand")
        tmp256 = C.ring(st, 4, [128, 256], F32, "tmp256")
        tsv = C.ring(st, 2, [128, 8, 16], F32, "tsv")
        pos = C.ring(st, 2, [128, 8, 16], U32, "pos")
        k12i = C.ring(st, 2, [128, 2, 128], I32, "k12i")
        k12f = C.ring(st, 2, [128, 2, 128], F32, "k12f")
        eq = C.ring(st, 2, [128, 128, 16], F32, "eq")
        IG = C.ring(st, 2, [128, 3, 128], BF16, "IG")
        IGf = C.ring(st, 2, [128, 3, 128], F32, "IGf")
        IGT = C.ring(st, 2, [128, 3, 128], BF16, "IGT")
        ex = C.ring(st, 2, [128, 8, 16], F32, "ex")
        st8 = C.ring(st, 2, [128, 8], F32, "st8")
        A4 = C.ring(st, 3, [128, 16, 128], BF16, "A4")
        B4 = C.ring(st, 3, [128, 16, 128], BF16, "B4")
        Gst = C.ring(st, 1, [128, 128, 256], BF16, "Gst")
        est = {}

        def stageXc(grp, j2):
            ti = grp * 2 + j2
            h2 = h2r.next()
            P.dma("sp", h2.t[:], Sc["h2T"].t[ti], reads=[Sc["h2T"].reg(ti)], writes=[h2])
            qp, sa = qpT.next(), s_all.next()
            for c4 in range(4):
                p_ = pq.next()
                for cc in range(4):
                    cq = c4 * 4 + cc
                    for k in range(8):
                        mm(C, p_, p_.t[:, cc, :], wq, wq.t[:, k, cq * 128:(cq + 1) * 128], h2, h2.t[:, k, :], k == 0, k == 7)
                cp(C, "act", qp, qp.t[:, c4 * 4:(c4 + 1) * 4, :], p_, p_.t[:])
            for c4 in range(4):
                p_ = psc.next()
                for cc in range(4):
                    cq = c4 * 4 + cc
                    mm(C, p_, p_.t[:, cc, :], qp, qp.t[:, cq, :], skT, skT.t[:, cq, :], True, True)
                cp(C, "act", sa, sa.t[:, c4 * 4:(c4 + 1) * 4, :], p_, p_.t[:])
            if "dbgs" in Sc:
                P.dma("pool", Sc["dbgs"].t[ti], sa.t[:], reads=[sa], writes=[Sc["dbgs"].reg(ti)])
            est["sa", grp, j2] = sa

        def stageXt(grp, j2):
            ti = grp * 2 + j2
            sa = est.pop(("sa", grp, j2))
            v_, i_ = v16.next(), i16.next()

            def top16(src_t, src_ap, vdst_t, vdst_ap, idst_t, idst_ap, tmp):
                P.op("dve", lambda e: e.max(out=vdst_ap[:, 0:8], in_=src_ap), reads=[src_t], writes=[vdst_t])
                P.op("dve", lambda e: e.match_replace(out=tmp.t[:], in_to_replace=vdst_ap[:, 0:8], in_values=src_ap,
                                                      imm_value=-1e30), reads=[src_t, vdst_t], writes=[tmp])
                P.op("dve", lambda e: e.max(out=vdst_ap[:, 8:16], in_=tmp.t[:]), reads=[tmp, vdst_t], writes=[vdst_t])
                P.op("dve", lambda e: e.max_index(out=idst_ap[:, 0:8], in_max=vdst_ap[:, 0:8], in_values=src_ap),
                     reads=[src_t, vdst_t], writes=[idst_t])
                P.op("dve", lambda e: e.max_index(out=idst_ap[:, 8:16], in_max=vdst_ap[:, 8:16], in_values=src_ap),
                     reads=[src_t, vdst_t, idst_t], writes=[idst_t])

            for cq in range(16):
                top16(sa, sa.t[:, cq, :], v_.reg(cq), v_.t[:, cq, :], i_.reg(cq), i_.t[:, cq, :], tmp128.next())
            if_ = i16f.next()
            cp(C, "dve", if_, if_.t[:], i_.regl(range(16)), i_.t[:])
            cd = cand.next()
            vv = v_.t[:].rearrange("p (h a) k -> p h a k", a=2)
            tt(C, "dve", cd, cd.t[:].rearrange("p h (a b) -> p h a b", b=16),
               v_.regl(range(16)), vv[:, :, 0, :].unsqueeze(3).to_broadcast([128, 8, 16, 16]),
               v_.regl(range(16)), vv[:, :, 1, :].unsqueeze(2).to_broadcast([128, 8, 16, 16]), ALU.add)
            ts_, ps_ = tsv.next(), pos.next()
            for h in range(8):
                top16(cd, cd.t[:, h, :], ts_.reg(h), ts_.t[:, h, :], ps_.reg(h), ps_.t[:, h, :], tmp256.next())
            ki, kf = k12i.next(), k12f.next()
            posf = ps_.t[:].rearrange("p h k -> p (h k)").bitcast(I32)
            P.op("dve", lambda e, ki=ki, posf=posf: e.tensor_single_scalar(out=ki.t[:, 0, :], in_=posf, scalar=4, op=ALU.arith_shift_right),
                 reads=ps_.regl(range(8)), writes=[ki])
            P.op("dve", lambda e, ki=ki, posf=posf: e.tensor_single_scalar(out=ki.t[:, 1, :], in_=posf, scalar=15, op=ALU.bitwise_and),
                 reads=ps_.regl(range(8)) + [ki], writes=[ki])
            cp(C, "dve", kf, kf.t[:], ki, ki.t[:])
            ig = IGf.next()
            iv = if_.t[:].rearrange("p (h a) k -> p h a k", a=2)
            for a in range(2):
                e_ = eq.next()
                tt(C, "dve", e_, e_.t[:], kf, kf.t[:, a, :].unsqueeze(2).to_broadcast([128, 128, 16]),
                   io_f, io_f.t[:, 0:16].unsqueeze(1).to_broadcast([128, 128, 16]), ALU.is_equal)
                e4 = e_.t[:].rearrange("p (h k) c -> p h k c", h=8)
                tt(C, "dve", e_, e4, e_, e4, if_, iv[:, :, a, :].unsqueeze(2).to_broadcast([128, 8, 16, 16]), ALU.mult)
                P.op("dve", lambda e, e_=e_, ig=ig, a=a: e.tensor_reduce(out=ig.t[:, a, :], in_=e_.t[:], axis=AX.X, op=ALU.add),
                     reads=[e_], writes=[ig])
            x_ = ex.next()
            tt(C, "dve", x_, x_.t[:], ts_.regl(range(8)), ts_.t[:], ts_.regl(range(8)), ts_.t[:, :, 0:1].to_broadcast([128, 8, 16]), ALU.subtract)
            act(C, x_, x_.t[:], x_, x_.t[:], AF.Exp)
            s8 = st8.next()
            P.op("dve", lambda e, s8=s8, x_=x_: e.tensor_reduce(out=s8.t[:], in_=x_.t[:], axis=AX.X, op=ALU.add), reads=[x_], writes=[s8])
            P.op("dve", lambda e, s8=s8: e.reciprocal(out=s8.t[:], in_=s8.t[:]), reads=[s8], writes=[s8])
            tt(C, "dve", ig, ig.t[:, 2, :].rearrange("p (h k) -> p h k", h=8), x_, x_.t[:],
               s8, s8.t[:].unsqueeze(2).to_broadcast([128, 8, 16]), ALU.mult)
            igf_ = ig
            ig = IG.next()
            cp(C, "dve", ig, ig.t[:], igf_, igf_.t[:])
            if "dbgig" in Sc:
                P.dma("pool", Sc["dbgig"].t[ti], ig.t[:], reads=[ig], writes=[Sc["dbgig"].reg(ti)])
            for a in range(3):
                tr(C, pIG, pIG.t[:, a, :], ig, ig.t[:, a, :], K["ident"])
            igt = IGT.next()
            cp(C, "dve", igt, igt.t[:], pIG, pIG.t[:, 0:3, :])
            est["igt", grp, j2] = igt

        def stageY(grp, j2):
            if j2 == 0:
                est["G", grp] = Gst.next()
            G_ = est["G", grp]
            igt = est.pop(("igt", grp, j2))
            TB = 16
            for b16 in range(128 // TB):
                a4, bb4 = A4.next(), B4.next()
                tsl = slice(b16 * TB, (b16 + 1) * TB)
                av = a4.t[:].rearrange("p t i -> p (t i)").rearrange("p (i t) -> p i t", t=TB)
                bv = bb4.t[:].rearrange("p t i -> p (t i)").rearrange("p (i t) -> p i t", t=TB)
                tt(C, "dve", a4, av, io_rep, io_rep.t[:], igt, igt.t[:, 0, tsl].unsqueeze(1).to_broadcast([128, 128, TB]), ALU.is_equal)
                tt(C, "dve", bb4, bv, io_rep, io_rep.t[:], igt, igt.t[:, 1, tsl].unsqueeze(1).to_broadcast([128, 128, TB]), ALU.is_equal)
                tt(C, "pool", a4, av, a4, av, igt, igt.t[:, 2, tsl].unsqueeze(1).to_broadcast([128, 128, TB]), ALU.mult)
                for q4 in range(TB // 4):
                    pg = pG.next()
                    for q_ in range(4):
                        mm(C, pg, pg.t[:, q_, :], a4, av[:, :, q4 * 4 + q_], bb4, bv[:, :, q4 * 4 + q_], True, True)
                    t0 = j2 * 128 + b16 * TB + q4 * 4
                    cp(C, "act", G_, G_.t[:, :, t0:t0 + 4].rearrange("p i t -> p t i"), pg, pg.t[:])
            if j2 == 1:
                hc = slice((grp % 2) * 256, (grp % 2 + 1) * 256)
                for i0 in range(0, 128, 32):
                    P.dma("pool", Sc["G"].t[grp // 2, :, i0:i0 + 32, hc], G_.t[:, i0:i0 + 32, :], reads=[G_], writes=[Sc["G"].reg(grp)])

        tl = [(grp, j2) for grp in range(ngroups) for j2 in range(2)]
        for it in range(len(tl) + 2):
            if it < len(tl):
                stageXc(*tl[it])
            if 1 <= it < len(tl) + 1:
                stageXt(*tl[it - 1])
            if it >= 2:
                stageY(*tl[it - 2])


def phase_e0(C, K):
    P = C.P
    jobs = []
    for i2 in range(128):
        jobs.append(lambda i2=i2: P.dma("pool", C.scr["uTb"].t[i2], C.ins["uT"].t[i2], reads=[C.ins["uT"]], writes=[C.scr["uTb"].reg(i2)]))
        jobs.append(lambda i2=i2: P.dma("pool", C.scr["vLb"].t[i2], C.ins["vL"].t[i2], reads=[C.ins["vL"]], writes=[C.scr["vLb"].reg(i2)]))
    return jobs


def phase_e2(C, K, ngroups=8, ni2=128):
    nc, P = C.nc, C.P
    Sc = C.scr
    with contextlib.ExitStack() as st:
        h2r = C.ring(st, 1, [128, 8, 512], BF16, "h2g")
        po = [C.alloc(st, [128, 512], F32, "po", psum=True) for _ in range(4)]
        par = C.ring(st, 4, [128, 512], F32, "pa", psum=True)
        uch = C.ring(st, 3, [128, 2, 8, 128], BF16, "uch")
        vch = C.ring(st, 4, [128, 2, 512], BF16, "vch")
        gch = C.ring(st, 3, [128, 2, 512], BF16, "gch")
        sqr = C.ring(st, 3, [128, 512], F32, "sqe")
        t2r = C.ring(st, 3, [128, 512], F32, "t2e")
        sgr = C.ring(st, 3, [128, 512], BF16, "sge")
        agr = C.ring(st, 3, [128, 512], BF16, "age")
        Wall = C.alloc(st, [128, ni2, 512], BF16, "Wall")
        xs = C.ring(st, 2, [128, 512], F32, "xs")
        x1h = C.ring(st, 2, [128, 512], F32, "x1h")
        LAG = 3
        state = {}

        def s1(grp, i2):
            h2 = state["h2"]
            if i2 % 2 == 0:
                u_, v_, g_ = uch.next(), vch.next(), gch.next()
                P.dma("sp", u_.t[:], Sc["uTb"].t[i2:i2 + 2].rearrange("i p k c -> p i k c"),
                      reads=Sc["uTb"].regl([i2, i2 + 1]), writes=[u_])
                P.dma("sp", v_.t[:], Sc["vLb"].t[i2:i2 + 2, :, 0:512].rearrange("i p d -> p i d"),
                      reads=Sc["vLb"].regl([i2, i2 + 1]), writes=[v_])
                P.dma("sp", g_.t[:], Sc["G"].t[grp, :, i2:i2 + 2, :], reads=Sc["G"].regl([2 * grp, 2 * grp + 1]), writes=[g_])
                state["uvg"] = (u_, v_, g_)
            u_, v_, g_ = state["uvg"]
            e_ = i2 % 2
            pa = par.next()
            for k in range(8):
                mm(C, pa, pa.t[:], u_, u_.t[:, e_, k, :], h2, h2.t[:, k, :], k == 0, k == 7)
            sq, t2, sg, ag = sqr.next(), t2r.next(), sgr.next(), agr.next()
            Wb = Wall.reg(i2)
            act(C, sq, sq.t[:], pa, pa.t[:], AF.Square, scale=0.21145921592448583)
            stt(C, "dve", t2, t2.t[:], sq, sq.t[:], 1.0, pa, pa.t[:], ALU.add, ALU.mult)
            tt(C, "dve", ag, ag.t[:], pa, pa.t[:], g_, g_.t[:, e_, :], ALU.mult)
            act(C, sg, sg.t[:], t2, t2.t[:], AF.Sigmoid, scale=1.5957691216057308)
            P.op("pool", lambda e: e.tensor_tensor(out=Wall.t[:, i2, :], in0=sg.t[:], in1=ag.t[:], op=ALU.mult),
                 reads=[sg, ag], writes=[Wb])
            state["v", i2] = (v_, e_)

        def s2(grp, i2, half):
            v_, e_ = state.pop(("v", i2)) if half == 0 else state.pop(("v2", i2))
            for j in range(4):
                P.op("pe", lambda e, j=j: e.matmul(po[j].t[:], lhsT=Wall.t[:, i2, j * 128:(j + 1) * 128], rhs=v_.t[:, e_, :],
                                                    start=(i2 == 0), stop=(i2 == ni2 - 1)),
                     reads=[Wall.reg(i2), v_], writes=[po[j]], pe_accum=True)

        def evac(grp, half):
            hs = slice(half * 512, (half + 1) * 512)
            for j in range(4):
                ti = grp * 4 + j
                rows = slice(ti * 128, (ti + 1) * 128)
                x1_ = x1h.next()
                P.dma("sp", x1_.t[:], Sc["x1"].t[rows, hs], reads=[Sc["x1"].reg(ti)], writes=[x1_])
                x_ = xs.next()
                tt(C, "dve", x_, x_.t[:], po[j], po[j].t[:], x1_, x1_.t[:], ALU.add)
                P.dma("pool", Sc["x2"].t[rows, hs], x_.t[:], reads=[x_], writes=[Sc["x2"].reg((ti, half))])

        for grp in range(ngroups):
            h2 = h2r.next()
            for j in range(4):
                ti = grp * 4 + j
                P.dma("sp", h2.t[:, :, j * 128:(j + 1) * 128], Sc["h2T"].t[ti], reads=[Sc["h2T"].reg(ti)], writes=[h2])
            state["h2"] = h2
            for it in range(ni2 + LAG):
                if it < ni2:
                    s1(grp, it)
                if it >= LAG:
                    s2(grp, it - LAG, 0)
            evac(grp, 0)
            for it in range(ni2 + LAG):
                if it < ni2:
                    if it % 2 == 0:
                        v_ = vch.next()
                        P.dma("sp", v_.t[:], Sc["vLb"].t[it:it + 2, :, 512:1024].rearrange("i p d -> p i d"),
                              reads=Sc["vLb"].regl([it, it + 1]), writes=[v_])
                        state["vp"] = v_
                    state["v2", it] = (state["vp"], it % 2)
                if it >= LAG:
                    s2(grp, it - LAG, 1)
            evac(grp, 1)


def phase_f(C, K, ntiles=NT):
    nc, P = C.nc, C.P
    Sc = C.scr
    I = C.ins
    with contextlib.ExitStack() as st:
        wg = load_w(C, st, "ple_gate", 8, 1024)
        wp = load_w(C, st, "ple_proj", 2, 1024)
        gple = C.alloc(st, [128, 1024], F32, "gple")
        P.dma("sp", gple.t[:], I["norm_ple"].t.to_broadcast([128, 1024]), writes=[gple])
        x2r = C.ring(st, 3, [128, 1024], F32, "x2f")
        pr_ = C.ring(st, 2, [128, 256], F32, "pf32")
        pbr = C.ring(st, 2, [128, 256], BF16, "pbf")
        junk = C.alloc(st, [128, 1024], BF16, "junkf")
        stat = C.ring(st, 2, [128, 8], F32, "statf")
        hbr = C.ring(st, 2, [128, 1024], BF16, "hbf")
        pt = C.ring(st, 2, [128, 8, 128], BF16, "ptf", psum=True)
        ptp = C.alloc(st, [128, 8, 128], BF16, "ptp", psum=True)
        h3r = C.ring(st, 3, [128, 8, 128], BF16, "h3T")
        pTr = C.ring(st, 3, [128, 2, 128], BF16, "pT")
        pgr = C.ring(st, 2, [128, 512], F32, "pgate", psum=True)
        ppr = C.ring(st, 2, [128, 512], F32, "pproj", psum=True)
        sgr = C.ring(st, 2, [128, 512], F32, "sgf")
        tr_ = C.ring(st, 2, [128, 512], F32, "tf")
        outr = C.ring(st, 2, [128, 1024], F32, "outf")
        def pro(ti):
            rows = slice(ti * 128, (ti + 1) * 128)
            x2 = x2r.next()
            P.dma("sp", x2.t[:], Sc["x2"].t[rows, :], reads=[Sc["x2"].reg((ti, 0)), Sc["x2"].reg((ti, 1))], writes=[x2])
            pf = pr_.next()
            P.dma("sp", pf.t[:], I["p"].t[rows, :], reads=[I["p"]], writes=[pf])
            pb = pbr.next()
            cp(C, "pool", pb, pb.t[:], pf, pf.t[:])
            for k in range(2):
                tr(C, ptp, ptp.t[:, k, :], pb, pb.t[:, k * 128:(k + 1) * 128], K["ident"])
            pT = pTr.next()
            cp(C, "act", pT, pT.t[:], ptp, ptp.t[:, 0:2, :])
            h3 = h3r.next()
            norm_transpose(C, K, x2, gple, stat.next(), junk, hbr.next(), pt.next(), h3, h3.t[:])
            return x2, pT, h3

        def body(ti, x2, pT, h3):
            rows = slice(ti * 128, (ti + 1) * 128)
            o_ = outr.next()
            for hf in range(2):
                hs = slice(hf * 512, (hf + 1) * 512)
                pg, pp = pgr.next(), ppr.next()
                for k in range(8):
                    mm(C, pg, pg.t[:], h3, h3.t[:, k, :], wg, wg.t[:, k, hs], k == 0, k == 7)
                for k in range(2):
                    mm(C, pp, pp.t[:], pT, pT.t[:, k, :], wp, wp.t[:, k, hs], k == 0, k == 1)
                sg, t_ = sgr.next(), tr_.next()
                act(C, sg, sg.t[:], pg, pg.t[:], AF.Sigmoid)
                tt(C, "dve", t_, t_.t[:], pp, pp.t[:], sg, sg.t[:], ALU.mult)
                tt(C, "pool", o_, o_.t[:, hs], t_, t_.t[:], x2, x2.t[:, hs], ALU.add)
            P.dma("sp", C.y.t[rows, :], o_.t[:], reads=[o_], writes=[C.y.reg(ti)])

        pend = pro(0)
        for ti in range(ntiles):
            nxt = pro(ti + 1) if ti + 1 < ntiles else None
            body(ti, *pend)
            pend = nxt


def declare(C):
    C.din("x", [S, D])
    C.din("p", [S, 256])
    C.din("norm_mix", [1, D])
    C.din("w_in", [D, 5376])
    C.din("q_norm", [1, 64])
    C.din("k_norm", [1, 64])
    C.din("hg_lb_raw", [2, 2, 512])
    C.din("hg_out_norm", [1, 128])
    C.din("w_up_att", [512, D])
    C.din("w_up_hg", [512, D])
    C.din("w_out", [D, D])
    C.din("norm_ffn", [1, D])
    C.din("peer_wq", [D, 2048])
    C.din("skT", [128, 16, 128])
    C.din("uT", [128, 128, 8, 128])
    C.din("vL", [128, 128, D])
    C.din("norm_ple", [1, D])
    C.din("ple_gate", [D, D])
    C.din("ple_proj", [256, D])
    sc = C.scratch
    sc("hqT", [4, 128, S], BF16)
    sc("kT", [2, 4, 128, S], BF16)
    sc("gtsT", [16, 128, S], BF16)
    sc("qT", [4, 128, S], BF16)
    sc("kTa", [2, 128, S], BF16)
    sc("v", [NT, 128, 2, 128], BF16)
    sc("g", [2, S, 512], F32)
    sc("k", [2, S, 512], BF16)
    sc("hi", [S, 512], BF16)
    sc("sg", [S, 512], BF16)
    sc("attoT", [4, 128, S], BF16)
    sc("ofwd", [S, 512], F32)
    sc("uTb", [128, 128, 8, 128], BF16)
    sc("vLb", [128, 128, D], BF16)
    sc("x2", [S, D], F32)
    sc("G", [8, 128, 128, 512], BF16)
    if "dbgs" in C.dbg:
        sc("dbgs", [NT, 128, 16, 128], F32)
        sc("dbgig", [NT, 128, 3, 128], BF16)
    sc("x1", [S, D], F32)
    sc("h2T", [NT, 128, 8, 128], BF16)
    sc("hgoT", [4, 128, S], BF16)
    if "dbgo" in C.dbg:
        sc("dbgo", [S, 512], F32)
    if "dbgacc" in C.dbg:
        sc("dbgacc", [128, 512], F32)
        sc("dbgp", [2, 128, 512], BF16)
        sc("dbgvs", [128, NT, 256], BF16)


def build(dbg=(), phases="A", ntiles=NT, **kw):
    nc = bass.Bass("TRN2", target_bir_lowering=False)
    C = Ctx(nc, dbg)
    declare(C)
    C.y = T(nc.dram_tensor("y", [S, D], F32, kind="ExternalOutput").ap(), "y")
    C.outs.append(C.y)
    with contextlib.ExitStack() as st:
        K = build_consts(C, st)
        if "A" in phases:
            phase_a(C, K, ntiles)
        if "C" in phases:
            C.P.barrier()
            phase_c(C, K, kw.get("c_tiles", NT), kw.get("c_dirs", (0, 1)))
        C.P.barrier()
        bg = phase_e0(C, K) if "2" in phases else []
        if "B" in phases:
            phase_b(C, K, kw.get("b_groups", 8), kw.get("hhs", (0, 1)), bg)
        else:
            for job in bg:
                job()
        if "D" in phases:
            C.P.barrier()
            phase_d(C, K, kw.get("d_groups", 8))
        if "E" in phases:
            C.P.barrier()
            phase_e1(C, K, kw.get("e1_groups", 16))
        if "2" in phases:
            C.P.barrier()
            phase_e2(C, K, kw.get("e2_groups", 8), kw.get("ni2", 128))
        if "F" in phases:
            C.P.barrier()
            phase_f(C, K, kw.get("f_tiles", NT))
        fin = []
        for t in C.outs:
            fin.append(t.b)
            fin.extend(t.regs.values())
        C.P.emit(final_bufs=fin)
    return nc, C


def _in_maps(inp, ncores):
    shared = {}
    for k in ["norm_mix", "q_norm", "k_norm", "hg_out_norm", "norm_ffn", "norm_ple"]:
        shared[k] = np.ascontiguousarray(np.asarray(inp[k], np.float32)[0][None])
    for k in ["w_in", "w_up_att", "w_up_hg", "w_out", "peer_wq", "ple_gate", "ple_proj"]:
        shared[k] = np.ascontiguousarray(np.asarray(inp[k], np.float32)[0])
    shared["hg_lb_raw"] = np.ascontiguousarray(np.asarray(inp["hg_lb_raw"], np.float32))
    sk = np.asarray(inp["peer_subkeys"], np.float32)[0]
    shared["skT"] = np.ascontiguousarray(sk.transpose(3, 0, 1, 2).reshape(128, 16, 128))
    u = np.asarray(inp["peer_u"], np.float32)[0].reshape(128, 128, 8, 128)
    shared["uT"] = np.ascontiguousarray(u.transpose(1, 3, 2, 0))
    v = np.asarray(inp["peer_v"], np.float32)[0].reshape(128, 128, D)
    shared["vL"] = np.ascontiguousarray(v.transpose(1, 0, 2))
    x = np.asarray(inp["x"], np.float32)
    p = np.asarray(inp["p"], np.float32)
    maps = []
    for b in range(ncores):
        m = dict(shared)
        m["x"] = np.ascontiguousarray(x[b])
        m["p"] = np.ascontiguousarray(p[0, b])
        maps.append(m)
    return maps


_NC_CACHE = {}


def kernel(**inputs):
    ncores = 8
    if "nc" not in _NC_CACHE:
        _NC_CACHE["nc"] = build(phases="ABCDE2F")[0]
    nc = _NC_CACHE["nc"]
    maps = _in_maps(inputs, ncores)
    res = run_bass_kernel_spmd(nc, maps, core_ids=list(range(ncores)))
    out = np.stack([np.asarray(res.results[b]["y"], np.float32) for b in range(ncores)], 0)
    return out
```

```python
import contextlib
import numpy as np
import concourse.bass as bass
import concourse.mybir as mybir
from concourse.bass_utils import run_bass_kernel_spmd

F32 = mybir.dt.float32
BF16 = mybir.dt.bfloat16
I32 = mybir.dt.int32
U32 = mybir.dt.uint32
ALU = mybir.AluOpType
AF = mybir.ActivationFunctionType
AX = mybir.AxisListType

S = 4096
D = 1024
NT = S // 128
EPS = 1e-6
EPOCH = 16000
NDMA_SLOTS = 48


class Buf:
    __slots__ = ("name", "last_w", "readers")

    def __init__(self, name=""):
        self.name = name
        self.last_w = None
        self.readers = {}


class T:
    __slots__ = ("t", "b", "regs")

    def __init__(self, t, name=""):
        self.t = t
        self.b = Buf(name)
        self.regs = {}

    def reg(self, key):
        if key not in self.regs:
            self.regs[key] = Buf(f"{self.b.name}[{key}]")
        return self.regs[key]

    def regl(self, keys):
        return [self.reg(k) for k in keys]


class Prog:
    ENGS = ("pe", "act", "dve", "pool", "sp")

    def __init__(self, nc):
        self.nc = nc
        self.ops = {e: [] for e in self.ENGS}
        self.count = {e: 0 for e in self.ENGS}
        self.known = {e: {} for e in self.ENGS}
        self.dma_count = [0] * NDMA_SLOTS
        self.dma_rr = {"hw": 0, "sw": 0}
        self.n_instr = 0
        self.pending = {e: [] for e in self.ENGS}

    def barrier(self):
        prods = [(e, self.count[e]) for e in self.ENGS if self.count[e] > 0]
        prods += [(("dma", s), c) for s, c in enumerate(self.dma_count) if c > 0]
        for e in self.ENGS:
            kn = self.known[e]
            for p, c in prods:
                if kn.get(p, 0) < c:
                    kn[p] = c
                    self.pending[e].append((p, c))

    def _deps(self, eng, reads, writes, pe_accum=False):
        deps = {}

        def add(p, s):
            if deps.get(p, 0) < s:
                deps[p] = s

        for b in reads:
            if b.last_w is not None:
                add(*b.last_w)
        for b in writes:
            if b.last_w is not None:
                if not (pe_accum and b.last_w[0] == "pe" and eng == "pe"):
                    add(*b.last_w)
            for p, s in b.readers.items():
                add(p, s)
        waits = []
        kn = self.known[eng]
        for p, s in deps.items():
            if kn.get(p, 0) < s:
                kn[p] = s
                waits.append((p, s))
        return waits

    def _commit(self, prod, seq, reads, writes):
        for b in writes:
            b.last_w = (prod, seq)
            b.readers = {}
        for b in reads:
            b.readers[prod] = max(b.readers.get(prod, 0), seq)

    @staticmethod
    def _bufs(xs):
        out = []
        for x in xs:
            if isinstance(x, T):
                out.append(x.b)
            elif isinstance(x, (list, tuple)):
                out.extend(Prog._bufs(x))
            else:
                out.append(x)
        return out

    def op(self, eng, fn, reads=(), writes=(), pe_accum=False):
        reads = self._bufs(reads)
        writes = self._bufs(writes)
        waits = self._deps(eng, reads, writes, pe_accum)
        waits = [w for w in self.pending[eng] if w not in waits] + waits
        self.pending[eng] = []
        self.count[eng] += 1
        seq = self.count[eng]
        self.ops[eng].append((waits, fn, (eng, seq)))
        self._commit(eng, seq, reads, writes)
        self.n_instr += 1

    def dma(self, q, out, in_, reads=(), writes=(), **kw):
        reads = self._bufs(reads)
        writes = self._bufs(writes)
        waits = self._deps(q, reads, writes)
        waits = [w for w in self.pending[q] if w not in waits] + waits
        self.pending[q] = []
        half = NDMA_SLOTS // 2
        kind = "sw" if q == "pool" else "hw"
        slot = self.dma_rr[kind] + (half if kind == "sw" else 0)
        self.dma_rr[kind] = (self.dma_rr[kind] + 1) % half
        prod = ("dma", slot)
        prev = self.dma_count[slot]
        kn = self.known[q]
        if prev and kn.get(prod, 0) < prev:
            kn[prod] = prev
            waits.append((prod, prev))
        self.dma_count[slot] += 1
        seq = self.dma_count[slot]
        self.ops[q].append((waits, (lambda e: e.dma_start(out=out, in_=in_, **kw)), (prod, seq)))
        self._commit(prod, seq, reads, writes)
        self.n_instr += 1

    def emit(self, final_bufs=()):
        nc = self.nc
        final_bufs = self._bufs(final_bufs)
        with contextlib.ExitStack() as st:
            sems = {}
            for e in self.ENGS:
                nep = self.count[e] // EPOCH + 1
                sems[e] = [st.enter_context(nc.semaphore(f"s_{e}_{k}")) for k in range(nep)]
            for s in range(NDMA_SLOTS):
                sems[("dma", s)] = [st.enter_context(nc.semaphore(f"s_dma{s}"))]
            fin = {}
            for b in final_bufs:
                if b.last_w is not None:
                    p, s = b.last_w
                    fin[p] = max(fin.get(p, 0), s)

            def wait(eng, p, s):
                if isinstance(p, tuple):
                    eng.wait_ge(sems[p][0], 16 * s)
                else:
                    k = (s - 1) // EPOCH
                    eng.wait_ge(sems[p][k], s - k * EPOCH)

            def run(ename, eng):
                for waits, fn, (prod, seq) in self.ops[ename]:
                    for p, s in waits:
                        wait(eng, p, s)
                    ins = fn(eng)
                    if isinstance(prod, tuple):
                        ins.then_inc(sems[prod][0], 16)
                    else:
                        k = (seq - 1) // EPOCH
                        ins.then_inc(sems[prod][k], 1)
                if ename == "sp":
                    for p, s in fin.items():
                        wait(eng, p, s)

            with nc.Block() as block:
                @block.sync
                def _(e):
                    run("sp", e)

                @block.tensor
                def _(e):
                    run("pe", e)

                @block.scalar
                def _(e):
                    run("act", e)

                @block.vector
                def _(e):
                    run("dve", e)

                @block.gpsimd
                def _(e):
                    run("pool", e)


class Ring:
    def __init__(self, items):
        self.items = items
        self.i = 0

    def next(self):
        x = self.items[self.i % len(self.items)]
        self.i += 1
        return x


class Ctx:
    def __init__(self, nc, dbg=()):
        self.nc = nc
        self.P = Prog(nc)
        self.dbg = set(dbg)
        self.ins = {}
        self.scr = {}
        self.outs = []
        self.uid = 0

    def din(self, name, shape, dt=F32):
        ap = self.nc.dram_tensor(name, list(shape), dt, kind="ExternalInput").ap()
        self.ins[name] = T(ap, name)
        return self.ins[name]

    def scratch(self, name, shape, dt):
        if name in self.dbg:
            ap = self.nc.dram_tensor(name, list(shape), dt, kind="ExternalOutput").ap()
        else:
            ap = self.nc.dram_tensor(name, list(shape), dt).ap()
        t = T(ap, name)
        self.scr[name] = t
        if name in self.dbg:
            self.outs.append(t)
        return t

    def alloc(self, st, shape, dt, name=None, psum=False):
        self.uid += 1
        name = f"{name or 't'}_{self.uid}"
        if psum:
            t = st.enter_context(self.nc.psum_tensor(name, list(shape), dt))
        else:
            t = st.enter_context(self.nc.sbuf_tensor(name, list(shape), dt))
        return T(t, name)

    def ring(self, st, n, shape, dt, name=None, psum=False):
        return Ring([self.alloc(st, shape, dt, name, psum) for _ in range(n)])


def mm(C, out_t, out_ap, lhsT_t, lhsT_ap, rhs_t, rhs_ap, start, stop):
    C.P.op("pe", lambda e: e.matmul(out_ap, lhsT=lhsT_ap, rhs=rhs_ap, start=start, stop=stop),
           reads=[lhsT_t, rhs_t], writes=[out_t], pe_accum=True)


def tr(C, out_t, out_ap, in_t, in_ap, ident):
    C.P.op("pe", lambda e: e.transpose(out=out_ap, in_=in_ap, identity=ident.t[:]),
           reads=[in_t, ident], writes=[out_t], pe_accum=True)


def act(C, out_t, out_ap, in_t, in_ap, func, reads=(), extra_w=(), **kw):
    C.P.op("act", lambda e: e.activation(out=out_ap, in_=in_ap, func=func, **kw),
           reads=[in_t] + list(reads), writes=[out_t] + list(extra_w))


def tt(C, eng, out_t, out_ap, a_t, a_ap, b_t, b_ap, op):
    C.P.op(eng, lambda e: e.tensor_tensor(out=out_ap, in0=a_ap, in1=b_ap, op=op),
           reads=[a_t, b_t], writes=[out_t])


def ts(C, eng, out_t, out_ap, a_t, a_ap, s1, s2, op0, op1=None, reads=()):
    if op1 is None:
        C.P.op(eng, lambda e: e.tensor_scalar(out=out_ap, in0=a_ap, scalar1=s1, scalar2=None, op0=op0),
               reads=[a_t] + list(reads), writes=[out_t])
    else:
        C.P.op(eng, lambda e: e.tensor_scalar(out=out_ap, in0=a_ap, scalar1=s1, scalar2=s2, op0=op0, op1=op1),
               reads=[a_t] + list(reads), writes=[out_t])


def stt(C, eng, out_t, out_ap, a_t, a_ap, scalar, b_t, b_ap, op0, op1, reads=()):
    C.P.op(eng, lambda e: e.scalar_tensor_tensor(out=out_ap, in0=a_ap, scalar=scalar, in1=b_ap, op0=op0, op1=op1),
           reads=[a_t, b_t] + list(reads), writes=[out_t])


def cp(C, eng, out_t, out_ap, in_t, in_ap):
    if eng == "act":
        C.P.op("act", lambda e: e.copy(out=out_ap, in_=in_ap), reads=[in_t], writes=[out_t])
    else:
        C.P.op(eng, lambda e: e.tensor_copy(out=out_ap, in_=in_ap), reads=[in_t], writes=[out_t])


def rsqrt_mean(C, st_t, src_ap_fn, n, scale):
    ap = src_ap_fn()
    ts(C, "dve", st_t, ap, st_t, ap, scale, EPS, ALU.mult, ALU.add)
    C.P.op("act", lambda e: e.activation(out=ap, in_=ap, func=AF.Sqrt), reads=[st_t], writes=[st_t])
    C.P.op("dve", lambda e: e.reciprocal(out=ap, in_=ap), reads=[st_t], writes=[st_t])


def build_consts(C, st):
    P = C.P
    K = {}
    idf = C.alloc(st, [128, 128], F32, "idf")
    P.op("pool", lambda e: e.memset(idf.t[:], 0.0), writes=[idf])
    P.op("pool", lambda e: e.affine_select(out=idf.t[:], in_=idf.t[:], pattern=[[-1, 128]],
                                           compare_op=ALU.not_equal, fill=1.0, base=0, channel_multiplier=1),
         reads=[idf], writes=[idf])
    ident = C.alloc(st, [128, 128], BF16, "ident")
    cp(C, "dve", ident, ident.t[:], idf, idf.t[:])
    K["ident"] = ident
    K["identf"] = idf
    return K


def build_rope(C, st):
    import math
    P = C.P
    rope = C.alloc(st, [128, NT, 2, 32], F32, "rope")
    with contextlib.ExitStack() as tmp:
        pf = C.alloc(tmp, [128, 1], F32, "pf")
        P.op("pool", lambda e: e.iota(pf.t[:], pattern=[[0, 1]], base=0, channel_multiplier=1,
                                      allow_small_or_imprecise_dtypes=True), writes=[pf])
        pi = C.alloc(tmp, [128, 2], I32, "pi")
        hl = C.alloc(tmp, [128, 2], I32, "hl")
        hlf = C.alloc(tmp, [128, 2], F32, "hlf")
        cp(C, "dve", pi, pi.t[:, 0:1], pf, pf.t[:])
        P.op("dve", lambda e: e.tensor_single_scalar(out=hl.t[:, 0:1], in_=pi.t[:, 0:1], scalar=6, op=ALU.arith_shift_right),
             reads=[pi], writes=[hl])
        P.op("dve", lambda e: e.tensor_single_scalar(out=hl.t[:, 1:2], in_=pi.t[:, 0:1], scalar=63, op=ALU.bitwise_and),
             reads=[pi, hl], writes=[hl])
        cp(C, "dve", hlf, hlf.t[:], hl, hl.t[:])
        jf = C.alloc(tmp, [128, 16], F32, "jf")
        P.op("pool", lambda e: e.iota(jf.t[:], pattern=[[1, 16]], base=0, channel_multiplier=0,
                                      allow_small_or_imprecise_dtypes=True), writes=[jf])
        inv = C.alloc(tmp, [128, 16], F32, "inv")
        act(C, inv, inv.t[:], jf, jf.t[:], AF.Exp, scale=-math.log(10000.0) / 16.0)
        rowp = C.alloc(tmp, [128, NT], F32, "rowp")
        P.op("pool", lambda e: e.iota(rowp.t[:], pattern=[[2, NT]], base=0, channel_multiplier=0,
                                      allow_small_or_imprecise_dtypes=True), writes=[rowp])
        ts(C, "dve", rowp, rowp.t[:], rowp, rowp.t[:], hlf.t[:, 0:1], None, ALU.add, reads=[hlf])
        ang = C.alloc(tmp, [128, NT, 32], F32, "ang")
        tt(C, "dve", ang, ang.t[:, :, 0:16], rowp, rowp.t[:].unsqueeze(2).to_broadcast([128, NT, 16]),
           inv, inv.t[:].unsqueeze(1).to_broadcast([128, NT, 16]), ALU.mult)
        colang = C.alloc(tmp, [128, 16], F32, "colang")
        ts(C, "dve", colang, colang.t[:], inv, inv.t[:], hlf.t[:, 1:2], None, ALU.mult, reads=[hlf])
        cp(C, "dve", ang, ang.t[:, :, 16:32], colang, colang.t[:].unsqueeze(1).to_broadcast([128, NT, 16]))
        ni = C.alloc(tmp, [128, NT, 32], I32, "ni")
        nf = C.alloc(tmp, [128, NT, 32], F32, "nf")
        red = C.alloc(tmp, [128, NT, 32], F32, "red")
        two_pi = 2.0 * math.pi
        for which, shift in ((1, 0.0), (0, math.pi / 2.0)):
            ts(C, "dve", nf, nf.t[:], ang, ang.t[:], shift, 1.0 / two_pi, ALU.add, ALU.mult)
            cp(C, "dve", ni, ni.t[:], nf, nf.t[:])
            cp(C, "dve", nf, nf.t[:], ni, ni.t[:])
            stt(C, "dve", red, red.t[:], nf, nf.t[:], -two_pi, ang, ang.t[:], ALU.mult, ALU.add)
            ts(C, "dve", red, red.t[:], red, red.t[:], shift, 3.1415925, ALU.add, ALU.min)
            ts(C, "dve", red, red.t[:], red, red.t[:], -3.1415925, None, ALU.max)
            act(C, rope, rope.t[:, :, which, :], red, red.t[:], AF.Sin)
    return rope


OFF = dict(aq=0, ak=512, av=640, hq=768, hff=1280, hfb=1792, hi=2304, hg=2816, gate=3328)


def phase_a(C, K, ntiles=NT):
    nc, P = C.nc, C.P
    I = C.ins
    Sc = C.scr
    with contextlib.ExitStack() as st:
        K = dict(K)
        K["rope"] = build_rope(C, st)
        P.barrier()
        w_in = C.alloc(st, [128, 8, 5376], BF16, "w_in")
        wv = I["w_in"].t.rearrange("(k p) n -> p k n", p=128)
        for c0 in range(0, 5376, 672):
            P.dma("pool", w_in.t[:, :, c0:c0 + 672], wv[:, :, c0:c0 + 672], reads=[I["w_in"]], writes=[w_in])
        gmix = C.alloc(st, [128, 1024], F32, "gmix")
        P.dma("sp", gmix.t[:], I["norm_mix"].t.to_broadcast([128, 1024]), writes=[gmix])
        qg = C.alloc(st, [128, 8, 64], F32, "qg")
        P.dma("sp", qg.t[:], I["q_norm"].t.unsqueeze(1).to_broadcast([128, 8, 64]), writes=[qg])
        kg = C.alloc(st, [128, 2, 64], F32, "kg")
        P.dma("sp", kg.t[:], I["k_norm"].t.unsqueeze(1).to_broadcast([128, 2, 64]), writes=[kg])
        raw = C.alloc(st, [128, 2, 2, 512], F32, "raw")
        P.dma("sp", raw.t[:], I["hg_lb_raw"].t.unsqueeze(0).to_broadcast([128, 2, 2, 512]), writes=[raw])
        lbb = C.alloc(st, [128, 2, 512], F32, "lbb")
        omlb = C.alloc(st, [128, 2, 512], F32, "omlb")
        tt(C, "dve", lbb, lbb.t[:], raw, raw.t[:, 0], raw, raw.t[:, 1], ALU.subtract)
        act(C, omlb, omlb.t[:], lbb, lbb.t[:], AF.Sigmoid, scale=-1.0)
        act(C, lbb, lbb.t[:], lbb, lbb.t[:], AF.Sigmoid)
        rawT = C.alloc(st, [128, 2, 2, 4], F32, "rawT")
        P.dma("sp", rawT.t[:], I["hg_lb_raw"].t.rearrange("s r (c p) -> p s r c", p=128), writes=[rawT],
              allow_slow_non_contiguous=True)
        omlT = C.alloc(st, [128, 2, 4], F32, "omlT")
        tt(C, "dve", omlT, omlT.t[:], rawT, rawT.t[:, 0], rawT, rawT.t[:, 1], ALU.subtract)
        act(C, omlT, omlT.t[:], omlT, omlT.t[:], AF.Sigmoid, scale=-1.0)

        xr = C.ring(st, 3, [128, 1024], F32, "xt")
        junk = C.alloc(st, [128, 1024], BF16, "junk")
        stat = C.ring(st, 8, [128, 16], F32, "stat")
        hbr = C.ring(st, 2, [128, 1024], BF16, "hb")
        hTr = C.ring(st, 2, [128, 8, 512], BF16, "hT")
        ptr = C.ring(st, 2, [128, 8, 128], BF16, "ptr", psum=True)
        pfr = C.ring(st, 2, [128, 512], F32, "pf", psum=True)
        ptk = C.ring(st, 3, [128, 512], F32, "ptk", psum=True)
        pqt = C.alloc(st, [128, 8, 128], BF16, "pqt", psum=True)
        stg_bf = C.ring(st, 4, [128, 512], BF16, "stgb")
        stg_f = C.ring(st, 3, [128, 512], F32, "stgf")
        tmpf = C.ring(st, 4, [128, 512], F32, "tmpf")
        qn = C.ring(st, 3, [128, 512], F32, "qn")
        qr = C.ring(st, 2, [128, 512], BF16, "qr")
        kk = C.ring(st, 2, [128, 4, 64], BF16, "kk")
        kn_ = C.ring(st, 2, [128, 128], F32, "kn")
        vst = C.ring(st, 2, [128, 2, 128], BF16, "vst")
        for v_ in vst.items:
            P.op("pool", lambda e, v_=v_: e.memset(v_.t[:], 1.0), writes=[v_])
        qTs = C.ring(st, 2, [128, 6, 128], BF16, "qTs")
        rt = C.ring(st, 8, [128, 8, 2, 16], F32, "rt")

        def run_rr(gens):
            gens = list(gens)
            while gens:
                for g_ in list(gens):
                    try:
                        next(g_)
                    except StopIteration:
                        gens.remove(g_)

        def rope_norm(src_ps, src_ap, nh, gain, cs, dst_t, dst_ap4):
            w = nh * 64
            sq = tmpf.next()
            act(C, sq, sq.t[:, 0:w], src_ps, src_ap, AF.Square)
            yield
            s8 = stat.next()
            P.op("dve", lambda e: e.tensor_reduce(out=s8.t[:, 0:nh], in_=sq.t[:, 0:w].rearrange("p (h d) -> p h d", d=64),
                                                  axis=AX.X, op=ALU.add), reads=[sq], writes=[s8])
            yield
            ap = s8.t[:, 0:nh]
            ts(C, "dve", s8, ap, s8, ap, 1.0 / 64, EPS, ALU.mult, ALU.add)
            yield
            C.P.op("act", lambda e: e.activation(out=ap, in_=ap, func=AF.Sqrt), reads=[s8], writes=[s8])
            yield
            C.P.op("dve", lambda e: e.reciprocal(out=ap, in_=ap), reads=[s8], writes=[s8])
            yield
            n_ = qn.next()
            nv = n_.t[:, 0:w].rearrange("p (h d) -> p h d", d=64)
            tt(C, "dve", n_, nv, src_ps, src_ap.rearrange("p (h d) -> p h d", d=64),
               s8, s8.t[:, 0:nh].unsqueeze(2).to_broadcast([128, nh, 64]), ALU.mult)
            yield
            tt(C, "pool", n_, nv, n_, nv, gain, gain.t[:, 0:nh, :], ALU.mult)
            yield
            v5 = n_.t[:, 0:w].rearrange("p (h a b f) -> p h a b f", a=2, b=2, f=16)
            x1 = v5[:, :, :, 0, :]
            x2 = v5[:, :, :, 1, :]
            cb = cs.t[:, 0, :].rearrange("p (a f) -> p a f", a=2).unsqueeze(1).to_broadcast([128, nh, 2, 16])
            sb_ = cs.t[:, 1, :].rearrange("p (a f) -> p a f", a=2).unsqueeze(1).to_broadcast([128, nh, 2, 16])
            t1, t2 = rt.next(), rt.next()
            tt(C, "dve", t1, t1.t[:, 0:nh], n_, x1, cs, cb, ALU.mult)
            tt(C, "pool", t2, t2.t[:, 0:nh], n_, x2, cs, sb_, ALU.mult)
            yield
            t3, t4 = rt.next(), rt.next()
            tt(C, "dve", t3, t3.t[:, 0:nh], n_, x1, cs, sb_, ALU.mult)
            tt(C, "pool", t4, t4.t[:, 0:nh], n_, x2, cs, cb, ALU.mult)
            yield
            tt(C, "dve", dst_t, dst_ap4[:, :, :, 0, :], t1, t1.t[:, 0:nh], t2, t2.t[:, 0:nh], ALU.subtract)
            yield
            tt(C, "pool", dst_t, dst_ap4[:, :, :, 1, :], t3, t3.t[:, 0:nh], t4, t4.t[:, 0:nh], ALU.add)
            yield

        def prep_tile(g, j, hT):
            ti = g * 4 + j
            xt = xr.next()
            P.dma("sp", xt.t[:], I["x"].t[ti * 128:(ti + 1) * 128, :], reads=[I["x"]], writes=[xt])
            s_ = stat.next()
            act(C, junk, junk.t[:], xt, xt.t[:], AF.Square, accum_out=s_.t[:, 0:1], extra_w=[s_])
            yield
            ap = s_.t[:, 0:1]
            ts(C, "dve", s_, ap, s_, ap, 1.0 / D, EPS, ALU.mult, ALU.add)
            yield
            C.P.op("act", lambda e: e.activation(out=ap, in_=ap, func=AF.Sqrt), reads=[s_], writes=[s_])
            yield
            C.P.op("dve", lambda e: e.reciprocal(out=ap, in_=ap), reads=[s_], writes=[s_])
            yield
            hb = hbr.next()
            stt(C, "dve", hb, hb.t[:], xt, xt.t[:], s_.t[:, 0:1], gmix, gmix.t[:], ALU.mult, ALU.mult, reads=[s_])
            yield
            pt = ptr.next()
            for k in range(8):
                tr(C, pt, pt.t[:, k, :], hb, hb.t[:, k * 128:(k + 1) * 128], K["ident"])
            yield
            cp(C, "act", hT, hT.t[:, :, j * 128:(j + 1) * 128], pt, pt.t[:])
            yield

        ngr = ntiles // 4
        hTs = {0: hTr.next()}
        for j in range(4):
            run_rr([prep_tile(0, j, hTs[0])])
        for g in range(ngr):
            hT = hTs[g]
            if g + 1 < ngr:
                hTs[g + 1] = hTr.next()
            cols = slice(g * 512, (g + 1) * 512)
            fm = [("hq", OFF["hq"] + c * 128, c) for c in range(4)] + \
                 [("hff", OFF["hff"] + c * 128, c) for c in range(4)] + \
                 [("hfb", OFF["hfb"] + c * 128, c) for c in range(4)] + \
                 [("gate", OFF["gate"] + c * 128, c) for c in range(16)]
            for kind, c0, c in fm:
                pf = pfr.next()
                for k in range(8):
                    mm(C, pf, pf.t[:], w_in, w_in.t[:, k, c0:c0 + 128], hT, hT.t[:, k, :], k == 0, k == 7)
                sg = stg_bf.next()
                if kind == "hq":
                    act(C, sg, sg.t[:], pf, pf.t[:], AF.Silu)
                    dst = Sc["hqT"]
                    P.dma("pool", dst.t[c, :, cols], sg.t[:], reads=[sg], writes=[dst.reg((c, g))])
                elif kind in ("hff", "hfb"):
                    d_ = 0 if kind == "hff" else 1
                    tf = tmpf.next()
                    act(C, tf, tf.t[:], pf, pf.t[:], AF.Sigmoid, scale=-1.0)
                    ts(C, "dve", sg, sg.t[:], tf, tf.t[:], omlT.t[:, d_, c:c + 1], None, ALU.mult, reads=[omlT])
                    dst = Sc["kT"]
                    P.dma("pool", dst.t[d_, c, :, cols], sg.t[:], reads=[sg], writes=[dst.reg((d_, c, g))])
                else:
                    act(C, sg, sg.t[:], pf, pf.t[:], AF.Sigmoid)
                    dst = Sc["gtsT"]
                    P.dma("pool", dst.t[c, :, cols], sg.t[:], reads=[sg], writes=[dst.reg((c, g))])
            for j in range(4):
                ti = g * 4 + j
                rows = slice(ti * 128, (ti + 1) * 128)
                lhs = lambda k, j=j: hT.t[:, k, j * 128:(j + 1) * 128]
                cs = T(K["rope"].t[:, ti], "rope_v")
                cs.b = K["rope"].b
                q_ = qr.next()
                k_ = kk.next()

                def chain_q():
                    pq = ptk.next()
                    for k in range(8):
                        mm(C, pq, pq.t[:], hT, lhs(k), w_in, w_in.t[:, k, 0:512], k == 0, k == 7)
                    yield
                    yield from rope_norm(pq, pq.t[:], 8, qg, cs, q_, q_.t[:].rearrange("p (h a b f) -> p h a b f", a=2, b=2, f=16))

                def chain_kv():
                    pkv = ptk.next()
                    for k in range(8):
                        mm(C, pkv, pkv.t[:, 0:256], hT, lhs(k), w_in, w_in.t[:, k, 512:768], k == 0, k == 7)
                    yield
                    v_ = vst.next()
                    cp(C, "act", v_, v_.t[:, :, 0:64], pkv, pkv.t[:, 128:256].rearrange("p (h d) -> p h d", d=64))
                    P.dma("pool", Sc["v"].t[ti], v_.t[:], reads=[v_], writes=[Sc["v"].reg(ti)])
                    yield
                    kview = k_.t[:].rearrange("p (h r) d -> p h r d", r=2)
                    yield from rope_norm(pkv, pkv.t[:, 0:128], 2, kg, cs, k_,
                                         kview[:, :, 0, :].rearrange("p h (a b f) -> p h a b f", a=2, b=2, f=16))
                    cp(C, "pool", k_, kview[:, :, 1, :], k_, kview[:, :, 0, :])
                    yield

                def chain_gate(d_, key):
                    pg = ptk.next()
                    for k in range(8):
                        mm(C, pg, pg.t[:], hT, lhs(k), w_in, w_in.t[:, k, OFF[key]:OFF[key] + 512], k == 0, k == 7)
                    yield
                    tf = tmpf.next()
                    act(C, tf, tf.t[:], pg, pg.t[:], AF.Sigmoid)
                    yield
                    tt(C, "dve", tf, tf.t[:], tf, tf.t[:], omlb, omlb.t[:, d_, :], ALU.mult)
                    yield
                    tt(C, "dve", tf, tf.t[:], tf, tf.t[:], lbb, lbb.t[:, d_, :], ALU.add)
                    yield
                    gf = stg_f.next()
                    act(C, gf, gf.t[:], tf, tf.t[:], AF.Ln)
                    P.dma("pool", Sc["g"].t[d_, rows, :], gf.t[:], reads=[gf], writes=[Sc["g"].reg((d_, ti))])
                    kb = stg_bf.next()
                    ts(C, "pool", kb, kb.t[:], tf, tf.t[:], -1.0, 1.0, ALU.mult, ALU.add)
                    P.dma("pool", Sc["k"].t[d_, rows, :], kb.t[:], reads=[kb], writes=[Sc["k"].reg((d_, ti))])
                    yield

                def chain_h(key, dstn, fn):
                    ph = ptk.next()
                    for k in range(8):
                        mm(C, ph, ph.t[:], hT, lhs(k), w_in, w_in.t[:, k, OFF[key]:OFF[key] + 512], k == 0, k == 7)
                    yield
                    sb_ = stg_bf.next()
                    if fn is None:
                        cp(C, "act", sb_, sb_.t[:], ph, ph.t[:])
                    else:
                        act(C, sb_, sb_.t[:], ph, ph.t[:], fn)
                    P.dma("pool", Sc[dstn].t[rows, :], sb_.t[:], reads=[sb_], writes=[Sc[dstn].reg(ti)])
                    yield

                chains = [chain_q(), chain_kv(), chain_gate(0, "hff")]
                if g + 1 < ngr:
                    chains.append(prep_tile(g + 1, j, hTs[g + 1]))
                run_rr(chains)
                run_rr([chain_gate(1, "hfb"), chain_h("hi", "hi", None), chain_h("hg", "sg", AF.Silu)])
                for pr in range(4):
                    tr(C, pqt, pqt.t[:, pr, :], q_, q_.t[:, pr * 128:(pr + 1) * 128], K["ident"])
                kflat = k_.t[:].rearrange("p a d -> p (a d)")
                for kv in range(2):
                    tr(C, pqt, pqt.t[:, 4 + kv, :], k_, kflat[:, kv * 128:(kv + 1) * 128], K["ident"])
                qs = qTs.next()
                cp(C, "act", qs, qs.t[:], pqt, pqt.t[:, 0:6, :])
                P.dma("pool", Sc["qT"].t[:, :, rows].rearrange("r p t -> p r t"), qs.t[:, 0:4, :], reads=[qs], writes=[Sc["qT"].reg(ti)])
                P.dma("pool", Sc["kTa"].t[:, :, rows].rearrange("r p t -> p r t"), qs.t[:, 4:6, :], reads=[qs], writes=[Sc["kTa"].reg(ti)])


def phase_b(C, K, ngroups=8, hhs=(0, 1), bg=()):
    nc, P = C.nc, C.P
    Sc = C.scr
    with contextlib.ExitStack() as st:
        kT = [C.alloc(st, [128, S], BF16, "kTsb") for _ in range(2)]
        for kv in range(2):
            for hf in range(2):
                cs_ = slice(hf * 2048, (hf + 1) * 2048)
                P.dma("sp", kT[kv].t[:, cs_], Sc["kTa"].t[kv, :, cs_],
                      reads=Sc["kTa"].regl(range(hf * 16, hf * 16 + 16)), writes=[kT[kv]])
        vs = C.alloc(st, [128, NT, 256], BF16, "vsb")
        for hf in range(4):
            P.dma("sp", vs.t[:, hf * 8:(hf + 1) * 8, :], Sc["v"].t[hf * 8:(hf + 1) * 8].rearrange("t p h c -> p t (h c)"),
                  reads=Sc["v"].regl(range(hf * 8, hf * 8 + 8)), writes=[vs])
        if "dbgvs" in Sc:
            P.dma("pool", Sc["dbgvs"].t[:], vs.t[:], reads=[vs], writes=[Sc["dbgvs"]])
        qr_ = C.ring(st, 2, [128, 512], BF16, "qTg")
        psS = C.ring(st, 4, [128, 512], F32, "psS", psum=True)
        acc = [C.alloc(st, [128, 512], F32, "acc", psum=True) for _ in range(2)]
        ptr_ = C.ring(st, 8, [128, 512], BF16, "pT")
        rl = C.ring(st, 2, [128, 512], F32, "rl")
        obr = C.ring(st, 2, [128, 512], BF16, "ob")
        LAG = 3
        steps = [(g, pr, kt, hh) for g in range(ngroups) for pr in range(4) for kt in range(NT) for hh in hhs]
        state = {}
        accs = [acc, [C.alloc(st, [128, 512], F32, "acc2", psum=True) for _ in range(2)]]

        def stage1(g, pr, kt, hh):
            kv = pr // 2
            if kt == 0 and hh == hhs[0]:
                q = qr_.next()
                P.dma("sp", q.t[:], Sc["qT"].t[pr, :, g * 512:(g + 1) * 512], reads=Sc["qT"].regl(range(4 * g, 4 * g + 4)), writes=[q])
                state["q", g, pr] = q
            q = state["q", g, pr]
            rows = slice(hh * 64, (hh + 1) * 64)
            s_ = psS.next()
            mm(C, s_, s_.t[:], kT[kv], kT[kv].t[rows, kt * 128:(kt + 1) * 128], q, q.t[rows, :], True, True)
            p_ = ptr_.next()
            act(C, p_, p_.t[:], s_, s_.t[:], AF.Exp, scale=0.125)
            state["p", g, pr, kt, hh] = p_

        def stage2(g, pr, kt, hh):
            kv = pr // 2
            p_ = state.pop(("p", g, pr, kt, hh))
            ac = accs[(g * 4 + pr) % 2]
            mm(C, ac[hh], ac[hh].t[:], vs, vs.t[:, kt, kv * 128:(kv + 1) * 128], p_, p_.t[:], kt == 0, kt == NT - 1)
            if kt == NT - 1 and hh == hhs[-1]:
                ob = obr.next()
                for h2_ in hhs:
                    r_ = rl.next()
                    C.P.op("dve", lambda e, r_=r_, h2_=h2_, ac=ac: e.reciprocal(out=r_.t[64:128, :], in_=ac[h2_].t[64:128, :]),
                           reads=[ac[h2_]], writes=[r_])
                    tt(C, "dve", ob, ob.t[h2_ * 64:(h2_ + 1) * 64, :], ac[h2_], ac[h2_].t[0:64, :], r_, r_.t[64:128, :], ALU.mult)
                P.dma("pool", Sc["attoT"].t[pr, :, g * 512:(g + 1) * 512], ob.t[:], reads=[ob], writes=[Sc["attoT"].reg((pr, g))])

        bg = list(bg)
        LAG = 4
        for it in range(0, len(steps) + LAG, 2):
            for i_ in (it, it + 1):
                if i_ < len(steps):
                    stage1(*steps[i_])
            for i_ in (it - LAG, it - LAG + 1):
                if 0 <= i_ < len(steps):
                    stage2(*steps[i_])
            if bg and (it // 2) % 3 == 2:
                bg.pop(0)()
        for job in bg:
            job()


def build_masks(C, st):
    P = C.P
    M = {}
    specs = {
        "f_incl": (ALU.is_ge, 0, 1, -1),
        "f_excl": (ALU.is_gt, 0, -1, 1),
        "b_incl": (ALU.is_ge, 0, -1, 1),
        "b_excl": (ALU.is_gt, 0, 1, -1),
    }
    for name, (op, base, tmul, pmul) in specs.items():
        m = C.alloc(st, [128, 128], F32, "m_" + name)
        P.op("pool", lambda e, m=m: e.memset(m.t[:], 1.0), writes=[m])
        P.op("pool", lambda e, m=m, op=op, base=base, tmul=tmul, pmul=pmul: e.affine_select(
            out=m.t[:], in_=m.t[:], pattern=[[tmul, 128]], compare_op=op, fill=0.0, base=base, channel_multiplier=pmul),
            reads=[m], writes=[m])
        P.op("pool", lambda e, m=m: e.memset(m.t[0:64, 64:128], 0.0), reads=[m], writes=[m])
        P.op("pool", lambda e, m=m: e.memset(m.t[64:128, 0:64], 0.0), reads=[m], writes=[m])
        M[name] = m
    return M


def phase_c(C, K, ntiles=NT, dirs=(0, 1)):
    nc, P = C.nc, C.P
    Sc = C.scr
    I = C.ins
    with contextlib.ExitStack() as st:
        M = build_masks(C, st)
        gon = C.alloc(st, [128, 4, 128], F32, "gon")
        P.dma("sp", gon.t[:], I["hg_out_norm"].t.unsqueeze(1).to_broadcast([128, 4, 128]), writes=[gon])
        gr = C.ring(st, 2, [128, 512], F32, "g_t")
        kdr = C.ring(st, 2, [128, 512], BF16, "kd_t")
        kTr = C.ring(st, 2, [128, 4, 128], BF16, "kT_t")
        qTr = C.ring(st, 2, [128, 4, 128], BF16, "hqT_t")
        vr = C.ring(st, 2, [128, 512], BF16, "v_t")
        ofr = C.ring(st, 2, [128, 512], F32, "of_t")
        sgr = C.ring(st, 2, [128, 512], BF16, "sg_t")
        prx = C.alloc(st, [128, 512], F32, "prx", psum=True)
        pbT = C.alloc(st, [128, 4, 128], F32, "pbT", psum=True)
        pX = [C.alloc(st, [128, 4, 128], F32, "pX", psum=True) for _ in range(2)]
        pOs = [C.alloc(st, [128, 4, 128], F32, "pOs", psum=True) for _ in range(2)]
        pTr = C.alloc(st, [128, 8, 128], BF16, "pTr", psum=True)
        ebT = C.ring(st, 2, [128, 4, 128], F32, "ebT")
        enbT = C.ring(st, 2, [128, 4, 128], F32, "enbT")
        er = C.ring(st, 2, [128, 512], F32, "er")
        qfull = C.ring(st, 2, [128, 4, 128], BF16, "qfull")
        qlo = C.ring(st, 2, [128, 4, 128], BF16, "qlo")
        qhi = C.ring(st, 2, [128, 4, 128], BF16, "qhi")
        for t_ in qlo.items + qhi.items:
            P.op("pool", lambda e, t_=t_: e.memset(t_.t[:], 0.0), writes=[t_])
        ktil = C.ring(st, 2, [128, 4, 128], BF16, "ktil")
        kdec = C.ring(st, 2, [128, 512], BF16, "kdec")
        atm = C.ring(st, 4, [128, 128], BF16, "atm")
        S32 = [C.alloc(st, [128, 128], F32, "S32") for _ in range(4)]
        Sbf = [C.alloc(st, [128, 128], BF16, "Sbf") for _ in range(4)]
        osb = C.ring(st, 2, [128, 512], F32, "osb")
        tot = C.ring(st, 2, [128, 512], F32, "tot")
        sqt = C.ring(st, 2, [128, 512], F32, "sqt")
        stat = C.ring(st, 2, [128, 8], F32, "statc")
        onb = C.ring(st, 2, [128, 512], BF16, "onb")
        oTs = C.ring(st, 2, [128, 4, 128], BF16, "oTs")

        for d_ in dirs:
            Mi = M["f_incl"] if d_ == 0 else M["b_incl"]
            Me = M["f_excl"] if d_ == 0 else M["b_excl"]
            for hd in range(4):
                P.op("pool", lambda e, hd=hd: e.memset(S32[hd].t[:], 0.0), writes=[S32[hd]])
                P.op("pool", lambda e, hd=hd: e.memset(Sbf[hd].t[:], 0.0), writes=[Sbf[hd]])
            order = list(range(ntiles)) if d_ == 0 else list(range(ntiles - 1, -1, -1))
            def pro(ti):
                rows = slice(ti * 128, (ti + 1) * 128)
                g_t, kd_t, kT_t, q_t, v_t = gr.next(), kdr.next(), kTr.next(), qTr.next(), vr.next()
                P.dma("sp", g_t.t[:], Sc["g"].t[d_, rows, :], reads=[Sc["g"].reg((d_, ti))], writes=[g_t])
                P.dma("sp", kd_t.t[:], Sc["k"].t[d_, rows, :], reads=[Sc["k"].reg((d_, ti))], writes=[kd_t])
                P.dma("sp", kT_t.t[:], Sc["kT"].t[d_, :, :, rows].rearrange("h p t -> p h t"),
                      reads=[Sc["kT"].reg((d_, c, ti // 4)) for c in range(4)], writes=[kT_t])
                P.dma("sp", q_t.t[:], Sc["hqT"].t[:, :, rows].rearrange("h p t -> p h t"),
                      reads=[Sc["hqT"].reg((c, ti // 4)) for c in range(4)], writes=[q_t])
                P.dma("sp", v_t.t[:], Sc["hi"].t[rows, :], reads=[Sc["hi"].reg(ti)], writes=[v_t])
                mm(C, prx, prx.t[:], Me, Me.t[:], g_t, g_t.t[:], True, True)
                for hd in range(4):
                    mm(C, pbT, pbT.t[:, hd, :], g_t, g_t.t[:, hd * 128:(hd + 1) * 128], Mi, Mi.t[:], True, True)
                eb, enb, er_ = ebT.next(), enbT.next(), er.next()
                act(C, eb, eb.t[:], pbT, pbT.t[:], AF.Exp)
                act(C, enb, enb.t[:], pbT, pbT.t[:], AF.Exp, scale=-1.0)
                act(C, er_, er_.t[:], prx, prx.t[:], AF.Exp)
                qf, ql, qh, kt_, kdc = qfull.next(), qlo.next(), qhi.next(), ktil.next(), kdec.next()
                tt(C, "dve", qf, qf.t[:], q_t, q_t.t[:], eb, eb.t[:], ALU.mult)
                cp(C, "pool", ql, ql.t[:, :, 0:64], qf, qf.t[:, :, 0:64])
                cp(C, "pool", qh, qh.t[:, :, 64:128], qf, qf.t[:, :, 64:128])
                tt(C, "dve", kt_, kt_.t[:], kT_t, kT_t.t[:], enb, enb.t[:], ALU.mult)
                tt(C, "pool", kdc, kdc.t[:], kd_t, kd_t.t[:], er_, er_.t[:], ALU.mult)
                return dict(kd_t=kd_t, v_t=v_t, eb=eb, qf=qf, ql=ql, qh=qh, kt_=kt_, kdc=kdc)

            def tile_body(ti, B_):
                rows = slice(ti * 128, (ti + 1) * 128)
                kd_t, v_t, eb, qf, ql, qh, kt_, kdc = (B_[k_] for k_ in ('kd_t', 'v_t', 'eb', 'qf', 'ql', 'qh', 'kt_', 'kdc'))
                if d_ == 0:
                    ca, cb, qa, qb, la, lb_ = 0, 1, ql, qh, 63, 127
                else:
                    ca, cb, qa, qb, la, lb_ = 1, 0, qh, ql, 64, 0
                ra = slice(ca * 64, (ca + 1) * 64)
                rb = slice(cb * 64, (cb + 1) * 64)
                def head_chain(hd):
                    hc = slice(hd * 128, (hd + 1) * 128)
                    X, O_ = pX[hd % 2], pOs[hd % 2]
                    oa = O_.t[:, hd // 2, :]
                    mm(C, X, X.t[:, 0, :], kt_, kt_.t[:, hd, :], qf, qf.t[:, hd, :], True, True)
                    mm(C, O_, oa, qa, qa.t[:, hd, :], Sbf[hd], Sbf[hd].t[:], True, False)
                    mm(C, X, X.t[:, 1, :], kdc, kdc.t[ra, hc], v_t, v_t.t[ra, hc], True, True)
                    yield
                    am = atm.next()
                    tt(C, "dve", am, am.t[:], X, X.t[:, 0, :], Mi, Mi.t[:], ALU.mult)
                    stt(C, "dve", Sbf[hd], Sbf[hd].t[:], S32[hd], S32[hd].t[:], eb.t[:, hd, la:la + 1],
                        X, X.t[:, 1, :], ALU.mult, ALU.add, reads=[eb])
                    stt(C, "dve", S32[hd], S32[hd].t[:], S32[hd], S32[hd].t[:], eb.t[:, hd, la:la + 1],
                        X, X.t[:, 1, :], ALU.mult, ALU.add, reads=[eb])
                    yield
                    mm(C, O_, oa, qb, qb.t[:, hd, :], Sbf[hd], Sbf[hd].t[:], False, False)
                    mm(C, O_, oa, am, am.t[:], v_t, v_t.t[:, hc], False, True)
                    mm(C, X, X.t[:, 2, :], kdc, kdc.t[rb, hc], v_t, v_t.t[rb, hc], True, True)
                    yield
                    stt(C, "dve", Sbf[hd], Sbf[hd].t[:], S32[hd], S32[hd].t[:], eb.t[:, hd, lb_:lb_ + 1],
                        X, X.t[:, 2, :], ALU.mult, ALU.add, reads=[eb])
                    stt(C, "dve", S32[hd], S32[hd].t[:], S32[hd], S32[hd].t[:], eb.t[:, hd, lb_:lb_ + 1],
                        X, X.t[:, 2, :], ALU.mult, ALU.add, reads=[eb])
                    yield

                for pair in ((0, 1), (2, 3)):
                    gens = [head_chain(hd) for hd in pair]
                    while gens:
                        for g_ in list(gens):
                            try:
                                next(g_)
                            except StopIteration:
                                gens.remove(g_)

                def ov(tile_ap, s_):
                    return tile_ap.rearrange("p (a s d) -> p a s d", s=2, d=128)[:, :, s_, :]

                if d_ == 0 and len(dirs) == 2:
                    o_ = osb.next()
                    for s_ in range(2):
                        cp(C, "act", o_, ov(o_.t[:], s_), pOs[s_], pOs[s_].t[:, 0:2, :])
                    P.dma("pool", Sc["ofwd"].t[rows, :], o_.t[:], reads=[o_], writes=[Sc["ofwd"].reg(ti)])
                    return
                t_ = tot.next()
                if len(dirs) == 2:
                    of_ = ofr.next()
                    P.dma("sp", of_.t[:], Sc["ofwd"].t[rows, :], reads=[Sc["ofwd"].reg(ti)], writes=[of_])
                    for s_ in range(2):
                        tt(C, "dve", t_, ov(t_.t[:], s_), pOs[s_], pOs[s_].t[:, 0:2, :], of_, ov(of_.t[:], s_), ALU.add)
                else:
                    for s_ in range(2):
                        cp(C, "dve", t_, ov(t_.t[:], s_), pOs[s_], pOs[s_].t[:, 0:2, :])
                if "dbgo" in Sc:
                    P.dma("pool", Sc["dbgo"].t[rows, :], t_.t[:], reads=[t_], writes=[Sc["dbgo"].reg(ti)])
                sg_ = sgr.next()
                P.dma("sp", sg_.t[:], Sc["sg"].t[rows, :], reads=[Sc["sg"].reg(ti)], writes=[sg_])
                sq = sqt.next()
                act(C, sq, sq.t[:], t_, t_.t[:], AF.Square)
                s4 = stat.next()
                P.op("dve", lambda e, s4=s4, sq=sq: e.tensor_reduce(out=s4.t[:, 0:4], in_=sq.t[:].rearrange("p (h d) -> p h d", d=128),
                                                              axis=AX.X, op=ALU.add), reads=[sq], writes=[s4])
                rsqrt_mean(C, s4, lambda s4=s4: s4.t[:, 0:4], 4, 1.0 / 128)
                t3 = t_.t[:].rearrange("p (h d) -> p h d", d=128)
                tt(C, "dve", t_, t3, t_, t3, s4, s4.t[:, 0:4].unsqueeze(2).to_broadcast([128, 4, 128]), ALU.mult)
                tt(C, "pool", t_, t3, t_, t3, gon, gon.t[:], ALU.mult)
                ob = onb.next()
                tt(C, "dve", ob, ob.t[:], t_, t_.t[:], sg_, sg_.t[:], ALU.mult)
                for hd in range(4):
                    tr(C, pTr, pTr.t[:, hd, :], ob, ob.t[:, hd * 128:(hd + 1) * 128], K["ident"])
                os_ = oTs.next()
                cp(C, "act", os_, os_.t[:], pTr, pTr.t[:, 0:4, :])
                P.dma("pool", Sc["hgoT"].t[:, :, rows].rearrange("h p t -> p h t"), os_.t[:], reads=[os_], writes=[Sc["hgoT"].reg(ti)])

            pend = pro(order[0])
            for idx_, ti in enumerate(order):
                nxt = pro(order[idx_ + 1]) if idx_ + 1 < len(order) else None
                tile_body(ti, pend)
                pend = nxt


def load_w(C, st, name, kchunks, ncols, q="pool"):
    w = C.alloc(st, [128, kchunks, ncols], BF16, name)
    src = C.ins[name].t.rearrange("(k p) n -> p k n", p=128)
    step = min(kchunks, max(1, 4096 // ncols))
    for k0 in range(0, kchunks, step):
        C.P.dma(q, w.t[:, k0:k0 + step, :], src[:, k0:k0 + step, :], reads=[C.ins[name]], writes=[w])
    return w


def norm_transpose(C, K, xt, gain, stat, junk, hb, pt, dst, dst_ap):
    s_ = stat
    act(C, junk, junk.t[:], xt, xt.t[:], AF.Square, accum_out=s_.t[:, 0:1], extra_w=[s_])
    rsqrt_mean(C, s_, lambda: s_.t[:, 0:1], 1, 1.0 / D)
    stt(C, "dve", hb, hb.t[:], xt, xt.t[:], s_.t[:, 0:1], gain, gain.t[:], ALU.mult, ALU.mult, reads=[s_])
    for k in range(8):
        tr(C, pt, pt.t[:, k, :], hb, hb.t[:, k * 128:(k + 1) * 128], K["ident"])
    cp(C, "act", dst, dst_ap, pt, pt.t[:])


def phase_d(C, K, ngroups=8):
    nc, P = C.nc, C.P
    Sc = C.scr
    I = C.ins
    with contextlib.ExitStack() as st:
        wua = load_w(C, st, "w_up_att", 4, 1024)
        wuh = load_w(C, st, "w_up_hg", 4, 1024)
        wo = load_w(C, st, "w_out", 8, 1024)
        gffn = C.alloc(st, [128, 1024], F32, "gffn")
        P.dma("sp", gffn.t[:], I["norm_ffn"].t.to_broadcast([128, 1024]), writes=[gffn])
        aTr = C.ring(st, 2, [128, 4, 512], BF16, "aT")
        hTr_ = C.ring(st, 2, [128, 4, 512], BF16, "hgT")
        gtr = C.ring(st, 2, [128, 16, 512], BF16, "gts")
        pya = C.ring(st, 2, [128, 512], F32, "pya", psum=True)
        pyh = C.ring(st, 2, [128, 512], F32, "pyh", psum=True)
        px = C.ring(st, 2, [128, 512], F32, "px", psum=True)
        pt = C.ring(st, 2, [128, 8, 128], BF16, "ptd", psum=True)
        t1r = C.ring(st, 2, [128, 512], F32, "t1")
        t2r = C.ring(st, 2, [128, 512], F32, "t2")
        mTr = C.ring(st, 2, [128, 8, 512], BF16, "mT")
        xr = C.ring(st, 2, [128, 1024], F32, "xtd")
        x1r = C.ring(st, 3, [128, 1024], F32, "x1t")
        junk = C.alloc(st, [128, 1024], BF16, "junkd")
        stat = C.ring(st, 2, [128, 8], F32, "statd")
        hbr = C.ring(st, 2, [128, 1024], BF16, "hbd")
        h2s = C.ring(st, 2, [128, 8, 128], BF16, "h2s")
        for g in range(ngroups):
            cols = slice(g * 512, (g + 1) * 512)
            aT, hT, gt = aTr.next(), hTr_.next(), gtr.next()
            P.dma("sp", aT.t[:], Sc["attoT"].t[:, :, cols].rearrange("r p t -> p r t"),
                  reads=[Sc["attoT"].reg((pr, g)) for pr in range(4)], writes=[aT])
            P.dma("sp", hT.t[:], Sc["hgoT"].t[:, :, cols].rearrange("r p t -> p r t"),
                  reads=Sc["hgoT"].regl(range(4 * g, 4 * g + 4)), writes=[hT])
            P.dma("sp", gt.t[:], Sc["gtsT"].t[:, :, cols].rearrange("r p t -> p r t"),
                  reads=[Sc["gtsT"].reg((c, g)) for c in range(16)], writes=[gt])
            mT = mTr.next()
            for m_ in range(8):
                ms = slice(m_ * 128, (m_ + 1) * 128)
                ya, yh = pya.next(), pyh.next()
                for kc in range(4):
                    mm(C, ya, ya.t[:], wua, wua.t[:, kc, ms], aT, aT.t[:, kc, :], kc == 0, kc == 3)
                for kc in range(4):
                    mm(C, yh, yh.t[:], wuh, wuh.t[:, kc, ms], hT, hT.t[:, kc, :], kc == 0, kc == 3)
                t1, t2 = t1r.next(), t2r.next()
                tt(C, "dve", t1, t1.t[:], ya, ya.t[:], gt, gt.t[:, m_, :], ALU.mult)
                tt(C, "dve", t2, t2.t[:], yh, yh.t[:], gt, gt.t[:, 8 + m_, :], ALU.mult)
                tt(C, "pool", mT, mT.t[:, m_, :], t1, t1.t[:], t2, t2.t[:], ALU.add)
            def part1(j):
                ti = g * 4 + j
                rows = slice(ti * 128, (ti + 1) * 128)
                xt = xr.next()
                P.dma("sp", xt.t[:], I["x"].t[rows, :], reads=[I["x"]], writes=[xt])
                x1 = x1r.next()
                for hf in range(2):
                    hs = slice(hf * 512, (hf + 1) * 512)
                    p_ = px.next()
                    for m_ in range(8):
                        mm(C, p_, p_.t[:], mT, mT.t[:, m_, j * 128:(j + 1) * 128], wo, wo.t[:, m_, hs], m_ == 0, m_ == 7)
                    tt(C, "dve", x1, x1.t[:, hs], p_, p_.t[:], xt, xt.t[:, hs], ALU.add)
                P.dma("pool", Sc["x1"].t[rows, :], x1.t[:], reads=[x1], writes=[Sc["x1"].reg(ti)])
                return x1

            def part2(j, x1):
                ti = g * 4 + j
                hs_ = h2s.next()
                norm_transpose(C, K, x1, gffn, stat.next(), junk, hbr.next(), pt.next(), hs_, hs_.t[:])
                P.dma("pool", Sc["h2T"].t[ti], hs_.t[:], reads=[hs_], writes=[Sc["h2T"].reg(ti)])

            pend = part1(0)
            for j in range(4):
                nxt = part1(j + 1) if j + 1 < 4 else None
                part2(j, pend)
                pend = nxt


def phase_e1(C, K, ngroups=16):
    nc, P = C.nc, C.P
    Sc = C.scr
    I = C.ins
    with contextlib.ExitStack() as st:
        wq = load_w(C, st, "peer_wq", 8, 2048)
        skT = C.alloc(st, [128, 16, 128], BF16, "skT")
        P.dma("pool", skT.t[:], I["skT"].t, reads=[I["skT"]], writes=[skT])
        io_f = C.alloc(st, [128, 128], F32, "io_f")
        P.op("pool", lambda e: e.iota(io_f.t[:], pattern=[[1, 128]], base=0, channel_multiplier=0,
                                      allow_small_or_imprecise_dtypes=True), writes=[io_f])
        io_b = C.alloc(st, [128, 128], BF16, "io_b")
        cp(C, "dve", io_b, io_b.t[:], io_f, io_f.t[:])
        io_rep = C.alloc(st, [128, 128, 16], BF16, "io_rep")
        cp(C, "dve", io_rep, io_rep.t[:], io_f, io_f.t[:].unsqueeze(2).to_broadcast([128, 128, 16]))
        h2r = C.ring(st, 2, [128, 8, 128], BF16, "h2e")
        pq = C.ring(st, 2, [128, 4, 128], F32, "pq", psum=True)
        psc = C.ring(st, 2, [128, 4, 128], F32, "psc", psum=True)
        pIG = C.alloc(st, [128, 8, 128], BF16, "pIG", psum=True)
        pG = C.ring(st, 3, [128, 4, 128], F32, "pG", psum=True)
        qpT = C.ring(st, 2, [128, 16, 128], BF16, "qpT")
        s_all = C.ring(st, 2, [128, 16, 128], F32, "s_all")
        tmp128 = C.ring(st, 4, [128, 128], F32, "tmp128")
        v16 = C.ring(st, 2, [128, 16, 16], F32, "v16")
        i16 = C.ring(st, 2, [128, 16, 16], U32, "i16")
        i16f = C.ring(st, 2, [128, 16, 16], F32, "i16f")
        cand = C.ring(st, 1, [128, 8, 256], F32, "cand")
        tmp256 = C.ring(st, 4, [128, 256], F32, "tmp256")
        tsv = C.ring(st, 2, [128, 8, 16], F32, "tsv")
        pos = C.ring(st, 2, [128, 8, 16], U32, "pos")
        k12i = C.ring(st, 2, [128, 2, 128], I32, "k12i")
        k12f = C.ring(st, 2, [128, 2, 128], F32, "k12f")
        eq = C.ring(st, 2, [128, 128, 16], F32, "eq")
        IG = C.ring(st, 2, [128, 3, 128], BF16, "IG")
        IGf = C.ring(st, 2, [128, 3, 128], F32, "IGf")
        IGT = C.ring(st, 2, [128, 3, 128], BF16, "IGT")
        ex = C.ring(st, 2, [128, 8, 16], F32, "ex")
        st8 = C.ring(st, 2, [128, 8], F32, "st8")
        A4 = C.ring(st, 3, [128, 16, 128], BF16, "A4")
        B4 = C.ring(st, 3, [128, 16, 128], BF16, "B4")
        Gst = C.ring(st, 1, [128, 128, 256], BF16, "Gst")
        est = {}

        def stageXc(grp, j2):
            ti = grp * 2 + j2
            h2 = h2r.next()
            P.dma("sp", h2.t[:], Sc["h2T"].t[ti], reads=[Sc["h2T"].reg(ti)], writes=[h2])
            qp, sa = qpT.next(), s_all.next()
            for c4 in range(4):
                p_ = pq.next()
                for cc in range(4):
                    cq = c4 * 4 + cc
                    for k in range(8):
                        mm(C, p_, p_.t[:, cc, :], wq, wq.t[:, k, cq * 128:(cq + 1) * 128], h2, h2.t[:, k, :], k == 0, k == 7)
                cp(C, "act", qp, qp.t[:, c4 * 4:(c4 + 1) * 4, :], p_, p_.t[:])
            for c4 in range(4):
                p_ = psc.next()
                for cc in range(4):
                    cq = c4 * 4 + cc
                    mm(C, p_, p_.t[:, cc, :], qp, qp.t[:, cq, :], skT, skT.t[:, cq, :], True, True)
                cp(C, "act", sa, sa.t[:, c4 * 4:(c4 + 1) * 4, :], p_, p_.t[:])
            if "dbgs" in Sc:
                P.dma("pool", Sc["dbgs"].t[ti], sa.t[:], reads=[sa], writes=[Sc["dbgs"].reg(ti)])
            est["sa", grp, j2] = sa

        def stageXt(grp, j2):
            ti = grp * 2 + j2
            sa = est.pop(("sa", grp, j2))
            v_, i_ = v16.next(), i16.next()

            def top16(src_t, src_ap, vdst_t, vdst_ap, idst_t, idst_ap, tmp):
                P.op("dve", lambda e: e.max(out=vdst_ap[:, 0:8], in_=src_ap), reads=[src_t], writes=[vdst_t])
                yield
                P.op("dve", lambda e: e.match_replace(out=tmp.t[:], in_to_replace=vdst_ap[:, 0:8], in_values=src_ap,
                                                      imm_value=-1e30), reads=[src_t, vdst_t], writes=[tmp])
                yield
                P.op("dve", lambda e: e.max(out=vdst_ap[:, 8:16], in_=tmp.t[:]), reads=[tmp, vdst_t], writes=[vdst_t])
                yield
                P.op("dve", lambda e: e.max_index(out=idst_ap[:, 0:8], in_max=vdst_ap[:, 0:8], in_values=src_ap),
                     reads=[src_t, vdst_t], writes=[idst_t])
                yield
                P.op("dve", lambda e: e.max_index(out=idst_ap[:, 8:16], in_max=vdst_ap[:, 8:16], in_values=src_ap),
                     reads=[src_t, vdst_t, idst_t], writes=[idst_t])
                yield

            def rr4(gens):
                gens = list(gens)
                while gens:
                    for g_ in list(gens):
                        try:
                            next(g_)
                        except StopIteration:
                            gens.remove(g_)

            for c0 in range(0, 16, 4):
                rr4([top16(sa, sa.t[:, cq, :], v_.reg(cq), v_.t[:, cq, :], i_.reg(cq), i_.t[:, cq, :], tmp128.next())
                     for cq in range(c0, c0 + 4)])
            if_ = i16f.next()
            cp(C, "dve", if_, if_.t[:], i_.regl(range(16)), i_.t[:])
            cd = cand.next()
            vv = v_.t[:].rearrange("p (h a) k -> p h a k", a=2)
            tt(C, "dve", cd, cd.t[:].rearrange("p h (a b) -> p h a b", b=16),
               v_.regl(range(16)), vv[:, :, 0, :].unsqueeze(3).to_broadcast([128, 8, 16, 16]),
               v_.regl(range(16)), vv[:, :, 1, :].unsqueeze(2).to_broadcast([128, 8, 16, 16]), ALU.add)
            ts_, ps_ = tsv.next(), pos.next()
            for h0 in range(0, 8, 4):
                rr4([top16(cd, cd.t[:, h, :], ts_.reg(h), ts_.t[:, h, :], ps_.reg(h), ps_.t[:, h, :], tmp256.next())
                     for h in range(h0, h0 + 4)])
            ki, kf = k12i.next(), k12f.next()
            posf = ps_.t[:].rearrange("p h k -> p (h k)").bitcast(I32)
            P.op("dve", lambda e, ki=ki, posf=posf: e.tensor_single_scalar(out=ki.t[:, 0, :], in_=posf, scalar=4, op=ALU.arith_shift_right),
                 reads=ps_.regl(range(8)), writes=[ki])
            P.op("dve", lambda e, ki=ki, posf=posf: e.tensor_single_scalar(out=ki.t[:, 1, :], in_=posf, scalar=15, op=ALU.bitwise_and),
                 reads=ps_.regl(range(8)) + [ki], writes=[ki])
            cp(C, "dve", kf, kf.t[:], ki, ki.t[:])
            ig = IGf.next()
            iv = if_.t[:].rearrange("p (h a) k -> p h a k", a=2)
            for a in range(2):
                e_ = eq.next()
                tt(C, "dve", e_, e_.t[:], kf, kf.t[:, a, :].unsqueeze(2).to_broadcast([128, 128, 16]),
                   io_f, io_f.t[:, 0:16].unsqueeze(1).to_broadcast([128, 128, 16]), ALU.is_equal)
                e4 = e_.t[:].rearrange("p (h k) c -> p h k c", h=8)
                tt(C, "dve", e_, e4, e_, e4, if_, iv[:, :, a, :].unsqueeze(2).to_broadcast([128, 8, 16, 16]), ALU.mult)
                P.op("dve", lambda e, e_=e_, ig=ig, a=a: e.tensor_reduce(out=ig.t[:, a, :], in_=e_.t[:], axis=AX.X, op=ALU.add),
                     reads=[e_], writes=[ig])
            x_ = ex.next()
            tt(C, "dve", x_, x_.t[:], ts_.regl(range(8)), ts_.t[:], ts_.regl(range(8)), ts_.t[:, :, 0:1].to_broadcast([128, 8, 16]), ALU.subtract)
            act(C, x_, x_.t[:], x_, x_.t[:], AF.Exp)
            s8 = st8.next()
            P.op("dve", lambda e, s8=s8, x_=x_: e.tensor_reduce(out=s8.t[:], in_=x_.t[:], axis=AX.X, op=ALU.add), reads=[x_], writes=[s8])
            P.op("dve", lambda e, s8=s8: e.reciprocal(out=s8.t[:], in_=s8.t[:]), reads=[s8], writes=[s8])
            tt(C, "dve", ig, ig.t[:, 2, :].rearrange("p (h k) -> p h k", h=8), x_, x_.t[:],
               s8, s8.t[:].unsqueeze(2).to_broadcast([128, 8, 16]), ALU.mult)
            igf_ = ig
            ig = IG.next()
            cp(C, "dve", ig, ig.t[:], igf_, igf_.t[:])
            if "dbgig" in Sc:
                P.dma("pool", Sc["dbgig"].t[ti], ig.t[:], reads=[ig], writes=[Sc["dbgig"].reg(ti)])
            for a in range(3):
                tr(C, pIG, pIG.t[:, a, :], ig, ig.t[:, a, :], K["ident"])
            igt = IGT.next()
            cp(C, "dve", igt, igt.t[:], pIG, pIG.t[:, 0:3, :])
            est["igt", grp, j2] = igt

        def stageY(grp, j2):
            if j2 == 0:
                est["G", grp] = Gst.next()
            G_ = est["G", grp]
            igt = est.pop(("igt", grp, j2))
            TB = 16
            for b16 in range(128 // TB):
                a4, bb4 = A4.next(), B4.next()
                tsl = slice(b16 * TB, (b16 + 1) * TB)
                av = a4.t[:].rearrange("p t i -> p (t i)").rearrange("p (i t) -> p i t", t=TB)
                bv = bb4.t[:].rearrange("p t i -> p (t i)").rearrange("p (i t) -> p i t", t=TB)
                tt(C, "dve", a4, av, io_rep, io_rep.t[:], igt, igt.t[:, 0, tsl].unsqueeze(1).to_broadcast([128, 128, TB]), ALU.is_equal)
                tt(C, "dve", bb4, bv, io_rep, io_rep.t[:], igt, igt.t[:, 1, tsl].unsqueeze(1).to_broadcast([128, 128, TB]), ALU.is_equal)
                tt(C, "pool", a4, av, a4, av, igt, igt.t[:, 2, tsl].unsqueeze(1).to_broadcast([128, 128, TB]), ALU.mult)
                for q4 in range(TB // 4):
                    pg = pG.next()
                    for q_ in range(4):
                        mm(C, pg, pg.t[:, q_, :], a4, av[:, :, q4 * 4 + q_], bb4, bv[:, :, q4 * 4 + q_], True, True)
                    t0 = j2 * 128 + b16 * TB + q4 * 4
                    cp(C, "act", G_, G_.t[:, :, t0:t0 + 4].rearrange("p i t -> p t i"), pg, pg.t[:])
            if j2 == 1:
                hc = slice((grp % 2) * 256, (grp % 2 + 1) * 256)
                for i0 in range(0, 128, 32):
                    P.dma("pool", Sc["G"].t[grp // 2, :, i0:i0 + 32, hc], G_.t[:, i0:i0 + 32, :], reads=[G_], writes=[Sc["G"].reg(grp)])

        tl = [(grp, j2) for grp in range(ngroups) for j2 in range(2)]
        for it in range(len(tl) + 2):
            if it < len(tl):
                stageXc(*tl[it])
            if 1 <= it < len(tl) + 1:
                stageXt(*tl[it - 1])
            if it >= 2:
                stageY(*tl[it - 2])


def phase_e0(C, K):
    P = C.P
    jobs = []
    for i2 in range(128):
        jobs.append(lambda i2=i2: P.dma("pool", C.scr["uTb"].t[i2], C.ins["uT"].t[i2], reads=[C.ins["uT"]], writes=[C.scr["uTb"].reg(i2)]))
        jobs.append(lambda i2=i2: P.dma("pool", C.scr["vLb"].t[i2], C.ins["vL"].t[i2], reads=[C.ins["vL"]], writes=[C.scr["vLb"].reg(i2)]))
    return jobs


def phase_e2(C, K, ngroups=8, ni2=128):
    nc, P = C.nc, C.P
    Sc = C.scr
    with contextlib.ExitStack() as st:
        h2r = C.ring(st, 1, [128, 8, 512], BF16, "h2g")
        po = [C.alloc(st, [128, 512], F32, "po", psum=True) for _ in range(4)]
        par = C.ring(st, 4, [128, 512], F32, "pa", psum=True)
        uch = C.ring(st, 3, [128, 2, 8, 128], BF16, "uch")
        vch = C.ring(st, 4, [128, 2, 512], BF16, "vch")
        gch = C.ring(st, 3, [128, 2, 512], BF16, "gch")
        sqr = C.ring(st, 3, [128, 512], F32, "sqe")
        t2r = C.ring(st, 3, [128, 512], F32, "t2e")
        sgr = C.ring(st, 3, [128, 512], BF16, "sge")
        agr = C.ring(st, 3, [128, 512], BF16, "age")
        Wall = C.alloc(st, [128, ni2, 512], BF16, "Wall")
        xs = C.ring(st, 2, [128, 512], F32, "xs")
        x1h = C.ring(st, 2, [128, 512], F32, "x1h")
        LAG = 3
        state = {}

        def s1(grp, i2):
            h2 = state["h2"]
            if i2 % 2 == 0:
                u_, v_, g_ = uch.next(), vch.next(), gch.next()
                P.dma("sp", u_.t[:], Sc["uTb"].t[i2:i2 + 2].rearrange("i p k c -> p i k c"),
                      reads=Sc["uTb"].regl([i2, i2 + 1]), writes=[u_])
                P.dma("sp", v_.t[:], Sc["vLb"].t[i2:i2 + 2, :, 0:512].rearrange("i p d -> p i d"),
                      reads=Sc["vLb"].regl([i2, i2 + 1]), writes=[v_])
                P.dma("sp", g_.t[:], Sc["G"].t[grp, :, i2:i2 + 2, :], reads=Sc["G"].regl([2 * grp, 2 * grp + 1]), writes=[g_])
                state["uvg"] = (u_, v_, g_)
            u_, v_, g_ = state["uvg"]
            e_ = i2 % 2
            pa = par.next()
            for k in range(8):
                mm(C, pa, pa.t[:], u_, u_.t[:, e_, k, :], h2, h2.t[:, k, :], k == 0, k == 7)
            sq, t2, sg, ag = sqr.next(), t2r.next(), sgr.next(), agr.next()
            Wb = Wall.reg(i2)
            act(C, sq, sq.t[:], pa, pa.t[:], AF.Square, scale=0.21145921592448583)
            stt(C, "dve", t2, t2.t[:], sq, sq.t[:], 1.0, pa, pa.t[:], ALU.add, ALU.mult)
            tt(C, "dve", ag, ag.t[:], pa, pa.t[:], g_, g_.t[:, e_, :], ALU.mult)
            act(C, sg, sg.t[:], t2, t2.t[:], AF.Sigmoid, scale=1.5957691216057308)
            P.op("pool", lambda e: e.tensor_tensor(out=Wall.t[:, i2, :], in0=sg.t[:], in1=ag.t[:], op=ALU.mult),
                 reads=[sg, ag], writes=[Wb])
            state["v", i2] = (v_, e_)

        def s2(grp, i2, half):
            v_, e_ = state.pop(("v", i2)) if half == 0 else state.pop(("v2", i2))
            for j in range(4):
                P.op("pe", lambda e, j=j: e.matmul(po[j].t[:], lhsT=Wall.t[:, i2, j * 128:(j + 1) * 128], rhs=v_.t[:, e_, :],
                                                    start=(i2 == 0), stop=(i2 == ni2 - 1)),
                     reads=[Wall.reg(i2), v_], writes=[po[j]], pe_accum=True)

        def evac(grp, half):
            hs = slice(half * 512, (half + 1) * 512)
            for j in range(4):
                ti = grp * 4 + j
                rows = slice(ti * 128, (ti + 1) * 128)
                x1_ = x1h.next()
                P.dma("sp", x1_.t[:], Sc["x1"].t[rows, hs], reads=[Sc["x1"].reg(ti)], writes=[x1_])
                x_ = xs.next()
                tt(C, "dve", x_, x_.t[:], po[j], po[j].t[:], x1_, x1_.t[:], ALU.add)
                P.dma("pool", Sc["x2"].t[rows, hs], x_.t[:], reads=[x_], writes=[Sc["x2"].reg((ti, half))])

        for grp in range(ngroups):
            h2 = h2r.next()
            for j in range(4):
                ti = grp * 4 + j
                P.dma("sp", h2.t[:, :, j * 128:(j + 1) * 128], Sc["h2T"].t[ti], reads=[Sc["h2T"].reg(ti)], writes=[h2])
            state["h2"] = h2
            for it in range(ni2 + LAG):
                if it < ni2:
                    s1(grp, it)
                if it >= LAG:
                    s2(grp, it - LAG, 0)
            evac(grp, 0)
            for it in range(ni2 + LAG):
                if it < ni2:
                    if it % 2 == 0:
                        v_ = vch.next()
                        P.dma("sp", v_.t[:], Sc["vLb"].t[it:it + 2, :, 512:1024].rearrange("i p d -> p i d"),
                              reads=Sc["vLb"].regl([it, it + 1]), writes=[v_])
                        state["vp"] = v_
                    state["v2", it] = (state["vp"], it % 2)
                if it >= LAG:
                    s2(grp, it - LAG, 1)
            evac(grp, 1)


def phase_f(C, K, ntiles=NT):
    nc, P = C.nc, C.P
    Sc = C.scr
    I = C.ins
    with contextlib.ExitStack() as st:
        wg = load_w(C, st, "ple_gate", 8, 1024)
        wp = load_w(C, st, "ple_proj", 2, 1024)
        gple = C.alloc(st, [128, 1024], F32, "gple")
        P.dma("sp", gple.t[:], I["norm_ple"].t.to_broadcast([128, 1024]), writes=[gple])
        x2r = C.ring(st, 3, [128, 1024], F32, "x2f")
        pr_ = C.ring(st, 2, [128, 256], F32, "pf32")
        pbr = C.ring(st, 2, [128, 256], BF16, "pbf")
        junk = C.alloc(st, [128, 1024], BF16, "junkf")
        stat = C.ring(st, 2, [128, 8], F32, "statf")
        hbr = C.ring(st, 2, [128, 1024], BF16, "hbf")
        pt = C.ring(st, 2, [128, 8, 128], BF16, "ptf", psum=True)
        ptp = C.alloc(st, [128, 8, 128], BF16, "ptp", psum=True)
        h3r = C.ring(st, 3, [128, 8, 128], BF16, "h3T")
        pTr = C.ring(st, 3, [128, 2, 128], BF16, "pT")
        pgr = C.ring(st, 2, [128, 512], F32, "pgate", psum=True)
        ppr = C.ring(st, 2, [128, 512], F32, "pproj", psum=True)
        sgr = C.ring(st, 2, [128, 512], F32, "sgf")
        tr_ = C.ring(st, 2, [128, 512], F32, "tf")
        outr = C.ring(st, 2, [128, 1024], F32, "outf")
        def pro(ti):
            rows = slice(ti * 128, (ti + 1) * 128)
            x2 = x2r.next()
            P.dma("sp", x2.t[:], Sc["x2"].t[rows, :], reads=[Sc["x2"].reg((ti, 0)), Sc["x2"].reg((ti, 1))], writes=[x2])
            pf = pr_.next()
            P.dma("sp", pf.t[:], I["p"].t[rows, :], reads=[I["p"]], writes=[pf])
            pb = pbr.next()
            cp(C, "pool", pb, pb.t[:], pf, pf.t[:])
            for k in range(2):
                tr(C, ptp, ptp.t[:, k, :], pb, pb.t[:, k * 128:(k + 1) * 128], K["ident"])
            pT = pTr.next()
            cp(C, "act", pT, pT.t[:], ptp, ptp.t[:, 0:2, :])
            h3 = h3r.next()
            norm_transpose(C, K, x2, gple, stat.next(), junk, hbr.next(), pt.next(), h3, h3.t[:])
            return x2, pT, h3

        def body(ti, x2, pT, h3):
            rows = slice(ti * 128, (ti + 1) * 128)
            o_ = outr.next()
            for hf in range(2):
                hs = slice(hf * 512, (hf + 1) * 512)
                pg, pp = pgr.next(), ppr.next()
                for k in range(8):
                    mm(C, pg, pg.t[:], h3, h3.t[:, k, :], wg, wg.t[:, k, hs], k == 0, k == 7)
                for k in range(2):
                    mm(C, pp, pp.t[:], pT, pT.t[:, k, :], wp, wp.t[:, k, hs], k == 0, k == 1)
                sg, t_ = sgr.next(), tr_.next()
                act(C, sg, sg.t[:], pg, pg.t[:], AF.Sigmoid)
                tt(C, "dve", t_, t_.t[:], pp, pp.t[:], sg, sg.t[:], ALU.mult)
                tt(C, "pool", o_, o_.t[:, hs], t_, t_.t[:], x2, x2.t[:, hs], ALU.add)
            P.dma("sp", C.y.t[rows, :], o_.t[:], reads=[o_], writes=[C.y.reg(ti)])

        pend = pro(0)
        for ti in range(ntiles):
            nxt = pro(ti + 1) if ti + 1 < ntiles else None
            body(ti, *pend)
            pend = nxt


def declare(C):
    C.din("x", [S, D])
    C.din("p", [S, 256])
    C.din("norm_mix", [1, D])
    C.din("w_in", [D, 5376])
    C.din("q_norm", [1, 64])
    C.din("k_norm", [1, 64])
    C.din("hg_lb_raw", [2, 2, 512])
    C.din("hg_out_norm", [1, 128])
    C.din("w_up_att", [512, D])
    C.din("w_up_hg", [512, D])
    C.din("w_out", [D, D])
    C.din("norm_ffn", [1, D])
    C.din("peer_wq", [D, 2048])
    C.din("skT", [128, 16, 128])
    C.din("uT", [128, 128, 8, 128])
    C.din("vL", [128, 128, D])
    C.din("norm_ple", [1, D])
    C.din("ple_gate", [D, D])
    C.din("ple_proj", [256, D])
    sc = C.scratch
    sc("hqT", [4, 128, S], BF16)
    sc("kT", [2, 4, 128, S], BF16)
    sc("gtsT", [16, 128, S], BF16)
    sc("qT", [4, 128, S], BF16)
    sc("kTa", [2, 128, S], BF16)
    sc("v", [NT, 128, 2, 128], BF16)
    sc("g", [2, S, 512], F32)
    sc("k", [2, S, 512], BF16)
    sc("hi", [S, 512], BF16)
    sc("sg", [S, 512], BF16)
    sc("attoT", [4, 128, S], BF16)
    sc("ofwd", [S, 512], F32)
    sc("uTb", [128, 128, 8, 128], BF16)
    sc("vLb", [128, 128, D], BF16)
    sc("x2", [S, D], F32)
    sc("G", [8, 128, 128, 512], BF16)
    if "dbgs" in C.dbg:
        sc("dbgs", [NT, 128, 16, 128], F32)
        sc("dbgig", [NT, 128, 3, 128], BF16)
    sc("x1", [S, D], F32)
    sc("h2T", [NT, 128, 8, 128], BF16)
    sc("hgoT", [4, 128, S], BF16)
    if "dbgo" in C.dbg:
        sc("dbgo", [S, 512], F32)
    if "dbgacc" in C.dbg:
        sc("dbgacc", [128, 512], F32)
        sc("dbgp", [2, 128, 512], BF16)
        sc("dbgvs", [128, NT, 256], BF16)


def build(dbg=(), phases="A", ntiles=NT, **kw):
    nc = bass.Bass("TRN2", target_bir_lowering=False)
    C = Ctx(nc, dbg)
    declare(C)
    C.y = T(nc.dram_tensor("y", [S, D], F32, kind="ExternalOutput").ap(), "y")
    C.outs.append(C.y)
    with contextlib.ExitStack() as st:
        K = build_consts(C, st)
        if "A" in phases:
            phase_a(C, K, ntiles)
        if "C" in phases:
            C.P.barrier()
            phase_c(C, K, kw.get("c_tiles", NT), kw.get("c_dirs", (0, 1)))
        C.P.barrier()
        bg = phase_e0(C, K) if "2" in phases else []
        if "B" in phases:
            phase_b(C, K, kw.get("b_groups", 8), kw.get("hhs", (0, 1)), bg)
        else:
            for job in bg:
                job()
        if "D" in phases:
            C.P.barrier()
            phase_d(C, K, kw.get("d_groups", 8))
        if "E" in phases:
            C.P.barrier()
            phase_e1(C, K, kw.get("e1_groups", 16))
        if "2" in phases:
            C.P.barrier()
            phase_e2(C, K, kw.get("e2_groups", 8), kw.get("ni2", 128))
        if "F" in phases:
            C.P.barrier()
            phase_f(C, K, kw.get("f_tiles", NT))
        fin = []
        for t in C.outs:
            fin.append(t.b)
            fin.extend(t.regs.values())
        C.P.emit(final_bufs=fin)
    return nc, C


def _in_maps(inp, ncores):
    shared = {}
    for k in ["norm_mix", "q_norm", "k_norm", "hg_out_norm", "norm_ffn", "norm_ple"]:
        shared[k] = np.ascontiguousarray(np.asarray(inp[k], np.float32)[0][None])
    for k in ["w_in", "w_up_att", "w_up_hg", "w_out", "peer_wq", "ple_gate", "ple_proj"]:
        shared[k] = np.ascontiguousarray(np.asarray(inp[k], np.float32)[0])
    shared["hg_lb_raw"] = np.ascontiguousarray(np.asarray(inp["hg_lb_raw"], np.float32))
    sk = np.asarray(inp["peer_subkeys"], np.float32)[0]
    shared["skT"] = np.ascontiguousarray(sk.transpose(3, 0, 1, 2).reshape(128, 16, 128))
    u = np.asarray(inp["peer_u"], np.float32)[0].reshape(128, 128, 8, 128)
    shared["uT"] = np.ascontiguousarray(u.transpose(1, 3, 2, 0))
    v = np.asarray(inp["peer_v"], np.float32)[0].reshape(128, 128, D)
    shared["vL"] = np.ascontiguousarray(v.transpose(1, 0, 2))
    x = np.asarray(inp["x"], np.float32)
    p = np.asarray(inp["p"], np.float32)
    maps = []
    for b in range(ncores):
        m = dict(shared)
        m["x"] = np.ascontiguousarray(x[b])
        m["p"] = np.ascontiguousarray(p[0, b])
        maps.append(m)
    return maps


_NC_CACHE = {}


def kernel(**inputs):
    ncores = 8
    if "nc" not in _NC_CACHE:
        _NC_CACHE["nc"] = build(phases="ABCDE2F")[0]
    nc = _NC_CACHE["nc"]
    maps = _in_maps(inputs, ncores)
    res = run_bass_kernel_spmd(nc, maps, core_ids=list(range(ncores)))
    out = np.stack([np.asarray(res.results[b]["y"], np.float32) for b in range(ncores)], 0)
    return out
```

```python
import contextlib
import numpy as np
import concourse.bass as bass
import concourse.mybir as mybir
from concourse.bass_utils import run_bass_kernel_spmd

F32 = mybir.dt.float32
BF16 = mybir.dt.bfloat16
I32 = mybir.dt.int32
U32 = mybir.dt.uint32
ALU = mybir.AluOpType
AF = mybir.ActivationFunctionType
AX = mybir.AxisListType

S = 4096
D = 1024
NT = S // 128
EPS = 1e-6
EPOCH = 16000
NDMA_SLOTS = 48


class Buf:
    __slots__ = ("name", "last_w", "readers")

    def __init__(self, name=""):
        self.name = name
        self.last_w = None
        self.readers = {}


class T:
    __slots__ = ("t", "b", "regs")

    def __init__(self, t, name=""):
        self.t = t
        self.b = Buf(name)
        self.regs = {}

    def reg(self, key):
        if key not in self.regs:
            self.regs[key] = Buf(f"{self.b.name}[{key}]")
        return self.regs[key]

    def regl(self, keys):
        return [self.reg(k) for k in keys]


class Prog:
    ENGS = ("pe", "act", "dve", "pool", "sp")

    def __init__(self, nc):
        self.nc = nc
        self.ops = {e: [] for e in self.ENGS}
        self.count = {e: 0 for e in self.ENGS}
        self.known = {e: {} for e in self.ENGS}
        self.dma_count = [0] * NDMA_SLOTS
        self.dma_rr = {"hw": 0, "sw": 0}
        self.n_instr = 0
        self.pending = {e: [] for e in self.ENGS}

    def barrier(self):
        prods = [(e, self.count[e]) for e in self.ENGS if self.count[e] > 0]
        prods += [(("dma", s), c) for s, c in enumerate(self.dma_count) if c > 0]
        for e in self.ENGS:
            kn = self.known[e]
            for p, c in prods:
                if kn.get(p, 0) < c:
                    kn[p] = c
                    self.pending[e].append((p, c))

    def _deps(self, eng, reads, writes, pe_accum=False):
        deps = {}

        def add(p, s):
            if deps.get(p, 0) < s:
                deps[p] = s

        for b in reads:
            if b.last_w is not None:
                add(*b.last_w)
        for b in writes:
            if b.last_w is not None:
                if not (pe_accum and b.last_w[0] == "pe" and eng == "pe"):
                    add(*b.last_w)
            for p, s in b.readers.items():
                add(p, s)
        waits = []
        kn = self.known[eng]
        for p, s in deps.items():
            if kn.get(p, 0) < s:
                kn[p] = s
                waits.append((p, s))
        return waits

    def _commit(self, prod, seq, reads, writes):
        for b in writes:
            b.last_w = (prod, seq)
            b.readers = {}
        for b in reads:
            b.readers[prod] = max(b.readers.get(prod, 0), seq)

    @staticmethod
    def _bufs(xs):
        out = []
        for x in xs:
            if isinstance(x, T):
                out.append(x.b)
            elif isinstance(x, (list, tuple)):
                out.extend(Prog._bufs(x))
            else:
                out.append(x)
        return out

    def op(self, eng, fn, reads=(), writes=(), pe_accum=False):
        reads = self._bufs(reads)
        writes = self._bufs(writes)
        waits = self._deps(eng, reads, writes, pe_accum)
        waits = [w for w in self.pending[eng] if w not in waits] + waits
        self.pending[eng] = []
        self.count[eng] += 1
        seq = self.count[eng]
        self.ops[eng].append((waits, fn, (eng, seq)))
        self._commit(eng, seq, reads, writes)
        self.n_instr += 1

    def dma(self, q, out, in_, reads=(), writes=(), **kw):
        reads = self._bufs(reads)
        writes = self._bufs(writes)
        waits = self._deps(q, reads, writes)
        waits = [w for w in self.pending[q] if w not in waits] + waits
        self.pending[q] = []
        half = NDMA_SLOTS // 2
        kind = "sw" if q == "pool" else "hw"
        slot = self.dma_rr[kind] + (half if kind == "sw" else 0)
        self.dma_rr[kind] = (self.dma_rr[kind] + 1) % half
        prod = ("dma", slot)
        prev = self.dma_count[slot]
        kn = self.known[q]
        if prev and kn.get(prod, 0) < prev:
            kn[prod] = prev
            waits.append((prod, prev))
        self.dma_count[slot] += 1
        seq = self.dma_count[slot]
        self.ops[q].append((waits, (lambda e: e.dma_start(out=out, in_=in_, **kw)), (prod, seq)))
        self._commit(prod, seq, reads, writes)
        self.n_instr += 1

    def emit(self, final_bufs=()):
        nc = self.nc
        final_bufs = self._bufs(final_bufs)
        with contextlib.ExitStack() as st:
            sems = {}
            for e in self.ENGS:
                nep = self.count[e] // EPOCH + 1
                sems[e] = [st.enter_context(nc.semaphore(f"s_{e}_{k}")) for k in range(nep)]
            for s in range(NDMA_SLOTS):
                sems[("dma", s)] = [st.enter_context(nc.semaphore(f"s_dma{s}"))]
            fin = {}
            for b in final_bufs:
                if b.last_w is not None:
                    p, s = b.last_w
                    fin[p] = max(fin.get(p, 0), s)

            def wait(eng, p, s):
                if isinstance(p, tuple):
                    eng.wait_ge(sems[p][0], 16 * s)
                else:
                    k = (s - 1) // EPOCH
                    eng.wait_ge(sems[p][k], s - k * EPOCH)

            def run(ename, eng):
                for waits, fn, (prod, seq) in self.ops[ename]:
                    for p, s in waits:
                        wait(eng, p, s)
                    ins = fn(eng)
                    if isinstance(prod, tuple):
                        ins.then_inc(sems[prod][0], 16)
                    else:
                        k = (seq - 1) // EPOCH
                        ins.then_inc(sems[prod][k], 1)
                if ename == "sp":
                    for p, s in fin.items():
                        wait(eng, p, s)

            with nc.Block() as block:
                @block.sync
                def _(e):
                    run("sp", e)

                @block.tensor
                def _(e):
                    run("pe", e)

                @block.scalar
                def _(e):
                    run("act", e)

                @block.vector
                def _(e):
                    run("dve", e)

                @block.gpsimd
                def _(e):
                    run("pool", e)


class Ring:
    def __init__(self, items):
        self.items = items
        self.i = 0

    def next(self):
        x = self.items[self.i % len(self.items)]
        self.i += 1
        return x


class Ctx:
    def __init__(self, nc, dbg=()):
        self.nc = nc
        self.P = Prog(nc)
        self.dbg = set(dbg)
        self.ins = {}
        self.scr = {}
        self.outs = []
        self.uid = 0

    def din(self, name, shape, dt=F32):
        ap = self.nc.dram_tensor(name, list(shape), dt, kind="ExternalInput").ap()
        self.ins[name] = T(ap, name)
        return self.ins[name]

    def scratch(self, name, shape, dt):
        if name in self.dbg:
            ap = self.nc.dram_tensor(name, list(shape), dt, kind="ExternalOutput").ap()
        else:
            ap = self.nc.dram_tensor(name, list(shape), dt).ap()
        t = T(ap, name)
        self.scr[name] = t
        if name in self.dbg:
            self.outs.append(t)
        return t

    def alloc(self, st, shape, dt, name=None, psum=False):
        self.uid += 1
        name = f"{name or 't'}_{self.uid}"
        if psum:
            t = st.enter_context(self.nc.psum_tensor(name, list(shape), dt))
        else:
            t = st.enter_context(self.nc.sbuf_tensor(name, list(shape), dt))
        return T(t, name)

    def ring(self, st, n, shape, dt, name=None, psum=False):
        return Ring([self.alloc(st, shape, dt, name, psum) for _ in range(n)])


def mm(C, out_t, out_ap, lhsT_t, lhsT_ap, rhs_t, rhs_ap, start, stop):
    C.P.op("pe", lambda e: e.matmul(out_ap, lhsT=lhsT_ap, rhs=rhs_ap, start=start, stop=stop),
           reads=[lhsT_t, rhs_t], writes=[out_t], pe_accum=True)


def tr(C, out_t, out_ap, in_t, in_ap, ident):
    C.P.op("pe", lambda e: e.transpose(out=out_ap, in_=in_ap, identity=ident.t[:]),
           reads=[in_t, ident], writes=[out_t], pe_accum=True)


def act(C, out_t, out_ap, in_t, in_ap, func, reads=(), extra_w=(), **kw):
    C.P.op("act", lambda e: e.activation(out=out_ap, in_=in_ap, func=func, **kw),
           reads=[in_t] + list(reads), writes=[out_t] + list(extra_w))


def tt(C, eng, out_t, out_ap, a_t, a_ap, b_t, b_ap, op):
    C.P.op(eng, lambda e: e.tensor_tensor(out=out_ap, in0=a_ap, in1=b_ap, op=op),
           reads=[a_t, b_t], writes=[out_t])


def ts(C, eng, out_t, out_ap, a_t, a_ap, s1, s2, op0, op1=None, reads=()):
    if op1 is None:
        C.P.op(eng, lambda e: e.tensor_scalar(out=out_ap, in0=a_ap, scalar1=s1, scalar2=None, op0=op0),
               reads=[a_t] + list(reads), writes=[out_t])
    else:
        C.P.op(eng, lambda e: e.tensor_scalar(out=out_ap, in0=a_ap, scalar1=s1, scalar2=s2, op0=op0, op1=op1),
               reads=[a_t] + list(reads), writes=[out_t])


def stt(C, eng, out_t, out_ap, a_t, a_ap, scalar, b_t, b_ap, op0, op1, reads=()):
    C.P.op(eng, lambda e: e.scalar_tensor_tensor(out=out_ap, in0=a_ap, scalar=scalar, in1=b_ap, op0=op0, op1=op1),
           reads=[a_t, b_t] + list(reads), writes=[out_t])


def cp(C, eng, out_t, out_ap, in_t, in_ap):
    if eng == "act":
        C.P.op("act", lambda e: e.copy(out=out_ap, in_=in_ap), reads=[in_t], writes=[out_t])
    else:
        C.P.op(eng, lambda e: e.tensor_copy(out=out_ap, in_=in_ap), reads=[in_t], writes=[out_t])


def rsqrt_mean(C, st_t, src_ap_fn, n, scale):
    ap = src_ap_fn()
    ts(C, "dve", st_t, ap, st_t, ap, scale, EPS, ALU.mult, ALU.add)
    C.P.op("act", lambda e: e.activation(out=ap, in_=ap, func=AF.Sqrt), reads=[st_t], writes=[st_t])
    C.P.op("dve", lambda e: e.reciprocal(out=ap, in_=ap), reads=[st_t], writes=[st_t])


def build_consts(C, st):
    P = C.P
    K = {}
    idf = C.alloc(st, [128, 128], F32, "idf")
    P.op("pool", lambda e: e.memset(idf.t[:], 0.0), writes=[idf])
    P.op("pool", lambda e: e.affine_select(out=idf.t[:], in_=idf.t[:], pattern=[[-1, 128]],
                                           compare_op=ALU.not_equal, fill=1.0, base=0, channel_multiplier=1),
         reads=[idf], writes=[idf])
    ident = C.alloc(st, [128, 128], BF16, "ident")
    cp(C, "dve", ident, ident.t[:], idf, idf.t[:])
    K["ident"] = ident
    K["identf"] = idf
    return K


def build_rope(C, st):
    import math
    P = C.P
    rope = C.alloc(st, [128, NT, 2, 32], F32, "rope")
    with contextlib.ExitStack() as tmp:
        pf = C.alloc(tmp, [128, 1], F32, "pf")
        P.op("pool", lambda e: e.iota(pf.t[:], pattern=[[0, 1]], base=0, channel_multiplier=1,
                                      allow_small_or_imprecise_dtypes=True), writes=[pf])
        pi = C.alloc(tmp, [128, 2], I32, "pi")
        hl = C.alloc(tmp, [128, 2], I32, "hl")
        hlf = C.alloc(tmp, [128, 2], F32, "hlf")
        cp(C, "dve", pi, pi.t[:, 0:1], pf, pf.t[:])
        P.op("dve", lambda e: e.tensor_single_scalar(out=hl.t[:, 0:1], in_=pi.t[:, 0:1], scalar=6, op=ALU.arith_shift_right),
             reads=[pi], writes=[hl])
        P.op("dve", lambda e: e.tensor_single_scalar(out=hl.t[:, 1:2], in_=pi.t[:, 0:1], scalar=63, op=ALU.bitwise_and),
             reads=[pi, hl], writes=[hl])
        cp(C, "dve", hlf, hlf.t[:], hl, hl.t[:])
        jf = C.alloc(tmp, [128, 16], F32, "jf")
        P.op("pool", lambda e: e.iota(jf.t[:], pattern=[[1, 16]], base=0, channel_multiplier=0,
                                      allow_small_or_imprecise_dtypes=True), writes=[jf])
        inv = C.alloc(tmp, [128, 16], F32, "inv")
        act(C, inv, inv.t[:], jf, jf.t[:], AF.Exp, scale=-math.log(10000.0) / 16.0)
        rowp = C.alloc(tmp, [128, NT], F32, "rowp")
        P.op("pool", lambda e: e.iota(rowp.t[:], pattern=[[2, NT]], base=0, channel_multiplier=0,
                                      allow_small_or_imprecise_dtypes=True), writes=[rowp])
        ts(C, "dve", rowp, rowp.t[:], rowp, rowp.t[:], hlf.t[:, 0:1], None, ALU.add, reads=[hlf])
        ang = C.alloc(tmp, [128, NT, 32], F32, "ang")
        tt(C, "dve", ang, ang.t[:, :, 0:16], rowp, rowp.t[:].unsqueeze(2).to_broadcast([128, NT, 16]),
           inv, inv.t[:].unsqueeze(1).to_broadcast([128, NT, 16]), ALU.mult)
        colang = C.alloc(tmp, [128, 16], F32, "colang")
        ts(C, "dve", colang, colang.t[:], inv, inv.t[:], hlf.t[:, 1:2], None, ALU.mult, reads=[hlf])
        cp(C, "dve", ang, ang.t[:, :, 16:32], colang, colang.t[:].unsqueeze(1).to_broadcast([128, NT, 16]))
        ni = C.alloc(tmp, [128, NT, 32], I32, "ni")
        nf = C.alloc(tmp, [128, NT, 32], F32, "nf")
        red = C.alloc(tmp, [128, NT, 32], F32, "red")
        two_pi = 2.0 * math.pi
        for which, shift in ((1, 0.0), (0, math.pi / 2.0)):
            ts(C, "dve", nf, nf.t[:], ang, ang.t[:], shift, 1.0 / two_pi, ALU.add, ALU.mult)
            cp(C, "dve", ni, ni.t[:], nf, nf.t[:])
            cp(C, "dve", nf, nf.t[:], ni, ni.t[:])
            stt(C, "dve", red, red.t[:], nf, nf.t[:], -two_pi, ang, ang.t[:], ALU.mult, ALU.add)
            ts(C, "dve", red, red.t[:], red, red.t[:], shift, 3.1415925, ALU.add, ALU.min)
            ts(C, "dve", red, red.t[:], red, red.t[:], -3.1415925, None, ALU.max)
            act(C, rope, rope.t[:, :, which, :], red, red.t[:], AF.Sin)
    return rope


OFF = dict(aq=0, ak=512, av=640, hq=768, hff=1280, hfb=1792, hi=2304, hg=2816, gate=3328)


def phase_a(C, K, ntiles=NT):
    nc, P = C.nc, C.P
    I = C.ins
    Sc = C.scr
    with contextlib.ExitStack() as st:
        K = dict(K)
        K["rope"] = build_rope(C, st)
        P.barrier()
        w_in = C.alloc(st, [128, 8, 5376], BF16, "w_in")
        wv = I["w_in"].t.rearrange("(k p) n -> p k n", p=128)
        for c0 in range(0, 5376, 672):
            P.dma("pool", w_in.t[:, :, c0:c0 + 672], wv[:, :, c0:c0 + 672], reads=[I["w_in"]], writes=[w_in])
        gmix = C.alloc(st, [128, 1024], F32, "gmix")
        P.dma("sp", gmix.t[:], I["norm_mix"].t.to_broadcast([128, 1024]), writes=[gmix])
        qg = C.alloc(st, [128, 8, 64], F32, "qg")
        P.dma("sp", qg.t[:], I["q_norm"].t.unsqueeze(1).to_broadcast([128, 8, 64]), writes=[qg])
        kg = C.alloc(st, [128, 2, 64], F32, "kg")
        P.dma("sp", kg.t[:], I["k_norm"].t.unsqueeze(1).to_broadcast([128, 2, 64]), writes=[kg])
        raw = C.alloc(st, [128, 2, 2, 512], F32, "raw")
        P.dma("sp", raw.t[:], I["hg_lb_raw"].t.unsqueeze(0).to_broadcast([128, 2, 2, 512]), writes=[raw])
        lbb = C.alloc(st, [128, 2, 512], F32, "lbb")
        omlb = C.alloc(st, [128, 2, 512], F32, "omlb")
        tt(C, "dve", lbb, lbb.t[:], raw, raw.t[:, 0], raw, raw.t[:, 1], ALU.subtract)
        act(C, omlb, omlb.t[:], lbb, lbb.t[:], AF.Sigmoid, scale=-1.0)
        act(C, lbb, lbb.t[:], lbb, lbb.t[:], AF.Sigmoid)
        rawT = C.alloc(st, [128, 2, 2, 4], F32, "rawT")
        P.dma("sp", rawT.t[:], I["hg_lb_raw"].t.rearrange("s r (c p) -> p s r c", p=128), writes=[rawT],
              allow_slow_non_contiguous=True)
        omlT = C.alloc(st, [128, 2, 4], F32, "omlT")
        tt(C, "dve", omlT, omlT.t[:], rawT, rawT.t[:, 0], rawT, rawT.t[:, 1], ALU.subtract)
        act(C, omlT, omlT.t[:], omlT, omlT.t[:], AF.Sigmoid, scale=-1.0)

        xr = C.ring(st, 3, [128, 1024], F32, "xt")
        junk = C.alloc(st, [128, 1024], BF16, "junk")
        stat = C.ring(st, 8, [128, 16], F32, "stat")
        hbr = C.ring(st, 2, [128, 1024], BF16, "hb")
        hTr = C.ring(st, 2, [128, 8, 512], BF16, "hT")
        ptr = C.ring(st, 2, [128, 8, 128], BF16, "ptr", psum=True)
        pfr = C.ring(st, 2, [128, 512], F32, "pf", psum=True)
        ptk = C.ring(st, 3, [128, 512], F32, "ptk", psum=True)
        pqt = C.alloc(st, [128, 8, 128], BF16, "pqt", psum=True)
        stg_bf = C.ring(st, 4, [128, 512], BF16, "stgb")
        stg_f = C.ring(st, 3, [128, 512], F32, "stgf")
        tmpf = C.ring(st, 4, [128, 512], F32, "tmpf")
        qn = C.ring(st, 3, [128, 512], F32, "qn")
        qr = C.ring(st, 2, [128, 512], BF16, "qr")
        kk = C.ring(st, 2, [128, 4, 64], BF16, "kk")
        kn_ = C.ring(st, 2, [128, 128], F32, "kn")
        vst = C.ring(st, 2, [128, 2, 128], BF16, "vst")
        for v_ in vst.items:
            P.op("pool", lambda e, v_=v_: e.memset(v_.t[:], 1.0), writes=[v_])
        qTs = C.ring(st, 2, [128, 6, 128], BF16, "qTs")
        rt = C.ring(st, 8, [128, 8, 2, 16], F32, "rt")

        def run_rr(gens):
            gens = list(gens)
            while gens:
                for g_ in list(gens):
                    try:
                        next(g_)
                    except StopIteration:
                        gens.remove(g_)

        def rope_norm(src_ps, src_ap, nh, gain, cs, dst_t, dst_ap4):
            w = nh * 64
            sq = tmpf.next()
            act(C, sq, sq.t[:, 0:w], src_ps, src_ap, AF.Square)
            yield
            s8 = stat.next()
            P.op("dve", lambda e: e.tensor_reduce(out=s8.t[:, 0:nh], in_=sq.t[:, 0:w].rearrange("p (h d) -> p h d", d=64),
                                                  axis=AX.X, op=ALU.add), reads=[sq], writes=[s8])
            yield
            ap = s8.t[:, 0:nh]
            ts(C, "dve", s8, ap, s8, ap, 1.0 / 64, EPS, ALU.mult, ALU.add)
            yield
            C.P.op("act", lambda e: e.activation(out=ap, in_=ap, func=AF.Sqrt), reads=[s8], writes=[s8])
            yield
            C.P.op("dve", lambda e: e.reciprocal(out=ap, in_=ap), reads=[s8], writes=[s8])
            yield
            n_ = qn.next()
            nv = n_.t[:, 0:w].rearrange("p (h d) -> p h d", d=64)
            tt(C, "dve", n_, nv, src_ps, src_ap.rearrange("p (h d) -> p h d", d=64),
               s8, s8.t[:, 0:nh].unsqueeze(2).to_broadcast([128, nh, 64]), ALU.mult)
            yield
            tt(C, "pool", n_, nv, n_, nv, gain, gain.t[:, 0:nh, :], ALU.mult)
            yield
            v5 = n_.t[:, 0:w].rearrange("p (h a b f) -> p h a b f", a=2, b=2, f=16)
            x1 = v5[:, :, :, 0, :]
            x2 = v5[:, :, :, 1, :]
            cb = cs.t[:, 0, :].rearrange("p (a f) -> p a f", a=2).unsqueeze(1).to_broadcast([128, nh, 2, 16])
            sb_ = cs.t[:, 1, :].rearrange("p (a f) -> p a f", a=2).unsqueeze(1).to_broadcast([128, nh, 2, 16])
            t1, t2 = rt.next(), rt.next()
            tt(C, "dve", t1, t1.t[:, 0:nh], n_, x1, cs, cb, ALU.mult)
            tt(C, "pool", t2, t2.t[:, 0:nh], n_, x2, cs, sb_, ALU.mult)
            yield
            t3, t4 = rt.next(), rt.next()
            tt(C, "dve", t3, t3.t[:, 0:nh], n_, x1, cs, sb_, ALU.mult)
            tt(C, "pool", t4, t4.t[:, 0:nh], n_, x2, cs, cb, ALU.mult)
            yield
            tt(C, "dve", dst_t, dst_ap4[:, :, :, 0, :], t1, t1.t[:, 0:nh], t2, t2.t[:, 0:nh], ALU.subtract)
            yield
            tt(C, "pool", dst_t, dst_ap4[:, :, :, 1, :], t3, t3.t[:, 0:nh], t4, t4.t[:, 0:nh], ALU.add)
            yield

        def prep_tile(g, j, hT):
            ti = g * 4 + j
            xt = xr.next()
            P.dma("sp", xt.t[:], I["x"].t[ti * 128:(ti + 1) * 128, :], reads=[I["x"]], writes=[xt])
            s_ = stat.next()
            act(C, junk, junk.t[:], xt, xt.t[:], AF.Square, accum_out=s_.t[:, 0:1], extra_w=[s_])
            yield
            ap = s_.t[:, 0:1]
            ts(C, "dve", s_, ap, s_, ap, 1.0 / D, EPS, ALU.mult, ALU.add)
            yield
            C.P.op("act", lambda e: e.activation(out=ap, in_=ap, func=AF.Sqrt), reads=[s_], writes=[s_])
            yield
            C.P.op("dve", lambda e: e.reciprocal(out=ap, in_=ap), reads=[s_], writes=[s_])
            yield
            hb = hbr.next()
            stt(C, "dve", hb, hb.t[:], xt, xt.t[:], s_.t[:, 0:1], gmix, gmix.t[:], ALU.mult, ALU.mult, reads=[s_])
            yield
            pt = ptr.next()
            for k in range(8):
                tr(C, pt, pt.t[:, k, :], hb, hb.t[:, k * 128:(k + 1) * 128], K["ident"])
            yield
            cp(C, "act", hT, hT.t[:, :, j * 128:(j + 1) * 128], pt, pt.t[:])
            yield

        ngr = ntiles // 4
        hTs = {0: hTr.next()}
        for j in range(4):
            run_rr([prep_tile(0, j, hTs[0])])
        for g in range(ngr):
            hT = hTs[g]
            if g + 1 < ngr:
                hTs[g + 1] = hTr.next()
            cols = slice(g * 512, (g + 1) * 512)
            fm = [("hq", OFF["hq"] + c * 128, c) for c in range(4)] + \
                 [("hff", OFF["hff"] + c * 128, c) for c in range(4)] + \
                 [("hfb", OFF["hfb"] + c * 128, c) for c in range(4)] + \
                 [("gate", OFF["gate"] + c * 128, c) for c in range(16)]
            for kind, c0, c in fm:
                pf = pfr.next()
                for k in range(8):
                    mm(C, pf, pf.t[:], w_in, w_in.t[:, k, c0:c0 + 128], hT, hT.t[:, k, :], k == 0, k == 7)
                sg = stg_bf.next()
                if kind == "hq":
                    act(C, sg, sg.t[:], pf, pf.t[:], AF.Silu)
                    dst = Sc["hqT"]
                    P.dma("pool", dst.t[c, :, cols], sg.t[:], reads=[sg], writes=[dst.reg((c, g))])
                elif kind in ("hff", "hfb"):
                    d_ = 0 if kind == "hff" else 1
                    tf = tmpf.next()
                    act(C, tf, tf.t[:], pf, pf.t[:], AF.Sigmoid, scale=-1.0)
                    ts(C, "dve", sg, sg.t[:], tf, tf.t[:], omlT.t[:, d_, c:c + 1], None, ALU.mult, reads=[omlT])
                    dst = Sc["kT"]
                    P.dma("pool", dst.t[d_, c, :, cols], sg.t[:], reads=[sg], writes=[dst.reg((d_, c, g))])
                else:
                    act(C, sg, sg.t[:], pf, pf.t[:], AF.Sigmoid)
                    dst = Sc["gtsT"]
                    P.dma("pool", dst.t[c, :, cols], sg.t[:], reads=[sg], writes=[dst.reg((c, g))])
            for j in range(4):
                ti = g * 4 + j
                rows = slice(ti * 128, (ti + 1) * 128)
                lhs = lambda k, j=j: hT.t[:, k, j * 128:(j + 1) * 128]
                cs = T(K["rope"].t[:, ti], "rope_v")
                cs.b = K["rope"].b
                q_ = qr.next()
                k_ = kk.next()

                def chain_q():
                    pq = ptk.next()
                    for k in range(8):
                        mm(C, pq, pq.t[:], hT, lhs(k), w_in, w_in.t[:, k, 0:512], k == 0, k == 7)
                    yield
                    yield from rope_norm(pq, pq.t[:], 8, qg, cs, q_, q_.t[:].rearrange("p (h a b f) -> p h a b f", a=2, b=2, f=16))

                def chain_kv():
                    pkv = ptk.next()
                    for k in range(8):
                        mm(C, pkv, pkv.t[:, 0:256], hT, lhs(k), w_in, w_in.t[:, k, 512:768], k == 0, k == 7)
                    yield
                    v_ = vst.next()
                    cp(C, "act", v_, v_.t[:, :, 0:64], pkv, pkv.t[:, 128:256].rearrange("p (h d) -> p h d", d=64))
                    P.dma("pool", Sc["v"].t[ti], v_.t[:], reads=[v_], writes=[Sc["v"].reg(ti)])
                    yield
                    kview = k_.t[:].rearrange("p (h r) d -> p h r d", r=2)
                    yield from rope_norm(pkv, pkv.t[:, 0:128], 2, kg, cs, k_,
                                         kview[:, :, 0, :].rearrange("p h (a b f) -> p h a b f", a=2, b=2, f=16))
                    cp(C, "pool", k_, kview[:, :, 1, :], k_, kview[:, :, 0, :])
                    yield

                def chain_gate(d_, key):
                    pg = ptk.next()
                    for k in range(8):
                        mm(C, pg, pg.t[:], hT, lhs(k), w_in, w_in.t[:, k, OFF[key]:OFF[key] + 512], k == 0, k == 7)
                    yield
                    tf = tmpf.next()
                    act(C, tf, tf.t[:], pg, pg.t[:], AF.Sigmoid)
                    yield
                    tt(C, "dve", tf, tf.t[:], tf, tf.t[:], omlb, omlb.t[:, d_, :], ALU.mult)
                    yield
                    tt(C, "dve", tf, tf.t[:], tf, tf.t[:], lbb, lbb.t[:, d_, :], ALU.add)
                    yield
                    gf = stg_f.next()
                    act(C, gf, gf.t[:], tf, tf.t[:], AF.Ln)
                    P.dma("pool", Sc["g"].t[d_, rows, :], gf.t[:], reads=[gf], writes=[Sc["g"].reg((d_, ti))])
                    kb = stg_bf.next()
                    ts(C, "pool", kb, kb.t[:], tf, tf.t[:], -1.0, 1.0, ALU.mult, ALU.add)
                    P.dma("pool", Sc["k"].t[d_, rows, :], kb.t[:], reads=[kb], writes=[Sc["k"].reg((d_, ti))])
                    yield

                def chain_h(key, dstn, fn):
                    ph = ptk.next()
                    for k in range(8):
                        mm(C, ph, ph.t[:], hT, lhs(k), w_in, w_in.t[:, k, OFF[key]:OFF[key] + 512], k == 0, k == 7)
                    yield
                    sb_ = stg_bf.next()
                    if fn is None:
                        cp(C, "act", sb_, sb_.t[:], ph, ph.t[:])
                    else:
                        act(C, sb_, sb_.t[:], ph, ph.t[:], fn)
                    P.dma("pool", Sc[dstn].t[rows, :], sb_.t[:], reads=[sb_], writes=[Sc[dstn].reg(ti)])
                    yield

                chains = [chain_q(), chain_kv(), chain_gate(0, "hff")]
                if g + 1 < ngr:
                    chains.append(prep_tile(g + 1, j, hTs[g + 1]))
                run_rr(chains)
                run_rr([chain_gate(1, "hfb"), chain_h("hi", "hi", None), chain_h("hg", "sg", AF.Silu)])
                for pr in range(4):
                    tr(C, pqt, pqt.t[:, pr, :], q_, q_.t[:, pr * 128:(pr + 1) * 128], K["ident"])
                kflat = k_.t[:].rearrange("p a d -> p (a d)")
                for kv in range(2):
                    tr(C, pqt, pqt.t[:, 4 + kv, :], k_, kflat[:, kv * 128:(kv + 1) * 128], K["ident"])
                qs = qTs.next()
                cp(C, "act", qs, qs.t[:], pqt, pqt.t[:, 0:6, :])
                P.dma("pool", Sc["qT"].t[:, :, rows].rearrange("r p t -> p r t"), qs.t[:, 0:4, :], reads=[qs], writes=[Sc["qT"].reg(ti)])
                P.dma("pool", Sc["kTa"].t[:, :, rows].rearrange("r p t -> p r t"), qs.t[:, 4:6, :], reads=[qs], writes=[Sc["kTa"].reg(ti)])


def phase_b(C, K, ngroups=8, hhs=(0, 1), bg=()):
    nc, P = C.nc, C.P
    Sc = C.scr
    with contextlib.ExitStack() as st:
        kT = [C.alloc(st, [128, S], BF16, "kTsb") for _ in range(2)]
        for kv in range(2):
            for hf in range(2):
                cs_ = slice(hf * 2048, (hf + 1) * 2048)
                P.dma("sp", kT[kv].t[:, cs_], Sc["kTa"].t[kv, :, cs_],
                      reads=Sc["kTa"].regl(range(hf * 16, hf * 16 + 16)), writes=[kT[kv]])
        vs = C.alloc(st, [128, NT, 256], BF16, "vsb")
        for hf in range(4):
            P.dma("sp", vs.t[:, hf * 8:(hf + 1) * 8, :], Sc["v"].t[hf * 8:(hf + 1) * 8].rearrange("t p h c -> p t (h c)"),
                  reads=Sc["v"].regl(range(hf * 8, hf * 8 + 8)), writes=[vs])
        if "dbgvs" in Sc:
            P.dma("pool", Sc["dbgvs"].t[:], vs.t[:], reads=[vs], writes=[Sc["dbgvs"]])
        qr_ = C.ring(st, 2, [128, 512], BF16, "qTg")
        psS = C.ring(st, 4, [128, 512], F32, "psS", psum=True)
        acc = [C.alloc(st, [128, 512], F32, "acc", psum=True) for _ in range(2)]
        ptr_ = C.ring(st, 8, [128, 512], BF16, "pT")
        rl = C.ring(st, 2, [128, 512], F32, "rl")
        obr = C.ring(st, 2, [128, 512], BF16, "ob")
        LAG = 3
        steps = [(g, pr, kt, hh) for g in range(ngroups) for pr in range(4) for kt in range(NT) for hh in hhs]
        state = {}
        accs = [acc, [C.alloc(st, [128, 512], F32, "acc2", psum=True) for _ in range(2)]]

        def stage1(g, pr, kt, hh):
            kv = pr // 2
            if kt == 0 and hh == hhs[0]:
                q = qr_.next()
                P.dma("sp", q.t[:], Sc["qT"].t[pr, :, g * 512:(g + 1) * 512], reads=Sc["qT"].regl(range(4 * g, 4 * g + 4)), writes=[q])
                state["q", g, pr] = q
            q = state["q", g, pr]
            rows = slice(hh * 64, (hh + 1) * 64)
            s_ = psS.next()
            mm(C, s_, s_.t[:], kT[kv], kT[kv].t[rows, kt * 128:(kt + 1) * 128], q, q.t[rows, :], True, True)
            p_ = ptr_.next()
            act(C, p_, p_.t[:], s_, s_.t[:], AF.Exp, scale=0.125)
            state["p", g, pr, kt, hh] = p_

        def stage2(g, pr, kt, hh):
            kv = pr // 2
            p_ = state.pop(("p", g, pr, kt, hh))
            ac = accs[(g * 4 + pr) % 2]
            mm(C, ac[hh], ac[hh].t[:], vs, vs.t[:, kt, kv * 128:(kv + 1) * 128], p_, p_.t[:], kt == 0, kt == NT - 1)
            if kt == NT - 1 and hh == hhs[-1]:
                ob = obr.next()
                for h2_ in hhs:
                    r_ = rl.next()
                    C.P.op("dve", lambda e, r_=r_, h2_=h2_, ac=ac: e.reciprocal(out=r_.t[64:128, :], in_=ac[h2_].t[64:128, :]),
                           reads=[ac[h2_]], writes=[r_])
                    tt(C, "dve", ob, ob.t[h2_ * 64:(h2_ + 1) * 64, :], ac[h2_], ac[h2_].t[0:64, :], r_, r_.t[64:128, :], ALU.mult)
                P.dma("pool", Sc["attoT"].t[pr, :, g * 512:(g + 1) * 512], ob.t[:], reads=[ob], writes=[Sc["attoT"].reg((pr, g))])

        bg = list(bg)
        LAG = 4
        for it in range(0, len(steps) + LAG, 2):
            for i_ in (it, it + 1):
                if i_ < len(steps):
                    stage1(*steps[i_])
            for i_ in (it - LAG, it - LAG + 1):
                if 0 <= i_ < len(steps):
                    stage2(*steps[i_])
            if bg and (it // 2) % 3 == 2:
                bg.pop(0)()
        for job in bg:
            job()


def build_masks(C, st):
    P = C.P
    M = {}
    specs = {
        "f_incl": (ALU.is_ge, 0, 1, -1),
        "f_excl": (ALU.is_gt, 0, -1, 1),
        "b_incl": (ALU.is_ge, 0, -1, 1),
        "b_excl": (ALU.is_gt, 0, 1, -1),
    }
    for name, (op, base, tmul, pmul) in specs.items():
        m = C.alloc(st, [128, 128], F32, "m_" + name)
        P.op("pool", lambda e, m=m: e.memset(m.t[:], 1.0), writes=[m])
        P.op("pool", lambda e, m=m, op=op, base=base, tmul=tmul, pmul=pmul: e.affine_select(
            out=m.t[:], in_=m.t[:], pattern=[[tmul, 128]], compare_op=op, fill=0.0, base=base, channel_multiplier=pmul),
            reads=[m], writes=[m])
        P.op("pool", lambda e, m=m: e.memset(m.t[0:64, 64:128], 0.0), reads=[m], writes=[m])
        P.op("pool", lambda e, m=m: e.memset(m.t[64:128, 0:64], 0.0), reads=[m], writes=[m])
        M[name] = m
    return M


def phase_c(C, K, ntiles=NT, dirs=(0, 1)):
    nc, P = C.nc, C.P
    Sc = C.scr
    I = C.ins
    with contextlib.ExitStack() as st:
        M = build_masks(C, st)
        gon = C.alloc(st, [128, 4, 128], F32, "gon")
        P.dma("sp", gon.t[:], I["hg_out_norm"].t.unsqueeze(1).to_broadcast([128, 4, 128]), writes=[gon])
        gr = C.ring(st, 2, [128, 512], F32, "g_t")
        kdr = C.ring(st, 2, [128, 512], BF16, "kd_t")
        kTr = C.ring(st, 2, [128, 4, 128], BF16, "kT_t")
        qTr = C.ring(st, 2, [128, 4, 128], BF16, "hqT_t")
        vr = C.ring(st, 2, [128, 512], BF16, "v_t")
        ofr = C.ring(st, 2, [128, 512], F32, "of_t")
        sgr = C.ring(st, 2, [128, 512], BF16, "sg_t")
        prx = C.alloc(st, [128, 512], F32, "prx", psum=True)
        pbT = C.alloc(st, [128, 4, 128], F32, "pbT", psum=True)
        pX = [C.alloc(st, [128, 4, 128], F32, "pX", psum=True) for _ in range(2)]
        pOs = [C.alloc(st, [128, 4, 128], F32, "pOs", psum=True) for _ in range(2)]
        pTr = C.alloc(st, [128, 8, 128], BF16, "pTr", psum=True)
        ebT = C.ring(st, 2, [128, 4, 128], F32, "ebT")
        enbT = C.ring(st, 2, [128, 4, 128], F32, "enbT")
        er = C.ring(st, 2, [128, 512], F32, "er")
        qfull = C.ring(st, 2, [128, 4, 128], BF16, "qfull")
        qlo = C.ring(st, 2, [128, 4, 128], BF16, "qlo")
        qhi = C.ring(st, 2, [128, 4, 128], BF16, "qhi")
        for t_ in qlo.items + qhi.items:
            P.op("pool", lambda e, t_=t_: e.memset(t_.t[:], 0.0), writes=[t_])
        ktil = C.ring(st, 2, [128, 4, 128], BF16, "ktil")
        kdec = C.ring(st, 2, [128, 512], BF16, "kdec")
        atm = C.ring(st, 4, [128, 128], BF16, "atm")
        S32 = [C.alloc(st, [128, 128], F32, "S32") for _ in range(4)]
        Sbf = [C.alloc(st, [128, 128], BF16, "Sbf") for _ in range(4)]
        osb = C.ring(st, 2, [128, 512], F32, "osb")
        tot = C.ring(st, 2, [128, 512], F32, "tot")
        sqt = C.ring(st, 2, [128, 512], F32, "sqt")
        stat = C.ring(st, 2, [128, 8], F32, "statc")
        onb = C.ring(st, 2, [128, 512], BF16, "onb")
        oTs = C.ring(st, 2, [128, 4, 128], BF16, "oTs")

        for d_ in dirs:
            Mi = M["f_incl"] if d_ == 0 else M["b_incl"]
            Me = M["f_excl"] if d_ == 0 else M["b_excl"]
            for hd in range(4):
                P.op("pool", lambda e, hd=hd: e.memset(S32[hd].t[:], 0.0), writes=[S32[hd]])
                P.op("pool", lambda e, hd=hd: e.memset(Sbf[hd].t[:], 0.0), writes=[Sbf[hd]])
            order = list(range(ntiles)) if d_ == 0 else list(range(ntiles - 1, -1, -1))
            def pro(ti):
                rows = slice(ti * 128, (ti + 1) * 128)
                g_t, kd_t, kT_t, q_t, v_t = gr.next(), kdr.next(), kTr.next(), qTr.next(), vr.next()
                P.dma("sp", g_t.t[:], Sc["g"].t[d_, rows, :], reads=[Sc["g"].reg((d_, ti))], writes=[g_t])
                P.dma("sp", kd_t.t[:], Sc["k"].t[d_, rows, :], reads=[Sc["k"].reg((d_, ti))], writes=[kd_t])
                P.dma("sp", kT_t.t[:], Sc["kT"].t[d_, :, :, rows].rearrange("h p t -> p h t"),
                      reads=[Sc["kT"].reg((d_, c, ti // 4)) for c in range(4)], writes=[kT_t])
                P.dma("sp", q_t.t[:], Sc["hqT"].t[:, :, rows].rearrange("h p t -> p h t"),
                      reads=[Sc["hqT"].reg((c, ti // 4)) for c in range(4)], writes=[q_t])
                P.dma("sp", v_t.t[:], Sc["hi"].t[rows, :], reads=[Sc["hi"].reg(ti)], writes=[v_t])
                mm(C, prx, prx.t[:], Me, Me.t[:], g_t, g_t.t[:], True, True)
                for hd in range(4):
                    mm(C, pbT, pbT.t[:, hd, :], g_t, g_t.t[:, hd * 128:(hd + 1) * 128], Mi, Mi.t[:], True, True)
                eb, enb, er_ = ebT.next(), enbT.next(), er.next()
                act(C, eb, eb.t[:], pbT, pbT.t[:], AF.Exp)
                act(C, enb, enb.t[:], pbT, pbT.t[:], AF.Exp, scale=-1.0)
                act(C, er_, er_.t[:], prx, prx.t[:], AF.Exp)
                qf, ql, qh, kt_, kdc = qfull.next(), qlo.next(), qhi.next(), ktil.next(), kdec.next()
                tt(C, "dve", qf, qf.t[:], q_t, q_t.t[:], eb, eb.t[:], ALU.mult)
                cp(C, "pool", ql, ql.t[:, :, 0:64], qf, qf.t[:, :, 0:64])
                cp(C, "pool", qh, qh.t[:, :, 64:128], qf, qf.t[:, :, 64:128])
                tt(C, "dve", kt_, kt_.t[:], kT_t, kT_t.t[:], enb, enb.t[:], ALU.mult)
                tt(C, "pool", kdc, kdc.t[:], kd_t, kd_t.t[:], er_, er_.t[:], ALU.mult)
                return dict(kd_t=kd_t, v_t=v_t, eb=eb, qf=qf, ql=ql, qh=qh, kt_=kt_, kdc=kdc)

            def tile_body(ti, B_):
                rows = slice(ti * 128, (ti + 1) * 128)
                kd_t, v_t, eb, qf, ql, qh, kt_, kdc = (B_[k_] for k_ in ('kd_t', 'v_t', 'eb', 'qf', 'ql', 'qh', 'kt_', 'kdc'))
                if d_ == 0:
                    ca, cb, qa, qb, la, lb_ = 0, 1, ql, qh, 63, 127
                else:
                    ca, cb, qa, qb, la, lb_ = 1, 0, qh, ql, 64, 0
                ra = slice(ca * 64, (ca + 1) * 64)
                rb = slice(cb * 64, (cb + 1) * 64)
                def head_chain(hd):
                    hc = slice(hd * 128, (hd + 1) * 128)
                    X, O_ = pX[hd % 2], pOs[hd % 2]
                    oa = O_.t[:, hd // 2, :]
                    mm(C, X, X.t[:, 0, :], kt_, kt_.t[:, hd, :], qf, qf.t[:, hd, :], True, True)
                    mm(C, O_, oa, qa, qa.t[:, hd, :], Sbf[hd], Sbf[hd].t[:], True, False)
                    mm(C, X, X.t[:, 1, :], kdc, kdc.t[ra, hc], v_t, v_t.t[ra, hc], True, True)
                    yield
                    am = atm.next()
                    tt(C, "dve", am, am.t[:], X, X.t[:, 0, :], Mi, Mi.t[:], ALU.mult)
                    stt(C, "dve", S32[hd], S32[hd].t[:], S32[hd], S32[hd].t[:], eb.t[:, hd, la:la + 1],
                        X, X.t[:, 1, :], ALU.mult, ALU.add, reads=[eb])
                    yield
                    cp(C, "act", Sbf[hd], Sbf[hd].t[:], S32[hd], S32[hd].t[:])
                    yield
                    mm(C, O_, oa, qb, qb.t[:, hd, :], Sbf[hd], Sbf[hd].t[:], False, False)
                    mm(C, O_, oa, am, am.t[:], v_t, v_t.t[:, hc], False, True)
                    mm(C, X, X.t[:, 2, :], kdc, kdc.t[rb, hc], v_t, v_t.t[rb, hc], True, True)
                    yield
                    stt(C, "dve", S32[hd], S32[hd].t[:], S32[hd], S32[hd].t[:], eb.t[:, hd, lb_:lb_ + 1],
                        X, X.t[:, 2, :], ALU.mult, ALU.add, reads=[eb])
                    yield
                    cp(C, "act", Sbf[hd], Sbf[hd].t[:], S32[hd], S32[hd].t[:])
                    yield

                for pair in ((0, 1), (2, 3)):
                    gens = [head_chain(hd) for hd in pair]
                    while gens:
                        for g_ in list(gens):
                            try:
                                next(g_)
                            except StopIteration:
                                gens.remove(g_)

                def ov(tile_ap, s_):
                    return tile_ap.rearrange("p (a s d) -> p a s d", s=2, d=128)[:, :, s_, :]

                if d_ == 0 and len(dirs) == 2:
                    o_ = osb.next()
                    for s_ in range(2):
                        cp(C, "act", o_, ov(o_.t[:], s_), pOs[s_], pOs[s_].t[:, 0:2, :])
                    P.dma("pool", Sc["ofwd"].t[rows, :], o_.t[:], reads=[o_], writes=[Sc["ofwd"].reg(ti)])
                    return
                t_ = tot.next()
                if len(dirs) == 2:
                    of_ = ofr.next()
                    P.dma("sp", of_.t[:], Sc["ofwd"].t[rows, :], reads=[Sc["ofwd"].reg(ti)], writes=[of_])
                    for s_ in range(2):
                        tt(C, "dve", t_, ov(t_.t[:], s_), pOs[s_], pOs[s_].t[:, 0:2, :], of_, ov(of_.t[:], s_), ALU.add)
                else:
                    for s_ in range(2):
                        cp(C, "dve", t_, ov(t_.t[:], s_), pOs[s_], pOs[s_].t[:, 0:2, :])
                if "dbgo" in Sc:
                    P.dma("pool", Sc["dbgo"].t[rows, :], t_.t[:], reads=[t_], writes=[Sc["dbgo"].reg(ti)])
                sg_ = sgr.next()
                P.dma("sp", sg_.t[:], Sc["sg"].t[rows, :], reads=[Sc["sg"].reg(ti)], writes=[sg_])
                sq = sqt.next()
                act(C, sq, sq.t[:], t_, t_.t[:], AF.Square)
                s4 = stat.next()
                P.op("dve", lambda e, s4=s4, sq=sq: e.tensor_reduce(out=s4.t[:, 0:4], in_=sq.t[:].rearrange("p (h d) -> p h d", d=128),
                                                              axis=AX.X, op=ALU.add), reads=[sq], writes=[s4])
                rsqrt_mean(C, s4, lambda s4=s4: s4.t[:, 0:4], 4, 1.0 / 128)
                t3 = t_.t[:].rearrange("p (h d) -> p h d", d=128)
                tt(C, "dve", t_, t3, t_, t3, s4, s4.t[:, 0:4].unsqueeze(2).to_broadcast([128, 4, 128]), ALU.mult)
                tt(C, "pool", t_, t3, t_, t3, gon, gon.t[:], ALU.mult)
                ob = onb.next()
                tt(C, "dve", ob, ob.t[:], t_, t_.t[:], sg_, sg_.t[:], ALU.mult)
                for hd in range(4):
                    tr(C, pTr, pTr.t[:, hd, :], ob, ob.t[:, hd * 128:(hd + 1) * 128], K["ident"])
                os_ = oTs.next()
                cp(C, "act", os_, os_.t[:], pTr, pTr.t[:, 0:4, :])
                P.dma("pool", Sc["hgoT"].t[:, :, rows].rearrange("h p t -> p h t"), os_.t[:], reads=[os_], writes=[Sc["hgoT"].reg(ti)])

            pend = pro(order[0])
            for idx_, ti in enumerate(order):
                nxt = pro(order[idx_ + 1]) if idx_ + 1 < len(order) else None
                tile_body(ti, pend)
                pend = nxt


def load_w(C, st, name, kchunks, ncols, q="pool"):
    w = C.alloc(st, [128, kchunks, ncols], BF16, name)
    src = C.ins[name].t.rearrange("(k p) n -> p k n", p=128)
    step = min(kchunks, max(1, 4096 // ncols))
    for k0 in range(0, kchunks, step):
        C.P.dma(q, w.t[:, k0:k0 + step, :], src[:, k0:k0 + step, :], reads=[C.ins[name]], writes=[w])
    return w


def norm_transpose(C, K, xt, gain, stat, junk, hb, pt, dst, dst_ap):
    s_ = stat
    act(C, junk, junk.t[:], xt, xt.t[:], AF.Square, accum_out=s_.t[:, 0:1], extra_w=[s_])
    rsqrt_mean(C, s_, lambda: s_.t[:, 0:1], 1, 1.0 / D)
    stt(C, "dve", hb, hb.t[:], xt, xt.t[:], s_.t[:, 0:1], gain, gain.t[:], ALU.mult, ALU.mult, reads=[s_])
    for k in range(8):
        tr(C, pt, pt.t[:, k, :], hb, hb.t[:, k * 128:(k + 1) * 128], K["ident"])
    cp(C, "act", dst, dst_ap, pt, pt.t[:])


def phase_d(C, K, ngroups=8):
    nc, P = C.nc, C.P
    Sc = C.scr
    I = C.ins
    with contextlib.ExitStack() as st:
        wua = load_w(C, st, "w_up_att", 4, 1024)
        wuh = load_w(C, st, "w_up_hg", 4, 1024)
        wo = load_w(C, st, "w_out", 8, 1024)
        gffn = C.alloc(st, [128, 1024], F32, "gffn")
        P.dma("sp", gffn.t[:], I["norm_ffn"].t.to_broadcast([128, 1024]), writes=[gffn])
        aTr = C.ring(st, 2, [128, 4, 512], BF16, "aT")
        hTr_ = C.ring(st, 2, [128, 4, 512], BF16, "hgT")
        gtr = C.ring(st, 2, [128, 16, 512], BF16, "gts")
        pya = C.ring(st, 2, [128, 512], F32, "pya", psum=True)
        pyh = C.ring(st, 2, [128, 512], F32, "pyh", psum=True)
        px = C.ring(st, 2, [128, 512], F32, "px", psum=True)
        pt = C.ring(st, 2, [128, 8, 128], BF16, "ptd", psum=True)
        t1r = C.ring(st, 2, [128, 512], F32, "t1")
        t2r = C.ring(st, 2, [128, 512], F32, "t2")
        mTr = C.ring(st, 2, [128, 8, 512], BF16, "mT")
        xr = C.ring(st, 2, [128, 1024], F32, "xtd")
        x1r = C.ring(st, 3, [128, 1024], F32, "x1t")
        junk = C.alloc(st, [128, 1024], BF16, "junkd")
        stat = C.ring(st, 2, [128, 8], F32, "statd")
        hbr = C.ring(st, 2, [128, 1024], BF16, "hbd")
        h2s = C.ring(st, 2, [128, 8, 128], BF16, "h2s")
        for g in range(ngroups):
            cols = slice(g * 512, (g + 1) * 512)
            aT, hT, gt = aTr.next(), hTr_.next(), gtr.next()
            P.dma("sp", aT.t[:], Sc["attoT"].t[:, :, cols].rearrange("r p t -> p r t"),
                  reads=[Sc["attoT"].reg((pr, g)) for pr in range(4)], writes=[aT])
            P.dma("sp", hT.t[:], Sc["hgoT"].t[:, :, cols].rearrange("r p t -> p r t"),
                  reads=Sc["hgoT"].regl(range(4 * g, 4 * g + 4)), writes=[hT])
            P.dma("sp", gt.t[:], Sc["gtsT"].t[:, :, cols].rearrange("r p t -> p r t"),
                  reads=[Sc["gtsT"].reg((c, g)) for c in range(16)], writes=[gt])
            mT = mTr.next()
            for m_ in range(8):
                ms = slice(m_ * 128, (m_ + 1) * 128)
                ya, yh = pya.next(), pyh.next()
                for kc in range(4):
                    mm(C, ya, ya.t[:], wua, wua.t[:, kc, ms], aT, aT.t[:, kc, :], kc == 0, kc == 3)
                for kc in range(4):
                    mm(C, yh, yh.t[:], wuh, wuh.t[:, kc, ms], hT, hT.t[:, kc, :], kc == 0, kc == 3)
                t1, t2 = t1r.next(), t2r.next()
                tt(C, "dve", t1, t1.t[:], ya, ya.t[:], gt, gt.t[:, m_, :], ALU.mult)
                tt(C, "dve", t2, t2.t[:], yh, yh.t[:], gt, gt.t[:, 8 + m_, :], ALU.mult)
                tt(C, "pool", mT, mT.t[:, m_, :], t1, t1.t[:], t2, t2.t[:], ALU.add)
            def part1(j):
                ti = g * 4 + j
                rows = slice(ti * 128, (ti + 1) * 128)
                xt = xr.next()
                P.dma("sp", xt.t[:], I["x"].t[rows, :], reads=[I["x"]], writes=[xt])
                x1 = x1r.next()
                for hf in range(2):
                    hs = slice(hf * 512, (hf + 1) * 512)
                    p_ = px.next()
                    for m_ in range(8):
                        mm(C, p_, p_.t[:], mT, mT.t[:, m_, j * 128:(j + 1) * 128], wo, wo.t[:, m_, hs], m_ == 0, m_ == 7)
                    tt(C, "dve", x1, x1.t[:, hs], p_, p_.t[:], xt, xt.t[:, hs], ALU.add)
                P.dma("pool", Sc["x1"].t[rows, :], x1.t[:], reads=[x1], writes=[Sc["x1"].reg(ti)])
                return x1

            def part2(j, x1):
                ti = g * 4 + j
                hs_ = h2s.next()
                norm_transpose(C, K, x1, gffn, stat.next(), junk, hbr.next(), pt.next(), hs_, hs_.t[:])
                P.dma("pool", Sc["h2T"].t[ti], hs_.t[:], reads=[hs_], writes=[Sc["h2T"].reg(ti)])

            pend = part1(0)
            for j in range(4):
                nxt = part1(j + 1) if j + 1 < 4 else None
                part2(j, pend)
                pend = nxt


def phase_e1(C, K, ngroups=16):
    nc, P = C.nc, C.P
    Sc = C.scr
    I = C.ins
    with contextlib.ExitStack() as st:
        wq = load_w(C, st, "peer_wq", 8, 2048)
        skT = C.alloc(st, [128, 16, 128], BF16, "skT")
        P.dma("pool", skT.t[:], I["skT"].t, reads=[I["skT"]], writes=[skT])
        io_f = C.alloc(st, [128, 128], F32, "io_f")
        P.op("pool", lambda e: e.iota(io_f.t[:], pattern=[[1, 128]], base=0, channel_multiplier=0,
                                      allow_small_or_imprecise_dtypes=True), writes=[io_f])
        io_b = C.alloc(st, [128, 128], BF16, "io_b")
        cp(C, "dve", io_b, io_b.t[:], io_f, io_f.t[:])
        io_rep = C.alloc(st, [128, 128, 16], BF16, "io_rep")
        cp(C, "dve", io_rep, io_rep.t[:], io_f, io_f.t[:].unsqueeze(2).to_broadcast([128, 128, 16]))
        h2r = C.ring(st, 2, [128, 8, 128], BF16, "h2e")
        pq = C.ring(st, 2, [128, 4, 128], F32, "pq", psum=True)
        psc = C.ring(st, 2, [128, 4, 128], F32, "psc", psum=True)
        pIG = C.alloc(st, [128, 8, 128], BF16, "pIG", psum=True)
        pG = C.ring(st, 3, [128, 4, 128], F32, "pG", psum=True)
        qpT = C.ring(st, 2, [128, 16, 128], BF16, "qpT")
        s_all = C.ring(st, 2, [128, 16, 128], F32, "s_all")
        tmp128 = C.ring(st, 6, [128, 128], F32, "tmp128")
        v16 = C.ring(st, 2, [128, 16, 16], F32, "v16")
        i16 = C.ring(st, 2, [128, 16, 16], U32, "i16")
        i16f = C.ring(st, 2, [128, 16, 16], F32, "i16f")
        cand = C.ring(st, 1, [128, 8, 256], F32, "cand")
        tmp256 = C.ring(st, 4, [128, 256], F32, "tmp256")
        tsv = C.ring(st, 2, [128, 8, 16], F32, "tsv")
        pos = C.ring(st, 2, [128, 8, 16], U32, "pos")
        k12i = C.ring(st, 2, [128, 2, 128], I32, "k12i")
        k12f = C.ring(st, 2, [128, 2, 128], F32, "k12f")
        eq = C.ring(st, 2, [128, 128, 16], F32, "eq")
        IG = C.ring(st, 2, [128, 3, 128], BF16, "IG")
        IGf = C.ring(st, 2, [128, 3, 128], F32, "IGf")
        IGT = C.ring(st, 2, [128, 3, 128], BF16, "IGT")
        ex = C.ring(st, 2, [128, 8, 16], F32, "ex")
        st8 = C.ring(st, 2, [128, 8], F32, "st8")
        A4 = C.ring(st, 3, [128, 16, 128], BF16, "A4")
        B4 = C.ring(st, 3, [128, 16, 128], BF16, "B4")
        Gst = C.ring(st, 1, [128, 128, 256], BF16, "Gst")
        est = {}

        def stageXc(grp, j2):
            ti = grp * 2 + j2
            h2 = h2r.next()
            P.dma("sp", h2.t[:], Sc["h2T"].t[ti], reads=[Sc["h2T"].reg(ti)], writes=[h2])
            qp, sa = qpT.next(), s_all.next()
            for c4 in range(4):
                p_ = pq.next()
                for cc in range(4):
                    cq = c4 * 4 + cc
                    for k in range(8):
                        mm(C, p_, p_.t[:, cc, :], wq, wq.t[:, k, cq * 128:(cq + 1) * 128], h2, h2.t[:, k, :], k == 0, k == 7)
                cp(C, "act", qp, qp.t[:, c4 * 4:(c4 + 1) * 4, :], p_, p_.t[:])
            for c4 in range(4):
                p_ = psc.next()
                for cc in range(4):
                    cq = c4 * 4 + cc
                    mm(C, p_, p_.t[:, cc, :], qp, qp.t[:, cq, :], skT, skT.t[:, cq, :], True, True)
                cp(C, "act", sa, sa.t[:, c4 * 4:(c4 + 1) * 4, :], p_, p_.t[:])
            if "dbgs" in Sc:
                P.dma("pool", Sc["dbgs"].t[ti], sa.t[:], reads=[sa], writes=[Sc["dbgs"].reg(ti)])
            est["sa", grp, j2] = sa

        def stageXt(grp, j2):
            ti = grp * 2 + j2
            sa = est.pop(("sa", grp, j2))
            v_, i_ = v16.next(), i16.next()

            def top16(src_t, src_ap, vdst_t, vdst_ap, idst_t, idst_ap, tmp):
                P.op("dve", lambda e: e.max(out=vdst_ap[:, 0:8], in_=src_ap), reads=[src_t], writes=[vdst_t])
                yield
                P.op("dve", lambda e: e.match_replace(out=tmp.t[:], in_to_replace=vdst_ap[:, 0:8], in_values=src_ap,
                                                      imm_value=-1e30), reads=[src_t, vdst_t], writes=[tmp])
                yield
                P.op("dve", lambda e: e.max(out=vdst_ap[:, 8:16], in_=tmp.t[:]), reads=[tmp, vdst_t], writes=[vdst_t])
                yield
                P.op("dve", lambda e: e.max_index(out=idst_ap[:, 0:8], in_max=vdst_ap[:, 0:8], in_values=src_ap),
                     reads=[src_t, vdst_t], writes=[idst_t])
                yield
                P.op("dve", lambda e: e.max_index(out=idst_ap[:, 8:16], in_max=vdst_ap[:, 8:16], in_values=src_ap),
                     reads=[src_t, vdst_t, idst_t], writes=[idst_t])
                yield

            def rr4(gens):
                gens = list(gens)
                while gens:
                    for g_ in list(gens):
                        try:
                            next(g_)
                        except StopIteration:
                            gens.remove(g_)

            for c0, c1 in ((0, 6), (6, 12), (12, 16)):
                rr4([top16(sa, sa.t[:, cq, :], v_.reg(cq), v_.t[:, cq, :], i_.reg(cq), i_.t[:, cq, :], tmp128.next())
                     for cq in range(c0, c1)])
            if_ = i16f.next()
            cp(C, "dve", if_, if_.t[:], i_.regl(range(16)), i_.t[:])
            cd = cand.next()
            vv = v_.t[:].rearrange("p (h a) k -> p h a k", a=2)
            tt(C, "dve", cd, cd.t[:].rearrange("p h (a b) -> p h a b", b=16),
               v_.regl(range(16)), vv[:, :, 0, :].unsqueeze(3).to_broadcast([128, 8, 16, 16]),
               v_.regl(range(16)), vv[:, :, 1, :].unsqueeze(2).to_broadcast([128, 8, 16, 16]), ALU.add)
            ts_, ps_ = tsv.next(), pos.next()
            for h0 in range(0, 8, 4):
                rr4([top16(cd, cd.t[:, h, :], ts_.reg(h), ts_.t[:, h, :], ps_.reg(h), ps_.t[:, h, :], tmp256.next())
                     for h in range(h0, h0 + 4)])
            ki, kf = k12i.next(), k12f.next()
            posf = ps_.t[:].rearrange("p h k -> p (h k)").bitcast(I32)
            P.op("dve", lambda e, ki=ki, posf=posf: e.tensor_single_scalar(out=ki.t[:, 0, :], in_=posf, scalar=4, op=ALU.arith_shift_right),
                 reads=ps_.regl(range(8)), writes=[ki])
            P.op("dve", lambda e, ki=ki, posf=posf: e.tensor_single_scalar(out=ki.t[:, 1, :], in_=posf, scalar=15, op=ALU.bitwise_and),
                 reads=ps_.regl(range(8)) + [ki], writes=[ki])
            cp(C, "dve", kf, kf.t[:], ki, ki.t[:])
            ig = IGf.next()
            iv = if_.t[:].rearrange("p (h a) k -> p h a k", a=2)
            for a in range(2):
                e_ = eq.next()
                tt(C, "dve", e_, e_.t[:], kf, kf.t[:, a, :].unsqueeze(2).to_broadcast([128, 128, 16]),
                   io_f, io_f.t[:, 0:16].unsqueeze(1).to_broadcast([128, 128, 16]), ALU.is_equal)
                e4 = e_.t[:].rearrange("p (h k) c -> p h k c", h=8)
                tt(C, "dve", e_, e4, e_, e4, if_, iv[:, :, a, :].unsqueeze(2).to_broadcast([128, 8, 16, 16]), ALU.mult)
                P.op("dve", lambda e, e_=e_, ig=ig, a=a: e.tensor_reduce(out=ig.t[:, a, :], in_=e_.t[:], axis=AX.X, op=ALU.add),
                     reads=[e_], writes=[ig])
            x_ = ex.next()
            tt(C, "dve", x_, x_.t[:], ts_.regl(range(8)), ts_.t[:], ts_.regl(range(8)), ts_.t[:, :, 0:1].to_broadcast([128, 8, 16]), ALU.subtract)
            act(C, x_, x_.t[:], x_, x_.t[:], AF.Exp)
            s8 = st8.next()
            P.op("dve", lambda e, s8=s8, x_=x_: e.tensor_reduce(out=s8.t[:], in_=x_.t[:], axis=AX.X, op=ALU.add), reads=[x_], writes=[s8])
            P.op("dve", lambda e, s8=s8: e.reciprocal(out=s8.t[:], in_=s8.t[:]), reads=[s8], writes=[s8])
            tt(C, "dve", ig, ig.t[:, 2, :].rearrange("p (h k) -> p h k", h=8), x_, x_.t[:],
               s8, s8.t[:].unsqueeze(2).to_broadcast([128, 8, 16]), ALU.mult)
            igf_ = ig
            ig = IG.next()
            cp(C, "dve", ig, ig.t[:], igf_, igf_.t[:])
            if "dbgig" in Sc:
                P.dma("pool", Sc["dbgig"].t[ti], ig.t[:], reads=[ig], writes=[Sc["dbgig"].reg(ti)])
            for a in range(3):
                tr(C, pIG, pIG.t[:, a, :], ig, ig.t[:, a, :], K["ident"])
            igt = IGT.next()
            cp(C, "dve", igt, igt.t[:], pIG, pIG.t[:, 0:3, :])
            est["igt", grp, j2] = igt

        def stageY(grp, j2):
            if j2 == 0:
                est["G", grp] = Gst.next()
            G_ = est["G", grp]
            igt = est.pop(("igt", grp, j2))
            TB = 16
            for b16 in range(128 // TB):
                a4, bb4 = A4.next(), B4.next()
                tsl = slice(b16 * TB, (b16 + 1) * TB)
                av = a4.t[:].rearrange("p t i -> p (t i)").rearrange("p (i t) -> p i t", t=TB)
                bv = bb4.t[:].rearrange("p t i -> p (t i)").rearrange("p (i t) -> p i t", t=TB)
                tt(C, "dve", a4, av, io_rep, io_rep.t[:], igt, igt.t[:, 0, tsl].unsqueeze(1).to_broadcast([128, 128, TB]), ALU.is_equal)
                tt(C, "dve", bb4, bv, io_rep, io_rep.t[:], igt, igt.t[:, 1, tsl].unsqueeze(1).to_broadcast([128, 128, TB]), ALU.is_equal)
                tt(C, "pool", a4, av, a4, av, igt, igt.t[:, 2, tsl].unsqueeze(1).to_broadcast([128, 128, TB]), ALU.mult)
                for q4 in range(TB // 4):
                    pg = pG.next()
                    for q_ in range(4):
                        mm(C, pg, pg.t[:, q_, :], a4, av[:, :, q4 * 4 + q_], bb4, bv[:, :, q4 * 4 + q_], True, True)
                    t0 = j2 * 128 + b16 * TB + q4 * 4
                    cp(C, "act", G_, G_.t[:, :, t0:t0 + 4].rearrange("p i t -> p t i"), pg, pg.t[:])
            if j2 == 1:
                hc = slice((grp % 2) * 256, (grp % 2 + 1) * 256)
                for i0 in range(0, 128, 32):
                    P.dma("pool", Sc["G"].t[grp // 2, :, i0:i0 + 32, hc], G_.t[:, i0:i0 + 32, :], reads=[G_], writes=[Sc["G"].reg(grp)])

        tl = [(grp, j2) for grp in range(ngroups) for j2 in range(2)]
        for it in range(len(tl) + 2):
            if it < len(tl):
                stageXc(*tl[it])
            if 1 <= it < len(tl) + 1:
                stageXt(*tl[it - 1])
            if it >= 2:
                stageY(*tl[it - 2])


def phase_e0(C, K):
    P = C.P
    jobs = []
    for i2 in range(128):
        jobs.append(lambda i2=i2: P.dma("pool", C.scr["uTb"].t[i2], C.ins["uT"].t[i2], reads=[C.ins["uT"]], writes=[C.scr["uTb"].reg(i2)]))
        jobs.append(lambda i2=i2: P.dma("pool", C.scr["vLb"].t[i2], C.ins["vL"].t[i2], reads=[C.ins["vL"]], writes=[C.scr["vLb"].reg(i2)]))
    return jobs


def phase_e2(C, K, ngroups=8, ni2=128):
    nc, P = C.nc, C.P
    Sc = C.scr
    with contextlib.ExitStack() as st:
        h2r = C.ring(st, 1, [128, 8, 512], BF16, "h2g")
        po = [C.alloc(st, [128, 512], F32, "po", psum=True) for _ in range(4)]
        par = C.ring(st, 4, [128, 512], F32, "pa", psum=True)
        uch = C.ring(st, 3, [128, 2, 8, 128], BF16, "uch")
        vch = C.ring(st, 4, [128, 2, 512], BF16, "vch")
        gch = C.ring(st, 3, [128, 2, 512], BF16, "gch")
        sqr = C.ring(st, 3, [128, 512], F32, "sqe")
        t2r = C.ring(st, 3, [128, 512], F32, "t2e")
        sgr = C.ring(st, 3, [128, 512], BF16, "sge")
        agr = C.ring(st, 3, [128, 512], BF16, "age")
        Wall = C.alloc(st, [128, ni2, 512], BF16, "Wall")
        xs = C.ring(st, 2, [128, 512], F32, "xs")
        x1h = C.ring(st, 2, [128, 512], F32, "x1h")
        LAG = 3
        state = {}

        def s1(grp, i2):
            h2 = state["h2"]
            if i2 % 2 == 0:
                u_, v_, g_ = uch.next(), vch.next(), gch.next()
                P.dma("sp", u_.t[:], Sc["uTb"].t[i2:i2 + 2].rearrange("i p k c -> p i k c"),
                      reads=Sc["uTb"].regl([i2, i2 + 1]), writes=[u_])
                P.dma("sp", v_.t[:], Sc["vLb"].t[i2:i2 + 2, :, 0:512].rearrange("i p d -> p i d"),
                      reads=Sc["vLb"].regl([i2, i2 + 1]), writes=[v_])
                P.dma("sp", g_.t[:], Sc["G"].t[grp, :, i2:i2 + 2, :], reads=Sc["G"].regl([2 * grp, 2 * grp + 1]), writes=[g_])
                state["uvg"] = (u_, v_, g_)
            u_, v_, g_ = state["uvg"]
            e_ = i2 % 2
            pa = par.next()
            for k in range(8):
                mm(C, pa, pa.t[:], u_, u_.t[:, e_, k, :], h2, h2.t[:, k, :], k == 0, k == 7)
            sq, t2, sg, ag = sqr.next(), t2r.next(), sgr.next(), agr.next()
            Wb = Wall.reg(i2)
            act(C, sq, sq.t[:], pa, pa.t[:], AF.Square, scale=0.21145921592448583)
            stt(C, "dve", t2, t2.t[:], sq, sq.t[:], 1.0, pa, pa.t[:], ALU.add, ALU.mult)
            tt(C, "dve", ag, ag.t[:], pa, pa.t[:], g_, g_.t[:, e_, :], ALU.mult)
            act(C, sg, sg.t[:], t2, t2.t[:], AF.Sigmoid, scale=1.5957691216057308)
            P.op("pool", lambda e: e.tensor_tensor(out=Wall.t[:, i2, :], in0=sg.t[:], in1=ag.t[:], op=ALU.mult),
                 reads=[sg, ag], writes=[Wb])
            state["v", i2] = (v_, e_)

        def s2(grp, i2, half):
            v_, e_ = state.pop(("v", i2)) if half == 0 else state.pop(("v2", i2))
            for j in range(4):
                P.op("pe", lambda e, j=j: e.matmul(po[j].t[:], lhsT=Wall.t[:, i2, j * 128:(j + 1) * 128], rhs=v_.t[:, e_, :],
                                                    start=(i2 == 0), stop=(i2 == ni2 - 1)),
                     reads=[Wall.reg(i2), v_], writes=[po[j]], pe_accum=True)

        def evac(grp, half):
            hs = slice(half * 512, (half + 1) * 512)
            for j in range(4):
                ti = grp * 4 + j
                rows = slice(ti * 128, (ti + 1) * 128)
                x1_ = x1h.next()
                P.dma("sp", x1_.t[:], Sc["x1"].t[rows, hs], reads=[Sc["x1"].reg(ti)], writes=[x1_])
                x_ = xs.next()
                tt(C, "dve", x_, x_.t[:], po[j], po[j].t[:], x1_, x1_.t[:], ALU.add)
                P.dma("pool", Sc["x2"].t[rows, hs], x_.t[:], reads=[x_], writes=[Sc["x2"].reg((ti, half))])

        for grp in range(ngroups):
            h2 = h2r.next()
            for j in range(4):
                ti = grp * 4 + j
                P.dma("sp", h2.t[:, :, j * 128:(j + 1) * 128], Sc["h2T"].t[ti], reads=[Sc["h2T"].reg(ti)], writes=[h2])
            state["h2"] = h2
            for it in range(ni2 + LAG):
                if it < ni2:
                    s1(grp, it)
                if it >= LAG:
                    s2(grp, it - LAG, 0)
            evac(grp, 0)
            for it in range(ni2 + LAG):
                if it < ni2:
                    if it % 2 == 0:
                        v_ = vch.next()
                        P.dma("sp", v_.t[:], Sc["vLb"].t[it:it + 2, :, 512:1024].rearrange("i p d -> p i d"),
                              reads=Sc["vLb"].regl([it, it + 1]), writes=[v_])
                        state["vp"] = v_
                    state["v2", it] = (state["vp"], it % 2)
                if it >= LAG:
                    s2(grp, it - LAG, 1)
            evac(grp, 1)


def phase_f(C, K, ntiles=NT):
    nc, P = C.nc, C.P
    Sc = C.scr
    I = C.ins
    with contextlib.ExitStack() as st:
        wg = load_w(C, st, "ple_gate", 8, 1024)
        wp = load_w(C, st, "ple_proj", 2, 1024)
        gple = C.alloc(st, [128, 1024], F32, "gple")
        P.dma("sp", gple.t[:], I["norm_ple"].t.to_broadcast([128, 1024]), writes=[gple])
        x2r = C.ring(st, 3, [128, 1024], F32, "x2f")
        pr_ = C.ring(st, 2, [128, 256], F32, "pf32")
        pbr = C.ring(st, 2, [128, 256], BF16, "pbf")
        junk = C.alloc(st, [128, 1024], BF16, "junkf")
        stat = C.ring(st, 2, [128, 8], F32, "statf")
        hbr = C.ring(st, 2, [128, 1024], BF16, "hbf")
        pt = C.ring(st, 2, [128, 8, 128], BF16, "ptf", psum=True)
        ptp = C.alloc(st, [128, 8, 128], BF16, "ptp", psum=True)
        h3r = C.ring(st, 3, [128, 8, 128], BF16, "h3T")
        pTr = C.ring(st, 3, [128, 2, 128], BF16, "pT")
        pgr = C.ring(st, 2, [128, 512], F32, "pgate", psum=True)
        ppr = C.ring(st, 2, [128, 512], F32, "pproj", psum=True)
        sgr = C.ring(st, 2, [128, 512], F32, "sgf")
        tr_ = C.ring(st, 2, [128, 512], F32, "tf")
        outr = C.ring(st, 2, [128, 1024], F32, "outf")
        def pro(ti):
            rows = slice(ti * 128, (ti + 1) * 128)
            x2 = x2r.next()
            P.dma("sp", x2.t[:], Sc["x2"].t[rows, :], reads=[Sc["x2"].reg((ti, 0)), Sc["x2"].reg((ti, 1))], writes=[x2])
            pf = pr_.next()
            P.dma("sp", pf.t[:], I["p"].t[rows, :], reads=[I["p"]], writes=[pf])
            pb = pbr.next()
            cp(C, "pool", pb, pb.t[:], pf, pf.t[:])
            for k in range(2):
                tr(C, ptp, ptp.t[:, k, :], pb, pb.t[:, k * 128:(k + 1) * 128], K["ident"])
            pT = pTr.next()
            cp(C, "act", pT, pT.t[:], ptp, ptp.t[:, 0:2, :])
            h3 = h3r.next()
            norm_transpose(C, K, x2, gple, stat.next(), junk, hbr.next(), pt.next(), h3, h3.t[:])
            return x2, pT, h3

        def body(ti, x2, pT, h3):
            rows = slice(ti * 128, (ti + 1) * 128)
            o_ = outr.next()
            for hf in range(2):
                hs = slice(hf * 512, (hf + 1) * 512)
                pg, pp = pgr.next(), ppr.next()
                for k in range(8):
                    mm(C, pg, pg.t[:], h3, h3.t[:, k, :], wg, wg.t[:, k, hs], k == 0, k == 7)
                for k in range(2):
                    mm(C, pp, pp.t[:], pT, pT.t[:, k, :], wp, wp.t[:, k, hs], k == 0, k == 1)
                sg, t_ = sgr.next(), tr_.next()
                act(C, sg, sg.t[:], pg, pg.t[:], AF.Sigmoid)
                tt(C, "dve", t_, t_.t[:], pp, pp.t[:], sg, sg.t[:], ALU.mult)
                tt(C, "pool", o_, o_.t[:, hs], t_, t_.t[:], x2, x2.t[:, hs], ALU.add)
            P.dma("sp", C.y.t[rows, :], o_.t[:], reads=[o_], writes=[C.y.reg(ti)])

        pend = pro(0)
        for ti in range(ntiles):
            nxt = pro(ti + 1) if ti + 1 < ntiles else None
            body(ti, *pend)
            pend = nxt


def declare(C):
    C.din("x", [S, D])
    C.din("p", [S, 256])
    C.din("norm_mix", [1, D])
    C.din("w_in", [D, 5376])
    C.din("q_norm", [1, 64])
    C.din("k_norm", [1, 64])
    C.din("hg_lb_raw", [2, 2, 512])
    C.din("hg_out_norm", [1, 128])
    C.din("w_up_att", [512, D])
    C.din("w_up_hg", [512, D])
    C.din("w_out", [D, D])
    C.din("norm_ffn", [1, D])
    C.din("peer_wq", [D, 2048])
    C.din("skT", [128, 16, 128])
    C.din("uT", [128, 128, 8, 128])
    C.din("vL", [128, 128, D])
    C.din("norm_ple", [1, D])
    C.din("ple_gate", [D, D])
    C.din("ple_proj", [256, D])
    sc = C.scratch
    sc("hqT", [4, 128, S], BF16)
    sc("kT", [2, 4, 128, S], BF16)
    sc("gtsT", [16, 128, S], BF16)
    sc("qT", [4, 128, S], BF16)
    sc("kTa", [2, 128, S], BF16)
    sc("v", [NT, 128, 2, 128], BF16)
    sc("g", [2, S, 512], F32)
    sc("k", [2, S, 512], BF16)
    sc("hi", [S, 512], BF16)
    sc("sg", [S, 512], BF16)
    sc("attoT", [4, 128, S], BF16)
    sc("ofwd", [S, 512], F32)
    sc("uTb", [128, 128, 8, 128], BF16)
    sc("vLb", [128, 128, D], BF16)
    sc("x2", [S, D], F32)
    sc("G", [8, 128, 128, 512], BF16)
    if "dbgs" in C.dbg:
        sc("dbgs", [NT, 128, 16, 128], F32)
        sc("dbgig", [NT, 128, 3, 128], BF16)
    sc("x1", [S, D], F32)
    sc("h2T", [NT, 128, 8, 128], BF16)
    sc("hgoT", [4, 128, S], BF16)
    if "dbgo" in C.dbg:
        sc("dbgo", [S, 512], F32)
    if "dbgacc" in C.dbg:
        sc("dbgacc", [128, 512], F32)
        sc("dbgp", [2, 128, 512], BF16)
        sc("dbgvs", [128, NT, 256], BF16)


def build(dbg=(), phases="A", ntiles=NT, **kw):
    nc = bass.Bass("TRN2", target_bir_lowering=False)
    C = Ctx(nc, dbg)
    declare(C)
    C.y = T(nc.dram_tensor("y", [S, D], F32, kind="ExternalOutput").ap(), "y")
    C.outs.append(C.y)
    with contextlib.ExitStack() as st:
        K = build_consts(C, st)
        if "A" in phases:
            phase_a(C, K, ntiles)
        if "C" in phases:
            C.P.barrier()
            phase_c(C, K, kw.get("c_tiles", NT), kw.get("c_dirs", (0, 1)))
        C.P.barrier()
        bg = phase_e0(C, K) if "2" in phases else []
        if "B" in phases:
            phase_b(C, K, kw.get("b_groups", 8), kw.get("hhs", (0, 1)), bg)
        else:
            for job in bg:
                job()
        if "D" in phases:
            C.P.barrier()
            phase_d(C, K, kw.get("d_groups", 8))
        if "E" in phases:
            C.P.barrier()
            phase_e1(C, K, kw.get("e1_groups", 16))
        if "2" in phases:
            C.P.barrier()
            phase_e2(C, K, kw.get("e2_groups", 8), kw.get("ni2", 128))
        if "F" in phases:
            C.P.barrier()
            phase_f(C, K, kw.get("f_tiles", NT))
        fin = []
        for t in C.outs:
            fin.append(t.b)
            fin.extend(t.regs.values())
        C.P.emit(final_bufs=fin)
    return nc, C


def _in_maps(inp, ncores):
    shared = {}
    for k in ["norm_mix", "q_norm", "k_norm", "hg_out_norm", "norm_ffn", "norm_ple"]:
        shared[k] = np.ascontiguousarray(np.asarray(inp[k], np.float32)[0][None])
    for k in ["w_in", "w_up_att", "w_up_hg", "w_out", "peer_wq", "ple_gate", "ple_proj"]:
        shared[k] = np.ascontiguousarray(np.asarray(inp[k], np.float32)[0])
    shared["hg_lb_raw"] = np.ascontiguousarray(np.asarray(inp["hg_lb_raw"], np.float32))
    sk = np.asarray(inp["peer_subkeys"], np.float32)[0]
    shared["skT"] = np.ascontiguousarray(sk.transpose(3, 0, 1, 2).reshape(128, 16, 128))
    u = np.asarray(inp["peer_u"], np.float32)[0].reshape(128, 128, 8, 128)
    shared["uT"] = np.ascontiguousarray(u.transpose(1, 3, 2, 0))
    v = np.asarray(inp["peer_v"], np.float32)[0].reshape(128, 128, D)
    shared["vL"] = np.ascontiguousarray(v.transpose(1, 0, 2))
    x = np.asarray(inp["x"], np.float32)
    p = np.asarray(inp["p"], np.float32)
    maps = []
    for b in range(ncores):
        m = dict(shared)
        m["x"] = np.ascontiguousarray(x[b])
        m["p"] = np.ascontiguousarray(p[0, b])
        maps.append(m)
    return maps


_NC_CACHE = {}


def kernel(**inputs):
    ncores = 8
    if "nc" not in _NC_CACHE:
        _NC_CACHE["nc"] = build(phases="ABCDE2F")[0]
    nc = _NC_CACHE["nc"]
    maps = _in_maps(inputs, ncores)
    res = run_bass_kernel_spmd(nc, maps, core_ids=list(range(ncores)))
    out = np.stack([np.asarray(res.results[b]["y"], np.float32) for b in range(ncores)], 0)
    return out
```

```python
import contextlib
import numpy as np
import concourse.bass as bass
import concourse.mybir as mybir
from concourse.bass_utils import run_bass_kernel_spmd

F32 = mybir.dt.float32
BF16 = mybir.dt.bfloat16
I32 = mybir.dt.int32
U32 = mybir.dt.uint32
ALU = mybir.AluOpType
AF = mybir.ActivationFunctionType
AX = mybir.AxisListType

S = 4096
D = 1024
NT = S // 128
EPS = 1e-6
EPOCH = 16000
NDMA_SLOTS = 48


class Buf:
    __slots__ = ("name", "last_w", "readers")

    def __init__(self, name=""):
        self.name = name
        self.last_w = None
        self.readers = {}


class T:
    __slots__ = ("t", "b", "regs")

    def __init__(self, t, name=""):
        self.t = t
        self.b = Buf(name)
        self.regs = {}

    def reg(self, key):
        if key not in self.regs:
            self.regs[key] = Buf(f"{self.b.name}[{key}]")
        return self.regs[key]

    def regl(self, keys):
        return [self.reg(k) for k in keys]


class Prog:
    ENGS = ("pe", "act", "dve", "pool", "sp")

    def __init__(self, nc):
        self.nc = nc
        self.ops = {e: [] for e in self.ENGS}
        self.count = {e: 0 for e in self.ENGS}
        self.known = {e: {} for e in self.ENGS}
        self.dma_count = [0] * NDMA_SLOTS
        self.dma_rr = {"hw": 0, "sw": 0}
        self.n_instr = 0
        self.pending = {e: [] for e in self.ENGS}

    def barrier(self):
        prods = [(e, self.count[e]) for e in self.ENGS if self.count[e] > 0]
        prods += [(("dma", s), c) for s, c in enumerate(self.dma_count) if c > 0]
        for e in self.ENGS:
            kn = self.known[e]
            for p, c in prods:
                if kn.get(p, 0) < c:
                    kn[p] = c
                    self.pending[e].append((p, c))

    def _deps(self, eng, reads, writes, pe_accum=False):
        deps = {}

        def add(p, s):
            if deps.get(p, 0) < s:
                deps[p] = s

        for b in reads:
            if b.last_w is not None:
                add(*b.last_w)
        for b in writes:
            if b.last_w is not None:
                if not (pe_accum and b.last_w[0] == "pe" and eng == "pe"):
                    add(*b.last_w)
            for p, s in b.readers.items():
                add(p, s)
        waits = []
        kn = self.known[eng]
        for p, s in deps.items():
            if kn.get(p, 0) < s:
                kn[p] = s
                waits.append((p, s))
        return waits

    def _commit(self, prod, seq, reads, writes):
        for b in writes:
            b.last_w = (prod, seq)
            b.readers = {}
        for b in reads:
            b.readers[prod] = max(b.readers.get(prod, 0), seq)

    @staticmethod
    def _bufs(xs):
        out = []
        for x in xs:
            if isinstance(x, T):
                out.append(x.b)
            elif isinstance(x, (list, tuple)):
                out.extend(Prog._bufs(x))
            else:
                out.append(x)
        return out

    def op(self, eng, fn, reads=(), writes=(), pe_accum=False):
        reads = self._bufs(reads)
        writes = self._bufs(writes)
        waits = self._deps(eng, reads, writes, pe_accum)
        waits = [w for w in self.pending[eng] if w not in waits] + waits
        self.pending[eng] = []
        self.count[eng] += 1
        seq = self.count[eng]
        self.ops[eng].append((waits, fn, (eng, seq)))
        self._commit(eng, seq, reads, writes)
        self.n_instr += 1

    def dma(self, q, out, in_, reads=(), writes=(), **kw):
        reads = self._bufs(reads)
        writes = self._bufs(writes)
        waits = self._deps(q, reads, writes)
        waits = [w for w in self.pending[q] if w not in waits] + waits
        self.pending[q] = []
        half = NDMA_SLOTS // 2
        kind = "sw" if q == "pool" else "hw"
        slot = self.dma_rr[kind] + (half if kind == "sw" else 0)
        self.dma_rr[kind] = (self.dma_rr[kind] + 1) % half
        prod = ("dma", slot)
        prev = self.dma_count[slot]
        kn = self.known[q]
        if prev and kn.get(prod, 0) < prev:
            kn[prod] = prev
            waits.append((prod, prev))
        self.dma_count[slot] += 1
        seq = self.dma_count[slot]
        self.ops[q].append((waits, (lambda e: e.dma_start(out=out, in_=in_, **kw)), (prod, seq)))
        self._commit(prod, seq, reads, writes)
        self.n_instr += 1

    def emit(self, final_bufs=()):
        nc = self.nc
        final_bufs = self._bufs(final_bufs)
        with contextlib.ExitStack() as st:
            sems = {}
            for e in self.ENGS:
                nep = self.count[e] // EPOCH + 1
                sems[e] = [st.enter_context(nc.semaphore(f"s_{e}_{k}")) for k in range(nep)]
            for s in range(NDMA_SLOTS):
                sems[("dma", s)] = [st.enter_context(nc.semaphore(f"s_dma{s}"))]
            fin = {}
            for b in final_bufs:
                if b.last_w is not None:
                    p, s = b.last_w
                    fin[p] = max(fin.get(p, 0), s)

            def wait(eng, p, s):
                if isinstance(p, tuple):
                    eng.wait_ge(sems[p][0], 16 * s)
                else:
                    k = (s - 1) // EPOCH
                    eng.wait_ge(sems[p][k], s - k * EPOCH)

            def run(ename, eng):
                for waits, fn, (prod, seq) in self.ops[ename]:
                    for p, s in waits:
                        wait(eng, p, s)
                    ins = fn(eng)
                    if isinstance(prod, tuple):
                        ins.then_inc(sems[prod][0], 16)
                    else:
                        k = (seq - 1) // EPOCH
                        ins.then_inc(sems[prod][k], 1)
                if ename == "sp":
                    for p, s in fin.items():
                        wait(eng, p, s)

            with nc.Block() as block:
                @block.sync
                def _(e):
                    run("sp", e)

                @block.tensor
                def _(e):
                    run("pe", e)

                @block.scalar
                def _(e):
                    run("act", e)

                @block.vector
                def _(e):
                    run("dve", e)

                @block.gpsimd
                def _(e):
                    run("pool", e)


class Ring:
    def __init__(self, items):
        self.items = items
        self.i = 0

    def next(self):
        x = self.items[self.i % len(self.items)]
        self.i += 1
        return x


class Ctx:
    def __init__(self, nc, dbg=()):
        self.nc = nc
        self.P = Prog(nc)
        self.dbg = set(dbg)
        self.ins = {}
        self.scr = {}
        self.outs = []
        self.uid = 0

    def din(self, name, shape, dt=F32):
        ap = self.nc.dram_tensor(name, list(shape), dt, kind="ExternalInput").ap()
        self.ins[name] = T(ap, name)
        return self.ins[name]

    def scratch(self, name, shape, dt):
        if name in self.dbg:
            ap = self.nc.dram_tensor(name, list(shape), dt, kind="ExternalOutput").ap()
        else:
            ap = self.nc.dram_tensor(name, list(shape), dt).ap()
        t = T(ap, name)
        self.scr[name] = t
        if name in self.dbg:
            self.outs.append(t)
        return t

    def alloc(self, st, shape, dt, name=None, psum=False):
        self.uid += 1
        name = f"{name or 't'}_{self.uid}"
        if psum:
            t = st.enter_context(self.nc.psum_tensor(name, list(shape), dt))
        else:
            t = st.enter_context(self.nc.sbuf_tensor(name, list(shape), dt))
        return T(t, name)

    def ring(self, st, n, shape, dt, name=None, psum=False):
        return Ring([self.alloc(st, shape, dt, name, psum) for _ in range(n)])


def mm(C, out_t, out_ap, lhsT_t, lhsT_ap, rhs_t, rhs_ap, start, stop):
    C.P.op("pe", lambda e: e.matmul(out_ap, lhsT=lhsT_ap, rhs=rhs_ap, start=start, stop=stop),
           reads=[lhsT_t, rhs_t], writes=[out_t], pe_accum=True)


def tr(C, out_t, out_ap, in_t, in_ap, ident):
    C.P.op("pe", lambda e: e.transpose(out=out_ap, in_=in_ap, identity=ident.t[:]),
           reads=[in_t, ident], writes=[out_t], pe_accum=True)


def act(C, out_t, out_ap, in_t, in_ap, func, reads=(), extra_w=(), **kw):
    C.P.op("act", lambda e: e.activation(out=out_ap, in_=in_ap, func=func, **kw),
           reads=[in_t] + list(reads), writes=[out_t] + list(extra_w))


def tt(C, eng, out_t, out_ap, a_t, a_ap, b_t, b_ap, op):
    C.P.op(eng, lambda e: e.tensor_tensor(out=out_ap, in0=a_ap, in1=b_ap, op=op),
           reads=[a_t, b_t], writes=[out_t])


def ts(C, eng, out_t, out_ap, a_t, a_ap, s1, s2, op0, op1=None, reads=()):
    if op1 is None:
        C.P.op(eng, lambda e: e.tensor_scalar(out=out_ap, in0=a_ap, scalar1=s1, scalar2=None, op0=op0),
               reads=[a_t] + list(reads), writes=[out_t])
    else:
        C.P.op(eng, lambda e: e.tensor_scalar(out=out_ap, in0=a_ap, scalar1=s1, scalar2=s2, op0=op0, op1=op1),
               reads=[a_t] + list(reads), writes=[out_t])


def stt(C, eng, out_t, out_ap, a_t, a_ap, scalar, b_t, b_ap, op0, op1, reads=()):
    C.P.op(eng, lambda e: e.scalar_tensor_tensor(out=out_ap, in0=a_ap, scalar=scalar, in1=b_ap, op0=op0, op1=op1),
           reads=[a_t, b_t] + list(reads), writes=[out_t])


def cp(C, eng, out_t, out_ap, in_t, in_ap):
    if eng == "act":
        C.P.op("act", lambda e: e.copy(out=out_ap, in_=in_ap), reads=[in_t], writes=[out_t])
    else:
        C.P.op(eng, lambda e: e.tensor_copy(out=out_ap, in_=in_ap), reads=[in_t], writes=[out_t])


def rsqrt_mean(C, st_t, src_ap_fn, n, scale):
    ap = src_ap_fn()
    ts(C, "dve", st_t, ap, st_t, ap, scale, EPS, ALU.mult, ALU.add)
    C.P.op("act", lambda e: e.activation(out=ap, in_=ap, func=AF.Sqrt), reads=[st_t], writes=[st_t])
    C.P.op("dve", lambda e: e.reciprocal(out=ap, in_=ap), reads=[st_t], writes=[st_t])


def build_consts(C, st):
    P = C.P
    K = {}
    idf = C.alloc(st, [128, 128], F32, "idf")
    P.op("pool", lambda e: e.memset(idf.t[:], 0.0), writes=[idf])
    P.op("pool", lambda e: e.affine_select(out=idf.t[:], in_=idf.t[:], pattern=[[-1, 128]],
                                           compare_op=ALU.not_equal, fill=1.0, base=0, channel_multiplier=1),
         reads=[idf], writes=[idf])
    ident = C.alloc(st, [128, 128], BF16, "ident")
    cp(C, "dve", ident, ident.t[:], idf, idf.t[:])
    K["ident"] = ident
    K["identf"] = idf
    return K


def build_rope(C, st):
    import math
    P = C.P
    rope = C.alloc(st, [128, NT, 2, 32], F32, "rope")
    with contextlib.ExitStack() as tmp:
        pf = C.alloc(tmp, [128, 1], F32, "pf")
        P.op("pool", lambda e: e.iota(pf.t[:], pattern=[[0, 1]], base=0, channel_multiplier=1,
                                      allow_small_or_imprecise_dtypes=True), writes=[pf])
        pi = C.alloc(tmp, [128, 2], I32, "pi")
        hl = C.alloc(tmp, [128, 2], I32, "hl")
        hlf = C.alloc(tmp, [128, 2], F32, "hlf")
        cp(C, "dve", pi, pi.t[:, 0:1], pf, pf.t[:])
        P.op("dve", lambda e: e.tensor_single_scalar(out=hl.t[:, 0:1], in_=pi.t[:, 0:1], scalar=6, op=ALU.arith_shift_right),
             reads=[pi], writes=[hl])
        P.op("dve", lambda e: e.tensor_single_scalar(out=hl.t[:, 1:2], in_=pi.t[:, 0:1], scalar=63, op=ALU.bitwise_and),
             reads=[pi, hl], writes=[hl])
        cp(C, "dve", hlf, hlf.t[:], hl, hl.t[:])
        jf = C.alloc(tmp, [128, 16], F32, "jf")
        P.op("pool", lambda e: e.iota(jf.t[:], pattern=[[1, 16]], base=0, channel_multiplier=0,
                                      allow_small_or_imprecise_dtypes=True), writes=[jf])
        inv = C.alloc(tmp, [128, 16], F32, "inv")
        act(C, inv, inv.t[:], jf, jf.t[:], AF.Exp, scale=-math.log(10000.0) / 16.0)
        rowp = C.alloc(tmp, [128, NT], F32, "rowp")
        P.op("pool", lambda e: e.iota(rowp.t[:], pattern=[[2, NT]], base=0, channel_multiplier=0,
                                      allow_small_or_imprecise_dtypes=True), writes=[rowp])
        ts(C, "dve", rowp, rowp.t[:], rowp, rowp.t[:], hlf.t[:, 0:1], None, ALU.add, reads=[hlf])
        ang = C.alloc(tmp, [128, NT, 32], F32, "ang")
        tt(C, "dve", ang, ang.t[:, :, 0:16], rowp, rowp.t[:].unsqueeze(2).to_broadcast([128, NT, 16]),
           inv, inv.t[:].unsqueeze(1).to_broadcast([128, NT, 16]), ALU.mult)
        colang = C.alloc(tmp, [128, 16], F32, "colang")
        ts(C, "dve", colang, colang.t[:], inv, inv.t[:], hlf.t[:, 1:2], None, ALU.mult, reads=[hlf])
        cp(C, "dve", ang, ang.t[:, :, 16:32], colang, colang.t[:].unsqueeze(1).to_broadcast([128, NT, 16]))
        ni = C.alloc(tmp, [128, NT, 32], I32, "ni")
        nf = C.alloc(tmp, [128, NT, 32], F32, "nf")
        red = C.alloc(tmp, [128, NT, 32], F32, "red")
        two_pi = 2.0 * math.pi
        for which, shift in ((1, 0.0), (0, math.pi / 2.0)):
            ts(C, "dve", nf, nf.t[:], ang, ang.t[:], shift, 1.0 / two_pi, ALU.add, ALU.mult)
            cp(C, "dve", ni, ni.t[:], nf, nf.t[:])
            cp(C, "dve", nf, nf.t[:], ni, ni.t[:])
            stt(C, "dve", red, red.t[:], nf, nf.t[:], -two_pi, ang, ang.t[:], ALU.mult, ALU.add)
            ts(C, "dve", red, red.t[:], red, red.t[:], shift, 3.1415925, ALU.add, ALU.min)
            ts(C, "dve", red, red.t[:], red, red.t[:], -3.1415925, None, ALU.max)
            act(C, rope, rope.t[:, :, which, :], red, red.t[:], AF.Sin)
    return rope


OFF = dict(aq=0, ak=512, av=640, hq=768, hff=1280, hfb=1792, hi=2304, hg=2816, gate=3328)


def phase_a(C, K, ntiles=NT):
    nc, P = C.nc, C.P
    I = C.ins
    Sc = C.scr
    with contextlib.ExitStack() as st:
        K = dict(K)
        K["rope"] = build_rope(C, st)
        P.barrier()
        w_in = C.alloc(st, [128, 8, 5376], BF16, "w_in")
        wv = I["w_in"].t.rearrange("(k p) n -> p k n", p=128)
        for c0 in range(0, 5376, 672):
            P.dma("pool", w_in.t[:, :, c0:c0 + 672], wv[:, :, c0:c0 + 672], reads=[I["w_in"]], writes=[w_in])
        gmix = C.alloc(st, [128, 1024], F32, "gmix")
        P.dma("sp", gmix.t[:], I["norm_mix"].t.to_broadcast([128, 1024]), writes=[gmix])
        qg = C.alloc(st, [128, 8, 64], F32, "qg")
        P.dma("sp", qg.t[:], I["q_norm"].t.unsqueeze(1).to_broadcast([128, 8, 64]), writes=[qg])
        kg = C.alloc(st, [128, 2, 64], F32, "kg")
        P.dma("sp", kg.t[:], I["k_norm"].t.unsqueeze(1).to_broadcast([128, 2, 64]), writes=[kg])
        raw = C.alloc(st, [128, 2, 2, 512], F32, "raw")
        P.dma("sp", raw.t[:], I["hg_lb_raw"].t.unsqueeze(0).to_broadcast([128, 2, 2, 512]), writes=[raw])
        lbb = C.alloc(st, [128, 2, 512], F32, "lbb")
        omlb = C.alloc(st, [128, 2, 512], F32, "omlb")
        tt(C, "dve", lbb, lbb.t[:], raw, raw.t[:, 0], raw, raw.t[:, 1], ALU.subtract)
        act(C, omlb, omlb.t[:], lbb, lbb.t[:], AF.Sigmoid, scale=-1.0)
        act(C, lbb, lbb.t[:], lbb, lbb.t[:], AF.Sigmoid)
        rawT = C.alloc(st, [128, 2, 2, 4], F32, "rawT")
        P.dma("sp", rawT.t[:], I["hg_lb_raw"].t.rearrange("s r (c p) -> p s r c", p=128), writes=[rawT],
              allow_slow_non_contiguous=True)
        omlT = C.alloc(st, [128, 2, 4], F32, "omlT")
        tt(C, "dve", omlT, omlT.t[:], rawT, rawT.t[:, 0], rawT, rawT.t[:, 1], ALU.subtract)
        act(C, omlT, omlT.t[:], omlT, omlT.t[:], AF.Sigmoid, scale=-1.0)

        xr = C.ring(st, 3, [128, 1024], F32, "xt")
        junk = C.alloc(st, [128, 1024], BF16, "junk")
        stat = C.ring(st, 8, [128, 16], F32, "stat")
        hbr = C.ring(st, 2, [128, 1024], BF16, "hb")
        hTr = C.ring(st, 2, [128, 8, 512], BF16, "hT")
        ptr = C.ring(st, 2, [128, 8, 128], BF16, "ptr", psum=True)
        pfr = C.ring(st, 2, [128, 512], F32, "pf", psum=True)
        ptk = C.ring(st, 3, [128, 512], F32, "ptk", psum=True)
        pqt = C.alloc(st, [128, 8, 128], BF16, "pqt", psum=True)
        stg_bf = C.ring(st, 4, [128, 512], BF16, "stgb")
        stg_f = C.ring(st, 3, [128, 512], F32, "stgf")
        tmpf = C.ring(st, 4, [128, 512], F32, "tmpf")
        qn = C.ring(st, 3, [128, 512], F32, "qn")
        qr = C.ring(st, 2, [128, 512], BF16, "qr")
        kk = C.ring(st, 2, [128, 4, 64], BF16, "kk")
        kn_ = C.ring(st, 2, [128, 128], F32, "kn")
        vst = C.ring(st, 2, [128, 2, 128], BF16, "vst")
        for v_ in vst.items:
            P.op("pool", lambda e, v_=v_: e.memset(v_.t[:], 1.0), writes=[v_])
        qTs = C.ring(st, 2, [128, 6, 128], BF16, "qTs")
        rt = C.ring(st, 8, [128, 8, 2, 16], F32, "rt")

        def run_rr(gens):
            gens = list(gens)
            while gens:
                for g_ in list(gens):
                    try:
                        next(g_)
                    except StopIteration:
                        gens.remove(g_)

        def rope_norm(src_ps, src_ap, nh, gain, cs, dst_t, dst_ap4):
            w = nh * 64
            sq = tmpf.next()
            act(C, sq, sq.t[:, 0:w], src_ps, src_ap, AF.Square)
            yield
            s8 = stat.next()
            P.op("dve", lambda e: e.tensor_reduce(out=s8.t[:, 0:nh], in_=sq.t[:, 0:w].rearrange("p (h d) -> p h d", d=64),
                                                  axis=AX.X, op=ALU.add), reads=[sq], writes=[s8])
            yield
            ap = s8.t[:, 0:nh]
            ts(C, "dve", s8, ap, s8, ap, 1.0 / 64, EPS, ALU.mult, ALU.add)
            yield
            C.P.op("act", lambda e: e.activation(out=ap, in_=ap, func=AF.Sqrt), reads=[s8], writes=[s8])
            yield
            C.P.op("dve", lambda e: e.reciprocal(out=ap, in_=ap), reads=[s8], writes=[s8])
            yield
            n_ = qn.next()
            nv = n_.t[:, 0:w].rearrange("p (h d) -> p h d", d=64)
            tt(C, "dve", n_, nv, src_ps, src_ap.rearrange("p (h d) -> p h d", d=64),
               s8, s8.t[:, 0:nh].unsqueeze(2).to_broadcast([128, nh, 64]), ALU.mult)
            yield
            tt(C, "pool", n_, nv, n_, nv, gain, gain.t[:, 0:nh, :], ALU.mult)
            yield
            v5 = n_.t[:, 0:w].rearrange("p (h a b f) -> p h a b f", a=2, b=2, f=16)
            x1 = v5[:, :, :, 0, :]
            x2 = v5[:, :, :, 1, :]
            cb = cs.t[:, 0, :].rearrange("p (a f) -> p a f", a=2).unsqueeze(1).to_broadcast([128, nh, 2, 16])
            sb_ = cs.t[:, 1, :].rearrange("p (a f) -> p a f", a=2).unsqueeze(1).to_broadcast([128, nh, 2, 16])
            t1, t2 = rt.next(), rt.next()
            tt(C, "dve", t1, t1.t[:, 0:nh], n_, x1, cs, cb, ALU.mult)
            tt(C, "pool", t2, t2.t[:, 0:nh], n_, x2, cs, sb_, ALU.mult)
            yield
            t3, t4 = rt.next(), rt.next()
            tt(C, "dve", t3, t3.t[:, 0:nh], n_, x1, cs, sb_, ALU.mult)
            tt(C, "pool", t4, t4.t[:, 0:nh], n_, x2, cs, cb, ALU.mult)
            yield
            tt(C, "dve", dst_t, dst_ap4[:, :, :, 0, :], t1, t1.t[:, 0:nh], t2, t2.t[:, 0:nh], ALU.subtract)
            yield
            tt(C, "pool", dst_t, dst_ap4[:, :, :, 1, :], t3, t3.t[:, 0:nh], t4, t4.t[:, 0:nh], ALU.add)
            yield

        def prep_tile(g, j, hT):
            ti = g * 4 + j
            xt = xr.next()
            P.dma("sp", xt.t[:], I["x"].t[ti * 128:(ti + 1) * 128, :], reads=[I["x"]], writes=[xt])
            s_ = stat.next()
            act(C, junk, junk.t[:], xt, xt.t[:], AF.Square, accum_out=s_.t[:, 0:1], extra_w=[s_])
            yield
            ap = s_.t[:, 0:1]
            ts(C, "dve", s_, ap, s_, ap, 1.0 / D, EPS, ALU.mult, ALU.add)
            yield
            C.P.op("act", lambda e: e.activation(out=ap, in_=ap, func=AF.Sqrt), reads=[s_], writes=[s_])
            yield
            C.P.op("dve", lambda e: e.reciprocal(out=ap, in_=ap), reads=[s_], writes=[s_])
            yield
            hb = hbr.next()
            stt(C, "dve", hb, hb.t[:], xt, xt.t[:], s_.t[:, 0:1], gmix, gmix.t[:], ALU.mult, ALU.mult, reads=[s_])
            yield
            pt = ptr.next()
            for k in range(8):
                tr(C, pt, pt.t[:, k, :], hb, hb.t[:, k * 128:(k + 1) * 128], K["ident"])
            yield
            cp(C, "act", hT, hT.t[:, :, j * 128:(j + 1) * 128], pt, pt.t[:])
            yield

        ngr = ntiles // 4
        hTs = {0: hTr.next()}
        for j in range(4):
            run_rr([prep_tile(0, j, hTs[0])])
        for g in range(ngr):
            hT = hTs[g]
            if g + 1 < ngr:
                hTs[g + 1] = hTr.next()
            cols = slice(g * 512, (g + 1) * 512)
            fm = [("hq", OFF["hq"] + c * 128, c) for c in range(4)] + \
                 [("hff", OFF["hff"] + c * 128, c) for c in range(4)] + \
                 [("hfb", OFF["hfb"] + c * 128, c) for c in range(4)] + \
                 [("gate", OFF["gate"] + c * 128, c) for c in range(16)]
            for kind, c0, c in fm:
                pf = pfr.next()
                for k in range(8):
                    mm(C, pf, pf.t[:], w_in, w_in.t[:, k, c0:c0 + 128], hT, hT.t[:, k, :], k == 0, k == 7)
                sg = stg_bf.next()
                if kind == "hq":
                    act(C, sg, sg.t[:], pf, pf.t[:], AF.Silu)
                    dst = Sc["hqT"]
                    P.dma("pool", dst.t[c, :, cols], sg.t[:], reads=[sg], writes=[dst.reg((c, g))])
                elif kind in ("hff", "hfb"):
                    d_ = 0 if kind == "hff" else 1
                    tf = tmpf.next()
                    act(C, tf, tf.t[:], pf, pf.t[:], AF.Sigmoid, scale=-1.0)
                    ts(C, "dve", sg, sg.t[:], tf, tf.t[:], omlT.t[:, d_, c:c + 1], None, ALU.mult, reads=[omlT])
                    dst = Sc["kT"]
                    P.dma("pool", dst.t[d_, c, :, cols], sg.t[:], reads=[sg], writes=[dst.reg((d_, c, g))])
                else:
                    act(C, sg, sg.t[:], pf, pf.t[:], AF.Sigmoid)
                    dst = Sc["gtsT"]
                    P.dma("pool", dst.t[c, :, cols], sg.t[:], reads=[sg], writes=[dst.reg((c, g))])
            for j in range(4):
                ti = g * 4 + j
                rows = slice(ti * 128, (ti + 1) * 128)
                lhs = lambda k, j=j: hT.t[:, k, j * 128:(j + 1) * 128]
                cs = T(K["rope"].t[:, ti], "rope_v")
                cs.b = K["rope"].b
                q_ = qr.next()
                k_ = kk.next()

                def chain_q():
                    pq = ptk.next()
                    for k in range(8):
                        mm(C, pq, pq.t[:], hT, lhs(k), w_in, w_in.t[:, k, 0:512], k == 0, k == 7)
                    yield
                    yield from rope_norm(pq, pq.t[:], 8, qg, cs, q_, q_.t[:].rearrange("p (h a b f) -> p h a b f", a=2, b=2, f=16))

                def chain_kv():
                    pkv = ptk.next()
                    for k in range(8):
                        mm(C, pkv, pkv.t[:, 0:256], hT, lhs(k), w_in, w_in.t[:, k, 512:768], k == 0, k == 7)
                    yield
                    v_ = vst.next()
                    cp(C, "act", v_, v_.t[:, :, 0:64], pkv, pkv.t[:, 128:256].rearrange("p (h d) -> p h d", d=64))
                    P.dma("pool", Sc["v"].t[ti], v_.t[:], reads=[v_], writes=[Sc["v"].reg(ti)])
                    yield
                    kview = k_.t[:].rearrange("p (h r) d -> p h r d", r=2)
                    yield from rope_norm(pkv, pkv.t[:, 0:128], 2, kg, cs, k_,
                                         kview[:, :, 0, :].rearrange("p h (a b f) -> p h a b f", a=2, b=2, f=16))
                    cp(C, "pool", k_, kview[:, :, 1, :], k_, kview[:, :, 0, :])
                    yield

                def chain_gate(d_, key):
                    pg = ptk.next()
                    for k in range(8):
                        mm(C, pg, pg.t[:], hT, lhs(k), w_in, w_in.t[:, k, OFF[key]:OFF[key] + 512], k == 0, k == 7)
                    yield
                    tf = tmpf.next()
                    act(C, tf, tf.t[:], pg, pg.t[:], AF.Sigmoid)
                    yield
                    tt(C, "dve", tf, tf.t[:], tf, tf.t[:], omlb, omlb.t[:, d_, :], ALU.mult)
                    yield
                    tt(C, "dve", tf, tf.t[:], tf, tf.t[:], lbb, lbb.t[:, d_, :], ALU.add)
                    yield
                    gf = stg_f.next()
                    act(C, gf, gf.t[:], tf, tf.t[:], AF.Ln)
                    P.dma("pool", Sc["g"].t[d_, rows, :], gf.t[:], reads=[gf], writes=[Sc["g"].reg((d_, ti))])
                    kb = stg_bf.next()
                    ts(C, "pool", kb, kb.t[:], tf, tf.t[:], -1.0, 1.0, ALU.mult, ALU.add)
                    P.dma("pool", Sc["k"].t[d_, rows, :], kb.t[:], reads=[kb], writes=[Sc["k"].reg((d_, ti))])
                    yield

                def chain_h(key, dstn, fn):
                    ph = ptk.next()
                    for k in range(8):
                        mm(C, ph, ph.t[:], hT, lhs(k), w_in, w_in.t[:, k, OFF[key]:OFF[key] + 512], k == 0, k == 7)
                    yield
                    sb_ = stg_bf.next()
                    if fn is None:
                        cp(C, "act", sb_, sb_.t[:], ph, ph.t[:])
                    else:
                        act(C, sb_, sb_.t[:], ph, ph.t[:], fn)
                    P.dma("pool", Sc[dstn].t[rows, :], sb_.t[:], reads=[sb_], writes=[Sc[dstn].reg(ti)])
                    yield

                chains = [chain_q(), chain_kv(), chain_gate(0, "hff")]
                if g + 1 < ngr:
                    chains.append(prep_tile(g + 1, j, hTs[g + 1]))
                run_rr(chains)
                run_rr([chain_gate(1, "hfb"), chain_h("hi", "hi", None), chain_h("hg", "sg", AF.Silu)])
                for pr in range(4):
                    tr(C, pqt, pqt.t[:, pr, :], q_, q_.t[:, pr * 128:(pr + 1) * 128], K["ident"])
                kflat = k_.t[:].rearrange("p a d -> p (a d)")
                for kv in range(2):
                    tr(C, pqt, pqt.t[:, 4 + kv, :], k_, kflat[:, kv * 128:(kv + 1) * 128], K["ident"])
                qs = qTs.next()
                cp(C, "act", qs, qs.t[:], pqt, pqt.t[:, 0:6, :])
                P.dma("pool", Sc["qT"].t[:, :, rows].rearrange("r p t -> p r t"), qs.t[:, 0:4, :], reads=[qs], writes=[Sc["qT"].reg(ti)])
                P.dma("pool", Sc["kTa"].t[:, :, rows].rearrange("r p t -> p r t"), qs.t[:, 4:6, :], reads=[qs], writes=[Sc["kTa"].reg(ti)])


def phase_b(C, K, ngroups=8, hhs=(0, 1), bg=()):
    nc, P = C.nc, C.P
    Sc = C.scr
    with contextlib.ExitStack() as st:
        kT = [C.alloc(st, [128, S], BF16, "kTsb") for _ in range(2)]
        for kv in range(2):
            for hf in range(2):
                cs_ = slice(hf * 2048, (hf + 1) * 2048)
                P.dma("sp", kT[kv].t[:, cs_], Sc["kTa"].t[kv, :, cs_],
                      reads=Sc["kTa"].regl(range(hf * 16, hf * 16 + 16)), writes=[kT[kv]])
        vs = C.alloc(st, [128, NT, 256], BF16, "vsb")
        for hf in range(4):
            P.dma("sp", vs.t[:, hf * 8:(hf + 1) * 8, :], Sc["v"].t[hf * 8:(hf + 1) * 8].rearrange("t p h c -> p t (h c)"),
                  reads=Sc["v"].regl(range(hf * 8, hf * 8 + 8)), writes=[vs])
        if "dbgvs" in Sc:
            P.dma("pool", Sc["dbgvs"].t[:], vs.t[:], reads=[vs], writes=[Sc["dbgvs"]])
        qr_ = C.ring(st, 2, [128, 512], BF16, "qTg")
        psS = C.ring(st, 4, [128, 512], F32, "psS", psum=True)
        acc = [C.alloc(st, [128, 512], F32, "acc", psum=True) for _ in range(2)]
        ptr_ = C.ring(st, 8, [128, 512], BF16, "pT")
        rl = C.ring(st, 2, [128, 512], F32, "rl")
        obr = C.ring(st, 2, [128, 512], BF16, "ob")
        LAG = 3
        steps = [(g, pr, kt, hh) for g in range(ngroups) for pr in range(4) for kt in range(NT) for hh in hhs]
        state = {}
        accs = [acc, [C.alloc(st, [128, 512], F32, "acc2", psum=True) for _ in range(2)]]

        def stage1(g, pr, kt, hh):
            kv = pr // 2
            if kt == 0 and hh == hhs[0]:
                q = qr_.next()
                P.dma("sp", q.t[:], Sc["qT"].t[pr, :, g * 512:(g + 1) * 512], reads=Sc["qT"].regl(range(4 * g, 4 * g + 4)), writes=[q])
                state["q", g, pr] = q
            q = state["q", g, pr]
            rows = slice(hh * 64, (hh + 1) * 64)
            s_ = psS.next()
            mm(C, s_, s_.t[:], kT[kv], kT[kv].t[rows, kt * 128:(kt + 1) * 128], q, q.t[rows, :], True, True)
            p_ = ptr_.next()
            act(C, p_, p_.t[:], s_, s_.t[:], AF.Exp, scale=0.125)
            state["p", g, pr, kt, hh] = p_

        def stage2(g, pr, kt, hh):
            kv = pr // 2
            p_ = state.pop(("p", g, pr, kt, hh))
            ac = accs[(g * 4 + pr) % 2]
            mm(C, ac[hh], ac[hh].t[:], vs, vs.t[:, kt, kv * 128:(kv + 1) * 128], p_, p_.t[:], kt == 0, kt == NT - 1)
            if kt == NT - 1 and hh == hhs[-1]:
                ob = obr.next()
                for h2_ in hhs:
                    r_ = rl.next()
                    C.P.op("dve", lambda e, r_=r_, h2_=h2_, ac=ac: e.reciprocal(out=r_.t[64:128, :], in_=ac[h2_].t[64:128, :]),
                           reads=[ac[h2_]], writes=[r_])
                    tt(C, "dve", ob, ob.t[h2_ * 64:(h2_ + 1) * 64, :], ac[h2_], ac[h2_].t[0:64, :], r_, r_.t[64:128, :], ALU.mult)
                P.dma("pool", Sc["attoT"].t[pr, :, g * 512:(g + 1) * 512], ob.t[:], reads=[ob], writes=[Sc["attoT"].reg((pr, g))])

        bg = list(bg)
        LAG = 4
        for it in range(0, len(steps) + LAG, 2):
            for i_ in (it, it + 1):
                if i_ < len(steps):
                    stage1(*steps[i_])
            for i_ in (it - LAG, it - LAG + 1):
                if 0 <= i_ < len(steps):
                    stage2(*steps[i_])
            if bg and (it // 2) % 3 == 2:
                bg.pop(0)()
        for job in bg:
            job()


def build_masks(C, st):
    P = C.P
    M = {}
    specs = {
        "f_incl": (ALU.is_ge, 0, 1, -1),
        "f_excl": (ALU.is_gt, 0, -1, 1),
        "b_incl": (ALU.is_ge, 0, -1, 1),
        "b_excl": (ALU.is_gt, 0, 1, -1),
    }
    for name, (op, base, tmul, pmul) in specs.items():
        m = C.alloc(st, [128, 128], F32, "m_" + name)
        P.op("pool", lambda e, m=m: e.memset(m.t[:], 1.0), writes=[m])
        P.op("pool", lambda e, m=m, op=op, base=base, tmul=tmul, pmul=pmul: e.affine_select(
            out=m.t[:], in_=m.t[:], pattern=[[tmul, 128]], compare_op=op, fill=0.0, base=base, channel_multiplier=pmul),
            reads=[m], writes=[m])
        P.op("pool", lambda e, m=m: e.memset(m.t[0:64, 64:128], 0.0), reads=[m], writes=[m])
        P.op("pool", lambda e, m=m: e.memset(m.t[64:128, 0:64], 0.0), reads=[m], writes=[m])
        M[name] = m
    return M


def phase_c(C, K, ntiles=NT, dirs=(0, 1)):
    nc, P = C.nc, C.P
    Sc = C.scr
    I = C.ins
    with contextlib.ExitStack() as st:
        M = build_masks(C, st)
        gon = C.alloc(st, [128, 4, 128], F32, "gon")
        P.dma("sp", gon.t[:], I["hg_out_norm"].t.unsqueeze(1).to_broadcast([128, 4, 128]), writes=[gon])
        gr = C.ring(st, 2, [128, 512], F32, "g_t")
        kdr = C.ring(st, 2, [128, 512], BF16, "kd_t")
        kTr = C.ring(st, 2, [128, 4, 128], BF16, "kT_t")
        qTr = C.ring(st, 2, [128, 4, 128], BF16, "hqT_t")
        vr = C.ring(st, 2, [128, 512], BF16, "v_t")
        ofr = C.ring(st, 2, [128, 512], F32, "of_t")
        sgr = C.ring(st, 2, [128, 512], BF16, "sg_t")
        prx = C.alloc(st, [128, 512], F32, "prx", psum=True)
        pbT = C.alloc(st, [128, 4, 128], F32, "pbT", psum=True)
        pX = [C.alloc(st, [128, 4, 128], F32, "pX", psum=True) for _ in range(2)]
        pOs = [C.alloc(st, [128, 4, 128], F32, "pOs", psum=True) for _ in range(2)]
        pTr = C.alloc(st, [128, 8, 128], BF16, "pTr", psum=True)
        ebT = C.ring(st, 2, [128, 4, 128], F32, "ebT")
        enbT = C.ring(st, 2, [128, 4, 128], F32, "enbT")
        er = C.ring(st, 2, [128, 512], F32, "er")
        qfull = C.ring(st, 2, [128, 4, 128], BF16, "qfull")
        qlo = C.ring(st, 2, [128, 4, 128], BF16, "qlo")
        qhi = C.ring(st, 2, [128, 4, 128], BF16, "qhi")
        for t_ in qlo.items + qhi.items:
            P.op("pool", lambda e, t_=t_: e.memset(t_.t[:], 0.0), writes=[t_])
        ktil = C.ring(st, 2, [128, 4, 128], BF16, "ktil")
        kdec = C.ring(st, 2, [128, 512], BF16, "kdec")
        atm = C.ring(st, 4, [128, 128], BF16, "atm")
        S32 = [C.alloc(st, [128, 128], F32, "S32") for _ in range(4)]
        Sbf = [C.alloc(st, [128, 128], BF16, "Sbf") for _ in range(4)]
        osb = C.ring(st, 2, [128, 512], F32, "osb")
        tot = C.ring(st, 2, [128, 512], F32, "tot")
        sqt = C.ring(st, 2, [128, 512], F32, "sqt")
        stat = C.ring(st, 2, [128, 8], F32, "statc")
        onb = C.ring(st, 2, [128, 512], BF16, "onb")
        oTs = C.ring(st, 2, [128, 4, 128], BF16, "oTs")

        for d_ in dirs:
            Mi = M["f_incl"] if d_ == 0 else M["b_incl"]
            Me = M["f_excl"] if d_ == 0 else M["b_excl"]
            for hd in range(4):
                P.op("pool", lambda e, hd=hd: e.memset(S32[hd].t[:], 0.0), writes=[S32[hd]])
                P.op("pool", lambda e, hd=hd: e.memset(Sbf[hd].t[:], 0.0), writes=[Sbf[hd]])
            order = list(range(ntiles)) if d_ == 0 else list(range(ntiles - 1, -1, -1))
            def pro(ti):
                rows = slice(ti * 128, (ti + 1) * 128)
                g_t, kd_t, kT_t, q_t, v_t = gr.next(), kdr.next(), kTr.next(), qTr.next(), vr.next()
                P.dma("sp", g_t.t[:], Sc["g"].t[d_, rows, :], reads=[Sc["g"].reg((d_, ti))], writes=[g_t])
                P.dma("sp", kd_t.t[:], Sc["k"].t[d_, rows, :], reads=[Sc["k"].reg((d_, ti))], writes=[kd_t])
                P.dma("sp", kT_t.t[:], Sc["kT"].t[d_, :, :, rows].rearrange("h p t -> p h t"),
                      reads=[Sc["kT"].reg((d_, c, ti // 4)) for c in range(4)], writes=[kT_t])
                P.dma("sp", q_t.t[:], Sc["hqT"].t[:, :, rows].rearrange("h p t -> p h t"),
                      reads=[Sc["hqT"].reg((c, ti // 4)) for c in range(4)], writes=[q_t])
                P.dma("sp", v_t.t[:], Sc["hi"].t[rows, :], reads=[Sc["hi"].reg(ti)], writes=[v_t])
                mm(C, prx, prx.t[:], Me, Me.t[:], g_t, g_t.t[:], True, True)
                for hd in range(4):
                    mm(C, pbT, pbT.t[:, hd, :], g_t, g_t.t[:, hd * 128:(hd + 1) * 128], Mi, Mi.t[:], True, True)
                eb, enb, er_ = ebT.next(), enbT.next(), er.next()
                act(C, eb, eb.t[:], pbT, pbT.t[:], AF.Exp)
                act(C, enb, enb.t[:], pbT, pbT.t[:], AF.Exp, scale=-1.0)
                act(C, er_, er_.t[:], prx, prx.t[:], AF.Exp)
                qf, ql, qh, kt_, kdc = qfull.next(), qlo.next(), qhi.next(), ktil.next(), kdec.next()
                tt(C, "dve", qf, qf.t[:], q_t, q_t.t[:], eb, eb.t[:], ALU.mult)
                cp(C, "pool", ql, ql.t[:, :, 0:64], qf, qf.t[:, :, 0:64])
                cp(C, "pool", qh, qh.t[:, :, 64:128], qf, qf.t[:, :, 64:128])
                tt(C, "dve", kt_, kt_.t[:], kT_t, kT_t.t[:], enb, enb.t[:], ALU.mult)
                tt(C, "pool", kdc, kdc.t[:], kd_t, kd_t.t[:], er_, er_.t[:], ALU.mult)
                return dict(kd_t=kd_t, v_t=v_t, eb=eb, qf=qf, ql=ql, qh=qh, kt_=kt_, kdc=kdc)

            def tile_body(ti, B_):
                rows = slice(ti * 128, (ti + 1) * 128)
                kd_t, v_t, eb, qf, ql, qh, kt_, kdc = (B_[k_] for k_ in ('kd_t', 'v_t', 'eb', 'qf', 'ql', 'qh', 'kt_', 'kdc'))
                if d_ == 0:
                    ca, cb, qa, qb, la, lb_ = 0, 1, ql, qh, 63, 127
                else:
                    ca, cb, qa, qb, la, lb_ = 1, 0, qh, ql, 64, 0
                ra = slice(ca * 64, (ca + 1) * 64)
                rb = slice(cb * 64, (cb + 1) * 64)
                def head_chain(hd):
                    hc = slice(hd * 128, (hd + 1) * 128)
                    X, O_ = pX[hd % 2], pOs[hd % 2]
                    oa = O_.t[:, hd // 2, :]
                    mm(C, X, X.t[:, 0, :], kt_, kt_.t[:, hd, :], qf, qf.t[:, hd, :], True, True)
                    mm(C, O_, oa, qa, qa.t[:, hd, :], Sbf[hd], Sbf[hd].t[:], True, False)
                    mm(C, X, X.t[:, 1, :], kdc, kdc.t[ra, hc], v_t, v_t.t[ra, hc], True, True)
                    yield
                    am = atm.next()
                    tt(C, "dve", am, am.t[:], X, X.t[:, 0, :], Mi, Mi.t[:], ALU.mult)
                    stt(C, "dve", S32[hd], S32[hd].t[:], S32[hd], S32[hd].t[:], eb.t[:, hd, la:la + 1],
                        X, X.t[:, 1, :], ALU.mult, ALU.add, reads=[eb])
                    yield
                    cp(C, "act", Sbf[hd], Sbf[hd].t[:], S32[hd], S32[hd].t[:])
                    yield
                    mm(C, O_, oa, qb, qb.t[:, hd, :], Sbf[hd], Sbf[hd].t[:], False, False)
                    mm(C, O_, oa, am, am.t[:], v_t, v_t.t[:, hc], False, True)
                    mm(C, X, X.t[:, 2, :], kdc, kdc.t[rb, hc], v_t, v_t.t[rb, hc], True, True)
                    yield
                    stt(C, "dve", S32[hd], S32[hd].t[:], S32[hd], S32[hd].t[:], eb.t[:, hd, lb_:lb_ + 1],
                        X, X.t[:, 2, :], ALU.mult, ALU.add, reads=[eb])
                    yield
                    cp(C, "act", Sbf[hd], Sbf[hd].t[:], S32[hd], S32[hd].t[:])
                    yield

                for pair in ((0, 1), (2, 3)):
                    gens = [head_chain(hd) for hd in pair]
                    while gens:
                        for g_ in list(gens):
                            try:
                                next(g_)
                            except StopIteration:
                                gens.remove(g_)

                def ov(tile_ap, s_):
                    return tile_ap.rearrange("p (a s d) -> p a s d", s=2, d=128)[:, :, s_, :]

                if d_ == 0 and len(dirs) == 2:
                    o_ = osb.next()
                    for s_ in range(2):
                        cp(C, "act", o_, ov(o_.t[:], s_), pOs[s_], pOs[s_].t[:, 0:2, :])
                    P.dma("pool", Sc["ofwd"].t[rows, :], o_.t[:], reads=[o_], writes=[Sc["ofwd"].reg(ti)])
                    return
                t_ = tot.next()
                if len(dirs) == 2:
                    of_ = ofr.next()
                    P.dma("sp", of_.t[:], Sc["ofwd"].t[rows, :], reads=[Sc["ofwd"].reg(ti)], writes=[of_])
                    for s_ in range(2):
                        tt(C, "dve", t_, ov(t_.t[:], s_), pOs[s_], pOs[s_].t[:, 0:2, :], of_, ov(of_.t[:], s_), ALU.add)
                else:
                    for s_ in range(2):
                        cp(C, "dve", t_, ov(t_.t[:], s_), pOs[s_], pOs[s_].t[:, 0:2, :])
                if "dbgo" in Sc:
                    P.dma("pool", Sc["dbgo"].t[rows, :], t_.t[:], reads=[t_], writes=[Sc["dbgo"].reg(ti)])
                sg_ = sgr.next()
                P.dma("sp", sg_.t[:], Sc["sg"].t[rows, :], reads=[Sc["sg"].reg(ti)], writes=[sg_])
                sq = sqt.next()
                act(C, sq, sq.t[:], t_, t_.t[:], AF.Square)
                s4 = stat.next()
                P.op("dve", lambda e, s4=s4, sq=sq: e.tensor_reduce(out=s4.t[:, 0:4], in_=sq.t[:].rearrange("p (h d) -> p h d", d=128),
                                                              axis=AX.X, op=ALU.add), reads=[sq], writes=[s4])
                rsqrt_mean(C, s4, lambda s4=s4: s4.t[:, 0:4], 4, 1.0 / 128)
                t3 = t_.t[:].rearrange("p (h d) -> p h d", d=128)
                tt(C, "dve", t_, t3, t_, t3, s4, s4.t[:, 0:4].unsqueeze(2).to_broadcast([128, 4, 128]), ALU.mult)
                tt(C, "pool", t_, t3, t_, t3, gon, gon.t[:], ALU.mult)
                ob = onb.next()
                tt(C, "dve", ob, ob.t[:], t_, t_.t[:], sg_, sg_.t[:], ALU.mult)
                for hd in range(4):
                    tr(C, pTr, pTr.t[:, hd, :], ob, ob.t[:, hd * 128:(hd + 1) * 128], K["ident"])
                os_ = oTs.next()
                cp(C, "act", os_, os_.t[:], pTr, pTr.t[:, 0:4, :])
                P.dma("pool", Sc["hgoT"].t[:, :, rows].rearrange("h p t -> p h t"), os_.t[:], reads=[os_], writes=[Sc["hgoT"].reg(ti)])

            pend = pro(order[0])
            for idx_, ti in enumerate(order):
                nxt = pro(order[idx_ + 1]) if idx_ + 1 < len(order) else None
                tile_body(ti, pend)
                pend = nxt


def load_w(C, st, name, kchunks, ncols, q="pool"):
    w = C.alloc(st, [128, kchunks, ncols], BF16, name)
    src = C.ins[name].t.rearrange("(k p) n -> p k n", p=128)
    step = min(kchunks, max(1, 4096 // ncols))
    for k0 in range(0, kchunks, step):
        C.P.dma(q, w.t[:, k0:k0 + step, :], src[:, k0:k0 + step, :], reads=[C.ins[name]], writes=[w])
    return w


def norm_transpose(C, K, xt, gain, stat, junk, hb, pt, dst, dst_ap):
    s_ = stat
    act(C, junk, junk.t[:], xt, xt.t[:], AF.Square, accum_out=s_.t[:, 0:1], extra_w=[s_])
    rsqrt_mean(C, s_, lambda: s_.t[:, 0:1], 1, 1.0 / D)
    stt(C, "dve", hb, hb.t[:], xt, xt.t[:], s_.t[:, 0:1], gain, gain.t[:], ALU.mult, ALU.mult, reads=[s_])
    for k in range(8):
        tr(C, pt, pt.t[:, k, :], hb, hb.t[:, k * 128:(k + 1) * 128], K["ident"])
    cp(C, "act", dst, dst_ap, pt, pt.t[:])


def load_d_weights(C, st):
    wua = load_w(C, st, "w_up_att", 4, 1024)
    wuh = load_w(C, st, "w_up_hg", 4, 1024)
    wo = load_w(C, st, "w_out", 8, 1024)
    gffn = C.alloc(st, [128, 1024], F32, "gffn")
    C.P.dma("sp", gffn.t[:], C.ins["norm_ffn"].t.to_broadcast([128, 1024]), writes=[gffn])
    return wua, wuh, wo, gffn


def phase_d(C, K, ngroups=8, pre=None):
    nc, P = C.nc, C.P
    Sc = C.scr
    I = C.ins
    with contextlib.ExitStack() as st:
        wua, wuh, wo, gffn = pre if pre is not None else load_d_weights(C, st)
        aTr = C.ring(st, 2, [128, 4, 512], BF16, "aT")
        hTr_ = C.ring(st, 2, [128, 4, 512], BF16, "hgT")
        gtr = C.ring(st, 2, [128, 16, 512], BF16, "gts")
        pya = C.ring(st, 2, [128, 512], F32, "pya", psum=True)
        pyh = C.ring(st, 2, [128, 512], F32, "pyh", psum=True)
        px = C.ring(st, 2, [128, 512], F32, "px", psum=True)
        pt = C.ring(st, 2, [128, 8, 128], BF16, "ptd", psum=True)
        t1r = C.ring(st, 2, [128, 512], F32, "t1")
        t2r = C.ring(st, 2, [128, 512], F32, "t2")
        mTr = C.ring(st, 2, [128, 8, 512], BF16, "mT")
        xr = C.ring(st, 2, [128, 1024], F32, "xtd")
        x1r = C.ring(st, 3, [128, 1024], F32, "x1t")
        junk = C.alloc(st, [128, 1024], BF16, "junkd")
        stat = C.ring(st, 2, [128, 8], F32, "statd")
        hbr = C.ring(st, 2, [128, 1024], BF16, "hbd")
        h2s = C.ring(st, 2, [128, 8, 128], BF16, "h2s")
        for g in range(ngroups):
            cols = slice(g * 512, (g + 1) * 512)
            aT, hT, gt = aTr.next(), hTr_.next(), gtr.next()
            P.dma("sp", aT.t[:], Sc["attoT"].t[:, :, cols].rearrange("r p t -> p r t"),
                  reads=[Sc["attoT"].reg((pr, g)) for pr in range(4)], writes=[aT])
            P.dma("sp", hT.t[:], Sc["hgoT"].t[:, :, cols].rearrange("r p t -> p r t"),
                  reads=Sc["hgoT"].regl(range(4 * g, 4 * g + 4)), writes=[hT])
            P.dma("sp", gt.t[:], Sc["gtsT"].t[:, :, cols].rearrange("r p t -> p r t"),
                  reads=[Sc["gtsT"].reg((c, g)) for c in range(16)], writes=[gt])
            mT = mTr.next()
            for m_ in range(8):
                ms = slice(m_ * 128, (m_ + 1) * 128)
                ya, yh = pya.next(), pyh.next()
                for kc in range(4):
                    mm(C, ya, ya.t[:], wua, wua.t[:, kc, ms], aT, aT.t[:, kc, :], kc == 0, kc == 3)
                for kc in range(4):
                    mm(C, yh, yh.t[:], wuh, wuh.t[:, kc, ms], hT, hT.t[:, kc, :], kc == 0, kc == 3)
                t1, t2 = t1r.next(), t2r.next()
                tt(C, "dve", t1, t1.t[:], ya, ya.t[:], gt, gt.t[:, m_, :], ALU.mult)
                tt(C, "dve", t2, t2.t[:], yh, yh.t[:], gt, gt.t[:, 8 + m_, :], ALU.mult)
                tt(C, "pool", mT, mT.t[:, m_, :], t1, t1.t[:], t2, t2.t[:], ALU.add)
            def part1(j):
                ti = g * 4 + j
                rows = slice(ti * 128, (ti + 1) * 128)
                xt = xr.next()
                P.dma("sp", xt.t[:], I["x"].t[rows, :], reads=[I["x"]], writes=[xt])
                x1 = x1r.next()
                for hf in range(2):
                    hs = slice(hf * 512, (hf + 1) * 512)
                    p_ = px.next()
                    for m_ in range(8):
                        mm(C, p_, p_.t[:], mT, mT.t[:, m_, j * 128:(j + 1) * 128], wo, wo.t[:, m_, hs], m_ == 0, m_ == 7)
                    tt(C, "dve", x1, x1.t[:, hs], p_, p_.t[:], xt, xt.t[:, hs], ALU.add)
                P.dma("pool", Sc["x1"].t[rows, :], x1.t[:], reads=[x1], writes=[Sc["x1"].reg(ti)])
                return x1

            def part2(j, x1):
                ti = g * 4 + j
                hs_ = h2s.next()
                norm_transpose(C, K, x1, gffn, stat.next(), junk, hbr.next(), pt.next(), hs_, hs_.t[:])
                P.dma("pool", Sc["h2T"].t[ti], hs_.t[:], reads=[hs_], writes=[Sc["h2T"].reg(ti)])

            pend = part1(0)
            for j in range(4):
                nxt = part1(j + 1) if j + 1 < 4 else None
                part2(j, pend)
                pend = nxt


def phase_e1(C, K, ngroups=16):
    nc, P = C.nc, C.P
    Sc = C.scr
    I = C.ins
    with contextlib.ExitStack() as st:
        wq = load_w(C, st, "peer_wq", 8, 2048)
        skT = C.alloc(st, [128, 16, 128], BF16, "skT")
        P.dma("pool", skT.t[:], I["skT"].t, reads=[I["skT"]], writes=[skT])
        io_f = C.alloc(st, [128, 128], F32, "io_f")
        P.op("pool", lambda e: e.iota(io_f.t[:], pattern=[[1, 128]], base=0, channel_multiplier=0,
                                      allow_small_or_imprecise_dtypes=True), writes=[io_f])
        io_b = C.alloc(st, [128, 128], BF16, "io_b")
        cp(C, "dve", io_b, io_b.t[:], io_f, io_f.t[:])
        io_rep = C.alloc(st, [128, 128, 16], BF16, "io_rep")
        cp(C, "dve", io_rep, io_rep.t[:], io_f, io_f.t[:].unsqueeze(2).to_broadcast([128, 128, 16]))
        h2r = C.ring(st, 2, [128, 8, 128], BF16, "h2e")
        pq = C.ring(st, 2, [128, 4, 128], F32, "pq", psum=True)
        psc = C.ring(st, 2, [128, 4, 128], F32, "psc", psum=True)
        pIG = C.alloc(st, [128, 8, 128], BF16, "pIG", psum=True)
        pG = C.ring(st, 3, [128, 4, 128], F32, "pG", psum=True)
        qpT = C.ring(st, 2, [128, 16, 128], BF16, "qpT")
        s_all = C.ring(st, 2, [128, 16, 128], F32, "s_all")
        tmp128 = C.ring(st, 4, [128, 128], F32, "tmp128")
        v16 = C.ring(st, 2, [128, 16, 16], F32, "v16")
        i16 = C.ring(st, 2, [128, 16, 16], U32, "i16")
        i16f = C.ring(st, 2, [128, 16, 16], F32, "i16f")
        cand = C.ring(st, 1, [128, 8, 256], F32, "cand")
        tmp256 = C.ring(st, 4, [128, 256], F32, "tmp256")
        tsv = C.ring(st, 2, [128, 8, 16], F32, "tsv")
        pos = C.ring(st, 2, [128, 8, 16], U32, "pos")
        k12i = C.ring(st, 2, [128, 2, 128], I32, "k12i")
        k12f = C.ring(st, 2, [128, 2, 128], F32, "k12f")
        eq = C.ring(st, 2, [128, 128, 16], F32, "eq")
        IG = C.ring(st, 2, [128, 3, 128], BF16, "IG")
        IGf = C.ring(st, 2, [128, 3, 128], F32, "IGf")
        IGT = C.ring(st, 2, [128, 3, 128], BF16, "IGT")
        ex = C.ring(st, 2, [128, 8, 16], F32, "ex")
        st8 = C.ring(st, 2, [128, 8], F32, "st8")
        A4 = C.ring(st, 3, [128, 16, 128], BF16, "A4")
        B4 = C.ring(st, 3, [128, 16, 128], BF16, "B4")
        Gst = C.ring(st, 1, [128, 128, 256], BF16, "Gst")
        est = {}

        def stageXc(grp, j2):
            ti = grp * 2 + j2
            h2 = h2r.next()
            P.dma("sp", h2.t[:], Sc["h2T"].t[ti], reads=[Sc["h2T"].reg(ti)], writes=[h2])
            qp, sa = qpT.next(), s_all.next()
            for c4 in range(4):
                p_ = pq.next()
                for cc in range(4):
                    cq = c4 * 4 + cc
                    for k in range(8):
                        mm(C, p_, p_.t[:, cc, :], wq, wq.t[:, k, cq * 128:(cq + 1) * 128], h2, h2.t[:, k, :], k == 0, k == 7)
                cp(C, "act", qp, qp.t[:, c4 * 4:(c4 + 1) * 4, :], p_, p_.t[:])
            for c4 in range(4):
                p_ = psc.next()
                for cc in range(4):
                    cq = c4 * 4 + cc
                    mm(C, p_, p_.t[:, cc, :], qp, qp.t[:, cq, :], skT, skT.t[:, cq, :], True, True)
                cp(C, "act", sa, sa.t[:, c4 * 4:(c4 + 1) * 4, :], p_, p_.t[:])
            if "dbgs" in Sc:
                P.dma("pool", Sc["dbgs"].t[ti], sa.t[:], reads=[sa], writes=[Sc["dbgs"].reg(ti)])
            est["sa", grp, j2] = sa

        def stageXt(grp, j2):
            ti = grp * 2 + j2
            sa = est.pop(("sa", grp, j2))
            v_, i_ = v16.next(), i16.next()

            def top16(src_t, src_ap, vdst_t, vdst_ap, idst_t, idst_ap, tmp):
                P.op("dve", lambda e: e.max(out=vdst_ap[:, 0:8], in_=src_ap), reads=[src_t], writes=[vdst_t])
                yield
                P.op("dve", lambda e: e.match_replace(out=tmp.t[:], in_to_replace=vdst_ap[:, 0:8], in_values=src_ap,
                                                      imm_value=-1e30), reads=[src_t, vdst_t], writes=[tmp])
                yield
                P.op("dve", lambda e: e.max(out=vdst_ap[:, 8:16], in_=tmp.t[:]), reads=[tmp, vdst_t], writes=[vdst_t])
                yield
                P.op("dve", lambda e: e.max_index(out=idst_ap[:, 0:8], in_max=vdst_ap[:, 0:8], in_values=src_ap),
                     reads=[src_t, vdst_t], writes=[idst_t])
                yield
                P.op("dve", lambda e: e.max_index(out=idst_ap[:, 8:16], in_max=vdst_ap[:, 8:16], in_values=src_ap),
                     reads=[src_t, vdst_t, idst_t], writes=[idst_t])
                yield

            def rr4(gens):
                gens = list(gens)
                while gens:
                    for g_ in list(gens):
                        try:
                            next(g_)
                        except StopIteration:
                            gens.remove(g_)

            for c0 in range(0, 16, 4):
                rr4([top16(sa, sa.t[:, cq, :], v_.reg(cq), v_.t[:, cq, :], i_.reg(cq), i_.t[:, cq, :], tmp128.next())
                     for cq in range(c0, c0 + 4)])
            if_ = i16f.next()
            cp(C, "dve", if_, if_.t[:], i_.regl(range(16)), i_.t[:])
            cd = cand.next()
            vv = v_.t[:].rearrange("p (h a) k -> p h a k", a=2)
            tt(C, "dve", cd, cd.t[:].rearrange("p h (a b) -> p h a b", b=16),
               v_.regl(range(16)), vv[:, :, 0, :].unsqueeze(3).to_broadcast([128, 8, 16, 16]),
               v_.regl(range(16)), vv[:, :, 1, :].unsqueeze(2).to_broadcast([128, 8, 16, 16]), ALU.add)
            ts_, ps_ = tsv.next(), pos.next()
            for h0 in range(0, 8, 4):
                rr4([top16(cd, cd.t[:, h, :], ts_.reg(h), ts_.t[:, h, :], ps_.reg(h), ps_.t[:, h, :], tmp256.next())
                     for h in range(h0, h0 + 4)])
            ki, kf = k12i.next(), k12f.next()
            posf = ps_.t[:].rearrange("p h k -> p (h k)").bitcast(I32)
            P.op("dve", lambda e, ki=ki, posf=posf: e.tensor_single_scalar(out=ki.t[:, 0, :], in_=posf, scalar=4, op=ALU.arith_shift_right),
                 reads=ps_.regl(range(8)), writes=[ki])
            P.op("dve", lambda e, ki=ki, posf=posf: e.tensor_single_scalar(out=ki.t[:, 1, :], in_=posf, scalar=15, op=ALU.bitwise_and),
                 reads=ps_.regl(range(8)) + [ki], writes=[ki])
            cp(C, "dve", kf, kf.t[:], ki, ki.t[:])
            ig = IGf.next()
            iv = if_.t[:].rearrange("p (h a) k -> p h a k", a=2)
            for a in range(2):
                e_ = eq.next()
                tt(C, "dve", e_, e_.t[:], kf, kf.t[:, a, :].unsqueeze(2).to_broadcast([128, 128, 16]),
                   io_f, io_f.t[:, 0:16].unsqueeze(1).to_broadcast([128, 128, 16]), ALU.is_equal)
                e4 = e_.t[:].rearrange("p (h k) c -> p h k c", h=8)
                tt(C, "dve", e_, e4, e_, e4, if_, iv[:, :, a, :].unsqueeze(2).to_broadcast([128, 8, 16, 16]), ALU.mult)
                P.op("dve", lambda e, e_=e_, ig=ig, a=a: e.tensor_reduce(out=ig.t[:, a, :], in_=e_.t[:], axis=AX.X, op=ALU.add),
                     reads=[e_], writes=[ig])
            x_ = ex.next()
            tt(C, "dve", x_, x_.t[:], ts_.regl(range(8)), ts_.t[:], ts_.regl(range(8)), ts_.t[:, :, 0:1].to_broadcast([128, 8, 16]), ALU.subtract)
            act(C, x_, x_.t[:], x_, x_.t[:], AF.Exp)
            s8 = st8.next()
            P.op("dve", lambda e, s8=s8, x_=x_: e.tensor_reduce(out=s8.t[:], in_=x_.t[:], axis=AX.X, op=ALU.add), reads=[x_], writes=[s8])
            P.op("dve", lambda e, s8=s8: e.reciprocal(out=s8.t[:], in_=s8.t[:]), reads=[s8], writes=[s8])
            tt(C, "dve", ig, ig.t[:, 2, :].rearrange("p (h k) -> p h k", h=8), x_, x_.t[:],
               s8, s8.t[:].unsqueeze(2).to_broadcast([128, 8, 16]), ALU.mult)
            igf_ = ig
            ig = IG.next()
            cp(C, "dve", ig, ig.t[:], igf_, igf_.t[:])
            if "dbgig" in Sc:
                P.dma("pool", Sc["dbgig"].t[ti], ig.t[:], reads=[ig], writes=[Sc["dbgig"].reg(ti)])
            for a in range(3):
                tr(C, pIG, pIG.t[:, a, :], ig, ig.t[:, a, :], K["ident"])
            igt = IGT.next()
            cp(C, "dve", igt, igt.t[:], pIG, pIG.t[:, 0:3, :])
            est["igt", grp, j2] = igt

        def stageY(grp, j2):
            if j2 == 0:
                est["G", grp] = Gst.next()
            G_ = est["G", grp]
            igt = est.pop(("igt", grp, j2))
            TB = 16
            for b16 in range(128 // TB):
                a4, bb4 = A4.next(), B4.next()
                tsl = slice(b16 * TB, (b16 + 1) * TB)
                av = a4.t[:].rearrange("p t i -> p (t i)").rearrange("p (i t) -> p i t", t=TB)
                bv = bb4.t[:].rearrange("p t i -> p (t i)").rearrange("p (i t) -> p i t", t=TB)
                tt(C, "dve", a4, av, io_rep, io_rep.t[:], igt, igt.t[:, 0, tsl].unsqueeze(1).to_broadcast([128, 128, TB]), ALU.is_equal)
                tt(C, "dve", bb4, bv, io_rep, io_rep.t[:], igt, igt.t[:, 1, tsl].unsqueeze(1).to_broadcast([128, 128, TB]), ALU.is_equal)
                tt(C, "pool", a4, av, a4, av, igt, igt.t[:, 2, tsl].unsqueeze(1).to_broadcast([128, 128, TB]), ALU.mult)
                for q4 in range(TB // 4):
                    pg = pG.next()
                    for q_ in range(4):
                        mm(C, pg, pg.t[:, q_, :], a4, av[:, :, q4 * 4 + q_], bb4, bv[:, :, q4 * 4 + q_], True, True)
                    t0 = j2 * 128 + b16 * TB + q4 * 4
                    cp(C, "act", G_, G_.t[:, :, t0:t0 + 4].rearrange("p i t -> p t i"), pg, pg.t[:])
            if j2 == 1:
                hc = slice((grp % 2) * 256, (grp % 2 + 1) * 256)
                for i0 in range(0, 128, 32):
                    P.dma("pool", Sc["G"].t[grp // 2, :, i0:i0 + 32, hc], G_.t[:, i0:i0 + 32, :], reads=[G_], writes=[Sc["G"].reg(grp)])

        tl = [(grp, j2) for grp in range(ngroups) for j2 in range(2)]
        for it in range(len(tl) + 2):
            if it < len(tl):
                stageXc(*tl[it])
            if 1 <= it < len(tl) + 1:
                stageXt(*tl[it - 1])
            if it >= 2:
                stageY(*tl[it - 2])


def phase_e0(C, K):
    P = C.P
    jobs = []
    for i2 in range(128):
        jobs.append(lambda i2=i2: P.dma("pool", C.scr["uTb"].t[i2], C.ins["uT"].t[i2], reads=[C.ins["uT"]], writes=[C.scr["uTb"].reg(i2)]))
        jobs.append(lambda i2=i2: P.dma("pool", C.scr["vLb"].t[i2], C.ins["vL"].t[i2], reads=[C.ins["vL"]], writes=[C.scr["vLb"].reg(i2)]))
    return jobs


def phase_e2(C, K, ngroups=8, ni2=128):
    nc, P = C.nc, C.P
    Sc = C.scr
    with contextlib.ExitStack() as st:
        h2r = C.ring(st, 1, [128, 8, 512], BF16, "h2g")
        po = [C.alloc(st, [128, 512], F32, "po", psum=True) for _ in range(4)]
        par = C.ring(st, 4, [128, 512], F32, "pa", psum=True)
        uch = C.ring(st, 3, [128, 2, 8, 128], BF16, "uch")
        vch = C.ring(st, 4, [128, 2, 512], BF16, "vch")
        gch = C.ring(st, 3, [128, 2, 512], BF16, "gch")
        sqr = C.ring(st, 3, [128, 512], F32, "sqe")
        t2r = C.ring(st, 3, [128, 512], F32, "t2e")
        sgr = C.ring(st, 3, [128, 512], BF16, "sge")
        agr = C.ring(st, 3, [128, 512], BF16, "age")
        Wall = C.alloc(st, [128, ni2, 512], BF16, "Wall")
        xs = C.ring(st, 2, [128, 512], F32, "xs")
        x1h = C.ring(st, 2, [128, 512], F32, "x1h")
        LAG = 3
        state = {}

        def s1(grp, i2):
            h2 = state["h2"]
            if i2 % 2 == 0:
                u_, v_, g_ = uch.next(), vch.next(), gch.next()
                P.dma("sp", u_.t[:], Sc["uTb"].t[i2:i2 + 2].rearrange("i p k c -> p i k c"),
                      reads=Sc["uTb"].regl([i2, i2 + 1]), writes=[u_])
                P.dma("sp", v_.t[:], Sc["vLb"].t[i2:i2 + 2, :, 0:512].rearrange("i p d -> p i d"),
                      reads=Sc["vLb"].regl([i2, i2 + 1]), writes=[v_])
                P.dma("sp", g_.t[:], Sc["G"].t[grp, :, i2:i2 + 2, :], reads=Sc["G"].regl([2 * grp, 2 * grp + 1]), writes=[g_])
                state["uvg"] = (u_, v_, g_)
            u_, v_, g_ = state["uvg"]
            e_ = i2 % 2
            pa = par.next()
            for k in range(8):
                mm(C, pa, pa.t[:], u_, u_.t[:, e_, k, :], h2, h2.t[:, k, :], k == 0, k == 7)
            sq, t2, sg, ag = sqr.next(), t2r.next(), sgr.next(), agr.next()
            Wb = Wall.reg(i2)
            act(C, sq, sq.t[:], pa, pa.t[:], AF.Square, scale=0.21145921592448583)
            stt(C, "dve", t2, t2.t[:], sq, sq.t[:], 1.0, pa, pa.t[:], ALU.add, ALU.mult)
            tt(C, "dve", ag, ag.t[:], pa, pa.t[:], g_, g_.t[:, e_, :], ALU.mult)
            act(C, sg, sg.t[:], t2, t2.t[:], AF.Sigmoid, scale=1.5957691216057308)
            P.op("pool", lambda e: e.tensor_tensor(out=Wall.t[:, i2, :], in0=sg.t[:], in1=ag.t[:], op=ALU.mult),
                 reads=[sg, ag], writes=[Wb])
            state["v", i2] = (v_, e_)

        def s2(grp, i2, half):
            v_, e_ = state.pop(("v", i2)) if half == 0 else state.pop(("v2", i2))
            for j in range(4):
                P.op("pe", lambda e, j=j: e.matmul(po[j].t[:], lhsT=Wall.t[:, i2, j * 128:(j + 1) * 128], rhs=v_.t[:, e_, :],
                                                    start=(i2 == 0), stop=(i2 == ni2 - 1)),
                     reads=[Wall.reg(i2), v_], writes=[po[j]], pe_accum=True)

        def evac(grp, half):
            hs = slice(half * 512, (half + 1) * 512)
            for j in range(4):
                ti = grp * 4 + j
                rows = slice(ti * 128, (ti + 1) * 128)
                x1_ = x1h.next()
                P.dma("sp", x1_.t[:], Sc["x1"].t[rows, hs], reads=[Sc["x1"].reg(ti)], writes=[x1_])
                x_ = xs.next()
                tt(C, "dve", x_, x_.t[:], po[j], po[j].t[:], x1_, x1_.t[:], ALU.add)
                P.dma("pool", Sc["x2"].t[rows, hs], x_.t[:], reads=[x_], writes=[Sc["x2"].reg((ti, half))])

        for grp in range(ngroups):
            h2 = h2r.next()
            for j in range(4):
                ti = grp * 4 + j
                P.dma("sp", h2.t[:, :, j * 128:(j + 1) * 128], Sc["h2T"].t[ti], reads=[Sc["h2T"].reg(ti)], writes=[h2])
            state["h2"] = h2
            for it in range(ni2 + LAG):
                if it < ni2:
                    s1(grp, it)
                if it >= LAG:
                    s2(grp, it - LAG, 0)
            evac(grp, 0)
            for it in range(ni2 + LAG):
                if it < ni2:
                    if it % 2 == 0:
                        v_ = vch.next()
                        P.dma("sp", v_.t[:], Sc["vLb"].t[it:it + 2, :, 512:1024].rearrange("i p d -> p i d"),
                              reads=Sc["vLb"].regl([it, it + 1]), writes=[v_])
                        state["vp"] = v_
                    state["v2", it] = (state["vp"], it % 2)
                if it >= LAG:
                    s2(grp, it - LAG, 1)
            evac(grp, 1)


def phase_f(C, K, ntiles=NT):
    nc, P = C.nc, C.P
    Sc = C.scr
    I = C.ins
    with contextlib.ExitStack() as st:
        wg = load_w(C, st, "ple_gate", 8, 1024)
        wp = load_w(C, st, "ple_proj", 2, 1024)
        gple = C.alloc(st, [128, 1024], F32, "gple")
        P.dma("sp", gple.t[:], I["norm_ple"].t.to_broadcast([128, 1024]), writes=[gple])
        x2r = C.ring(st, 3, [128, 1024], F32, "x2f")
        pr_ = C.ring(st, 2, [128, 256], F32, "pf32")
        pbr = C.ring(st, 2, [128, 256], BF16, "pbf")
        junk = C.alloc(st, [128, 1024], BF16, "junkf")
        stat = C.ring(st, 2, [128, 8], F32, "statf")
        hbr = C.ring(st, 2, [128, 1024], BF16, "hbf")
        pt = C.ring(st, 2, [128, 8, 128], BF16, "ptf", psum=True)
        ptp = C.alloc(st, [128, 8, 128], BF16, "ptp", psum=True)
        h3r = C.ring(st, 3, [128, 8, 128], BF16, "h3T")
        pTr = C.ring(st, 3, [128, 2, 128], BF16, "pT")
        pgr = C.ring(st, 2, [128, 512], F32, "pgate", psum=True)
        ppr = C.ring(st, 2, [128, 512], F32, "pproj", psum=True)
        sgr = C.ring(st, 2, [128, 512], F32, "sgf")
        tr_ = C.ring(st, 2, [128, 512], F32, "tf")
        outr = C.ring(st, 2, [128, 1024], F32, "outf")
        def pro(ti):
            rows = slice(ti * 128, (ti + 1) * 128)
            x2 = x2r.next()
            P.dma("sp", x2.t[:], Sc["x2"].t[rows, :], reads=[Sc["x2"].reg((ti, 0)), Sc["x2"].reg((ti, 1))], writes=[x2])
            pf = pr_.next()
            P.dma("sp", pf.t[:], I["p"].t[rows, :], reads=[I["p"]], writes=[pf])
            pb = pbr.next()
            cp(C, "pool", pb, pb.t[:], pf, pf.t[:])
            for k in range(2):
                tr(C, ptp, ptp.t[:, k, :], pb, pb.t[:, k * 128:(k + 1) * 128], K["ident"])
            pT = pTr.next()
            cp(C, "act", pT, pT.t[:], ptp, ptp.t[:, 0:2, :])
            h3 = h3r.next()
            norm_transpose(C, K, x2, gple, stat.next(), junk, hbr.next(), pt.next(), h3, h3.t[:])
            return x2, pT, h3

        def body(ti, x2, pT, h3):
            rows = slice(ti * 128, (ti + 1) * 128)
            o_ = outr.next()
            for hf in range(2):
                hs = slice(hf * 512, (hf + 1) * 512)
                pg, pp = pgr.next(), ppr.next()
                for k in range(8):
                    mm(C, pg, pg.t[:], h3, h3.t[:, k, :], wg, wg.t[:, k, hs], k == 0, k == 7)
                for k in range(2):
                    mm(C, pp, pp.t[:], pT, pT.t[:, k, :], wp, wp.t[:, k, hs], k == 0, k == 1)
                sg, t_ = sgr.next(), tr_.next()
                act(C, sg, sg.t[:], pg, pg.t[:], AF.Sigmoid)
                tt(C, "dve", t_, t_.t[:], pp, pp.t[:], sg, sg.t[:], ALU.mult)
                tt(C, "pool", o_, o_.t[:, hs], t_, t_.t[:], x2, x2.t[:, hs], ALU.add)
            P.dma("sp", C.y.t[rows, :], o_.t[:], reads=[o_], writes=[C.y.reg(ti)])

        pend = pro(0)
        for ti in range(ntiles):
            nxt = pro(ti + 1) if ti + 1 < ntiles else None
            body(ti, *pend)
            pend = nxt


def declare(C):
    C.din("x", [S, D])
    C.din("p", [S, 256])
    C.din("norm_mix", [1, D])
    C.din("w_in", [D, 5376])
    C.din("q_norm", [1, 64])
    C.din("k_norm", [1, 64])
    C.din("hg_lb_raw", [2, 2, 512])
    C.din("hg_out_norm", [1, 128])
    C.din("w_up_att", [512, D])
    C.din("w_up_hg", [512, D])
    C.din("w_out", [D, D])
    C.din("norm_ffn", [1, D])
    C.din("peer_wq", [D, 2048])
    C.din("skT", [128, 16, 128])
    C.din("uT", [128, 128, 8, 128])
    C.din("vL", [128, 128, D])
    C.din("norm_ple", [1, D])
    C.din("ple_gate", [D, D])
    C.din("ple_proj", [256, D])
    sc = C.scratch
    sc("hqT", [4, 128, S], BF16)
    sc("kT", [2, 4, 128, S], BF16)
    sc("gtsT", [16, 128, S], BF16)
    sc("qT", [4, 128, S], BF16)
    sc("kTa", [2, 128, S], BF16)
    sc("v", [NT, 128, 2, 128], BF16)
    sc("g", [2, S, 512], F32)
    sc("k", [2, S, 512], BF16)
    sc("hi", [S, 512], BF16)
    sc("sg", [S, 512], BF16)
    sc("attoT", [4, 128, S], BF16)
    sc("ofwd", [S, 512], F32)
    sc("uTb", [128, 128, 8, 128], BF16)
    sc("vLb", [128, 128, D], BF16)
    sc("x2", [S, D], F32)
    sc("G", [8, 128, 128, 512], BF16)
    if "dbgs" in C.dbg:
        sc("dbgs", [NT, 128, 16, 128], F32)
        sc("dbgig", [NT, 128, 3, 128], BF16)
    sc("x1", [S, D], F32)
    sc("h2T", [NT, 128, 8, 128], BF16)
    sc("hgoT", [4, 128, S], BF16)
    if "dbgo" in C.dbg:
        sc("dbgo", [S, 512], F32)
    if "dbgacc" in C.dbg:
        sc("dbgacc", [128, 512], F32)
        sc("dbgp", [2, 128, 512], BF16)
        sc("dbgvs", [128, NT, 256], BF16)


def build(dbg=(), phases="A", ntiles=NT, **kw):
    nc = bass.Bass("TRN2", target_bir_lowering=False)
    C = Ctx(nc, dbg)
    declare(C)
    C.y = T(nc.dram_tensor("y", [S, D], F32, kind="ExternalOutput").ap(), "y")
    C.outs.append(C.y)
    with contextlib.ExitStack() as st:
        K = build_consts(C, st)
        if "A" in phases:
            phase_a(C, K, ntiles)
        if "C" in phases:
            C.P.barrier()
            phase_c(C, K, kw.get("c_tiles", NT), kw.get("c_dirs", (0, 1)))
        C.P.barrier()
        bg = phase_e0(C, K) if "2" in phases else []
        with contextlib.ExitStack() as st_bd:
            dW = load_d_weights(C, st_bd) if ("D" in phases and "B" in phases) else None
            if "B" in phases:
                phase_b(C, K, kw.get("b_groups", 8), kw.get("hhs", (0, 1)), bg)
            else:
                for job in bg:
                    job()
            if "D" in phases:
                C.P.barrier()
                phase_d(C, K, kw.get("d_groups", 8), dW)
        if "E" in phases:
            C.P.barrier()
            phase_e1(C, K, kw.get("e1_groups", 16))
        if "2" in phases:
            C.P.barrier()
            phase_e2(C, K, kw.get("e2_groups", 8), kw.get("ni2", 128))
        if "F" in phases:
            C.P.barrier()
            phase_f(C, K, kw.get("f_tiles", NT))
        fin = []
        for t in C.outs:
            fin.append(t.b)
            fin.extend(t.regs.values())
        C.P.emit(final_bufs=fin)
    return nc, C


def _in_maps(inp, ncores):
    shared = {}
    for k in ["norm_mix", "q_norm", "k_norm", "hg_out_norm", "norm_ffn", "norm_ple"]:
        shared[k] = np.ascontiguousarray(np.asarray(inp[k], np.float32)[0][None])
    for k in ["w_in", "w_up_att", "w_up_hg", "w_out", "peer_wq", "ple_gate", "ple_proj"]:
        shared[k] = np.ascontiguousarray(np.asarray(inp[k], np.float32)[0])
    shared["hg_lb_raw"] = np.ascontiguousarray(np.asarray(inp["hg_lb_raw"], np.float32))
    sk = np.asarray(inp["peer_subkeys"], np.float32)[0]
    shared["skT"] = np.ascontiguousarray(sk.transpose(3, 0, 1, 2).reshape(128, 16, 128))
    u = np.asarray(inp["peer_u"], np.float32)[0].reshape(128, 128, 8, 128)
    shared["uT"] = np.ascontiguousarray(u.transpose(1, 3, 2, 0))
    v = np.asarray(inp["peer_v"], np.float32)[0].reshape(128, 128, D)
    shared["vL"] = np.ascontiguousarray(v.transpose(1, 0, 2))
    x = np.asarray(inp["x"], np.float32)
    p = np.asarray(inp["p"], np.float32)
    maps = []
    for b in range(ncores):
        m = dict(shared)
        m["x"] = np.ascontiguousarray(x[b])
        m["p"] = np.ascontiguousarray(p[0, b])
        maps.append(m)
    return maps


_NC_CACHE = {}


def kernel(**inputs):
    ncores = 8
    if "nc" not in _NC_CACHE:
        _NC_CACHE["nc"] = build(phases="ABCDE2F")[0]
    nc = _NC_CACHE["nc"]
    maps = _in_maps(inputs, ncores)
    res = run_bass_kernel_spmd(nc, maps, core_ids=list(range(ncores)))
    out = np.stack([np.asarray(res.results[b]["y"], np.float32) for b in range(ncores)], 0)
    return out
```

```python
import contextlib
import numpy as np
import concourse.bass as bass
import concourse.mybir as mybir
from concourse.bass_utils import run_bass_kernel_spmd

F32 = mybir.dt.float32
BF16 = mybir.dt.bfloat16
I32 = mybir.dt.int32
U32 = mybir.dt.uint32
ALU = mybir.AluOpType
AF = mybir.ActivationFunctionType
AX = mybir.AxisListType

S = 4096
D = 1024
NT = S // 128
EPS = 1e-6
EPOCH = 16000
NDMA_SLOTS = 48


class Buf:
    __slots__ = ("name", "last_w", "readers")

    def __init__(self, name=""):
        self.name = name
        self.last_w = None
        self.readers = {}


class T:
    __slots__ = ("t", "b", "regs")

    def __init__(self, t, name=""):
        self.t = t
        self.b = Buf(name)
        self.regs = {}

    def reg(self, key):
        if key not in self.regs:
            self.regs[key] = Buf(f"{self.b.name}[{key}]")
        return self.regs[key]

    def regl(self, keys):
        return [self.reg(k) for k in keys]


class Prog:
    ENGS = ("pe", "act", "dve", "pool", "sp")

    def __init__(self, nc):
        self.nc = nc
        self.ops = {e: [] for e in self.ENGS}
        self.count = {e: 0 for e in self.ENGS}
        self.known = {e: {} for e in self.ENGS}
        self.dma_count = [0] * NDMA_SLOTS
        self.dma_rr = {"hw": 0, "sw": 0}
        self.n_instr = 0
        self.pending = {e: [] for e in self.ENGS}

    def barrier(self):
        prods = [(e, self.count[e]) for e in self.ENGS if self.count[e] > 0]
        prods += [(("dma", s), c) for s, c in enumerate(self.dma_count) if c > 0]
        for e in self.ENGS:
            kn = self.known[e]
            for p, c in prods:
                if kn.get(p, 0) < c:
                    kn[p] = c
                    self.pending[e].append((p, c))

    def _deps(self, eng, reads, writes, pe_accum=False):
        deps = {}

        def add(p, s):
            if deps.get(p, 0) < s:
                deps[p] = s

        for b in reads:
            if b.last_w is not None:
                add(*b.last_w)
        for b in writes:
            if b.last_w is not None:
                if not (pe_accum and b.last_w[0] == "pe" and eng == "pe"):
                    add(*b.last_w)
            for p, s in b.readers.items():
                add(p, s)
        waits = []
        kn = self.known[eng]
        for p, s in deps.items():
            if kn.get(p, 0) < s:
                kn[p] = s
                waits.append((p, s))
        return waits

    def _commit(self, prod, seq, reads, writes):
        for b in writes:
            b.last_w = (prod, seq)
            b.readers = {}
        for b in reads:
            b.readers[prod] = max(b.readers.get(prod, 0), seq)

    @staticmethod
    def _bufs(xs):
        out = []
        for x in xs:
            if isinstance(x, T):
                out.append(x.b)
            elif isinstance(x, (list, tuple)):
                out.extend(Prog._bufs(x))
            else:
                out.append(x)
        return out

    def op(self, eng, fn, reads=(), writes=(), pe_accum=False):
        reads = self._bufs(reads)
        writes = self._bufs(writes)
        waits = self._deps(eng, reads, writes, pe_accum)
        waits = [w for w in self.pending[eng] if w not in waits] + waits
        self.pending[eng] = []
        self.count[eng] += 1
        seq = self.count[eng]
        self.ops[eng].append((waits, fn, (eng, seq)))
        self._commit(eng, seq, reads, writes)
        self.n_instr += 1

    def dma(self, q, out, in_, reads=(), writes=(), **kw):
        reads = self._bufs(reads)
        writes = self._bufs(writes)
        waits = self._deps(q, reads, writes)
        waits = [w for w in self.pending[q] if w not in waits] + waits
        self.pending[q] = []
        half = NDMA_SLOTS // 2
        kind = "sw" if q == "pool" else "hw"
        slot = self.dma_rr[kind] + (half if kind == "sw" else 0)
        self.dma_rr[kind] = (self.dma_rr[kind] + 1) % half
        prod = ("dma", slot)
        prev = self.dma_count[slot]
        kn = self.known[q]
        if prev and kn.get(prod, 0) < prev:
            kn[prod] = prev
            waits.append((prod, prev))
        self.dma_count[slot] += 1
        seq = self.dma_count[slot]
        self.ops[q].append((waits, (lambda e: e.dma_start(out=out, in_=in_, **kw)), (prod, seq)))
        self._commit(prod, seq, reads, writes)
        self.n_instr += 1

    def emit(self, final_bufs=()):
        nc = self.nc
        final_bufs = self._bufs(final_bufs)
        with contextlib.ExitStack() as st:
            sems = {}
            for e in self.ENGS:
                nep = self.count[e] // EPOCH + 1
                sems[e] = [st.enter_context(nc.semaphore(f"s_{e}_{k}")) for k in range(nep)]
            for s in range(NDMA_SLOTS):
                sems[("dma", s)] = [st.enter_context(nc.semaphore(f"s_dma{s}"))]
            fin = {}
            for b in final_bufs:
                if b.last_w is not None:
                    p, s = b.last_w
                    fin[p] = max(fin.get(p, 0), s)

            def wait(eng, p, s):
                if isinstance(p, tuple):
                    eng.wait_ge(sems[p][0], 16 * s)
                else:
                    k = (s - 1) // EPOCH
                    eng.wait_ge(sems[p][k], s - k * EPOCH)

            def run(ename, eng):
                for waits, fn, (prod, seq) in self.ops[ename]:
                    for p, s in waits:
                        wait(eng, p, s)
                    ins = fn(eng)
                    if isinstance(prod, tuple):
                        ins.then_inc(sems[prod][0], 16)
                    else:
                        k = (seq - 1) // EPOCH
                        ins.then_inc(sems[prod][k], 1)
                if ename == "sp":
                    for p, s in fin.items():
                        wait(eng, p, s)

            with nc.Block() as block:
                @block.sync
                def _(e):
                    run("sp", e)

                @block.tensor
                def _(e):
                    run("pe", e)

                @block.scalar
                def _(e):
                    run("act", e)

                @block.vector
                def _(e):
                    run("dve", e)

                @block.gpsimd
                def _(e):
                    run("pool", e)


class Ring:
    def __init__(self, items):
        self.items = items
        self.i = 0

    def next(self):
        x = self.items[self.i % len(self.items)]
        self.i += 1
        return x


class Ctx:
    def __init__(self, nc, dbg=()):
        self.nc = nc
        self.P = Prog(nc)
        self.dbg = set(dbg)
        self.ins = {}
        self.scr = {}
        self.outs = []
        self.uid = 0

    def din(self, name, shape, dt=F32):
        ap = self.nc.dram_tensor(name, list(shape), dt, kind="ExternalInput").ap()
        self.ins[name] = T(ap, name)
        return self.ins[name]

    def scratch(self, name, shape, dt):
        if name in self.dbg:
            ap = self.nc.dram_tensor(name, list(shape), dt, kind="ExternalOutput").ap()
        else:
            ap = self.nc.dram_tensor(name, list(shape), dt).ap()
        t = T(ap, name)
        self.scr[name] = t
        if name in self.dbg:
            self.outs.append(t)
        return t

    def alloc(self, st, shape, dt, name=None, psum=False):
        self.uid += 1
        name = f"{name or 't'}_{self.uid}"
        if psum:
            t = st.enter_context(self.nc.psum_tensor(name, list(shape), dt))
        else:
            t = st.enter_context(self.nc.sbuf_tensor(name, list(shape), dt))
        return T(t, name)

    def ring(self, st, n, shape, dt, name=None, psum=False):
        return Ring([self.alloc(st, shape, dt, name, psum) for _ in range(n)])


def mm(C, out_t, out_ap, lhsT_t, lhsT_ap, rhs_t, rhs_ap, start, stop):
    C.P.op("pe", lambda e: e.matmul(out_ap, lhsT=lhsT_ap, rhs=rhs_ap, start=start, stop=stop),
           reads=[lhsT_t, rhs_t], writes=[out_t], pe_accum=True)


def tr(C, out_t, out_ap, in_t, in_ap, ident):
    C.P.op("pe", lambda e: e.transpose(out=out_ap, in_=in_ap, identity=ident.t[:]),
           reads=[in_t, ident], writes=[out_t], pe_accum=True)


def act(C, out_t, out_ap, in_t, in_ap, func, reads=(), extra_w=(), **kw):
    C.P.op("act", lambda e: e.activation(out=out_ap, in_=in_ap, func=func, **kw),
           reads=[in_t] + list(reads), writes=[out_t] + list(extra_w))


def tt(C, eng, out_t, out_ap, a_t, a_ap, b_t, b_ap, op):
    C.P.op(eng, lambda e: e.tensor_tensor(out=out_ap, in0=a_ap, in1=b_ap, op=op),
           reads=[a_t, b_t], writes=[out_t])


def ts(C, eng, out_t, out_ap, a_t, a_ap, s1, s2, op0, op1=None, reads=()):
    if op1 is None:
        C.P.op(eng, lambda e: e.tensor_scalar(out=out_ap, in0=a_ap, scalar1=s1, scalar2=None, op0=op0),
               reads=[a_t] + list(reads), writes=[out_t])
    else:
        C.P.op(eng, lambda e: e.tensor_scalar(out=out_ap, in0=a_ap, scalar1=s1, scalar2=s2, op0=op0, op1=op1),
               reads=[a_t] + list(reads), writes=[out_t])


def stt(C, eng, out_t, out_ap, a_t, a_ap, scalar, b_t, b_ap, op0, op1, reads=()):
    C.P.op(eng, lambda e: e.scalar_tensor_tensor(out=out_ap, in0=a_ap, scalar=scalar, in1=b_ap, op0=op0, op1=op1),
           reads=[a_t, b_t] + list(reads), writes=[out_t])


def cp(C, eng, out_t, out_ap, in_t, in_ap):
    if eng == "act":
        C.P.op("act", lambda e: e.copy(out=out_ap, in_=in_ap), reads=[in_t], writes=[out_t])
    else:
        C.P.op(eng, lambda e: e.tensor_copy(out=out_ap, in_=in_ap), reads=[in_t], writes=[out_t])


def rsqrt_mean(C, st_t, src_ap_fn, n, scale):
    ap = src_ap_fn()
    ts(C, "dve", st_t, ap, st_t, ap, scale, EPS, ALU.mult, ALU.add)
    C.P.op("act", lambda e: e.activation(out=ap, in_=ap, func=AF.Sqrt), reads=[st_t], writes=[st_t])
    C.P.op("dve", lambda e: e.reciprocal(out=ap, in_=ap), reads=[st_t], writes=[st_t])


def build_consts(C, st):
    P = C.P
    K = {}
    idf = C.alloc(st, [128, 128], F32, "idf")
    P.op("pool", lambda e: e.memset(idf.t[:], 0.0), writes=[idf])
    P.op("pool", lambda e: e.affine_select(out=idf.t[:], in_=idf.t[:], pattern=[[-1, 128]],
                                           compare_op=ALU.not_equal, fill=1.0, base=0, channel_multiplier=1),
         reads=[idf], writes=[idf])
    ident = C.alloc(st, [128, 128], BF16, "ident")
    cp(C, "dve", ident, ident.t[:], idf, idf.t[:])
    K["ident"] = ident
    K["identf"] = idf
    return K


def build_rope(C, st):
    import math
    P = C.P
    rope = C.alloc(st, [128, NT, 2, 32], F32, "rope")
    with contextlib.ExitStack() as tmp:
        pf = C.alloc(tmp, [128, 1], F32, "pf")
        P.op("pool", lambda e: e.iota(pf.t[:], pattern=[[0, 1]], base=0, channel_multiplier=1,
                                      allow_small_or_imprecise_dtypes=True), writes=[pf])
        pi = C.alloc(tmp, [128, 2], I32, "pi")
        hl = C.alloc(tmp, [128, 2], I32, "hl")
        hlf = C.alloc(tmp, [128, 2], F32, "hlf")
        cp(C, "dve", pi, pi.t[:, 0:1], pf, pf.t[:])
        P.op("dve", lambda e: e.tensor_single_scalar(out=hl.t[:, 0:1], in_=pi.t[:, 0:1], scalar=6, op=ALU.arith_shift_right),
             reads=[pi], writes=[hl])
        P.op("dve", lambda e: e.tensor_single_scalar(out=hl.t[:, 1:2], in_=pi.t[:, 0:1], scalar=63, op=ALU.bitwise_and),
             reads=[pi, hl], writes=[hl])
        cp(C, "dve", hlf, hlf.t[:], hl, hl.t[:])
        jf = C.alloc(tmp, [128, 16], F32, "jf")
        P.op("pool", lambda e: e.iota(jf.t[:], pattern=[[1, 16]], base=0, channel_multiplier=0,
                                      allow_small_or_imprecise_dtypes=True), writes=[jf])
        inv = C.alloc(tmp, [128, 16], F32, "inv")
        act(C, inv, inv.t[:], jf, jf.t[:], AF.Exp, scale=-math.log(10000.0) / 16.0)
        rowp = C.alloc(tmp, [128, NT], F32, "rowp")
        P.op("pool", lambda e: e.iota(rowp.t[:], pattern=[[2, NT]], base=0, channel_multiplier=0,
                                      allow_small_or_imprecise_dtypes=True), writes=[rowp])
        ts(C, "dve", rowp, rowp.t[:], rowp, rowp.t[:], hlf.t[:, 0:1], None, ALU.add, reads=[hlf])
        ang = C.alloc(tmp, [128, NT, 32], F32, "ang")
        tt(C, "dve", ang, ang.t[:, :, 0:16], rowp, rowp.t[:].unsqueeze(2).to_broadcast([128, NT, 16]),
           inv, inv.t[:].unsqueeze(1).to_broadcast([128, NT, 16]), ALU.mult)
        colang = C.alloc(tmp, [128, 16], F32, "colang")
        ts(C, "dve", colang, colang.t[:], inv, inv.t[:], hlf.t[:, 1:2], None, ALU.mult, reads=[hlf])
        cp(C, "dve", ang, ang.t[:, :, 16:32], colang, colang.t[:].unsqueeze(1).to_broadcast([128, NT, 16]))
        ni = C.alloc(tmp, [128, NT, 32], I32, "ni")
        nf = C.alloc(tmp, [128, NT, 32], F32, "nf")
        red = C.alloc(tmp, [128, NT, 32], F32, "red")
        two_pi = 2.0 * math.pi
        for which, shift in ((1, 0.0), (0, math.pi / 2.0)):
            ts(C, "dve", nf, nf.t[:], ang, ang.t[:], shift, 1.0 / two_pi, ALU.add, ALU.mult)
            cp(C, "dve", ni, ni.t[:], nf, nf.t[:])
            cp(C, "dve", nf, nf.t[:], ni, ni.t[:])
            stt(C, "dve", red, red.t[:], nf, nf.t[:], -two_pi, ang, ang.t[:], ALU.mult, ALU.add)
            ts(C, "dve", red, red.t[:], red, red.t[:], shift, 3.1415925, ALU.add, ALU.min)
            ts(C, "dve", red, red.t[:], red, red.t[:], -3.1415925, None, ALU.max)
            act(C, rope, rope.t[:, :, which, :], red, red.t[:], AF.Sin)
    return rope


OFF = dict(aq=0, ak=512, av=640, hq=768, hff=1280, hfb=1792, hi=2304, hg=2816, gate=3328)


def phase_a(C, K, ntiles=NT):
    nc, P = C.nc, C.P
    I = C.ins
    Sc = C.scr
    with contextlib.ExitStack() as st:
        K = dict(K)
        K["rope"] = build_rope(C, st)
        P.barrier()
        w_in = C.alloc(st, [128, 8, 5376], BF16, "w_in")
        wv = I["w_in"].t.rearrange("(k p) n -> p k n", p=128)
        for c0 in range(0, 5376, 672):
            P.dma("pool", w_in.t[:, :, c0:c0 + 672], wv[:, :, c0:c0 + 672], reads=[I["w_in"]], writes=[w_in])
        gmix = C.alloc(st, [128, 1024], F32, "gmix")
        P.dma("sp", gmix.t[:], I["norm_mix"].t.to_broadcast([128, 1024]), writes=[gmix])
        qg = C.alloc(st, [128, 8, 64], F32, "qg")
        P.dma("sp", qg.t[:], I["q_norm"].t.unsqueeze(1).to_broadcast([128, 8, 64]), writes=[qg])
        kg = C.alloc(st, [128, 2, 64], F32, "kg")
        P.dma("sp", kg.t[:], I["k_norm"].t.unsqueeze(1).to_broadcast([128, 2, 64]), writes=[kg])
        raw = C.alloc(st, [128, 2, 2, 512], F32, "raw")
        P.dma("sp", raw.t[:], I["hg_lb_raw"].t.unsqueeze(0).to_broadcast([128, 2, 2, 512]), writes=[raw])
        lbb = C.alloc(st, [128, 2, 512], F32, "lbb")
        omlb = C.alloc(st, [128, 2, 512], F32, "omlb")
        tt(C, "dve", lbb, lbb.t[:], raw, raw.t[:, 0], raw, raw.t[:, 1], ALU.subtract)
        act(C, omlb, omlb.t[:], lbb, lbb.t[:], AF.Sigmoid, scale=-1.0)
        act(C, lbb, lbb.t[:], lbb, lbb.t[:], AF.Sigmoid)
        rawT = C.alloc(st, [128, 2, 2, 4], F32, "rawT")
        P.dma("sp", rawT.t[:], I["hg_lb_raw"].t.rearrange("s r (c p) -> p s r c", p=128), writes=[rawT],
              allow_slow_non_contiguous=True)
        omlT = C.alloc(st, [128, 2, 4], F32, "omlT")
        tt(C, "dve", omlT, omlT.t[:], rawT, rawT.t[:, 0], rawT, rawT.t[:, 1], ALU.subtract)
        act(C, omlT, omlT.t[:], omlT, omlT.t[:], AF.Sigmoid, scale=-1.0)

        xr = C.ring(st, 3, [128, 1024], F32, "xt")
        junk = C.alloc(st, [128, 1024], BF16, "junk")
        stat = C.ring(st, 8, [128, 16], F32, "stat")
        hbr = C.ring(st, 2, [128, 1024], BF16, "hb")
        hTr = C.ring(st, 2, [128, 8, 512], BF16, "hT")
        ptr = C.ring(st, 2, [128, 8, 128], BF16, "ptr", psum=True)
        pfr = C.ring(st, 2, [128, 512], F32, "pf", psum=True)
        ptk = C.ring(st, 3, [128, 512], F32, "ptk", psum=True)
        pqt = C.alloc(st, [128, 8, 128], BF16, "pqt", psum=True)
        stg_bf = C.ring(st, 4, [128, 512], BF16, "stgb")
        stg_f = C.ring(st, 3, [128, 512], F32, "stgf")
        tmpf = C.ring(st, 4, [128, 512], F32, "tmpf")
        qn = C.ring(st, 3, [128, 512], F32, "qn")
        qr = C.ring(st, 2, [128, 512], BF16, "qr")
        kk = C.ring(st, 2, [128, 4, 64], BF16, "kk")
        kn_ = C.ring(st, 2, [128, 128], F32, "kn")
        vst = C.ring(st, 2, [128, 2, 128], BF16, "vst")
        for v_ in vst.items:
            P.op("pool", lambda e, v_=v_: e.memset(v_.t[:], 1.0), writes=[v_])
        qTs = C.ring(st, 2, [128, 6, 128], BF16, "qTs")
        rt = C.ring(st, 8, [128, 8, 2, 16], F32, "rt")

        def run_rr(gens):
            gens = list(gens)
            while gens:
                for g_ in list(gens):
                    try:
                        next(g_)
                    except StopIteration:
                        gens.remove(g_)

        def rope_norm(src_ps, src_ap, nh, gain, cs, dst_t, dst_ap4):
            w = nh * 64
            sq = tmpf.next()
            act(C, sq, sq.t[:, 0:w], src_ps, src_ap, AF.Square)
            yield
            s8 = stat.next()
            P.op("dve", lambda e: e.tensor_reduce(out=s8.t[:, 0:nh], in_=sq.t[:, 0:w].rearrange("p (h d) -> p h d", d=64),
                                                  axis=AX.X, op=ALU.add), reads=[sq], writes=[s8])
            yield
            ap = s8.t[:, 0:nh]
            ts(C, "dve", s8, ap, s8, ap, 1.0 / 64, EPS, ALU.mult, ALU.add)
            yield
            C.P.op("act", lambda e: e.activation(out=ap, in_=ap, func=AF.Sqrt), reads=[s8], writes=[s8])
            yield
            C.P.op("dve", lambda e: e.reciprocal(out=ap, in_=ap), reads=[s8], writes=[s8])
            yield
            n_ = qn.next()
            nv = n_.t[:, 0:w].rearrange("p (h d) -> p h d", d=64)
            tt(C, "dve", n_, nv, src_ps, src_ap.rearrange("p (h d) -> p h d", d=64),
               s8, s8.t[:, 0:nh].unsqueeze(2).to_broadcast([128, nh, 64]), ALU.mult)
            yield
            tt(C, "pool", n_, nv, n_, nv, gain, gain.t[:, 0:nh, :], ALU.mult)
            yield
            v5 = n_.t[:, 0:w].rearrange("p (h a b f) -> p h a b f", a=2, b=2, f=16)
            x1 = v5[:, :, :, 0, :]
            x2 = v5[:, :, :, 1, :]
            cb = cs.t[:, 0, :].rearrange("p (a f) -> p a f", a=2).unsqueeze(1).to_broadcast([128, nh, 2, 16])
            sb_ = cs.t[:, 1, :].rearrange("p (a f) -> p a f", a=2).unsqueeze(1).to_broadcast([128, nh, 2, 16])
            t1, t2 = rt.next(), rt.next()
            tt(C, "dve", t1, t1.t[:, 0:nh], n_, x1, cs, cb, ALU.mult)
            tt(C, "pool", t2, t2.t[:, 0:nh], n_, x2, cs, sb_, ALU.mult)
            yield
            t3, t4 = rt.next(), rt.next()
            tt(C, "dve", t3, t3.t[:, 0:nh], n_, x1, cs, sb_, ALU.mult)
            tt(C, "pool", t4, t4.t[:, 0:nh], n_, x2, cs, cb, ALU.mult)
            yield
            tt(C, "dve", dst_t, dst_ap4[:, :, :, 0, :], t1, t1.t[:, 0:nh], t2, t2.t[:, 0:nh], ALU.subtract)
            yield
            tt(C, "pool", dst_t, dst_ap4[:, :, :, 1, :], t3, t3.t[:, 0:nh], t4, t4.t[:, 0:nh], ALU.add)
            yield

        def prep_tile(g, j, hT):
            ti = g * 4 + j
            xt = xr.next()
            P.dma("sp", xt.t[:], I["x"].t[ti * 128:(ti + 1) * 128, :], reads=[I["x"]], writes=[xt])
            s_ = stat.next()
            act(C, junk, junk.t[:], xt, xt.t[:], AF.Square, accum_out=s_.t[:, 0:1], extra_w=[s_])
            yield
            ap = s_.t[:, 0:1]
            ts(C, "dve", s_, ap, s_, ap, 1.0 / D, EPS, ALU.mult, ALU.add)
            yield
            C.P.op("act", lambda e: e.activation(out=ap, in_=ap, func=AF.Sqrt), reads=[s_], writes=[s_])
            yield
            C.P.op("dve", lambda e: e.reciprocal(out=ap, in_=ap), reads=[s_], writes=[s_])
            yield
            hb = hbr.next()
            stt(C, "dve", hb, hb.t[:], xt, xt.t[:], s_.t[:, 0:1], gmix, gmix.t[:], ALU.mult, ALU.mult, reads=[s_])
            yield
            pt = ptr.next()
            for k in range(8):
                tr(C, pt, pt.t[:, k, :], hb, hb.t[:, k * 128:(k + 1) * 128], K["ident"])
            yield
            cp(C, "act", hT, hT.t[:, :, j * 128:(j + 1) * 128], pt, pt.t[:])
            yield

        ngr = ntiles // 4
        hTs = {0: hTr.next()}
        for j in range(4):
            run_rr([prep_tile(0, j, hTs[0])])
        for g in range(ngr):
            hT = hTs[g]
            if g + 1 < ngr:
                hTs[g + 1] = hTr.next()
            cols = slice(g * 512, (g + 1) * 512)
            fm = [("hq", OFF["hq"] + c * 128, c) for c in range(4)] + \
                 [("hff", OFF["hff"] + c * 128, c) for c in range(4)] + \
                 [("hfb", OFF["hfb"] + c * 128, c) for c in range(4)] + \
                 [("gate", OFF["gate"] + c * 128, c) for c in range(16)]
            for kind, c0, c in fm:
                pf = pfr.next()
                for k in range(8):
                    mm(C, pf, pf.t[:], w_in, w_in.t[:, k, c0:c0 + 128], hT, hT.t[:, k, :], k == 0, k == 7)
                sg = stg_bf.next()
                if kind == "hq":
                    act(C, sg, sg.t[:], pf, pf.t[:], AF.Silu)
                    dst = Sc["hqT"]
                    P.dma("pool", dst.t[c, :, cols], sg.t[:], reads=[sg], writes=[dst.reg((c, g))])
                elif kind in ("hff", "hfb"):
                    d_ = 0 if kind == "hff" else 1
                    tf = tmpf.next()
                    act(C, tf, tf.t[:], pf, pf.t[:], AF.Sigmoid, scale=-1.0)
                    ts(C, "dve", sg, sg.t[:], tf, tf.t[:], omlT.t[:, d_, c:c + 1], None, ALU.mult, reads=[omlT])
                    dst = Sc["kT"]
                    P.dma("pool", dst.t[d_, c, :, cols], sg.t[:], reads=[sg], writes=[dst.reg((d_, c, g))])
                else:
                    act(C, sg, sg.t[:], pf, pf.t[:], AF.Sigmoid)
                    dst = Sc["gtsT"]
                    P.dma("pool", dst.t[c, :, cols], sg.t[:], reads=[sg], writes=[dst.reg((c, g))])
            for j in range(4):
                ti = g * 4 + j
                rows = slice(ti * 128, (ti + 1) * 128)
                lhs = lambda k, j=j: hT.t[:, k, j * 128:(j + 1) * 128]
                cs = T(K["rope"].t[:, ti], "rope_v")
                cs.b = K["rope"].b
                q_ = qr.next()
                k_ = kk.next()

                def chain_q():
                    pq = ptk.next()
                    for k in range(8):
                        mm(C, pq, pq.t[:], hT, lhs(k), w_in, w_in.t[:, k, 0:512], k == 0, k == 7)
                    yield
                    yield from rope_norm(pq, pq.t[:], 8, qg, cs, q_, q_.t[:].rearrange("p (h a b f) -> p h a b f", a=2, b=2, f=16))

                def chain_kv():
                    pkv = ptk.next()
                    for k in range(8):
                        mm(C, pkv, pkv.t[:, 0:256], hT, lhs(k), w_in, w_in.t[:, k, 512:768], k == 0, k == 7)
                    yield
                    v_ = vst.next()
                    cp(C, "act", v_, v_.t[:, :, 0:64], pkv, pkv.t[:, 128:256].rearrange("p (h d) -> p h d", d=64))
                    P.dma("pool", Sc["v"].t[ti], v_.t[:], reads=[v_], writes=[Sc["v"].reg(ti)])
                    yield
                    kview = k_.t[:].rearrange("p (h r) d -> p h r d", r=2)
                    yield from rope_norm(pkv, pkv.t[:, 0:128], 2, kg, cs, k_,
                                         kview[:, :, 0, :].rearrange("p h (a b f) -> p h a b f", a=2, b=2, f=16))
                    cp(C, "pool", k_, kview[:, :, 1, :], k_, kview[:, :, 0, :])
                    yield

                def chain_gate(d_, key):
                    pg = ptk.next()
                    for k in range(8):
                        mm(C, pg, pg.t[:], hT, lhs(k), w_in, w_in.t[:, k, OFF[key]:OFF[key] + 512], k == 0, k == 7)
                    yield
                    tf = tmpf.next()
                    act(C, tf, tf.t[:], pg, pg.t[:], AF.Sigmoid)
                    yield
                    tt(C, "dve", tf, tf.t[:], tf, tf.t[:], omlb, omlb.t[:, d_, :], ALU.mult)
                    yield
                    tt(C, "dve", tf, tf.t[:], tf, tf.t[:], lbb, lbb.t[:, d_, :], ALU.add)
                    yield
                    gf = stg_f.next()
                    act(C, gf, gf.t[:], tf, tf.t[:], AF.Ln)
                    P.dma("pool", Sc["g"].t[d_, rows, :], gf.t[:], reads=[gf], writes=[Sc["g"].reg((d_, ti))])
                    kb = stg_bf.next()
                    ts(C, "pool", kb, kb.t[:], tf, tf.t[:], -1.0, 1.0, ALU.mult, ALU.add)
                    P.dma("pool", Sc["k"].t[d_, rows, :], kb.t[:], reads=[kb], writes=[Sc["k"].reg((d_, ti))])
                    yield

                def chain_h(key, dstn, fn):
                    ph = ptk.next()
                    for k in range(8):
                        mm(C, ph, ph.t[:], hT, lhs(k), w_in, w_in.t[:, k, OFF[key]:OFF[key] + 512], k == 0, k == 7)
                    yield
                    sb_ = stg_bf.next()
                    if fn is None:
                        cp(C, "act", sb_, sb_.t[:], ph, ph.t[:])
                    else:
                        act(C, sb_, sb_.t[:], ph, ph.t[:], fn)
                    P.dma("pool", Sc[dstn].t[rows, :], sb_.t[:], reads=[sb_], writes=[Sc[dstn].reg(ti)])
                    yield

                chains = [chain_q(), chain_kv(), chain_gate(0, "hff")]
                if g + 1 < ngr:
                    chains.append(prep_tile(g + 1, j, hTs[g + 1]))
                run_rr(chains)
                run_rr([chain_gate(1, "hfb"), chain_h("hi", "hi", None), chain_h("hg", "sg", AF.Silu)])
                for pr in range(4):
                    tr(C, pqt, pqt.t[:, pr, :], q_, q_.t[:, pr * 128:(pr + 1) * 128], K["ident"])
                kflat = k_.t[:].rearrange("p a d -> p (a d)")
                for kv in range(2):
                    tr(C, pqt, pqt.t[:, 4 + kv, :], k_, kflat[:, kv * 128:(kv + 1) * 128], K["ident"])
                qs = qTs.next()
                cp(C, "act", qs, qs.t[:], pqt, pqt.t[:, 0:6, :])
                P.dma("pool", Sc["qT"].t[:, :, rows].rearrange("r p t -> p r t"), qs.t[:, 0:4, :], reads=[qs], writes=[Sc["qT"].reg(ti)])
                P.dma("pool", Sc["kTa"].t[:, :, rows].rearrange("r p t -> p r t"), qs.t[:, 4:6, :], reads=[qs], writes=[Sc["kTa"].reg(ti)])


def phase_b(C, K, ngroups=8, hhs=(0, 1), bg=()):
    nc, P = C.nc, C.P
    Sc = C.scr
    with contextlib.ExitStack() as st:
        kT = [C.alloc(st, [128, S], BF16, "kTsb") for _ in range(2)]
        for kv in range(2):
            for hf in range(2):
                cs_ = slice(hf * 2048, (hf + 1) * 2048)
                P.dma("sp", kT[kv].t[:, cs_], Sc["kTa"].t[kv, :, cs_],
                      reads=Sc["kTa"].regl(range(hf * 16, hf * 16 + 16)), writes=[kT[kv]])
        vs = C.alloc(st, [128, NT, 256], BF16, "vsb")
        for hf in range(4):
            P.dma("sp", vs.t[:, hf * 8:(hf + 1) * 8, :], Sc["v"].t[hf * 8:(hf + 1) * 8].rearrange("t p h c -> p t (h c)"),
                  reads=Sc["v"].regl(range(hf * 8, hf * 8 + 8)), writes=[vs])
        if "dbgvs" in Sc:
            P.dma("pool", Sc["dbgvs"].t[:], vs.t[:], reads=[vs], writes=[Sc["dbgvs"]])
        qr_ = C.ring(st, 2, [128, 512], BF16, "qTg")
        psS = C.ring(st, 4, [128, 512], F32, "psS", psum=True)
        acc = [C.alloc(st, [128, 512], F32, "acc", psum=True) for _ in range(2)]
        ptr_ = C.ring(st, 8, [128, 512], BF16, "pT")
        rl = C.ring(st, 2, [128, 512], F32, "rl")
        obr = C.ring(st, 2, [128, 512], BF16, "ob")
        LAG = 3
        steps = [(g, pr, kt, hh) for g in range(ngroups) for pr in range(4) for kt in range(NT) for hh in hhs]
        state = {}
        accs = [acc, [C.alloc(st, [128, 512], F32, "acc2", psum=True) for _ in range(2)]]

        def stage1(g, pr, kt, hh):
            kv = pr // 2
            if kt == 0 and hh == hhs[0]:
                q = qr_.next()
                P.dma("sp", q.t[:], Sc["qT"].t[pr, :, g * 512:(g + 1) * 512], reads=Sc["qT"].regl(range(4 * g, 4 * g + 4)), writes=[q])
                state["q", g, pr] = q
            q = state["q", g, pr]
            rows = slice(hh * 64, (hh + 1) * 64)
            s_ = psS.next()
            mm(C, s_, s_.t[:], kT[kv], kT[kv].t[rows, kt * 128:(kt + 1) * 128], q, q.t[rows, :], True, True)
            p_ = ptr_.next()
            act(C, p_, p_.t[:], s_, s_.t[:], AF.Exp, scale=0.125)
            state["p", g, pr, kt, hh] = p_

        def stage2(g, pr, kt, hh):
            kv = pr // 2
            p_ = state.pop(("p", g, pr, kt, hh))
            ac = accs[(g * 4 + pr) % 2]
            mm(C, ac[hh], ac[hh].t[:], vs, vs.t[:, kt, kv * 128:(kv + 1) * 128], p_, p_.t[:], kt == 0, kt == NT - 1)
            if kt == NT - 1 and hh == hhs[-1]:
                ob = obr.next()
                for h2_ in hhs:
                    r_ = rl.next()
                    C.P.op("dve", lambda e, r_=r_, h2_=h2_, ac=ac: e.reciprocal(out=r_.t[64:128, :], in_=ac[h2_].t[64:128, :]),
                           reads=[ac[h2_]], writes=[r_])
                    tt(C, "dve", ob, ob.t[h2_ * 64:(h2_ + 1) * 64, :], ac[h2_], ac[h2_].t[0:64, :], r_, r_.t[64:128, :], ALU.mult)
                P.dma("pool", Sc["attoT"].t[pr, :, g * 512:(g + 1) * 512], ob.t[:], reads=[ob], writes=[Sc["attoT"].reg((pr, g))])

        bg = list(bg)
        LAG = 4
        for it in range(0, len(steps) + LAG, 2):
            for i_ in (it, it + 1):
                if i_ < len(steps):
                    stage1(*steps[i_])
            for i_ in (it - LAG, it - LAG + 1):
                if 0 <= i_ < len(steps):
                    stage2(*steps[i_])
            if bg and (it // 2) % 3 == 2:
                bg.pop(0)()
        for job in bg:
            job()


def build_masks(C, st):
    P = C.P
    M = {}
    specs = {
        "f_incl": (ALU.is_ge, 0, 1, -1),
        "f_excl": (ALU.is_gt, 0, -1, 1),
        "b_incl": (ALU.is_ge, 0, -1, 1),
        "b_excl": (ALU.is_gt, 0, 1, -1),
    }
    for name, (op, base, tmul, pmul) in specs.items():
        m = C.alloc(st, [128, 128], F32, "m_" + name)
        P.op("pool", lambda e, m=m: e.memset(m.t[:], 1.0), writes=[m])
        P.op("pool", lambda e, m=m, op=op, base=base, tmul=tmul, pmul=pmul: e.affine_select(
            out=m.t[:], in_=m.t[:], pattern=[[tmul, 128]], compare_op=op, fill=0.0, base=base, channel_multiplier=pmul),
            reads=[m], writes=[m])
        P.op("pool", lambda e, m=m: e.memset(m.t[0:64, 64:128], 0.0), reads=[m], writes=[m])
        P.op("pool", lambda e, m=m: e.memset(m.t[64:128, 0:64], 0.0), reads=[m], writes=[m])
        M[name] = m
    return M


def phase_c(C, K, ntiles=NT, dirs=(0, 1)):
    nc, P = C.nc, C.P
    Sc = C.scr
    I = C.ins
    with contextlib.ExitStack() as st:
        M = build_masks(C, st)
        gon = C.alloc(st, [128, 4, 128], F32, "gon")
        P.dma("sp", gon.t[:], I["hg_out_norm"].t.unsqueeze(1).to_broadcast([128, 4, 128]), writes=[gon])
        gr = C.ring(st, 2, [128, 512], F32, "g_t")
        kdr = C.ring(st, 2, [128, 512], BF16, "kd_t")
        kTr = C.ring(st, 2, [128, 4, 128], BF16, "kT_t")
        qTr = C.ring(st, 2, [128, 4, 128], BF16, "hqT_t")
        vr = C.ring(st, 2, [128, 512], BF16, "v_t")
        ofr = C.ring(st, 2, [128, 512], F32, "of_t")
        sgr = C.ring(st, 2, [128, 512], BF16, "sg_t")
        prx = C.alloc(st, [128, 512], F32, "prx", psum=True)
        pbT = C.alloc(st, [128, 4, 128], F32, "pbT", psum=True)
        pX = [C.alloc(st, [128, 4, 128], F32, "pX", psum=True) for _ in range(2)]
        pOs = [C.alloc(st, [128, 4, 128], F32, "pOs", psum=True) for _ in range(2)]
        pTr = C.alloc(st, [128, 8, 128], BF16, "pTr", psum=True)
        ebT = C.ring(st, 2, [128, 4, 128], F32, "ebT")
        enbT = C.ring(st, 2, [128, 4, 128], F32, "enbT")
        er = C.ring(st, 2, [128, 512], F32, "er")
        qfull = C.ring(st, 2, [128, 4, 128], BF16, "qfull")
        qlo = C.ring(st, 2, [128, 4, 128], BF16, "qlo")
        qhi = C.ring(st, 2, [128, 4, 128], BF16, "qhi")
        for t_ in qlo.items + qhi.items:
            P.op("pool", lambda e, t_=t_: e.memset(t_.t[:], 0.0), writes=[t_])
        ktil = C.ring(st, 2, [128, 4, 128], BF16, "ktil")
        kdec = C.ring(st, 2, [128, 512], BF16, "kdec")
        atm = C.ring(st, 4, [128, 128], BF16, "atm")
        S32 = [C.alloc(st, [128, 128], F32, "S32") for _ in range(4)]
        Sbf = [C.alloc(st, [128, 128], BF16, "Sbf") for _ in range(4)]
        osb = C.ring(st, 2, [128, 512], F32, "osb")
        tot = C.ring(st, 2, [128, 512], F32, "tot")
        sqt = C.ring(st, 2, [128, 512], F32, "sqt")
        stat = C.ring(st, 2, [128, 8], F32, "statc")
        onb = C.ring(st, 2, [128, 512], BF16, "onb")
        oTs = C.ring(st, 2, [128, 4, 128], BF16, "oTs")

        for d_ in dirs:
            Mi = M["f_incl"] if d_ == 0 else M["b_incl"]
            Me = M["f_excl"] if d_ == 0 else M["b_excl"]
            for hd in range(4):
                P.op("pool", lambda e, hd=hd: e.memset(S32[hd].t[:], 0.0), writes=[S32[hd]])
                P.op("pool", lambda e, hd=hd: e.memset(Sbf[hd].t[:], 0.0), writes=[Sbf[hd]])
            order = list(range(ntiles)) if d_ == 0 else list(range(ntiles - 1, -1, -1))
            def pro(ti):
                rows = slice(ti * 128, (ti + 1) * 128)
                g_t, kd_t, kT_t, q_t, v_t = gr.next(), kdr.next(), kTr.next(), qTr.next(), vr.next()
                P.dma("sp", g_t.t[:], Sc["g"].t[d_, rows, :], reads=[Sc["g"].reg((d_, ti))], writes=[g_t])
                P.dma("sp", kd_t.t[:], Sc["k"].t[d_, rows, :], reads=[Sc["k"].reg((d_, ti))], writes=[kd_t])
                P.dma("sp", kT_t.t[:], Sc["kT"].t[d_, :, :, rows].rearrange("h p t -> p h t"),
                      reads=[Sc["kT"].reg((d_, c, ti // 4)) for c in range(4)], writes=[kT_t])
                P.dma("sp", q_t.t[:], Sc["hqT"].t[:, :, rows].rearrange("h p t -> p h t"),
                      reads=[Sc["hqT"].reg((c, ti // 4)) for c in range(4)], writes=[q_t])
                P.dma("sp", v_t.t[:], Sc["hi"].t[rows, :], reads=[Sc["hi"].reg(ti)], writes=[v_t])
                mm(C, prx, prx.t[:], Me, Me.t[:], g_t, g_t.t[:], True, True)
                for hd in range(4):
                    mm(C, pbT, pbT.t[:, hd, :], g_t, g_t.t[:, hd * 128:(hd + 1) * 128], Mi, Mi.t[:], True, True)
                eb, enb, er_ = ebT.next(), enbT.next(), er.next()
                act(C, eb, eb.t[:], pbT, pbT.t[:], AF.Exp)
                act(C, enb, enb.t[:], pbT, pbT.t[:], AF.Exp, scale=-1.0)
                act(C, er_, er_.t[:], prx, prx.t[:], AF.Exp)
                qf, ql, qh, kt_, kdc = qfull.next(), qlo.next(), qhi.next(), ktil.next(), kdec.next()
                tt(C, "dve", qf, qf.t[:], q_t, q_t.t[:], eb, eb.t[:], ALU.mult)
                cp(C, "pool", ql, ql.t[:, :, 0:64], qf, qf.t[:, :, 0:64])
                cp(C, "pool", qh, qh.t[:, :, 64:128], qf, qf.t[:, :, 64:128])
                tt(C, "dve", kt_, kt_.t[:], kT_t, kT_t.t[:], enb, enb.t[:], ALU.mult)
                tt(C, "pool", kdc, kdc.t[:], kd_t, kd_t.t[:], er_, er_.t[:], ALU.mult)
                return dict(kd_t=kd_t, v_t=v_t, eb=eb, qf=qf, ql=ql, qh=qh, kt_=kt_, kdc=kdc)

            def tile_body(ti, B_):
                rows = slice(ti * 128, (ti + 1) * 128)
                kd_t, v_t, eb, qf, ql, qh, kt_, kdc = (B_[k_] for k_ in ('kd_t', 'v_t', 'eb', 'qf', 'ql', 'qh', 'kt_', 'kdc'))
                if d_ == 0:
                    ca, cb, qa, qb, la, lb_ = 0, 1, ql, qh, 63, 127
                else:
                    ca, cb, qa, qb, la, lb_ = 1, 0, qh, ql, 64, 0
                ra = slice(ca * 64, (ca + 1) * 64)
                rb = slice(cb * 64, (cb + 1) * 64)
                def head_chain(hd):
                    hc = slice(hd * 128, (hd + 1) * 128)
                    X, O_ = pX[hd % 2], pOs[hd % 2]
                    oa = O_.t[:, hd // 2, :]
                    mm(C, X, X.t[:, 0, :], kt_, kt_.t[:, hd, :], qf, qf.t[:, hd, :], True, True)
                    mm(C, O_, oa, qa, qa.t[:, hd, :], Sbf[hd], Sbf[hd].t[:], True, False)
                    mm(C, X, X.t[:, 1, :], kdc, kdc.t[ra, hc], v_t, v_t.t[ra, hc], True, True)
                    yield
                    am = atm.next()
                    tt(C, "dve", am, am.t[:], X, X.t[:, 0, :], Mi, Mi.t[:], ALU.mult)
                    stt(C, "dve", S32[hd], S32[hd].t[:], S32[hd], S32[hd].t[:], eb.t[:, hd, la:la + 1],
                        X, X.t[:, 1, :], ALU.mult, ALU.add, reads=[eb])
                    yield
                    cp(C, "act", Sbf[hd], Sbf[hd].t[:], S32[hd], S32[hd].t[:])
                    yield
                    mm(C, O_, oa, qb, qb.t[:, hd, :], Sbf[hd], Sbf[hd].t[:], False, False)
                    mm(C, O_, oa, am, am.t[:], v_t, v_t.t[:, hc], False, True)
                    mm(C, X, X.t[:, 2, :], kdc, kdc.t[rb, hc], v_t, v_t.t[rb, hc], True, True)
                    yield
                    stt(C, "dve", S32[hd], S32[hd].t[:], S32[hd], S32[hd].t[:], eb.t[:, hd, lb_:lb_ + 1],
                        X, X.t[:, 2, :], ALU.mult, ALU.add, reads=[eb])
                    yield
                    cp(C, "act", Sbf[hd], Sbf[hd].t[:], S32[hd], S32[hd].t[:])
                    yield

                for pair in ((0, 1), (2, 3)):
                    gens = [head_chain(hd) for hd in pair]
                    while gens:
                        for g_ in list(gens):
                            try:
                                next(g_)
                            except StopIteration:
                                gens.remove(g_)

                def ov(tile_ap, s_):
                    return tile_ap.rearrange("p (a s d) -> p a s d", s=2, d=128)[:, :, s_, :]

                if d_ == 0 and len(dirs) == 2:
                    o_ = osb.next()
                    for s_ in range(2):
                        cp(C, "act", o_, ov(o_.t[:], s_), pOs[s_], pOs[s_].t[:, 0:2, :])
                    P.dma("pool", Sc["ofwd"].t[rows, :], o_.t[:], reads=[o_], writes=[Sc["ofwd"].reg(ti)])
                    return
                t_ = tot.next()
                if len(dirs) == 2:
                    of_ = ofr.next()
                    P.dma("sp", of_.t[:], Sc["ofwd"].t[rows, :], reads=[Sc["ofwd"].reg(ti)], writes=[of_])
                    for s_ in range(2):
                        tt(C, "dve", t_, ov(t_.t[:], s_), pOs[s_], pOs[s_].t[:, 0:2, :], of_, ov(of_.t[:], s_), ALU.add)
                else:
                    for s_ in range(2):
                        cp(C, "dve", t_, ov(t_.t[:], s_), pOs[s_], pOs[s_].t[:, 0:2, :])
                if "dbgo" in Sc:
                    P.dma("pool", Sc["dbgo"].t[rows, :], t_.t[:], reads=[t_], writes=[Sc["dbgo"].reg(ti)])
                sg_ = sgr.next()
                P.dma("sp", sg_.t[:], Sc["sg"].t[rows, :], reads=[Sc["sg"].reg(ti)], writes=[sg_])
                sq = sqt.next()
                act(C, sq, sq.t[:], t_, t_.t[:], AF.Square)
                s4 = stat.next()
                P.op("dve", lambda e, s4=s4, sq=sq: e.tensor_reduce(out=s4.t[:, 0:4], in_=sq.t[:].rearrange("p (h d) -> p h d", d=128),
                                                              axis=AX.X, op=ALU.add), reads=[sq], writes=[s4])
                rsqrt_mean(C, s4, lambda s4=s4: s4.t[:, 0:4], 4, 1.0 / 128)
                t3 = t_.t[:].rearrange("p (h d) -> p h d", d=128)
                tt(C, "dve", t_, t3, t_, t3, s4, s4.t[:, 0:4].unsqueeze(2).to_broadcast([128, 4, 128]), ALU.mult)
                tt(C, "pool", t_, t3, t_, t3, gon, gon.t[:], ALU.mult)
                ob = onb.next()
                tt(C, "dve", ob, ob.t[:], t_, t_.t[:], sg_, sg_.t[:], ALU.mult)
                for hd in range(4):
                    tr(C, pTr, pTr.t[:, hd, :], ob, ob.t[:, hd * 128:(hd + 1) * 128], K["ident"])
                os_ = oTs.next()
                cp(C, "act", os_, os_.t[:], pTr, pTr.t[:, 0:4, :])
                P.dma("pool", Sc["hgoT"].t[:, :, rows].rearrange("h p t -> p h t"), os_.t[:], reads=[os_], writes=[Sc["hgoT"].reg(ti)])

            pend = pro(order[0])
            for idx_, ti in enumerate(order):
                nxt = pro(order[idx_ + 1]) if idx_ + 1 < len(order) else None
                tile_body(ti, pend)
                pend = nxt


def load_w(C, st, name, kchunks, ncols, q="pool"):
    w = C.alloc(st, [128, kchunks, ncols], BF16, name)
    src = C.ins[name].t.rearrange("(k p) n -> p k n", p=128)
    step = min(kchunks, max(1, 4096 // ncols))
    for k0 in range(0, kchunks, step):
        C.P.dma(q, w.t[:, k0:k0 + step, :], src[:, k0:k0 + step, :], reads=[C.ins[name]], writes=[w])
    return w


def norm_transpose(C, K, xt, gain, stat, junk, hb, pt, dst, dst_ap):
    s_ = stat
    act(C, junk, junk.t[:], xt, xt.t[:], AF.Square, accum_out=s_.t[:, 0:1], extra_w=[s_])
    rsqrt_mean(C, s_, lambda: s_.t[:, 0:1], 1, 1.0 / D)
    stt(C, "dve", hb, hb.t[:], xt, xt.t[:], s_.t[:, 0:1], gain, gain.t[:], ALU.mult, ALU.mult, reads=[s_])
    for k in range(8):
        tr(C, pt, pt.t[:, k, :], hb, hb.t[:, k * 128:(k + 1) * 128], K["ident"])
    cp(C, "act", dst, dst_ap, pt, pt.t[:])


def load_d_weights(C, st):
    wua = load_w(C, st, "w_up_att", 4, 1024)
    wuh = load_w(C, st, "w_up_hg", 4, 1024)
    wo = load_w(C, st, "w_out", 8, 1024)
    gffn = C.alloc(st, [128, 1024], F32, "gffn")
    C.P.dma("sp", gffn.t[:], C.ins["norm_ffn"].t.to_broadcast([128, 1024]), writes=[gffn])
    return wua, wuh, wo, gffn


def phase_d(C, K, ngroups=8, pre=None):
    nc, P = C.nc, C.P
    Sc = C.scr
    I = C.ins
    with contextlib.ExitStack() as st:
        wua, wuh, wo, gffn = pre if pre is not None else load_d_weights(C, st)
        aTr = C.ring(st, 2, [128, 4, 512], BF16, "aT")
        hTr_ = C.ring(st, 2, [128, 4, 512], BF16, "hgT")
        gtr = C.ring(st, 2, [128, 16, 512], BF16, "gts")
        pya = C.ring(st, 2, [128, 512], F32, "pya", psum=True)
        pyh = C.ring(st, 2, [128, 512], F32, "pyh", psum=True)
        px = C.ring(st, 2, [128, 512], F32, "px", psum=True)
        pt = C.ring(st, 2, [128, 8, 128], BF16, "ptd", psum=True)
        t1r = C.ring(st, 2, [128, 512], F32, "t1")
        t2r = C.ring(st, 2, [128, 512], F32, "t2")
        mTr = C.ring(st, 2, [128, 8, 512], BF16, "mT")
        xr = C.ring(st, 2, [128, 1024], F32, "xtd")
        x1r = C.ring(st, 3, [128, 1024], F32, "x1t")
        junk = C.alloc(st, [128, 1024], BF16, "junkd")
        stat = C.ring(st, 2, [128, 8], F32, "statd")
        hbr = C.ring(st, 2, [128, 1024], BF16, "hbd")
        h2s = C.ring(st, 2, [128, 8, 128], BF16, "h2s")
        for g in range(ngroups):
            cols = slice(g * 512, (g + 1) * 512)
            aT, hT, gt = aTr.next(), hTr_.next(), gtr.next()
            P.dma("sp", aT.t[:], Sc["attoT"].t[:, :, cols].rearrange("r p t -> p r t"),
                  reads=[Sc["attoT"].reg((pr, g)) for pr in range(4)], writes=[aT])
            P.dma("sp", hT.t[:], Sc["hgoT"].t[:, :, cols].rearrange("r p t -> p r t"),
                  reads=Sc["hgoT"].regl(range(4 * g, 4 * g + 4)), writes=[hT])
            P.dma("sp", gt.t[:], Sc["gtsT"].t[:, :, cols].rearrange("r p t -> p r t"),
                  reads=[Sc["gtsT"].reg((c, g)) for c in range(16)], writes=[gt])
            mT = mTr.next()
            for m_ in range(8):
                ms = slice(m_ * 128, (m_ + 1) * 128)
                ya, yh = pya.next(), pyh.next()
                for kc in range(4):
                    mm(C, ya, ya.t[:], wua, wua.t[:, kc, ms], aT, aT.t[:, kc, :], kc == 0, kc == 3)
                for kc in range(4):
                    mm(C, yh, yh.t[:], wuh, wuh.t[:, kc, ms], hT, hT.t[:, kc, :], kc == 0, kc == 3)
                t1, t2 = t1r.next(), t2r.next()
                tt(C, "dve", t1, t1.t[:], ya, ya.t[:], gt, gt.t[:, m_, :], ALU.mult)
                tt(C, "dve", t2, t2.t[:], yh, yh.t[:], gt, gt.t[:, 8 + m_, :], ALU.mult)
                tt(C, "pool", mT, mT.t[:, m_, :], t1, t1.t[:], t2, t2.t[:], ALU.add)
            def part1(j):
                ti = g * 4 + j
                rows = slice(ti * 128, (ti + 1) * 128)
                xt = xr.next()
                P.dma("sp", xt.t[:], I["x"].t[rows, :], reads=[I["x"]], writes=[xt])
                x1 = x1r.next()
                for hf in range(2):
                    hs = slice(hf * 512, (hf + 1) * 512)
                    p_ = px.next()
                    for m_ in range(8):
                        mm(C, p_, p_.t[:], mT, mT.t[:, m_, j * 128:(j + 1) * 128], wo, wo.t[:, m_, hs], m_ == 0, m_ == 7)
                    tt(C, "dve", x1, x1.t[:, hs], p_, p_.t[:], xt, xt.t[:, hs], ALU.add)
                P.dma("pool", Sc["x1"].t[rows, :], x1.t[:], reads=[x1], writes=[Sc["x1"].reg(ti)])
                return x1

            def part2(j, x1):
                ti = g * 4 + j
                hs_ = h2s.next()
                norm_transpose(C, K, x1, gffn, stat.next(), junk, hbr.next(), pt.next(), hs_, hs_.t[:])
                P.dma("pool", Sc["h2T"].t[ti], hs_.t[:], reads=[hs_], writes=[Sc["h2T"].reg(ti)])

            pend = part1(0)
            for j in range(4):
                nxt = part1(j + 1) if j + 1 < 4 else None
                part2(j, pend)
                pend = nxt


def phase_e1(C, K, ngroups=16):
    nc, P = C.nc, C.P
    Sc = C.scr
    I = C.ins
    with contextlib.ExitStack() as st:
        wq = load_w(C, st, "peer_wq", 8, 2048)
        skT = C.alloc(st, [128, 16, 128], BF16, "skT")
        P.dma("pool", skT.t[:], I["skT"].t, reads=[I["skT"]], writes=[skT])
        io_f = C.alloc(st, [128, 128], F32, "io_f")
        P.op("pool", lambda e: e.iota(io_f.t[:], pattern=[[1, 128]], base=0, channel_multiplier=0,
                                      allow_small_or_imprecise_dtypes=True), writes=[io_f])
        io_b = C.alloc(st, [128, 128], BF16, "io_b")
        cp(C, "dve", io_b, io_b.t[:], io_f, io_f.t[:])
        io_rep = C.alloc(st, [128, 128, 16], BF16, "io_rep")
        cp(C, "dve", io_rep, io_rep.t[:], io_f, io_f.t[:].unsqueeze(2).to_broadcast([128, 128, 16]))
        h2r = C.ring(st, 2, [128, 8, 128], BF16, "h2e")
        pq = C.ring(st, 2, [128, 4, 128], F32, "pq", psum=True)
        psc = C.ring(st, 2, [128, 4, 128], F32, "psc", psum=True)
        pIG = C.alloc(st, [128, 8, 128], BF16, "pIG", psum=True)
        pG = C.ring(st, 3, [128, 4, 128], F32, "pG", psum=True)
        qpT = C.ring(st, 2, [128, 16, 128], BF16, "qpT")
        s_all = C.ring(st, 2, [128, 16, 128], F32, "s_all")
        tmp128 = C.ring(st, 4, [128, 128], F32, "tmp128")
        v16 = C.ring(st, 2, [128, 16, 16], F32, "v16")
        i16 = C.ring(st, 2, [128, 16, 16], U32, "i16")
        i16f = C.ring(st, 2, [128, 16, 16], F32, "i16f")
        cand = C.ring(st, 1, [128, 8, 256], F32, "cand")
        tmp256 = C.ring(st, 4, [128, 256], F32, "tmp256")
        tsv = C.ring(st, 2, [128, 8, 16], F32, "tsv")
        pos = C.ring(st, 2, [128, 8, 16], U32, "pos")
        k12i = C.ring(st, 2, [128, 2, 128], I32, "k12i")
        k12f = C.ring(st, 2, [128, 2, 128], F32, "k12f")
        eq = C.ring(st, 2, [128, 128, 16], F32, "eq")
        IG = C.ring(st, 2, [128, 3, 128], BF16, "IG")
        IGf = C.ring(st, 2, [128, 3, 128], F32, "IGf")
        IGT = C.ring(st, 2, [128, 3, 128], BF16, "IGT")
        ex = C.ring(st, 2, [128, 8, 16], F32, "ex")
        st8 = C.ring(st, 2, [128, 8], F32, "st8")
        A4 = C.ring(st, 3, [128, 16, 128], BF16, "A4")
        B4 = C.ring(st, 3, [128, 16, 128], BF16, "B4")
        Gst = C.ring(st, 1, [128, 128, 256], BF16, "Gst")
        est = {}

        def stageXc(grp, j2):
            ti = grp * 2 + j2
            h2 = h2r.next()
            P.dma("sp", h2.t[:], Sc["h2T"].t[ti], reads=[Sc["h2T"].reg(ti)], writes=[h2])
            qp, sa = qpT.next(), s_all.next()
            for c4 in range(4):
                p_ = pq.next()
                for cc in range(4):
                    cq = c4 * 4 + cc
                    for k in range(8):
                        mm(C, p_, p_.t[:, cc, :], wq, wq.t[:, k, cq * 128:(cq + 1) * 128], h2, h2.t[:, k, :], k == 0, k == 7)
                cp(C, "act", qp, qp.t[:, c4 * 4:(c4 + 1) * 4, :], p_, p_.t[:])
            for c4 in range(4):
                p_ = psc.next()
                for cc in range(4):
                    cq = c4 * 4 + cc
                    mm(C, p_, p_.t[:, cc, :], qp, qp.t[:, cq, :], skT, skT.t[:, cq, :], True, True)
                cp(C, "act", sa, sa.t[:, c4 * 4:(c4 + 1) * 4, :], p_, p_.t[:])
            if "dbgs" in Sc:
                P.dma("pool", Sc["dbgs"].t[ti], sa.t[:], reads=[sa], writes=[Sc["dbgs"].reg(ti)])
            est["sa", grp, j2] = sa

        def stageXt(grp, j2):
            ti = grp * 2 + j2
            sa = est.pop(("sa", grp, j2))
            v_, i_ = v16.next(), i16.next()

            def top16(src_t, src_ap, vdst_t, vdst_ap, idst_t, idst_ap, tmp):
                P.op("dve", lambda e: e.max(out=vdst_ap[:, 0:8], in_=src_ap), reads=[src_t], writes=[vdst_t])
                yield
                P.op("dve", lambda e: e.match_replace(out=tmp.t[:], in_to_replace=vdst_ap[:, 0:8], in_values=src_ap,
                                                      imm_value=-1e30), reads=[src_t, vdst_t], writes=[tmp])
                yield
                P.op("dve", lambda e: e.max(out=vdst_ap[:, 8:16], in_=tmp.t[:]), reads=[tmp, vdst_t], writes=[vdst_t])
                yield
                P.op("dve", lambda e: e.max_index(out=idst_ap[:, 0:8], in_max=vdst_ap[:, 0:8], in_values=src_ap),
                     reads=[src_t, vdst_t], writes=[idst_t])
                yield
                P.op("dve", lambda e: e.max_index(out=idst_ap[:, 8:16], in_max=vdst_ap[:, 8:16], in_values=src_ap),
                     reads=[src_t, vdst_t, idst_t], writes=[idst_t])
                yield

            def rr4(gens):
                gens = list(gens)
                while gens:
                    for g_ in list(gens):
                        try:
                            next(g_)
                        except StopIteration:
                            gens.remove(g_)

            for c0 in range(0, 16, 4):
                rr4([top16(sa, sa.t[:, cq, :], v_.reg(cq), v_.t[:, cq, :], i_.reg(cq), i_.t[:, cq, :], tmp128.next())
                     for cq in range(c0, c0 + 4)])
            if_ = i16f.next()
            cp(C, "dve", if_, if_.t[:], i_.regl(range(16)), i_.t[:])
            cd = cand.next()
            vv = v_.t[:].rearrange("p (h a) k -> p h a k", a=2)
            tt(C, "dve", cd, cd.t[:].rearrange("p h (a b) -> p h a b", b=16),
               v_.regl(range(16)), vv[:, :, 0, :].unsqueeze(3).to_broadcast([128, 8, 16, 16]),
               v_.regl(range(16)), vv[:, :, 1, :].unsqueeze(2).to_broadcast([128, 8, 16, 16]), ALU.add)
            ts_, ps_ = tsv.next(), pos.next()
            for h0 in range(0, 8, 4):
                rr4([top16(cd, cd.t[:, h, :], ts_.reg(h), ts_.t[:, h, :], ps_.reg(h), ps_.t[:, h, :], tmp256.next())
                     for h in range(h0, h0 + 4)])
            ki, kf = k12i.next(), k12f.next()
            posf = ps_.t[:].rearrange("p h k -> p (h k)").bitcast(I32)
            P.op("dve", lambda e, ki=ki, posf=posf: e.tensor_single_scalar(out=ki.t[:, 0, :], in_=posf, scalar=4, op=ALU.arith_shift_right),
                 reads=ps_.regl(range(8)), writes=[ki])
            P.op("dve", lambda e, ki=ki, posf=posf: e.tensor_single_scalar(out=ki.t[:, 1, :], in_=posf, scalar=15, op=ALU.bitwise_and),
                 reads=ps_.regl(range(8)) + [ki], writes=[ki])
            cp(C, "dve", kf, kf.t[:], ki, ki.t[:])
            ig = IGf.next()
            iv = if_.t[:].rearrange("p (h a) k -> p h a k", a=2)
            for a in range(2):
                e_ = eq.next()
                tt(C, "dve", e_, e_.t[:], kf, kf.t[:, a, :].unsqueeze(2).to_broadcast([128, 128, 16]),
                   io_f, io_f.t[:, 0:16].unsqueeze(1).to_broadcast([128, 128, 16]), ALU.is_equal)
                e4 = e_.t[:].rearrange("p (h k) c -> p h k c", h=8)
                tt(C, "dve", e_, e4, e_, e4, if_, iv[:, :, a, :].unsqueeze(2).to_broadcast([128, 8, 16, 16]), ALU.mult)
                P.op("dve", lambda e, e_=e_, ig=ig, a=a: e.tensor_reduce(out=ig.t[:, a, :], in_=e_.t[:], axis=AX.X, op=ALU.add),
                     reads=[e_], writes=[ig])
            x_ = ex.next()
            tt(C, "dve", x_, x_.t[:], ts_.regl(range(8)), ts_.t[:], ts_.regl(range(8)), ts_.t[:, :, 0:1].to_broadcast([128, 8, 16]), ALU.subtract)
            act(C, x_, x_.t[:], x_, x_.t[:], AF.Exp)
            s8 = st8.next()
            P.op("dve", lambda e, s8=s8, x_=x_: e.tensor_reduce(out=s8.t[:], in_=x_.t[:], axis=AX.X, op=ALU.add), reads=[x_], writes=[s8])
            P.op("dve", lambda e, s8=s8: e.reciprocal(out=s8.t[:], in_=s8.t[:]), reads=[s8], writes=[s8])
            tt(C, "dve", ig, ig.t[:, 2, :].rearrange("p (h k) -> p h k", h=8), x_, x_.t[:],
               s8, s8.t[:].unsqueeze(2).to_broadcast([128, 8, 16]), ALU.mult)
            igf_ = ig
            ig = IG.next()
            cp(C, "dve", ig, ig.t[:], igf_, igf_.t[:])
            if "dbgig" in Sc:
                P.dma("pool", Sc["dbgig"].t[ti], ig.t[:], reads=[ig], writes=[Sc["dbgig"].reg(ti)])
            for a in range(3):
                tr(C, pIG, pIG.t[:, a, :], ig, ig.t[:, a, :], K["ident"])
            igt = IGT.next()
            cp(C, "dve", igt, igt.t[:], pIG, pIG.t[:, 0:3, :])
            est["igt", grp, j2] = igt

        def stageY(grp, j2):
            if j2 == 0:
                est["G", grp] = Gst.next()
            G_ = est["G", grp]
            igt = est.pop(("igt", grp, j2))
            TB = 16
            for b16 in range(128 // TB):
                a4, bb4 = A4.next(), B4.next()
                tsl = slice(b16 * TB, (b16 + 1) * TB)
                av = a4.t[:].rearrange("p t i -> p (t i)").rearrange("p (i t) -> p i t", t=TB)
                bv = bb4.t[:].rearrange("p t i -> p (t i)").rearrange("p (i t) -> p i t", t=TB)
                tt(C, "dve", a4, av, io_rep, io_rep.t[:], igt, igt.t[:, 0, tsl].unsqueeze(1).to_broadcast([128, 128, TB]), ALU.is_equal)
                tt(C, "dve", bb4, bv, io_rep, io_rep.t[:], igt, igt.t[:, 1, tsl].unsqueeze(1).to_broadcast([128, 128, TB]), ALU.is_equal)
                tt(C, "pool", a4, av, a4, av, igt, igt.t[:, 2, tsl].unsqueeze(1).to_broadcast([128, 128, TB]), ALU.mult)
                for q4 in range(TB // 4):
                    pg = pG.next()
                    for q_ in range(4):
                        mm(C, pg, pg.t[:, q_, :], a4, av[:, :, q4 * 4 + q_], bb4, bv[:, :, q4 * 4 + q_], True, True)
                    t0 = j2 * 128 + b16 * TB + q4 * 4
                    cp(C, "act", G_, G_.t[:, :, t0:t0 + 4].rearrange("p i t -> p t i"), pg, pg.t[:])
            if j2 == 1:
                hc = slice((grp % 2) * 256, (grp % 2 + 1) * 256)
                for i0 in range(0, 128, 32):
                    P.dma("pool", Sc["G"].t[grp // 2, :, i0:i0 + 32, hc], G_.t[:, i0:i0 + 32, :], reads=[G_], writes=[Sc["G"].reg(grp)])

        tl = [(grp, j2) for grp in range(ngroups) for j2 in range(2)]
        for it in range(len(tl) + 2):
            if it < len(tl):
                stageXc(*tl[it])
            if 1 <= it < len(tl) + 1:
                stageXt(*tl[it - 1])
            if it >= 2:
                stageY(*tl[it - 2])


def phase_e0(C, K):
    P = C.P
    jobs = []
    for i2 in range(128):
        jobs.append(lambda i2=i2: P.dma("pool", C.scr["uTb"].t[i2], C.ins["uT"].t[i2], reads=[C.ins["uT"]], writes=[C.scr["uTb"].reg(i2)]))
        jobs.append(lambda i2=i2: P.dma("pool", C.scr["vLb"].t[i2], C.ins["vL"].t[i2], reads=[C.ins["vL"]], writes=[C.scr["vLb"].reg(i2)]))
    return jobs


def phase_e2(C, K, ngroups=8, ni2=128):
    nc, P = C.nc, C.P
    Sc = C.scr
    with contextlib.ExitStack() as st:
        h2r = C.ring(st, 1, [128, 8, 512], BF16, "h2g")
        po = [C.alloc(st, [128, 512], F32, "po", psum=True) for _ in range(4)]
        par = C.ring(st, 4, [128, 512], F32, "pa", psum=True)
        uch = C.ring(st, 3, [128, 2, 8, 128], BF16, "uch")
        vch = C.ring(st, 4, [128, 2, 512], BF16, "vch")
        gch = C.ring(st, 3, [128, 2, 512], BF16, "gch")
        sqr = C.ring(st, 3, [128, 512], F32, "sqe")
        t2r = C.ring(st, 3, [128, 512], F32, "t2e")
        sgr = C.ring(st, 3, [128, 512], BF16, "sge")
        agr = C.ring(st, 3, [128, 512], BF16, "age")
        Wall = C.alloc(st, [128, ni2, 512], BF16, "Wall")
        xs = C.ring(st, 2, [128, 512], F32, "xs")
        x1h = C.ring(st, 2, [128, 512], F32, "x1h")
        LAG = 4
        state = {}

        def s1(grp, i2):
            h2 = state["h2"]
            if i2 % 2 == 0:
                u_, v_, g_ = uch.next(), vch.next(), gch.next()
                P.dma("sp", u_.t[:], Sc["uTb"].t[i2:i2 + 2].rearrange("i p k c -> p i k c"),
                      reads=Sc["uTb"].regl([i2, i2 + 1]), writes=[u_])
                P.dma("sp", v_.t[:], Sc["vLb"].t[i2:i2 + 2, :, 0:512].rearrange("i p d -> p i d"),
                      reads=Sc["vLb"].regl([i2, i2 + 1]), writes=[v_])
                P.dma("sp", g_.t[:], Sc["G"].t[grp, :, i2:i2 + 2, :], reads=Sc["G"].regl([2 * grp, 2 * grp + 1]), writes=[g_])
                state["uvg"] = (u_, v_, g_)
            u_, v_, g_ = state["uvg"]
            e_ = i2 % 2
            pa = par.next()
            for k in range(8):
                mm(C, pa, pa.t[:], u_, u_.t[:, e_, k, :], h2, h2.t[:, k, :], k == 0, k == 7)
            sq, t2, sg, ag = sqr.next(), t2r.next(), sgr.next(), agr.next()
            Wb = Wall.reg(i2)
            act(C, sq, sq.t[:], pa, pa.t[:], AF.Square, scale=0.21145921592448583)
            stt(C, "dve", t2, t2.t[:], sq, sq.t[:], 1.0, pa, pa.t[:], ALU.add, ALU.mult)
            tt(C, "dve", ag, ag.t[:], pa, pa.t[:], g_, g_.t[:, e_, :], ALU.mult)
            act(C, sg, sg.t[:], t2, t2.t[:], AF.Sigmoid, scale=1.5957691216057308)
            P.op("pool", lambda e: e.tensor_tensor(out=Wall.t[:, i2, :], in0=sg.t[:], in1=ag.t[:], op=ALU.mult),
                 reads=[sg, ag], writes=[Wb])
            state["v", i2] = (v_, e_)

        def s2(grp, i2, half):
            v_, e_ = state.pop(("v", i2)) if half == 0 else state.pop(("v2", i2))
            for j in range(4):
                P.op("pe", lambda e, j=j: e.matmul(po[j].t[:], lhsT=Wall.t[:, i2, j * 128:(j + 1) * 128], rhs=v_.t[:, e_, :],
                                                    start=(i2 == 0), stop=(i2 == ni2 - 1)),
                     reads=[Wall.reg(i2), v_], writes=[po[j]], pe_accum=True)

        def evac(grp, half):
            hs = slice(half * 512, (half + 1) * 512)
            for j in range(4):
                ti = grp * 4 + j
                rows = slice(ti * 128, (ti + 1) * 128)
                x1_ = x1h.next()
                P.dma("sp", x1_.t[:], Sc["x1"].t[rows, hs], reads=[Sc["x1"].reg(ti)], writes=[x1_])
                x_ = xs.next()
                tt(C, "dve", x_, x_.t[:], po[j], po[j].t[:], x1_, x1_.t[:], ALU.add)
                P.dma("pool", Sc["x2"].t[rows, hs], x_.t[:], reads=[x_], writes=[Sc["x2"].reg((ti, half))])

        for grp in range(ngroups):
            h2 = h2r.next()
            for j in range(4):
                ti = grp * 4 + j
                P.dma("sp", h2.t[:, :, j * 128:(j + 1) * 128], Sc["h2T"].t[ti], reads=[Sc["h2T"].reg(ti)], writes=[h2])
            state["h2"] = h2
            for it in range(ni2 + LAG):
                if it < ni2:
                    s1(grp, it)
                if it >= LAG:
                    s2(grp, it - LAG, 0)
            evac(grp, 0)
            for it in range(ni2 + LAG):
                if it < ni2:
                    if it % 2 == 0:
                        v_ = vch.next()
                        P.dma("sp", v_.t[:], Sc["vLb"].t[it:it + 2, :, 512:1024].rearrange("i p d -> p i d"),
                              reads=Sc["vLb"].regl([it, it + 1]), writes=[v_])
                        state["vp"] = v_
                    state["v2", it] = (state["vp"], it % 2)
                if it >= LAG:
                    s2(grp, it - LAG, 1)
            evac(grp, 1)


def phase_f(C, K, ntiles=NT):
    nc, P = C.nc, C.P
    Sc = C.scr
    I = C.ins
    with contextlib.ExitStack() as st:
        wg = load_w(C, st, "ple_gate", 8, 1024)
        wp = load_w(C, st, "ple_proj", 2, 1024)
        gple = C.alloc(st, [128, 1024], F32, "gple")
        P.dma("sp", gple.t[:], I["norm_ple"].t.to_broadcast([128, 1024]), writes=[gple])
        x2r = C.ring(st, 3, [128, 1024], F32, "x2f")
        pr_ = C.ring(st, 2, [128, 256], F32, "pf32")
        pbr = C.ring(st, 2, [128, 256], BF16, "pbf")
        junk = C.alloc(st, [128, 1024], BF16, "junkf")
        stat = C.ring(st, 2, [128, 8], F32, "statf")
        hbr = C.ring(st, 2, [128, 1024], BF16, "hbf")
        pt = C.ring(st, 2, [128, 8, 128], BF16, "ptf", psum=True)
        ptp = C.alloc(st, [128, 8, 128], BF16, "ptp", psum=True)
        h3r = C.ring(st, 3, [128, 8, 128], BF16, "h3T")
        pTr = C.ring(st, 3, [128, 2, 128], BF16, "pT")
        pgr = C.ring(st, 2, [128, 512], F32, "pgate", psum=True)
        ppr = C.ring(st, 2, [128, 512], F32, "pproj", psum=True)
        sgr = C.ring(st, 2, [128, 512], F32, "sgf")
        tr_ = C.ring(st, 2, [128, 512], F32, "tf")
        outr = C.ring(st, 2, [128, 1024], F32, "outf")
        def pro(ti):
            rows = slice(ti * 128, (ti + 1) * 128)
            x2 = x2r.next()
            P.dma("sp", x2.t[:], Sc["x2"].t[rows, :], reads=[Sc["x2"].reg((ti, 0)), Sc["x2"].reg((ti, 1))], writes=[x2])
            pf = pr_.next()
            P.dma("sp", pf.t[:], I["p"].t[rows, :], reads=[I["p"]], writes=[pf])
            pb = pbr.next()
            cp(C, "pool", pb, pb.t[:], pf, pf.t[:])
            for k in range(2):
                tr(C, ptp, ptp.t[:, k, :], pb, pb.t[:, k * 128:(k + 1) * 128], K["ident"])
            pT = pTr.next()
            cp(C, "act", pT, pT.t[:], ptp, ptp.t[:, 0:2, :])
            h3 = h3r.next()
            norm_transpose(C, K, x2, gple, stat.next(), junk, hbr.next(), pt.next(), h3, h3.t[:])
            return x2, pT, h3

        def body(ti, x2, pT, h3):
            rows = slice(ti * 128, (ti + 1) * 128)
            o_ = outr.next()
            for hf in range(2):
                hs = slice(hf * 512, (hf + 1) * 512)
                pg, pp = pgr.next(), ppr.next()
                for k in range(8):
                    mm(C, pg, pg.t[:], h3, h3.t[:, k, :], wg, wg.t[:, k, hs], k == 0, k == 7)
                for k in range(2):
                    mm(C, pp, pp.t[:], pT, pT.t[:, k, :], wp, wp.t[:, k, hs], k == 0, k == 1)
                sg, t_ = sgr.next(), tr_.next()
                act(C, sg, sg.t[:], pg, pg.t[:], AF.Sigmoid)
                tt(C, "dve", t_, t_.t[:], pp, pp.t[:], sg, sg.t[:], ALU.mult)
                tt(C, "pool", o_, o_.t[:, hs], t_, t_.t[:], x2, x2.t[:, hs], ALU.add)
            P.dma("sp", C.y.t[rows, :], o_.t[:], reads=[o_], writes=[C.y.reg(ti)])

        pend = pro(0)
        for ti in range(ntiles):
            nxt = pro(ti + 1) if ti + 1 < ntiles else None
            body(ti, *pend)
            pend = nxt


def declare(C):
    C.din("x", [S, D])
    C.din("p", [S, 256])
    C.din("norm_mix", [1, D])
    C.din("w_in", [D, 5376])
    C.din("q_norm", [1, 64])
    C.din("k_norm", [1, 64])
    C.din("hg_lb_raw", [2, 2, 512])
    C.din("hg_out_norm", [1, 128])
    C.din("w_up_att", [512, D])
    C.din("w_up_hg", [512, D])
    C.din("w_out", [D, D])
    C.din("norm_ffn", [1, D])
    C.din("peer_wq", [D, 2048])
    C.din("skT", [128, 16, 128])
    C.din("uT", [128, 128, 8, 128])
    C.din("vL", [128, 128, D])
    C.din("norm_ple", [1, D])
    C.din("ple_gate", [D, D])
    C.din("ple_proj", [256, D])
    sc = C.scratch
    sc("hqT", [4, 128, S], BF16)
    sc("kT", [2, 4, 128, S], BF16)
    sc("gtsT", [16, 128, S], BF16)
    sc("qT", [4, 128, S], BF16)
    sc("kTa", [2, 128, S], BF16)
    sc("v", [NT, 128, 2, 128], BF16)
    sc("g", [2, S, 512], F32)
    sc("k", [2, S, 512], BF16)
    sc("hi", [S, 512], BF16)
    sc("sg", [S, 512], BF16)
    sc("attoT", [4, 128, S], BF16)
    sc("ofwd", [S, 512], F32)
    sc("uTb", [128, 128, 8, 128], BF16)
    sc("vLb", [128, 128, D], BF16)
    sc("x2", [S, D], F32)
    sc("G", [8, 128, 128, 512], BF16)
    if "dbgs" in C.dbg:
        sc("dbgs", [NT, 128, 16, 128], F32)
        sc("dbgig", [NT, 128, 3, 128], BF16)
    sc("x1", [S, D], F32)
    sc("h2T", [NT, 128, 8, 128], BF16)
    sc("hgoT", [4, 128, S], BF16)
    if "dbgo" in C.dbg:
        sc("dbgo", [S, 512], F32)
    if "dbgacc" in C.dbg:
        sc("dbgacc", [128, 512], F32)
        sc("dbgp", [2, 128, 512], BF16)
        sc("dbgvs", [128, NT, 256], BF16)


def build(dbg=(), phases="A", ntiles=NT, **kw):
    nc = bass.Bass("TRN2", target_bir_lowering=False)
    C = Ctx(nc, dbg)
    declare(C)
    C.y = T(nc.dram_tensor("y", [S, D], F32, kind="ExternalOutput").ap(), "y")
    C.outs.append(C.y)
    with contextlib.ExitStack() as st:
        K = build_consts(C, st)
        if "A" in phases:
            phase_a(C, K, ntiles)
        if "C" in phases:
            C.P.barrier()
            phase_c(C, K, kw.get("c_tiles", NT), kw.get("c_dirs", (0, 1)))
        C.P.barrier()
        bg = phase_e0(C, K) if "2" in phases else []
        with contextlib.ExitStack() as st_bd:
            dW = load_d_weights(C, st_bd) if ("D" in phases and "B" in phases) else None
            if "B" in phases:
                phase_b(C, K, kw.get("b_groups", 8), kw.get("hhs", (0, 1)), bg)
            else:
                for job in bg:
                    job()
            if "D" in phases:
                C.P.barrier()
                phase_d(C, K, kw.get("d_groups", 8), dW)
        if "E" in phases:
            C.P.barrier()
            phase_e1(C, K, kw.get("e1_groups", 16))
        if "2" in phases:
            C.P.barrier()
            phase_e2(C, K, kw.get("e2_groups", 8), kw.get("ni2", 128))
        if "F" in phases:
            C.P.barrier()
            phase_f(C, K, kw.get("f_tiles", NT))
        fin = []
        for t in C.outs:
            fin.append(t.b)
            fin.extend(t.regs.values())
        C.P.emit(final_bufs=fin)
    return nc, C


def _in_maps(inp, ncores):
    shared = {}
    for k in ["norm_mix", "q_norm", "k_norm", "hg_out_norm", "norm_ffn", "norm_ple"]:
        shared[k] = np.ascontiguousarray(np.asarray(inp[k], np.float32)[0][None])
    for k in ["w_in", "w_up_att", "w_up_hg", "w_out", "peer_wq", "ple_gate", "ple_proj"]:
        shared[k] = np.ascontiguousarray(np.asarray(inp[k], np.float32)[0])
    shared["hg_lb_raw"] = np.ascontiguousarray(np.asarray(inp["hg_lb_raw"], np.float32))
    sk = np.asarray(inp["peer_subkeys"], np.float32)[0]
    shared["skT"] = np.ascontiguousarray(sk.transpose(3, 0, 1, 2).reshape(128, 16, 128))
    u = np.asarray(inp["peer_u"], np.float32)[0].reshape(128, 128, 8, 128)
    shared["uT"] = np.ascontiguousarray(u.transpose(1, 3, 2, 0))
    v = np.asarray(inp["peer_v"], np.float32)[0].reshape(128, 128, D)
    shared["vL"] = np.ascontiguousarray(v.transpose(1, 0, 2))
    x = np.asarray(inp["x"], np.float32)
    p = np.asarray(inp["p"], np.float32)
    maps = []
    for b in range(ncores):
        m = dict(shared)
        m["x"] = np.ascontiguousarray(x[b])
        m["p"] = np.ascontiguousarray(p[0, b])
        maps.append(m)
    return maps


_NC_CACHE = {}


def kernel(**inputs):
    ncores = 8
    if "nc" not in _NC_CACHE:
        _NC_CACHE["nc"] = build(phases="ABCDE2F")[0]
    nc = _NC_CACHE["nc"]
    maps = _in_maps(inputs, ncores)
    res = run_bass_kernel_spmd(nc, maps, core_ids=list(range(ncores)))
    out = np.stack([np.asarray(res.results[b]["y"], np.float32) for b in range(ncores)], 0)
    return out
```
